# Optimizing a Trainium2 kernel written in Bass

```python
import jax, jax.numpy as jnp
from jax import lax
import numpy as np

D_MODEL = 1024
BATCH = 16
SEQ = 2048
DEPTH = 1

CONV_DIM = 1024
CONV_KERNEL = 31
CONV_PAD = (CONV_KERNEL - 1) // 2
N_HEADS = 16
N_KV_HEADS = 4
HEAD_DIM = 64
ATTN_DIM = N_HEADS * HEAD_DIM
KV_DIM = N_KV_HEADS * HEAD_DIM
WINDOW = 128
BLOCK = 128
NEG_INF = -1e30
N_BRANCH = 2
IN_DIM = 2 * CONV_DIM + ATTN_DIM + 2 * KV_DIM + N_BRANCH * D_MODEL
PEER_HEADS = 8
PEER_KEY_DIM = 256
PEER_HALF = PEER_KEY_DIM // 2
N_KEYS = 128
N_EXPERTS = N_KEYS * N_KEYS
PEER_TOPK = 16
TOKEN_CHUNK = 128
EPS = 1e-6

kernel_name = "hybrid_conformer_swa_peer_block"


def rmsnorm(x, g):
    xf = x.astype(jnp.float32)
    y = xf * lax.rsqrt(jnp.mean(xf * xf, axis=-1, keepdims=True) + EPS)
    return (y * g.astype(jnp.float32)).astype(x.dtype)


def layernorm(x, g, b):
    xf = x.astype(jnp.float32)
    mu = jnp.mean(xf, axis=-1, keepdims=True)
    var = jnp.mean(jnp.square(xf - mu), axis=-1, keepdims=True)
    y = (xf - mu) * lax.rsqrt(var + EPS)
    return (y * g.astype(jnp.float32) + b.astype(jnp.float32)).astype(x.dtype)


def conformer_conv(a_in, dw_w, dw_b, ln_g, ln_b, w_pw):
    u = a_in[..., :CONV_DIM] * jax.nn.sigmoid(a_in[..., CONV_DIM:])
    u = lax.conv_general_dilated(
        u, dw_w[:, None, :], window_strides=(1,), padding=[(CONV_PAD, CONV_PAD)],
        dimension_numbers=('NWC', 'WIO', 'NWC'), feature_group_count=CONV_DIM) + dw_b
    u = layernorm(u, ln_g, ln_b)
    u = jax.nn.silu(u)
    return u @ w_pw


def windowed_gqa(q, k, v, sink):
    b, s = q.shape[0], q.shape[1]
    nb = s // BLOCK
    grp = N_HEADS // N_KV_HEADS
    span = BLOCK + 2 * WINDOW
    kp = jnp.pad(k, ((0, 0), (WINDOW, WINDOW), (0, 0), (0, 0)))
    vp = jnp.pad(v, ((0, 0), (WINDOW, WINDOW), (0, 0), (0, 0)))
    qb = q.reshape(b, nb, BLOCK, N_KV_HEADS, grp, HEAD_DIM).transpose(1, 0, 2, 3, 4, 5)
    slopes = jnp.exp2(-8.0 * jnp.arange(1, N_HEADS + 1, dtype=jnp.float32) / N_HEADS)
    slopes = slopes.reshape(N_KV_HEADS, grp)
    r = jnp.arange(BLOCK)[:, None]
    c = jnp.arange(span)[None, :]
    dist = jnp.abs(r + WINDOW - c)
    bias = -slopes[:, :, None, None] * dist.astype(jnp.float32)[None, None]
    in_window = dist <= WINDOW
    sink_f = sink.astype(jnp.float32).reshape(N_KV_HEADS, grp)[None, :, :, None]
    scale = HEAD_DIM ** -0.5

    def block_fn(args):
        i, qi = args
        start = i * BLOCK
        ks = lax.dynamic_slice_in_dim(kp, start, span, axis=1)
        vs = lax.dynamic_slice_in_dim(vp, start, span, axis=1)
        key_pos = start - WINDOW + jnp.arange(span)
        valid = in_window & ((key_pos >= 0) & (key_pos < s))[None, :]
        logits = jnp.einsum('bqkgd,bskd->bkgqs', qi, ks).astype(jnp.float32) * scale + bias
        logits = jnp.where(valid, logits, NEG_INF)
        m = jnp.maximum(logits.max(-1), sink_f)
        p = jnp.exp(logits - m[..., None])
        denom = p.sum(-1) + jnp.exp(sink_f - m)
        o = jnp.einsum('bkgqs,bskd->bqkgd', p, vs.astype(jnp.float32))
        o = o / denom.transpose(0, 3, 1, 2)[..., None]
        return o.astype(q.dtype)

    out = lax.map(block_fn, (jnp.arange(nb), qb))
    return out.transpose(1, 0, 2, 3, 4, 5).reshape(b, s, ATTN_DIM)


def peer_ffn(h, w_query, sub_keys, expert_u, expert_v):
    b, s, d = h.shape
    hc_all = h.reshape((b * s) // TOKEN_CHUNK, TOKEN_CHUNK, d)

    def chunk_fn(hc):
        q = (hc @ w_query).reshape(TOKEN_CHUNK, PEER_HEADS, 2, PEER_HALF)
        sc = jnp.einsum('thpd,pnd->thpn', q, sub_keys).astype(jnp.float32)
        vals, idx = lax.top_k(sc, PEER_TOPK)
        cand = vals[:, :, 0, :, None] + vals[:, :, 1, None, :]
        cand_idx = idx[:, :, 0, :, None] * N_KEYS + idx[:, :, 1, None, :]
        cand = cand.reshape(TOKEN_CHUNK, PEER_HEADS, PEER_TOPK * PEER_TOPK)
        cand_idx = cand_idx.reshape(TOKEN_CHUNK, PEER_HEADS, PEER_TOPK * PEER_TOPK)
        top_s, pos = lax.top_k(cand, PEER_TOPK)
        expert_idx = jnp.take_along_axis(cand_idx, pos, axis=-1)
        gate = jax.nn.softmax(top_s, axis=-1)
        u = expert_u[expert_idx]
        act = jax.nn.gelu(jnp.einsum('thkd,td->thk', u, hc).astype(jnp.float32), approximate=False)
        vv = expert_v[expert_idx]
        return jnp.einsum('thk,thkd->td', (gate * act).astype(hc.dtype), vv)

    return lax.map(chunk_fn, hc_all).reshape(b, s, d)


def setup_inputs(seed: int = 0) -> dict:
    key = jax.random.key(seed)
    ks = jax.random.split(key, 18)
    f32 = jnp.float32
    nrm = lambda k, shape, sc: jax.random.normal(k, shape, f32) * sc
    return {
        "x": nrm(ks[0], (BATCH, SEQ, D_MODEL), 1.0),
        "norm1_g": 1.0 + nrm(ks[1], (DEPTH, D_MODEL), 0.02),
        "w_in": nrm(ks[2], (DEPTH, D_MODEL, IN_DIM), D_MODEL ** -0.5),
        "conv_dw_w": nrm(ks[3], (DEPTH, CONV_KERNEL, CONV_DIM), CONV_KERNEL ** -0.5),
        "conv_dw_b": nrm(ks[4], (DEPTH, CONV_DIM), 0.02),
        "conv_ln_g": 1.0 + nrm(ks[5], (DEPTH, CONV_DIM), 0.02),
        "conv_ln_b": nrm(ks[6], (DEPTH, CONV_DIM), 0.02),
        "conv_w_pw": nrm(ks[7], (DEPTH, CONV_DIM, D_MODEL), CONV_DIM ** -0.5),
        "attn_sink": nrm(ks[8], (DEPTH, N_HEADS), 0.5),
        "attn_w_o": nrm(ks[9], (DEPTH, ATTN_DIM, D_MODEL), ATTN_DIM ** -0.5),
        "w_out": nrm(ks[10], (DEPTH, D_MODEL, D_MODEL), D_MODEL ** -0.5),
        "norm2_g": 1.0 + nrm(ks[11], (DEPTH, D_MODEL), 0.02),
        "peer_w_query": nrm(ks[12], (DEPTH, D_MODEL, PEER_HEADS * PEER_KEY_DIM), D_MODEL ** -0.5),
        "peer_sub_keys": nrm(ks[13], (DEPTH, 2, N_KEYS, PEER_HALF), PEER_HALF ** -0.5),
        "peer_u": nrm(ks[14], (DEPTH, N_EXPERTS, D_MODEL), D_MODEL ** -0.5),
        "peer_v": nrm(ks[15], (DEPTH, N_EXPERTS, D_MODEL), 0.25),
        "final_g": 1.0 + nrm(ks[16], (D_MODEL,), 0.02),
    }


def reference(x, norm1_g, w_in, conv_dw_w, conv_dw_b, conv_ln_g, conv_ln_b, conv_w_pw,
              attn_sink, attn_w_o, w_out, norm2_g, peer_w_query, peer_sub_keys,
              peer_u, peer_v, final_g):
    b, s, d = x.shape
    o_conv = 2 * CONV_DIM
    o_q = o_conv + ATTN_DIM
    o_k = o_q + KV_DIM
    o_v = o_k + KV_DIM
    for l in range(DEPTH):
        h = rmsnorm(x, norm1_g[l])
        z = h @ w_in[l]
        conv_in = z[..., :o_conv]
        q = z[..., o_conv:o_q].reshape(b, s, N_HEADS, HEAD_DIM)
        k = z[..., o_q:o_k].reshape(b, s, N_KV_HEADS, HEAD_DIM)
        v = z[..., o_k:o_v].reshape(b, s, N_KV_HEADS, HEAD_DIM)
        gates = jax.nn.sigmoid(z[..., o_v:].astype(jnp.float32)).astype(x.dtype)
        gates = gates.reshape(b, s, N_BRANCH, d)
        conv_out = conformer_conv(conv_in, conv_dw_w[l], conv_dw_b[l], conv_ln_g[l],
                                  conv_ln_b[l], conv_w_pw[l])
        attn_out = windowed_gqa(q, k, v, attn_sink[l]) @ attn_w_o[l]
        merged = gates[:, :, 0, :] * conv_out + gates[:, :, 1, :] * attn_out
        x = x + merged @ w_out[l]
        h2 = rmsnorm(x, norm2_g[l])
        x = x + peer_ffn(h2, peer_w_query[l], peer_sub_keys[l], peer_u[l], peer_v[l])
    return rmsnorm(x, final_g)
```

```python
import os
import numpy as np
from contextlib import ExitStack
import concourse.bass as bass
import concourse.mybir as mybir
from concourse.bass_utils import run_bass_kernel_spmd

F32 = mybir.dt.float32
BF16 = mybir.dt.bfloat16
U32 = mybir.dt.uint32
AF = mybir.ActivationFunctionType
ALU = mybir.AluOpType
AX = mybir.AxisListType

NCORES = 8
TOK = 4096
SEQ = 2048
D = 1024
EPS = 1e-6
TB = 256


class Tok:
    __slots__ = ("w", "r", "sem", "cnt", "name")

    def __init__(self, name=""):
        self.w = None
        self.r = {}
        self.sem = None
        self.cnt = 0
        self.name = name


class Buf:
    __slots__ = ("ap", "tok")

    def __init__(self, ap, name=""):
        self.ap = ap
        self.tok = Tok(name)


class Prog:
    ENG = ("pe", "dve", "act", "pool", "sp")

    def __init__(self, nc, es):
        self.nc = nc
        self.es = es
        self.ops = {e: [] for e in self.ENG}
        self.cnt = {e: 0 for e in self.ENG}
        self.sems = {}
        for e in self.ENG:
            self.sems["E_" + e] = es.enter_context(nc.semaphore("s_" + e))
        self.final = {}
        self.waited = {e: {} for e in self.ENG}
        self.ndma = 0

    def _collect(self, eng, reads, writes):
        need = {}

        def add(ev, raw):
            if ev is None:
                return
            key, val, src = ev
            if src == eng and eng == "pe":
                return
            if need.get(key, 0) < val:
                need[key] = val

        for t in reads:
            add(t.w, True)
        for t in writes:
            add(t.w, False)
            for key, (val, src) in t.r.items():
                add((key, val, src), False)
        waits = []
        wd = self.waited[eng]
        for key, val in need.items():
            if wd.get(key, 0) < val:
                wd[key] = val
                waits.append((key, val))
        return waits

    def _commit(self, ev, reads, writes):
        key, val, src = ev
        for t in reads:
            old = t.r.get(key)
            if old is None or old[0] < val:
                t.r[key] = (val, src)
        for t in writes:
            t.w = ev
            t.r = {}

    cap = None

    def capture(self):
        self.cap = []

    def end_capture(self):
        c, self.cap = self.cap, None
        return c

    def replay(self, lst, n):
        for _ in range(min(n, len(lst))):
            kind, args = lst.pop(0)
            getattr(self, kind)(*args)

    def op(self, eng, fn, reads=(), writes=()):
        if self.cap is not None:
            self.cap.append(("op", (eng, fn, list(reads), list(writes))))
            return
        reads = [b.tok if isinstance(b, Buf) else b for b in reads]
        writes = [b.tok if isinstance(b, Buf) else b for b in writes]
        waits = self._collect(eng, reads, writes)
        self.cnt[eng] += 1
        key = "E_" + eng
        ev = (key, self.cnt[eng], eng)
        self.final[key] = self.cnt[eng]
        self.ops[eng].append((waits, fn, key, 1))
        self._commit(ev, reads, writes)

    def opn(self, eng, fns, reads=(), writes=()):
        fns = list(fns)

        def run(e, fns=fns):
            last = None
            for f in fns:
                last = f(e)
            return last

        self.op(eng, run, reads, writes)

    def dma(self, q, out, in_, reads=(), writes=(), key=None):
        if self.cap is not None:
            self.cap.append(("dma", (q, out, in_, list(reads), list(writes), key)))
            return
        reads = [b.tok if isinstance(b, Buf) else b for b in reads]
        writes = [b.tok if isinstance(b, Buf) else b for b in writes]
        kt = key if key is not None else (writes[0] if writes else reads[0])
        if isinstance(kt, Buf):
            kt = kt.tok
        if kt.sem is None:
            self.ndma += 1
            kt.sem = "D_%d" % self.ndma
            self.sems[kt.sem] = self.es.enter_context(self.nc.semaphore("d%d" % self.ndma))
        waits = self._collect(q, reads, writes)
        kt.cnt += 16
        ev = (kt.sem, kt.cnt, None)
        self.final[kt.sem] = kt.cnt
        self.ops[q].append((waits, lambda e: e.dma_start(out=out, in_=in_), kt.sem, 16))
        self._commit(ev, reads, writes)

    def barrier(self):
        for e in self.ENG:
            waits = []
            wd = self.waited[e]
            for key, val in self.final.items():
                if key == "E_" + e:
                    continue
                if wd.get(key, 0) < val:
                    wd[key] = val
                    waits.append((key, val))
            if waits:
                self.ops[e].append((waits, None, None, 0))

    def emit(self):
        nc = self.nc
        self.barrier()
        engmap = {"pe": "tensor", "dve": "vector", "act": "scalar", "pool": "gpsimd", "sp": "sync"}
        with nc.Block() as block:
            for e in self.ENG:
                def body(engine, ops=self.ops[e], sems=self.sems):
                    for waits, fn, key, inc in ops:
                        for wk, wv in waits:
                            engine.wait_ge(sems[wk], wv)
                        if fn is not None:
                            fn(engine).then_inc(sems[key], inc)

                getattr(block, engmap[e])(body)


class Arena:
    def __init__(self, nc, nbytes):
        self.t32 = nc.alloc_sbuf_tensor("arena", [128, nbytes // 4], F32)
        self.t16 = self.t32.bitcast(BF16)
        self.tu = self.t32.bitcast(U32)
        self.off = 0
        self.cap = nbytes

    def alloc(self, nbytes):
        off = (self.off + 63) // 64 * 64
        self.off = off + nbytes
        assert self.off <= self.cap, (self.off, self.cap)
        return off

    def f32(self, n, name=""):
        o = self.alloc(n * 4)
        return Buf(self.t32[:, o // 4:o // 4 + n], name)

    def b16(self, n, name=""):
        o = self.alloc(n * 2)
        return Buf(self.t16[:, o // 2:o // 2 + n], name)

    def u32(self, n, name=""):
        o = self.alloc(n * 4)
        return Buf(self.tu[:, o // 4:o // 4 + n], name)


def bc(ap, dims):
    return bass.AP(ap.tensor, ap.offset, [list(ap.ap[0])] + [list(d) for d in dims])


def r3(ap, **kw):
    return ap.rearrange("p (a b) -> p a b", **kw)


KDBG = os.environ.get('KDBG', '')
SLOPES = [2.0 ** (-8.0 * (h + 1) / 16.0) for h in range(16)]


class _Stop(Exception):
    pass


def build_program(stop_after_a=False, stage=None):
    nc = bass.Bass("TRN2", target_bir_lowering=False)
    es = ExitStack()
    dt = lambda name, shape, dtype=F32, kind="ExternalInput": nc.dram_tensor(name, shape, dtype, kind=kind).ap()
    x_d = dt("x", [TOK, D])
    win_d = dt("win", [128, 8, 5632])
    wpw_d = dt("wpw", [128, 8, 1024])
    wo_d = dt("wo", [128, 8, 1024])
    wout_d = dt("wout", [128, 8, 1024])
    wq_d = dt("wq", [128, 8, 2048])
    vecs_d = dt("vecs", [128, 40])
    dww_d = dt("dww", [128, 248])
    sink_d = dt("sink", [128, 16])
    fg_d = dt("fg", [128, 1024])
    skT_d = dt("skT", [128, 256])
    pu_d = dt("pu", [16384, 1024])
    pv_d = dt("pv", [16384, 1024])
    out_d = dt("out", [TOK, D], F32, "ExternalOutput")
    ut_d = dt("ut_scr", [128, 128, 1024], BF16, "Internal")
    vb_d = dt("vb_scr", [16384, 1024], BF16, "Internal")
    wqb_d = dt("wqb_scr", [16, 128, 1024], BF16, "Internal")

    with es:
        P = Prog(nc, es)
        A = Arena(nc, 207 * 1024)
        banks = []
        psall = nc.alloc_psum_tensor("psall", [128, 4096], F32)
        psall16 = psall.bitcast(BF16)
        for i in range(8):
            banks.append((psall[:, i * 512:(i + 1) * 512], psall16[:, i * 1024:(i + 1) * 1024], Tok(f"bank{i}")))
        rr = {"i": 0}

        def nextbank(lst):
            rr["i"] += 1
            return banks[lst[rr["i"] % len(lst)]]

        ident = A.b16(128, "ident")
        identf = A.f32(128, "identf")
        ones16 = A.b16(128, "ones")
        vecs = A.f32(40, "vecs")
        dww = A.f32(248, "dww")
        esink = A.f32(16, "esink")
        small = A.f32(64, "small")
        out_tok = [Tok(f"out{i}") for i in range(TOK // 128)]

        P.dma("sp", vecs.ap, vecs_d, writes=[vecs])
        P.dma("sp", dww.ap, dww_d, writes=[dww])
        P.dma("sp", esink.ap, sink_d, writes=[esink])
        P.op("act", lambda e: e.activation(out=esink.ap, in_=esink.ap, func=AF.Exp), reads=[esink], writes=[esink])
        P.op("pool", lambda e: e.iota(identf.ap, [[1, 128]], base=0, channel_multiplier=-1,
                                      allow_small_or_imprecise_dtypes=True), writes=[identf])
        P.op("dve", lambda e: e.tensor_scalar(out=identf.ap, in0=identf.ap, scalar1=0.0, scalar2=None,
                                              op0=ALU.is_equal), reads=[identf], writes=[identf])
        P.op("dve", lambda e: e.tensor_copy(out=ident.ap, in_=identf.ap), reads=[identf], writes=[ident])
        P.op("pool", lambda e: e.memset(ones16.ap, 1.0 / 1024.0), writes=[ones16])
        G1, DWB, LNG, LNB, G2 = range(5)
        vec = lambda idx, k: vecs.ap[:, idx * 8 + k:idx * 8 + k + 1]

        mark0 = A.off
        Mtab = A.b16(3 * 16 * 128, "Mtab")
        Mv = Mtab.ap.rearrange("p (a h q) -> p a h q", a=3, h=16)
        hT = A.b16(8 * SEQ)
        hTv = r3(hT.ap, a=8)
        hT_tok = [Tok() for _ in range(16)]
        QT = A.b16(8 * SEQ)
        QTv = r3(QT.ap, a=8)
        QT_tok = [Tok() for _ in range(16)]
        cT = A.b16(8 * SEQ)
        cTv = r3(cT.ap, a=8)
        cT_tok = [[Tok() for _ in range(4)] for _ in range(8)]
        r4 = A.alloc(32768)
        KTv = r3(A.t16[:, r4 // 2:r4 // 2 + 4 * SEQ], a=4)
        KT_tok = Tok()
        Vo = r4 + 16384
        Vv = A.t16[:, Vo // 2:Vo // 2 + 16 * 4 * 65].rearrange("p (t g e) -> p t g e", t=16, g=4)
        V_tok = [Tok() for _ in range(16)]
        dgs = [Buf(A.t16[:, (r4 + i * 8192) // 2:(r4 + i * 8192) // 2 + 31 * 128]) for i in range(2)]
        Ub = [Buf(A.t16[:, (r4 + 16384 + i * 4224) // 2:(r4 + 16384 + i * 4224) // 2 + 2078]) for i in range(2)]
        mTv = r3(A.t16[:, r4 // 2:r4 // 2 + 8 * SEQ], a=8)
        mT_tok = [Tok() for _ in range(4)]
        wst = [A.f32(2048) for _ in range(2)]
        wbf = [A.b16(2048) for _ in range(4)]
        wst_h = [Buf(b.ap[:, h * 1024:(h + 1) * 1024]) for b in wst for h in range(2)]
        wbf_h = [Buf(b.ap[:, h * 1024:(h + 1) * 1024]) for b in wbf for h in range(2)]
        xts = [A.f32(1024) for _ in range(2)]
        xns = [A.b16(1024) for _ in range(2)]
        junk = A.b16(1024)
        w12 = A.alloc(12288)
        Ef = [Buf(A.t32[:, (w12 + i * 2048) // 4:(w12 + i * 2048) // 4 + 512]) for i in range(3)]
        PT = [Buf(A.t16[:, (w12 + 6144 + i * 1024) // 2:(w12 + 6144 + i * 1024) // 2 + 512]) for i in range(6)]
        lnm = Buf(A.t32[:, (w12) // 4:(w12) // 4 + 512])
        lnr = Buf(A.t32[:, (w12 + 2048) // 4:(w12 + 2048) // 4 + 512])
        lnt = [Buf(A.t32[:, (w12 + 4096 + i * 2048) // 4:(w12 + 4096 + i * 2048) // 4 + 512]) for i in range(2)]
        lnq = [Buf(A.t16[:, (w12 + 8192 + i * 1024) // 2:(w12 + 8192 + i * 1024) // 2 + 512]) for i in range(2)]
        wkd = Buf(A.t16[:, w12 // 2:w12 // 2 + 4096])
        wkdv = wkd.ap.rearrange("p (k g e) -> p k g e", k=8, g=4)
        AO = [A.b16(1024) for _ in range(2)]
        den = A.f32(16)
        rec = A.f32(16)
        ss_i = {"i": 0}
        small_cur = {"tok": None}
        wst_i = {"i": 0}
        wbf_i = {"i": 0}

        small_toks = [Tok() for _ in range(32)]

        def new_small():
            ss_i["i"] = (ss_i["i"] + 1) % 32
            i = ss_i["i"]
            small_cur["tok"] = small_toks[i]
            return small.ap[:, 2 * i:2 * i + 1], small.ap[:, 2 * i + 1:2 * i + 2]

        def load_w(src3, col0, ncols, scale_idx=None, eng="pool", half=False):
            wst_i["i"] += 1
            wbf_i["i"] += 1
            if half:
                st = wst_h[wst_i["i"] % 4]
                wb = wbf_h[wbf_i["i"] % 8]
            else:
                st = wst[wst_i["i"] % 2]
                wb = wbf[wbf_i["i"] % 4]
            stv = r3(st.ap[:, 0:8 * ncols], a=8)
            wbv = r3(wb.ap[:, 0:8 * ncols], a=8)
            P.dma("sp", stv, src3[:, :, col0:col0 + ncols], writes=[st])
            assert scale_idx is None
            P.op(eng, lambda e: e.tensor_copy(out=wb.ap[:, 0:8 * ncols], in_=st.ap[:, 0:8 * ncols]), reads=[st], writes=[wb])
            return wb, wbv

        def rms_tile(xt, xn):
            ss, rstd = new_small()
            sm = small_cur["tok"]
            P.op("act", lambda e: e.activation(out=junk.ap, in_=xt.ap, func=AF.Square, accum_out=ss),
                 reads=[xt], writes=[junk, sm])
            P.op("act", lambda e: e.activation(out=rstd, in_=ss, func=AF.Sqrt, scale=1.0 / D, bias=EPS),
                 reads=[sm], writes=[sm])
            P.op("dve", lambda e: e.reciprocal(out=rstd, in_=rstd), reads=[sm], writes=[sm])
            if xn is not None:
                P.op("dve", lambda e: e.tensor_scalar(out=xn.ap, in0=xt.ap, scalar1=rstd, scalar2=None, op0=ALU.mult),
                     reads=[xt, sm], writes=[xn])
            return rstd

        def transpose8(src, dst_fn, dst_toks, evac_eng="act", scale_idx=None, bl=(4,)):
            bt, bt16, btok = nextbank(list(bl))
            for k in range(8):
                P.op("pe", lambda e, k=k: e.transpose(out=bt16[:, k * 128:(k + 1) * 128], in_=src.ap[:, k * 128:(k + 1) * 128],
                                                      identity=ident.ap), reads=[src, ident], writes=[btok])
            if scale_idx is None:
                P.op(evac_eng, lambda e: (e.copy if evac_eng == "act" else e.tensor_copy)(
                    out=dst_fn(None), in_=r3(bt16[:, 0:1024], a=8)), reads=[btok], writes=dst_toks)
            else:
                for k in range(8):
                    if evac_eng == "act" or (evac_eng == "mix" and k % 2 == 0):
                        P.op("act", lambda e, k=k: e.activation(out=dst_fn(k), in_=bt16[:, k * 128:(k + 1) * 128], func=AF.Copy,
                                                                scale=vec(scale_idx, k)), reads=[btok, vecs], writes=dst_toks)
                    else:
                        P.op("dve", lambda e, k=k: e.tensor_scalar(out=dst_fn(k), in0=bt16[:, k * 128:(k + 1) * 128],
                                                                   scalar1=vec(scale_idx, k), scalar2=None, op0=ALU.mult),
                             reads=[btok, vecs], writes=dst_toks)

        Df = Ef[0]
        Am = Ef[1]
        Mf = Ef[2]
        for pos in range(3):
            P.op("pool", lambda e, pos=pos: e.iota(Df.ap[:, 0:128], [[1, 128]], base=128 * (1 - pos), channel_multiplier=-1,
                                                   allow_small_or_imprecise_dtypes=True), writes=[Df])
            P.op("act", lambda e: e.activation(out=Df.ap[:, 0:128], in_=Df.ap[:, 0:128], func=AF.Abs),
                 reads=[Df], writes=[Df])
            P.op("dve", lambda e: e.tensor_scalar(out=Am.ap[:, 0:128], in0=Df.ap[:, 0:128], scalar1=128.0, scalar2=None,
                                                  op0=ALU.is_le), reads=[Df], writes=[Am])
            for h in range(16):
                P.op("act", lambda e, h=h: e.activation(out=Mf.ap[:, 0:128], in_=Df.ap[:, 0:128], func=AF.Exp, scale=-SLOPES[h]),
                     reads=[Df], writes=[Mf])
                P.op("dve", lambda e, pos=pos, h=h: e.tensor_tensor(out=Mv[:, pos, h, :], in0=Mf.ap[:, 0:128], in1=Am.ap[:, 0:128],
                                                                    op=ALU.mult), reads=[Mf, Am], writes=[Mtab])
        P.barrier()

        def chk(name):
            if stage == name:
                raise _Stop()

        try:
          for s in range(2):
            tb0 = s * SEQ
            chk('S0')
            def s1_pre(tt):
                xt = xts[tt % 2]
                xn = xns[tt % 2]
                P.dma("sp", xt.ap, x_d[tb0 + tt * 128:tb0 + (tt + 1) * 128, :], writes=[xt])
                rms_tile(xt, xn)

            s1_pre(0)
            for tt in range(16):
                if tt + 1 < 16:
                    s1_pre(tt + 1)
                transpose8(xns[tt % 2], lambda k, tt=tt: hTv[:, k, tt * 128:(tt + 1) * 128], [hT_tok[tt]], evac_eng="mix", scale_idx=G1,
                           bl=(4, 5, 6, 7))

            chk('S1')
            ev_i = 0
            for cp in range(4):
                wb, wbv = load_w(win_d, 2048 + cp * 256, 256)
                for cc in range(2):
                    c = cp * 2 + cc
                    for b in range(4):
                        bt, _, btok = nextbank([0, 1, 2, 3])
                        P.opn("pe", [lambda e, k=k, cc=cc, b=b, wbv=wbv, bt=bt: e.matmul(
                            bt[:, :], lhsT=wbv[:, k, cc * 128:(cc + 1) * 128], rhs=hTv[:, k, b * 512:(b + 1) * 512],
                            start=(k == 0), stop=(k == 7)) for k in range(8)], reads=[wb] + hT_tok[4 * b:4 * b + 4], writes=[btok])
                        dst = QTv[:, c, b * 512:(b + 1) * 512]
                        ev_i += 1
                        if ev_i % 2:
                            P.op("act", lambda e, dst=dst, bt=bt: e.mul(out=dst, in_=bt[:, :], mul=0.125),
                                 reads=[btok], writes=QT_tok[4 * b:4 * b + 4])
                        else:
                            P.op("dve", lambda e, dst=dst, bt=bt: e.tensor_scalar(out=dst, in0=bt[:, :], scalar1=0.125, scalar2=None,
                                                                                  op0=ALU.mult), reads=[btok], writes=QT_tok[4 * b:4 * b + 4])
            wst_i["i"] += 1
            st = wst[wst_i["i"] % 2]
            stv = r3(st.ap, a=8)
            P.dma("sp", stv, win_d[:, :, 3072:3328], writes=[st])
            for half in range(2):
                P.op("pool", lambda e, half=half: e.tensor_copy(
                    out=wkdv[:, :, :, half * 64:(half + 1) * 64], in_=st.ap.rearrange("p (k g e) -> p k g e", k=8, g=4)),
                    reads=[st], writes=[wkd])
            for g in range(4):
                for b in range(4):
                    bt, _, btok = nextbank([0, 1, 2, 3])
                    for k in range(8):
                        P.op("pe", lambda e, k=k, g=g, b=b, bt=bt: e.matmul(
                            bt[:, :], lhsT=wkdv[:, k, g, :], rhs=hTv[:, k, b * 512:(b + 1) * 512],
                            start=(k == 0), stop=(k == 7)), reads=[wkd] + hT_tok[4 * b:4 * b + 4], writes=[btok])
                    P.op("act", lambda e, g=g, b=b, bt=bt: e.copy(out=KTv[:, g, b * 512:(b + 1) * 512], in_=bt[:, :]),
                         reads=[btok], writes=[KT_tok])
            wb, wbv = load_w(win_d, 3328, 256)
            P.op("pool", lambda e: e.memset(Vv[:, :, :, 64:65], 1.0), writes=V_tok)
            for tt in range(16):
                bt, _, btok = nextbank([0, 1, 2, 3])
                for k in range(8):
                    P.op("pe", lambda e, k=k, tt=tt, wbv=wbv, bt=bt: e.matmul(
                        bt[:, 0:256], lhsT=hTv[:, k, tt * 128:(tt + 1) * 128], rhs=wbv[:, k, :],
                        start=(k == 0), stop=(k == 7)), reads=[wb, hT_tok[tt]], writes=[btok])
                P.op("dve", lambda e, tt=tt, bt=bt: e.tensor_copy(out=Vv[:, tt, :, 0:64],
                                                                  in_=bt[:, 0:256].rearrange("p (g e) -> p g e", g=4)),
                     reads=[btok], writes=[V_tok[tt]])

            chk('S2')
            cnt3 = {"pt": 0, "ef": 0}
            po = [banks[5], banks[6], banks[7]]

            def st_phase(i, g):
                kbs = [kb for kb in (i - 1, i, i + 1) if 0 <= kb < 16]
                pts = []
                for kb in kbs:
                    pos = kb - i + 1
                    rr["pair"] = rr.get("pair", 0) + 1
                    b0 = 2 * (rr["pair"] % 2)
                    pair = (banks[b0], banks[b0 + 1])
                    for j in range(4):
                        h = 4 * g + j
                        c, p = h // 2, h % 2
                        bt, _, btok = pair[p]
                        jj = j // 2
                        P.op("pe", lambda e, jj=jj, g=g, kb=kb, c=c, p=p, i=i, bt=bt: e.matmul(
                            bt[:, jj * 128:(jj + 1) * 128], lhsT=KTv[64 * p:64 * p + 64, g, kb * 128:(kb + 1) * 128],
                            rhs=QTv[64 * p:64 * p + 64, c, i * 128:(i + 1) * 128], start=True, stop=True),
                            reads=[KT_tok, QT_tok[i]], writes=[btok])
                    cnt3["ef"] += 1
                    ef = Ef[cnt3["ef"] % 3]
                    src2 = bc(pair[0][0][:, 0:1], [[512, 2], [1, 256]])
                    P.op("act", lambda e, ef=ef, src2=src2: e.activation(out=ef.ap.rearrange("p (a b) -> p a b", a=2), in_=src2,
                                                                       func=AF.Exp),
                         reads=[pair[0][2], pair[1][2]], writes=[ef])
                    cnt3["pt"] += 1
                    pt = PT[cnt3["pt"] % 6]
                    m4 = bc(Mv[:, pos, 4 * g, :], [[128, 2], [256, 2], [1, 128]])
                    P.op("dve", lambda e, ef=ef, pt=pt, m4=m4: e.tensor_tensor(
                        out=pt.ap.rearrange("p (a b q) -> p a b q", a=2, b=2), in0=ef.ap.rearrange("p (a b q) -> p a b q", a=2, b=2),
                        in1=m4, op=ALU.mult), reads=[ef, Mtab], writes=[pt])
                    pts.append(pt)
                return pts

            def pv_phase(i, g, pts):
                kbs = [kb for kb in (i - 1, i, i + 1) if 0 <= kb < 16]
                for j in range(4):
                    h = 4 * g + j
                    pb, _, pbtok = po[h // 7]
                    o0 = (h % 7) * 65
                    P.opn("pe", [lambda e, j=j, g=g, kb=kb, n=n, pb=pb, o0=o0, pt=pts[n], last=len(kbs) - 1: e.matmul(
                        pb[:, o0:o0 + 65], lhsT=pt.ap[:, (j % 2) * 256 + (j // 2) * 128:(j % 2) * 256 + (j // 2) * 128 + 128], rhs=Vv[:, kb, g, :],
                        start=(n == 0), stop=(n == last)) for n, kb in enumerate(kbs)],
                        reads=list(pts) + [V_tok[kb] for kb in kbs], writes=[pbtok])

            items3 = [(i, g) for i in range(16) for g in range(4)]
            pend3 = st_phase(*items3[0])
            for idx3, (i, g) in enumerate(items3):
                nxt3 = st_phase(*items3[idx3 + 1]) if idx3 + 1 < len(items3) else None
                pv_phase(i, g, pend3)
                pend3 = nxt3
                if g != 3:
                    continue
                if stage in ('S3a', 'S3b'):
                    continue
                ao = AO[i % 2]
                for b3, (h0, nh) in enumerate(((0, 7), (7, 7), (14, 2))):
                    pb, _, pbtok = po[b3]
                    pv = pb[:, 0:nh * 65].rearrange("p (h e) -> p h e", e=65)
                    P.op("dve", lambda e, pv=pv, h0=h0, nh=nh: e.tensor_tensor(
                        out=den.ap[:, h0:h0 + nh], in0=pv[:, :, 64], in1=esink.ap[:, h0:h0 + nh], op=ALU.add),
                        reads=[pbtok, esink], writes=[den])
                P.op("dve", lambda e: e.reciprocal(out=rec.ap, in_=den.ap), reads=[den], writes=[rec])
                for b3, (h0, nh) in enumerate(((0, 7), (7, 7), (14, 2))):
                    pb, _, pbtok = po[b3]
                    pv = pb[:, 0:nh * 65].rearrange("p (h e) -> p h e", e=65)
                    P.op("dve", lambda e, pv=pv, h0=h0, nh=nh, ao=ao: e.tensor_tensor(
                        out=ao.ap[:, h0 * 64:(h0 + nh) * 64].rearrange("p (h e) -> p h e", e=64), in0=pv[:, :, 0:64],
                        in1=bc(rec.ap[:, h0:h0 + nh], [[1, nh], [0, 64]]), op=ALU.mult),
                        reads=[pbtok, rec], writes=[ao])
                if stage == 'S3c':
                    continue
                transpose8(ao, lambda k, i=i: QTv[:, :, i * 128:(i + 1) * 128], [QT_tok[i]])
            chk('S3'); chk('S3a'); chk('S3b'); chk('S3c')
            P.barrier()

            for u in Ub:
                P.op("pool", lambda e, u=u: e.memset(u.ap[:, 0:15], 0.0), writes=[u])
                P.op("pool", lambda e, u=u: e.memset(u.ap[:, 2063:2078], 0.0), writes=[u])
            sg_i = 0
            for cp in range(4):
                wa, wav = load_w(win_d, cp * 256, 256)
                wg, wgv = load_w(win_d, 1024 + cp * 256, 256)
                for cc in range(2):
                    c = cp * 2 + cc
                    u = Ub[c % 2]
                    for b in range(4):
                        ba, _, batok = nextbank([0, 1, 2, 3])
                        bg, _, bgtok = nextbank([0, 1, 2, 3])
                        for (wt, wtv, bt, btok) in ((wa, wav, ba, batok), (wg, wgv, bg, bgtok)):
                            P.opn("pe", [lambda e, k=k, cc=cc, b=b, wtv=wtv, bt=bt: e.matmul(
                                bt[:, :], lhsT=wtv[:, k, cc * 128:(cc + 1) * 128], rhs=hTv[:, k, b * 512:(b + 1) * 512],
                                start=(k == 0), stop=(k == 7)) for k in range(8)], reads=[wt] + hT_tok[4 * b:4 * b + 4], writes=[btok])
                        sg_i += 1
                        sg = Ef[sg_i % 3]
                        P.op("act", lambda e, sg=sg, bg=bg: e.activation(out=sg.ap, in_=bg[:, :], func=AF.Sigmoid),
                             reads=[bgtok], writes=[sg])
                        P.op("dve", lambda e, sg=sg, ba=ba, u=u, b=b: e.tensor_tensor(
                            out=u.ap[:, 15 + b * 512:15 + (b + 1) * 512], in0=ba[:, :], in1=sg.ap, op=ALU.mult),
                            reads=[batok, sg], writes=[u])
                    dg = dgs[c % 2]
                    P.op("dve", lambda e, dg=dg, c=c: e.tensor_tensor(
                        out=dg.ap.rearrange("p (t j) -> p t j", t=31), in0=bc(ident.ap[:, 0:1], [[0, 31], [1, 128]]),
                        in1=bc(dww.ap[:, c * 31:c * 31 + 1], [[1, 31], [0, 128]]), op=ALU.mult), reads=[ident, dww], writes=[dg])
                    for b in range(4):
                        bt, _, btok = nextbank([0, 1, 2, 3])
                        P.opn("pe", [lambda e, tap=tap, dg=dg, u=u, b=b, bt=bt: e.matmul(
                            bt[:, :], lhsT=dg.ap[:, tap * 128:(tap + 1) * 128], rhs=u.ap[:, tap + b * 512:tap + b * 512 + 512],
                            start=(tap == 0), stop=(tap == 30)) for tap in range(31)], reads=[dg, u], writes=[btok])
                        P.op("act", lambda e, c=c, b=b, bt=bt: e.activation(out=cTv[:, c, b * 512:(b + 1) * 512], in_=bt[:, :],
                                                                            func=AF.Identity, bias=vec(DWB, c)),
                             reads=[btok, vecs], writes=[cT_tok[c][b]])
            P.barrier()
            for b in range(4):
                bs_, _, bstok = nextbank([0, 1, 2, 3])
                bq_, _, bqtok = nextbank([0, 1, 2, 3])
                blk = slice(b * 512, (b + 1) * 512)
                for c in range(8):
                    sq = lnq[c % 2]
                    P.op("act", lambda e, sq=sq, c=c, blk=blk: e.activation(out=sq.ap, in_=cTv[:, c, blk], func=AF.Square),
                         reads=[cT_tok[c][b]], writes=[sq])
                    P.op("pe", lambda e, c=c, blk=blk, bs_=bs_: e.matmul(bs_[:, :], lhsT=ones16.ap, rhs=cTv[:, c, blk],
                                                                       start=(c == 0), stop=(c == 7)),
                         reads=[ones16, cT_tok[c][b]], writes=[bstok])
                    P.op("pe", lambda e, c=c, sq=sq, bq_=bq_: e.matmul(bq_[:, :], lhsT=ones16.ap, rhs=sq.ap,
                                                                     start=(c == 0), stop=(c == 7)),
                         reads=[ones16, sq], writes=[bqtok])
                P.op("act", lambda e, bs_=bs_: e.copy(out=lnm.ap, in_=bs_[:, :]), reads=[bstok], writes=[lnm])
                P.op("dve", lambda e: e.tensor_tensor(out=lnr.ap, in0=lnm.ap, in1=lnm.ap, op=ALU.mult), reads=[lnm], writes=[lnr])
                P.op("dve", lambda e, bq_=bq_: e.tensor_tensor(out=lnr.ap, in0=bq_[:, :], in1=lnr.ap, op=ALU.subtract),
                     reads=[bqtok, lnr], writes=[lnr])
                P.op("act", lambda e: e.activation(out=lnr.ap, in_=lnr.ap, func=AF.Sqrt, bias=EPS), reads=[lnr], writes=[lnr])
                P.op("dve", lambda e: e.reciprocal(out=lnr.ap, in_=lnr.ap), reads=[lnr], writes=[lnr])
                for c in range(8):
                    t1 = lnt[c % 2]
                    P.op("dve", lambda e, t1=t1, c=c, blk=blk: e.tensor_tensor(out=t1.ap, in0=cTv[:, c, blk], in1=lnm.ap, op=ALU.subtract),
                         reads=[cT_tok[c][b], lnm], writes=[t1])
                    P.op("dve", lambda e, t1=t1: e.tensor_tensor(out=t1.ap, in0=t1.ap, in1=lnr.ap, op=ALU.mult),
                         reads=[t1, lnr], writes=[t1])
                    P.op("act", lambda e, t1=t1, c=c, blk=blk: e.activation(out=cTv[:, c, blk], in_=t1.ap, func=AF.Silu,
                                                                           scale=vec(LNG, c), bias=vec(LNB, c)),
                         reads=[t1, vecs], writes=[cT_tok[c][b]])
            chk('S4')
            P.barrier()

            sg_i = 0
            for c in range(8):
                w1, w1v = load_w(wpw_d, c * 128, 128, half=True)
                w2, w2v = load_w(wo_d, c * 128, 128, half=True)
                w3, w3v = load_w(win_d, 3584 + c * 128, 128, half=True)
                w4, w4v = load_w(win_d, 4608 + c * 128, 128, half=True)
                for b in range(4):
                    blk = slice(b * 512, (b + 1) * 512)
                    bks = [nextbank([0, 1, 2, 3]) for _ in range(4)]
                    srcs = ((w1, w1v, cTv, [cT_tok[k][b] for k in range(8)]),
                            (w2, w2v, QTv, QT_tok[4 * b:4 * b + 4]),
                            (w3, w3v, hTv, hT_tok[4 * b:4 * b + 4]),
                            (w4, w4v, hTv, hT_tok[4 * b:4 * b + 4]))
                    for (wt, wtv, src, stoks), (bt, _, btok) in zip(srcs, bks):
                        P.opn("pe", [lambda e, k=k, wtv=wtv, src=src, bt=bt, blk=blk: e.matmul(
                            bt[:, :], lhsT=wtv[:, k, 0:128], rhs=src[:, k, blk], start=(k == 0), stop=(k == 7)) for k in range(8)],
                            reads=[wt] + list(stoks), writes=[btok])
                    sa = Ef[0]
                    sb_ = Ef[1]
                    m1 = Ef[2]
                    P.op("act", lambda e, bt=bks[2][0]: e.activation(out=sa.ap, in_=bt[:, :], func=AF.Sigmoid),
                         reads=[bks[2][2]], writes=[sa])
                    P.op("act", lambda e, bt=bks[3][0]: e.activation(out=sb_.ap, in_=bt[:, :], func=AF.Sigmoid),
                         reads=[bks[3][2]], writes=[sb_])
                    P.op("dve", lambda e, bt=bks[0][0]: e.tensor_tensor(out=m1.ap, in0=bt[:, :], in1=sa.ap, op=ALU.mult),
                         reads=[bks[0][2], sa], writes=[m1])
                    P.op("dve", lambda e, bt=bks[1][0]: e.tensor_tensor(out=sb_.ap, in0=bt[:, :], in1=sb_.ap, op=ALU.mult),
                         reads=[bks[1][2], sb_], writes=[sb_])
                    P.op("pool", lambda e, c=c, blk=blk: e.tensor_tensor(out=mTv[:, c, blk], in0=m1.ap, in1=sb_.ap, op=ALU.add),
                         reads=[m1, sb_], writes=[mT_tok[b]])

            chk('S5')
            P.barrier()
            wo4 = [load_w(wout_d, q4 * 256, 256) for q4 in range(4)]
            for tt in range(16):
                xt = xts[tt % 2]
                gt = (tb0 // 128) + tt
                P.dma("sp", xt.ap, x_d[tb0 + tt * 128:tb0 + (tt + 1) * 128, :], writes=[xt])
                for half in range(2):
                    bt, _, btok = nextbank([0, 1, 2, 3])
                    for q2 in range(2):
                        wb, wbv = wo4[half * 2 + q2]
                        P.opn("pe", [lambda e, k=k, tt=tt, q2=q2, wbv=wbv, bt=bt: e.matmul(
                            bt[:, q2 * 256:(q2 + 1) * 256], lhsT=mTv[:, k, tt * 128:(tt + 1) * 128], rhs=wbv[:, k, :],
                            start=(k == 0), stop=(k == 7)) for k in range(8)], reads=[wb, mT_tok[tt // 4]], writes=[btok])
                    P.op("dve", lambda e, half=half, bt=bt, xt=xt: e.tensor_tensor(
                        out=xt.ap[:, half * 512:(half + 1) * 512], in0=bt[:, :], in1=xt.ap[:, half * 512:(half + 1) * 512],
                        op=ALU.add), reads=[btok, xt], writes=[xt])
                P.dma("act", out_d[gt * 128:(gt + 1) * 128, :], xt.ap, reads=[xt], writes=[out_tok[gt]], key=xt)
            chk('S6')
            P.barrier()
        except _Stop:
            pass

        if not stop_after_a:
            A.off = mark0
            build_peer(nc, P, A, banks, nextbank, dict(
                ident=ident, identf=identf, vecs=vecs, vec=vec, G2=G2, small=small, new_small=new_small, small_cur=small_cur,
                out_tok=out_tok, out_d=out_d, fg_d=fg_d, skT_d=skT_d, pu_d=pu_d, pv_d=pv_d, wq_d=wq_d,
                ut_d=ut_d, vb_d=vb_d, wqb_d=wqb_d))
        P.emit()
    return nc


def build_peer(nc, P, A, banks, nextbank, C):
    ident, identf, vecs, vec, G2 = C["ident"], C["identf"], C["vecs"], C["vec"], C["G2"]
    small, new_small, out_tok, out_d = C["small"], C["new_small"], C["out_tok"], C["out_d"]
    small_cur = C["small_cur"]
    ut_d, vb_d, wqb_d = C["ut_d"], C["vb_d"], C["wqb_d"]
    NBLK = TOK // TB
    NT = TB // 128
    mark = A.off
    NPB = 4
    pst = [A.f32(1024) for _ in range(NPB)]
    pst2 = [A.f32(1024) for _ in range(NPB)]
    pbf = [A.b16(1024) for _ in range(NPB)]
    pbf2 = [A.b16(1024) for _ in range(NPB)]
    utb = [A.b16(1024) for _ in range(2)]
    assert A.off - mark <= 128 * TB * 2
    A.off = mark
    G = A.b16(128 * TB)
    Gv = G.ap.rearrange("p (i t) -> p i t", i=128)
    h2T = [A.b16(8 * TB) for _ in range(2)]
    h2T_toks = [[Tok() for _ in range(NT)] for _ in range(2)]
    qT = A.f32(16 * TB)
    qTv = r3(qT.ap, a=16)
    sc = [A.f32(2048)] * 2
    sc2 = A.f32(256)
    v16 = A.f32(256)
    v16v = r3(v16.ap, a=16)
    ix = A.u32(256)
    ixv = r3(ix.ap, a=16)
    ixf = A.f32(256)
    cand = sc[0]
    eqb = A.f32(2048)
    eq2 = cand
    ts = A.f32(128)
    tsv = r3(ts.ap, a=8)
    pos = A.u32(128)
    posv = r3(pos.ap, a=8)
    posf = A.f32(128)
    k1f = A.f32(128)
    k2f = A.f32(128)
    Ivs = [A.f32(128) for _ in range(2)]
    Jvs = [A.f32(128) for _ in range(2)]
    Wvs = [A.f32(128) for _ in range(2)]
    ew = A.f32(128)
    zz = A.f32(16)
    SM = A.f32(3 * TB)
    SMv = r3(SM.ap, a=3)
    iota128 = A.b16(128)
    iotaf = A.f32(128)
    skT = A.f32(256)
    skTv = r3(skT.ap, a=2)
    fg = A.f32(1024)
    ohB = [A.b16(16 * 128) for _ in range(2)]
    ohE = [A.b16(16 * 128) for _ in range(2)]
    OAI = A.b16(TB * 8)
    OAJ = A.b16(TB * 8)
    OBI = A.b16(TB * 16)
    OBJ = A.b16(TB * 16)
    xh = A.f32(TB)
    xl = A.f32(TB)
    ubr = [A.b16(1024) for _ in range(3)]
    vbr = [A.b16(1024) for _ in range(3)]
    gelr = [A.f32(TB) for _ in range(3)]
    ATr = [A.b16(TB) for _ in range(4)]
    xts = [A.f32(1024) for _ in range(2)]
    xns = [A.b16(1024) for _ in range(2)]
    wqc = [A.b16(1024) for _ in range(3)]
    ut_tok = [Tok() for _ in range(128)]
    vb_tok = [Tok() for _ in range(128)]
    wqb_tok = [Tok() for _ in range(16)]

    P.dma("sp", skT.ap, C["skT_d"], writes=[skT])
    P.dma("sp", fg.ap, C["fg_d"], writes=[fg])
    P.op("pool", lambda e: e.iota(iotaf.ap, [[1, 128]], base=0, channel_multiplier=0, allow_small_or_imprecise_dtypes=True),
         writes=[iotaf])
    P.op("dve", lambda e: e.tensor_copy(out=iota128.ap, in_=iotaf.ap), reads=[iotaf], writes=[iota128])
    k16 = A.f32(16)
    nhalf = A.f32(1)
    P.op("pool", lambda e: e.memset(nhalf.ap, -0.5), writes=[nhalf])
    P.op("dve", lambda e: e.tensor_scalar(out=k16.ap, in0=iotaf.ap[:, 0:16], scalar1=16.0, scalar2=None, op0=ALU.mult),
         reads=[iotaf], writes=[k16])

    for m in range(16):
        st = pst[m % NPB]
        pb = pbf[m % NPB]
        P.dma("sp", r3(st.ap, a=8), C["wq_d"][:, :, m * 128:(m + 1) * 128], writes=[st])
        P.op("pool", lambda e, st=st, pb=pb: e.tensor_copy(out=pb.ap, in_=st.ap), reads=[st], writes=[pb])
        P.dma("act", wqb_d[m], pb.ap, reads=[pb], writes=[wqb_tok[m]], key=pb)
    def prep_uv(i):
        st = pst[i % NPB]
        pb = pbf[i % NPB]
        P.dma("sp", st.ap, C["pu_d"][i * 128:(i + 1) * 128, :], writes=[st])
        P.op("pool", lambda e, st=st, pb=pb: e.tensor_copy(out=pb.ap, in_=st.ap), reads=[st], writes=[pb])
        bt, bt16, btok = nextbank([4, 5, 6, 7])
        for k in range(8):
            P.op("pe", lambda e, k=k, pb=pb, bt16=bt16: e.transpose(out=bt16[:, k * 128:(k + 1) * 128], in_=pb.ap[:, k * 128:(k + 1) * 128],
                                                             identity=ident.ap), reads=[pb, ident], writes=[btok])
        ut = utb[i % 2]
        P.op("act", lambda e, ut=ut, bt16=bt16: e.copy(out=ut.ap, in_=bt16[:, 0:1024]), reads=[btok], writes=[ut])
        P.dma("act", ut_d[i], ut.ap, reads=[ut], writes=[ut_tok[i]], key=ut)
        st2 = pst2[i % NPB]
        pb2 = pbf2[i % NPB]
        P.dma("pool", st2.ap, C["pv_d"][i * 128:(i + 1) * 128, :], writes=[st2])
        P.op("dve", lambda e, st2=st2, pb2=pb2: e.tensor_copy(out=pb2.ap, in_=st2.ap), reads=[st2], writes=[pb2])
        P.dma("act", vb_d[i * 128:(i + 1) * 128, :], pb2.ap, reads=[pb2], writes=[vb_tok[i]], key=pb2)

    cnt = {"oh": 0, "ev": 0}

    def routing(nb):
        h2Tv = r3(h2T[nb % 2].ap, a=8)
        h2T_tok = h2T_toks[nb % 2]
        for tt in range(NT):
            gt = nb * NT + tt
            xt = xts[tt]
            xn = xns[tt]
            P.dma("sp", xt.ap, out_d[gt * 128:(gt + 1) * 128, :], reads=[out_tok[gt]], writes=[xt])
            ss, rstd = new_small()
            smt = small_cur["tok"]
            P.op("dve", lambda e, xt=xt, ss=ss: e.scalar_tensor_tensor(out=eqb.ap[:, 0:1024], in0=xt.ap, scalar=1.0, in1=xt.ap,
                                                                       op0=ALU.mult, op1=ALU.mult, accum_out=ss),
                 reads=[xt], writes=[eqb, smt])
            P.op("pool", lambda e, ss=ss: e.tensor_scalar(out=ss, in0=ss, scalar1=1.0 / D, scalar2=EPS, op0=ALU.mult, op1=ALU.add),
                 reads=[smt], writes=[smt])
            P.op("pool", lambda e, ss=ss, rstd=rstd: e.tensor_tensor(out=rstd, in0=ss, in1=nhalf.ap, op=ALU.pow),
                 reads=[smt, nhalf], writes=[smt])
            P.op("dve", lambda e, xt=xt, xn=xn, rstd=rstd: e.tensor_scalar(out=xn.ap, in0=xt.ap, scalar1=rstd, scalar2=None, op0=ALU.mult),
                 reads=[xt, smt], writes=[xn])
            bt, bt16, btok = nextbank([7])
            for k in range(8):
                P.op("pe", lambda e, k=k, xn=xn, bt16=bt16: e.transpose(out=bt16[:, k * 128:(k + 1) * 128], in_=xn.ap[:, k * 128:(k + 1) * 128],
                                                                 identity=ident.ap), reads=[xn, ident], writes=[btok])
            for k in range(8):
                P.op("dve", lambda e, k=k, tt=tt, bt16=bt16: e.tensor_scalar(
                    out=h2Tv[:, k, tt * 128:(tt + 1) * 128], in0=bt16[:, k * 128:(k + 1) * 128], scalar1=vec(G2, k), scalar2=None,
                    op0=ALU.mult), reads=[btok, vecs], writes=[h2T_tok[tt]])
        for m in range(16):
            wq = wqc[m % 3]
            P.dma("sp", wq.ap, wqb_d[m], reads=[wqb_tok[m]], writes=[wq])
            bt, _, btok = nextbank([7])
            for k in range(8):
                P.op("pe", lambda e, k=k, wq=wq, bt=bt: e.matmul(bt[:, 0:TB], lhsT=r3(wq.ap, a=8)[:, k, :], rhs=h2Tv[:, k, :],
                                                              start=(k == 0), stop=(k == 7)), reads=[wq] + h2T_tok, writes=[btok])
            cnt["ev"] += 1
            if cnt["ev"] % 2:
                P.op("act", lambda e, m=m, bt=bt: e.copy(out=qTv[:, m, :], in_=bt[:, 0:TB]), reads=[btok], writes=[qT])
            else:
                P.op("dve", lambda e, m=m, bt=bt: e.tensor_copy(out=qTv[:, m, :], in_=bt[:, 0:TB]), reads=[btok], writes=[qT])
        for tt in range(NT):
            Iv, Jv, Wv = Ivs[tt], Jvs[tt], Wvs[tt]
            scb = sc[tt]
            for bq in range(4):
                bt, _, btok = nextbank([7])
                for mm in range(4):
                    m = bq * 4 + mm
                    P.op("pe", lambda e, mm=mm, m=m, tt=tt, bt=bt: e.matmul(
                        bt[:, mm * 128:(mm + 1) * 128], lhsT=qTv[:, m, tt * 128:(tt + 1) * 128], rhs=skTv[:, m % 2, :],
                        start=True, stop=True), reads=[qT, skT], writes=[btok])
                P.op("act", lambda e, bq=bq, bt=bt, scb=scb: e.copy(out=scb.ap[:, bq * 512:(bq + 1) * 512], in_=bt[:, :]),
                     reads=[btok], writes=[scb])
            for m in range(16):
                src = scb.ap[:, m * 128:(m + 1) * 128]
                P.op("dve", lambda e, m=m, src=src: e.max(out=v16v[:, m, 0:8], in_=src), reads=[scb], writes=[v16])
                P.op("dve", lambda e, m=m, src=src: e.max_index(out=ixv[:, m, 0:8], in_max=v16v[:, m, 0:8], in_values=src),
                     reads=[scb, v16], writes=[ix])
                P.op("dve", lambda e, m=m, src=src: e.match_replace(out=sc2.ap[:, 0:128], in_to_replace=v16v[:, m, 0:8], in_values=src,
                                                                    imm_value=-1e30), reads=[scb, v16], writes=[sc2])
                P.op("dve", lambda e, m=m: e.max(out=v16v[:, m, 8:16], in_=sc2.ap[:, 0:128]), reads=[sc2], writes=[v16])
                P.op("dve", lambda e, m=m: e.max_index(out=ixv[:, m, 8:16], in_max=v16v[:, m, 8:16], in_values=sc2.ap[:, 0:128]),
                     reads=[sc2, v16], writes=[ix])
            P.op("dve", lambda e: e.tensor_copy(out=ixf.ap, in_=ix.ap), reads=[ix], writes=[ixf])
            c4 = lambda b: b.ap.rearrange("p (h a b) -> p h a b", h=8, a=16)
            P.op("dve", lambda e: e.tensor_tensor(out=c4(cand), in0=bc(v16.ap[:, 0:1], [[32, 8], [1, 16], [0, 16]]),
                                                  in1=bc(v16.ap[:, 16:17], [[32, 8], [0, 16], [1, 16]]), op=ALU.add),
                 reads=[v16], writes=[cand])
            for h in range(8):
                src = cand.ap[:, h * 256:(h + 1) * 256]
                P.op("dve", lambda e, h=h, src=src: e.max(out=tsv[:, h, 0:8], in_=src), reads=[cand], writes=[ts])
                P.op("dve", lambda e, h=h, src=src: e.max_index(out=posv[:, h, 0:8], in_max=tsv[:, h, 0:8], in_values=src),
                     reads=[cand, ts], writes=[pos])
                P.op("dve", lambda e, h=h, src=src: e.match_replace(out=sc2.ap, in_to_replace=tsv[:, h, 0:8], in_values=src,
                                                                    imm_value=-1e30), reads=[cand, ts], writes=[sc2])
                P.op("dve", lambda e, h=h: e.max(out=tsv[:, h, 8:16], in_=sc2.ap), reads=[sc2], writes=[ts])
                P.op("dve", lambda e, h=h: e.max_index(out=posv[:, h, 8:16], in_max=tsv[:, h, 8:16], in_values=sc2.ap),
                     reads=[sc2, ts], writes=[pos])
            P.op("dve", lambda e: e.tensor_tensor(out=r3(ew.ap, a=8), in0=tsv, in1=bc(ts.ap[:, 0:1], [[16, 8], [0, 16]]), op=ALU.subtract),
                 reads=[ts], writes=[ew])
            P.op("act", lambda e: e.activation(out=ew.ap, in_=ew.ap, func=AF.Exp), reads=[ew], writes=[ew])
            P.op("dve", lambda e: e.tensor_reduce(out=zz.ap[:, 0:8], in_=r3(ew.ap, a=8), axis=AX.X, op=ALU.add), reads=[ew], writes=[zz])
            P.op("dve", lambda e: e.reciprocal(out=zz.ap[:, 8:16], in_=zz.ap[:, 0:8]), reads=[zz], writes=[zz])
            P.op("dve", lambda e, Wv=Wv: e.tensor_tensor(out=r3(Wv.ap, a=8), in0=r3(ew.ap, a=8), in1=bc(zz.ap[:, 8:9], [[1, 8], [0, 16]]), op=ALU.mult),
                 reads=[ew, zz], writes=[Wv])
            P.op("dve", lambda e: e.tensor_copy(out=posf.ap, in_=pos.ap), reads=[pos], writes=[posf])
            P.op("dve", lambda e: e.tensor_tensor(out=c4(eqb), in0=bc(posf.ap[:, 0:1], [[16, 8], [1, 16], [0, 16]]),
                                                  in1=bc(k16.ap[:, 0:1], [[0, 8], [0, 16], [1, 16]]), op=ALU.subtract),
                 reads=[k16, posf], writes=[eqb])
            P.op("dve", lambda e: e.tensor_scalar(out=eq2.ap, in0=eqb.ap, scalar1=0.0, scalar2=None, op0=ALU.is_ge),
                 reads=[eqb], writes=[eq2])
            P.op("dve", lambda e: e.scalar_tensor_tensor(out=eqb.ap, in0=eqb.ap, scalar=16.0, in1=eq2.ap, op0=ALU.is_lt, op1=ALU.mult),
                 reads=[eqb, eq2], writes=[eqb])
            P.op("dve", lambda e: e.tensor_tensor(out=c4(eq2), in0=c4(eqb), in1=bc(iotaf.ap[:, 0:1], [[0, 8], [0, 16], [1, 16]]), op=ALU.mult),
                 reads=[eqb, iotaf], writes=[eq2])
            P.op("dve", lambda e: e.tensor_reduce(out=k1f.ap, in_=c4(eq2), axis=AX.X, op=ALU.add), reads=[eq2], writes=[k1f])
            P.op("dve", lambda e: e.tensor_tensor(out=c4(eq2), in0=c4(eqb), in1=bc(ixf.ap[:, 0:1], [[32, 8], [0, 16], [1, 16]]), op=ALU.mult),
                 reads=[eqb, ixf], writes=[eq2])
            P.op("dve", lambda e, Iv=Iv: e.tensor_reduce(out=Iv.ap, in_=c4(eq2), axis=AX.X, op=ALU.add), reads=[eq2], writes=[Iv])
            P.op("dve", lambda e: e.scalar_tensor_tensor(out=k2f.ap, in0=k1f.ap, scalar=-16.0, in1=posf.ap, op0=ALU.mult, op1=ALU.add),
                 reads=[k1f, posf], writes=[k2f])
            P.op("dve", lambda e: e.tensor_tensor(out=c4(eqb), in0=bc(k2f.ap[:, 0:1], [[16, 8], [1, 16], [0, 16]]),
                                                  in1=bc(iotaf.ap[:, 0:1], [[0, 8], [0, 16], [1, 16]]), op=ALU.is_equal),
                 reads=[k2f, iotaf], writes=[eqb])
            P.op("dve", lambda e: e.tensor_tensor(out=c4(eq2), in0=c4(eqb), in1=bc(ixf.ap[:, 16:17], [[32, 8], [0, 16], [1, 16]]), op=ALU.mult),
                 reads=[eqb, ixf], writes=[eq2])
            P.op("dve", lambda e, Jv=Jv: e.tensor_reduce(out=Jv.ap, in_=c4(eq2), axis=AX.X, op=ALU.add), reads=[eq2], writes=[Jv])
        for tt in range(NT):
            Iv, Jv, Wv = Ivs[tt], Jvs[tt], Wvs[tt]
            bt, _, btok = nextbank([7])
            for qi, srcb in enumerate((Iv, Jv, Wv)):
                P.op("pe", lambda e, qi=qi, srcb=srcb, bt=bt: e.transpose(out=bt[:, qi * 128:(qi + 1) * 128], in_=srcb.ap, identity=identf.ap),
                     reads=[srcb, identf], writes=[btok])
            P.op("act", lambda e, tt=tt, bt=bt: e.copy(out=SMv[:, :, tt * 128:(tt + 1) * 128], in_=r3(bt[:, 0:384], a=3)),
                 reads=[btok], writes=[SM])
        t3 = lambda b, n: b.ap[:, 0:TB * n].rearrange("p (t a) -> p t a", a=n)
        for q, OA, OB in ((0, OAI, OBI), (1, OAJ, OBJ)):
            P.op("dve", lambda e, q=q: e.tensor_tensor(out=t3(eqb, 8), in0=bc(SMv[:, q, 0:1], [[1, TB], [0, 8]]),
                                                       in1=bc(k16.ap[:, 0:1], [[0, TB], [1, 8]]), op=ALU.subtract),
                 reads=[SM, k16], writes=[eqb])
            P.op("dve", lambda e: e.tensor_scalar(out=cand.ap, in0=eqb.ap, scalar1=0.0, scalar2=None, op0=ALU.is_ge),
                 reads=[eqb], writes=[cand])
            P.op("dve", lambda e, OA=OA: e.scalar_tensor_tensor(out=OA.ap, in0=eqb.ap, scalar=16.0, in1=cand.ap, op0=ALU.is_lt, op1=ALU.mult),
                 reads=[eqb, cand], writes=[OA])
            P.op("dve", lambda e, OA=OA: e.tensor_tensor(out=t3(eqb, 8), in0=t3(OA, 8), in1=bc(iotaf.ap[:, 0:1], [[0, TB], [1, 8]]), op=ALU.mult),
                 reads=[OA, iotaf], writes=[eqb])
            P.op("dve", lambda e: e.tensor_reduce(out=xh.ap, in_=t3(eqb, 8), axis=AX.X, op=ALU.add), reads=[eqb], writes=[xh])
            P.op("dve", lambda e, q=q: e.scalar_tensor_tensor(out=xl.ap, in0=xh.ap, scalar=-16.0, in1=SMv[:, q, :], op0=ALU.mult, op1=ALU.add),
                 reads=[xh, SM], writes=[xl])
            P.op("dve", lambda e, OB=OB: e.tensor_tensor(out=t3(OB, 16), in0=bc(xl.ap[:, 0:1], [[1, TB], [0, 16]]),
                                                         in1=bc(iotaf.ap[:, 0:1], [[0, TB], [1, 16]]), op=ALU.is_equal),
                 reads=[xl, iotaf], writes=[OB])
        P.op("dve", lambda e: e.tensor_tensor(out=t3(OAJ, 8), in0=t3(OAJ, 8), in1=bc(SMv[:, 2, 0:1], [[1, TB], [0, 8]]), op=ALU.mult),
             reads=[OAJ, SM], writes=[OAJ])

    def b5(nb):
        TG = 16
        for tg in range(TB // TG):
            t0 = tg * TG
            cnt["oh"] += 1
            ob, oc = ohB[cnt["oh"] % 2], ohE[cnt["oh"] % 2]
            o3 = lambda b: b.ap.rearrange("p (t i) -> p t i", t=TG)
            o4 = lambda b: b.ap.rearrange("p (t a c) -> p t a c", t=TG, a=8)
            P.op("dve", lambda e, oc=oc, t0=t0: e.tensor_tensor(
                out=o4(oc), in0=bc(OAI.ap[:, t0 * 8:t0 * 8 + 1], [[8, TG], [1, 8], [0, 16]]),
                in1=bc(OBI.ap[:, t0 * 16:t0 * 16 + 1], [[16, TG], [0, 8], [1, 16]]), op=ALU.mult), reads=[OAI, OBI], writes=[oc])
            P.op("pool", lambda e, ob=ob, t0=t0: e.tensor_tensor(
                out=o4(ob), in0=bc(OAJ.ap[:, t0 * 8:t0 * 8 + 1], [[8, TG], [1, 8], [0, 16]]),
                in1=bc(OBJ.ap[:, t0 * 16:t0 * 16 + 1], [[16, TG], [0, 8], [1, 16]]), op=ALU.mult), reads=[OAJ, OBJ], writes=[ob])
            for t4 in range(TG // 4):
                bt, _, btok = nextbank([6, 7])
                P.opn("pe", [lambda e, q=q, t4=t4, oc=oc, ob=ob, bt=bt: e.matmul(
                    bt[:, q * 128:(q + 1) * 128], lhsT=o3(ob)[:, t4 * 4 + q, :], rhs=o3(oc)[:, t4 * 4 + q, :], start=True, stop=True)
                    for q in range(4)], reads=[oc, ob], writes=[btok])
                ta = t0 + t4 * 4
                P.op("act", lambda e, ta=ta, bt=bt: e.copy(out=Gv[:, :, ta:ta + 4], in_=bc(bt[:, 0:1], [[1, 128], [128, 4]])),
                     reads=[btok], writes=[G])
    def b6(nb, pend):
        h2Tv = r3(h2T[nb % 2].ap, a=8)
        h2T_tok = h2T_toks[nb % 2]
        def u_side(i):
            ub = ubr[i % 3]
            vb = vbr[i % 3]
            P.dma("sp", ub.ap, ut_d[i], reads=[ut_tok[i]], writes=[ub])
            P.dma("sp", vb.ap, vb_d[i * 128:(i + 1) * 128, :], reads=[vb_tok[i]], writes=[vb])
            bs_, _, bstok = banks[4 + i % 3]
            P.opn("pe", [lambda e, k=k, ub=ub, bs_=bs_: e.matmul(bs_[:, 0:TB], lhsT=r3(ub.ap, a=8)[:, k, :], rhs=h2Tv[:, k, :],
                                                             start=(k == 0), stop=(k == 7)) for k in range(8)],
                  reads=[ub] + h2T_tok, writes=[bstok])
            return bs_, bstok, vb

        per = (len(pend) + 119) // 120 if pend else 0
        uq = [u_side(0), u_side(1)]
        for i in range(128):
            bs_, bstok, vb = uq.pop(0)
            if i + 2 < 128:
                uq.append(u_side(i + 2))
            gel = gelr[i % 3]
            P.op("act", lambda e, gel=gel, bs_=bs_: e.activation(out=gel.ap, in_=bs_[:, 0:TB], func=AF.Gelu), reads=[bstok], writes=[gel])
            at = ATr[i % 4]
            P.op("pool", lambda e, gel=gel, at=at, i=i: e.tensor_tensor(out=at.ap, in0=gel.ap, in1=Gv[:, i, :], op=ALU.mult),
                 reads=[gel, G], writes=[at])
            P.opn("pe", [lambda e, tt=tt, half=half, at=at, vb=vb, i=i: e.matmul(
                banks[tt * 2 + half][0][:, :], lhsT=at.ap[:, tt * 128:(tt + 1) * 128], rhs=vb.ap[:, half * 512:(half + 1) * 512],
                start=(i == 0), stop=(i == 127)) for tt in range(NT) for half in range(2)],
                reads=[at, vb], writes=[banks[b4][2] for b4 in range(2 * NT)])
            if pend:
                P.replay(pend, per)
        P.replay(pend, len(pend))

    def b7(nb):
        for tt in range(NT):
            gt = nb * NT + tt
            xt = xts[tt]
            P.dma("sp", xt.ap, out_d[gt * 128:(gt + 1) * 128, :], reads=[out_tok[gt]], writes=[xt])
            for half in range(2):
                ab, _, abtok = banks[tt * 2 + half]
                P.op("dve", lambda e, half=half, ab=ab, xt=xt: e.tensor_tensor(
                    out=xt.ap[:, half * 512:(half + 1) * 512], in0=ab[:, :], in1=xt.ap[:, half * 512:(half + 1) * 512], op=ALU.add),
                    reads=[abtok, xt], writes=[xt])
            ss, rstd = new_small()
            smt = small_cur["tok"]
            P.op("dve", lambda e, xt=xt, ss=ss: e.scalar_tensor_tensor(out=eqb.ap[:, 0:1024], in0=xt.ap, scalar=1.0, in1=xt.ap,
                                                                       op0=ALU.mult, op1=ALU.mult, accum_out=ss),
                 reads=[xt], writes=[eqb, smt])
            P.op("pool", lambda e, ss=ss: e.tensor_scalar(out=ss, in0=ss, scalar1=1.0 / D, scalar2=EPS, op0=ALU.mult, op1=ALU.add),
                 reads=[smt], writes=[smt])
            P.op("pool", lambda e, ss=ss, rstd=rstd: e.tensor_tensor(out=rstd, in0=ss, in1=nhalf.ap, op=ALU.pow),
                 reads=[smt, nhalf], writes=[smt])
            P.op("dve", lambda e, xt=xt, rstd=rstd: e.scalar_tensor_tensor(out=xt.ap, in0=xt.ap, scalar=rstd, in1=fg.ap,
                                                                           op0=ALU.mult, op1=ALU.mult), reads=[xt, smt, fg], writes=[xt])
            P.dma("act", out_d[gt * 128:(gt + 1) * 128, :], xt.ap, reads=[xt], writes=[out_tok[gt]], key=xt)


    routing(0)
    for i in range(128):
        prep_uv(i)
    P.barrier()
    for nb in range(NBLK):
        b5(nb)
        pend = []
        if nb + 1 < NBLK:
            P.capture()
            routing(nb + 1)
            pend = P.end_capture()
        b6(nb, pend)
        b7(nb)


def prep_inputs(inputs):
    f = lambda a: np.ascontiguousarray(np.asarray(a, dtype=np.float32))
    rk = lambda w: f(w.reshape(8, 128, -1).transpose(1, 0, 2))
    pv = lambda v: v.reshape(8, 128).T
    x = f(inputs["x"])
    vecs = np.concatenate([pv(np.asarray(inputs[n])[0]) for n in
                           ("norm1_g", "conv_dw_b", "conv_ln_g", "conv_ln_b", "norm2_g")], axis=1)
    dww = np.asarray(inputs["conv_dw_w"])[0].reshape(31, 8, 128).transpose(2, 1, 0).reshape(128, 248)
    shared = {
        "win": rk(np.asarray(inputs["w_in"])[0]),
        "wpw": rk(np.asarray(inputs["conv_w_pw"])[0]),
        "wo": rk(np.asarray(inputs["attn_w_o"])[0]),
        "wout": rk(np.asarray(inputs["w_out"])[0]),
        "wq": rk(np.asarray(inputs["peer_w_query"])[0]),
        "vecs": f(vecs),
        "dww": f(dww),
        "sink": f(np.broadcast_to(np.asarray(inputs["attn_sink"])[0][None, :], (128, 16))),
        "fg": f(np.broadcast_to(np.asarray(inputs["final_g"])[None, :], (128, 1024))),
        "skT": f(np.asarray(inputs["peer_sub_keys"])[0].transpose(2, 0, 1).reshape(128, 256)),
        "pu": f(np.asarray(inputs["peer_u"])[0]),
        "pv": f(np.asarray(inputs["peer_v"])[0]),
    }
    xs = x.reshape(NCORES, TOK, D)
    return [dict(shared, x=np.ascontiguousarray(xs[c])) for c in range(NCORES)]


_NC_CACHE = {}


def kernel(**inputs):
    in_maps = prep_inputs(inputs)
    if "nc" not in _NC_CACHE:
        _NC_CACHE["nc"] = build_program()
    res = run_bass_kernel_spmd(_NC_CACHE["nc"], in_maps, core_ids=list(range(NCORES)))
    out = np.stack([np.asarray(r["out"]) for r in res.results], axis=0)
    return out.reshape(16, SEQ, D).astype(np.float32)
```

```python
import os
import numpy as np
from contextlib import ExitStack
import concourse.bass as bass
import concourse.mybir as mybir
from concourse.bass_utils import run_bass_kernel_spmd

F32 = mybir.dt.float32
BF16 = mybir.dt.bfloat16
U32 = mybir.dt.uint32
AF = mybir.ActivationFunctionType
ALU = mybir.AluOpType
AX = mybir.AxisListType

NCORES = 8
TOK = 4096
SEQ = 2048
D = 1024
EPS = 1e-6
TB = 256


class Tok:
    __slots__ = ("w", "r", "sem", "cnt", "name")

    def __init__(self, name=""):
        self.w = None
        self.r = {}
        self.sem = None
        self.cnt = 0
        self.name = name


class Buf:
    __slots__ = ("ap", "tok")

    def __init__(self, ap, name=""):
        self.ap = ap
        self.tok = Tok(name)


class Prog:
    ENG = ("pe", "dve", "act", "pool", "sp")

    def __init__(self, nc, es):
        self.nc = nc
        self.es = es
        self.ops = {e: [] for e in self.ENG}
        self.cnt = {e: 0 for e in self.ENG}
        self.sems = {}
        for e in self.ENG:
            self.sems["E_" + e] = es.enter_context(nc.semaphore("s_" + e))
        self.final = {}
        self.waited = {e: {} for e in self.ENG}
        self.ndma = 0

    def _collect(self, eng, reads, writes):
        need = {}

        def add(ev, raw):
            if ev is None:
                return
            key, val, src = ev
            if src == eng and eng == "pe":
                return
            if need.get(key, 0) < val:
                need[key] = val

        for t in reads:
            add(t.w, True)
        for t in writes:
            add(t.w, False)
            for key, (val, src) in t.r.items():
                add((key, val, src), False)
        waits = []
        wd = self.waited[eng]
        for key, val in need.items():
            if wd.get(key, 0) < val:
                wd[key] = val
                waits.append((key, val))
        return waits

    def _commit(self, ev, reads, writes):
        key, val, src = ev
        for t in reads:
            old = t.r.get(key)
            if old is None or old[0] < val:
                t.r[key] = (val, src)
        for t in writes:
            t.w = ev
            t.r = {}

    cap = None

    def capture(self):
        self.cap = []

    def end_capture(self):
        c, self.cap = self.cap, None
        return c

    def replay(self, lst, n):
        for _ in range(min(n, len(lst))):
            kind, args = lst.pop(0)
            getattr(self, kind)(*args)

    def op(self, eng, fn, reads=(), writes=()):
        if self.cap is not None:
            self.cap.append(("op", (eng, fn, list(reads), list(writes))))
            return
        reads = [b.tok if isinstance(b, Buf) else b for b in reads]
        writes = [b.tok if isinstance(b, Buf) else b for b in writes]
        waits = self._collect(eng, reads, writes)
        self.cnt[eng] += 1
        key = "E_" + eng
        ev = (key, self.cnt[eng], eng)
        self.final[key] = self.cnt[eng]
        self.ops[eng].append((waits, fn, key, 1))
        self._commit(ev, reads, writes)

    def opn(self, eng, fns, reads=(), writes=()):
        fns = list(fns)

        def run(e, fns=fns):
            last = None
            for f in fns:
                last = f(e)
            return last

        self.op(eng, run, reads, writes)

    def dma(self, q, out, in_, reads=(), writes=(), key=None):
        if self.cap is not None:
            self.cap.append(("dma", (q, out, in_, list(reads), list(writes), key)))
            return
        reads = [b.tok if isinstance(b, Buf) else b for b in reads]
        writes = [b.tok if isinstance(b, Buf) else b for b in writes]
        kt = key if key is not None else (writes[0] if writes else reads[0])
        if isinstance(kt, Buf):
            kt = kt.tok
        if kt.sem is None:
            self.ndma += 1
            kt.sem = "D_%d" % self.ndma
            self.sems[kt.sem] = self.es.enter_context(self.nc.semaphore("d%d" % self.ndma))
        waits = self._collect(q, reads, writes)
        kt.cnt += 16
        ev = (kt.sem, kt.cnt, None)
        self.final[kt.sem] = kt.cnt
        self.ops[q].append((waits, lambda e: e.dma_start(out=out, in_=in_), kt.sem, 16))
        self._commit(ev, reads, writes)

    def barrier(self):
        for e in self.ENG:
            waits = []
            wd = self.waited[e]
            for key, val in self.final.items():
                if key == "E_" + e:
                    continue
                if wd.get(key, 0) < val:
                    wd[key] = val
                    waits.append((key, val))
            if waits:
                self.ops[e].append((waits, None, None, 0))

    def emit(self):
        nc = self.nc
        self.barrier()
        engmap = {"pe": "tensor", "dve": "vector", "act": "scalar", "pool": "gpsimd", "sp": "sync"}
        with nc.Block() as block:
            for e in self.ENG:
                def body(engine, ops=self.ops[e], sems=self.sems):
                    for waits, fn, key, inc in ops:
                        for wk, wv in waits:
                            engine.wait_ge(sems[wk], wv)
                        if fn is not None:
                            fn(engine).then_inc(sems[key], inc)

                getattr(block, engmap[e])(body)


class Arena:
    def __init__(self, nc, nbytes):
        self.t32 = nc.alloc_sbuf_tensor("arena", [128, nbytes // 4], F32)
        self.t16 = self.t32.bitcast(BF16)
        self.tu = self.t32.bitcast(U32)
        self.off = 0
        self.cap = nbytes

    def alloc(self, nbytes):
        off = (self.off + 63) // 64 * 64
        self.off = off + nbytes
        assert self.off <= self.cap, (self.off, self.cap)
        return off

    def f32(self, n, name=""):
        o = self.alloc(n * 4)
        return Buf(self.t32[:, o // 4:o // 4 + n], name)

    def b16(self, n, name=""):
        o = self.alloc(n * 2)
        return Buf(self.t16[:, o // 2:o // 2 + n], name)

    def u32(self, n, name=""):
        o = self.alloc(n * 4)
        return Buf(self.tu[:, o // 4:o // 4 + n], name)


def bc(ap, dims):
    return bass.AP(ap.tensor, ap.offset, [list(ap.ap[0])] + [list(d) for d in dims])


def r3(ap, **kw):
    return ap.rearrange("p (a b) -> p a b", **kw)


KDBG = os.environ.get('KDBG', '')
SLOPES = [2.0 ** (-8.0 * (h + 1) / 16.0) for h in range(16)]


class _Stop(Exception):
    pass


def build_program(stop_after_a=False, stage=None):
    nc = bass.Bass("TRN2", target_bir_lowering=False)
    es = ExitStack()
    dt = lambda name, shape, dtype=F32, kind="ExternalInput": nc.dram_tensor(name, shape, dtype, kind=kind).ap()
    x_d = dt("x", [TOK, D])
    win_d = dt("win", [128, 8, 5632])
    wpw_d = dt("wpw", [128, 8, 1024])
    wo_d = dt("wo", [128, 8, 1024])
    wout_d = dt("wout", [128, 8, 1024])
    wq_d = dt("wq", [128, 8, 2048])
    vecs_d = dt("vecs", [128, 40])
    dww_d = dt("dww", [128, 248])
    sink_d = dt("sink", [128, 16])
    fg_d = dt("fg", [128, 1024])
    skT_d = dt("skT", [128, 256])
    pu_d = dt("pu", [16384, 1024])
    pv_d = dt("pv", [16384, 1024])
    out_d = dt("out", [TOK, D], F32, "ExternalOutput")
    ut_d = dt("ut_scr", [128, 128, 1024], BF16, "Internal")
    vb_d = dt("vb_scr", [16384, 1024], BF16, "Internal")
    wqb_d = dt("wqb_scr", [16, 128, 1024], BF16, "Internal")

    with es:
        P = Prog(nc, es)
        A = Arena(nc, 207 * 1024)
        banks = []
        psall = nc.alloc_psum_tensor("psall", [128, 4096], F32)
        psall16 = psall.bitcast(BF16)
        for i in range(8):
            banks.append((psall[:, i * 512:(i + 1) * 512], psall16[:, i * 1024:(i + 1) * 1024], Tok(f"bank{i}")))
        rr = {"i": 0}

        def nextbank(lst):
            rr["i"] += 1
            return banks[lst[rr["i"] % len(lst)]]

        ident = A.b16(128, "ident")
        identf = A.f32(128, "identf")
        ones16 = A.b16(128, "ones")
        vecs = A.f32(40, "vecs")
        dww = A.f32(248, "dww")
        esink = A.f32(16, "esink")
        small = A.f32(64, "small")
        out_tok = [Tok(f"out{i}") for i in range(TOK // 128)]

        P.dma("sp", vecs.ap, vecs_d, writes=[vecs])
        P.dma("sp", dww.ap, dww_d, writes=[dww])
        P.dma("sp", esink.ap, sink_d, writes=[esink])
        P.op("act", lambda e: e.activation(out=esink.ap, in_=esink.ap, func=AF.Exp), reads=[esink], writes=[esink])
        P.op("pool", lambda e: e.iota(identf.ap, [[1, 128]], base=0, channel_multiplier=-1,
                                      allow_small_or_imprecise_dtypes=True), writes=[identf])
        P.op("dve", lambda e: e.tensor_scalar(out=identf.ap, in0=identf.ap, scalar1=0.0, scalar2=None,
                                              op0=ALU.is_equal), reads=[identf], writes=[identf])
        P.op("dve", lambda e: e.tensor_copy(out=ident.ap, in_=identf.ap), reads=[identf], writes=[ident])
        P.op("pool", lambda e: e.memset(ones16.ap, 1.0 / 1024.0), writes=[ones16])
        G1, DWB, LNG, LNB, G2 = range(5)
        vec = lambda idx, k: vecs.ap[:, idx * 8 + k:idx * 8 + k + 1]

        mark0 = A.off
        Mtab = A.b16(3 * 16 * 128, "Mtab")
        Mv = Mtab.ap.rearrange("p (a h q) -> p a h q", a=3, h=16)
        hT = A.b16(8 * SEQ)
        hTv = r3(hT.ap, a=8)
        hT_tok = [Tok() for _ in range(16)]
        QT = A.b16(8 * SEQ)
        QTv = r3(QT.ap, a=8)
        QT_tok = [Tok() for _ in range(16)]
        cT = A.b16(8 * SEQ)
        cTv = r3(cT.ap, a=8)
        cT_tok = [[Tok() for _ in range(4)] for _ in range(8)]
        r4 = A.alloc(32768)
        KTv = r3(A.t16[:, r4 // 2:r4 // 2 + 4 * SEQ], a=4)
        KT_tok = Tok()
        Vo = r4 + 16384
        Vv = A.t16[:, Vo // 2:Vo // 2 + 16 * 4 * 65].rearrange("p (t g e) -> p t g e", t=16, g=4)
        V_tok = [Tok() for _ in range(16)]
        dgs = [Buf(A.t16[:, (r4 + i * 8192) // 2:(r4 + i * 8192) // 2 + 31 * 128]) for i in range(2)]
        Ub = [Buf(A.t16[:, (r4 + 16384 + i * 4224) // 2:(r4 + 16384 + i * 4224) // 2 + 2078]) for i in range(2)]
        mTv = r3(A.t16[:, r4 // 2:r4 // 2 + 8 * SEQ], a=8)
        mT_tok = [Tok() for _ in range(4)]
        wst = [A.f32(2048) for _ in range(2)]
        wbf = [A.b16(2048) for _ in range(4)]
        wst_h = [Buf(b.ap[:, h * 1024:(h + 1) * 1024]) for b in wst for h in range(2)]
        wbf_h = [Buf(b.ap[:, h * 1024:(h + 1) * 1024]) for b in wbf for h in range(2)]
        xts = [A.f32(1024) for _ in range(2)]
        xns = [A.b16(1024) for _ in range(2)]
        junk = A.b16(1024)
        w12 = A.alloc(12288)
        Ef = [Buf(A.t32[:, (w12 + i * 2048) // 4:(w12 + i * 2048) // 4 + 512]) for i in range(3)]
        PT = [Buf(A.t16[:, (w12 + 6144 + i * 1024) // 2:(w12 + 6144 + i * 1024) // 2 + 512]) for i in range(6)]
        lnm = Buf(A.t32[:, (w12) // 4:(w12) // 4 + 512])
        lnr = Buf(A.t32[:, (w12 + 2048) // 4:(w12 + 2048) // 4 + 512])
        lnt = [Buf(A.t32[:, (w12 + 4096 + i * 2048) // 4:(w12 + 4096 + i * 2048) // 4 + 512]) for i in range(2)]
        lnq = [Buf(A.t16[:, (w12 + 8192 + i * 1024) // 2:(w12 + 8192 + i * 1024) // 2 + 512]) for i in range(2)]
        wkd = Buf(A.t16[:, w12 // 2:w12 // 2 + 4096])
        wkdv = wkd.ap.rearrange("p (k g e) -> p k g e", k=8, g=4)
        AO = [A.b16(1024) for _ in range(2)]
        den = A.f32(16)
        rec = A.f32(16)
        ss_i = {"i": 0}
        small_cur = {"tok": None}
        wst_i = {"i": 0}
        wbf_i = {"i": 0}

        small_toks = [Tok() for _ in range(32)]

        def new_small():
            ss_i["i"] = (ss_i["i"] + 1) % 32
            i = ss_i["i"]
            small_cur["tok"] = small_toks[i]
            return small.ap[:, 2 * i:2 * i + 1], small.ap[:, 2 * i + 1:2 * i + 2]

        def load_w(src3, col0, ncols, scale_idx=None, eng="pool", half=False):
            wst_i["i"] += 1
            wbf_i["i"] += 1
            if half:
                st = wst_h[wst_i["i"] % 4]
                wb = wbf_h[wbf_i["i"] % 8]
            else:
                st = wst[wst_i["i"] % 2]
                wb = wbf[wbf_i["i"] % 4]
            stv = r3(st.ap[:, 0:8 * ncols], a=8)
            wbv = r3(wb.ap[:, 0:8 * ncols], a=8)
            P.dma("sp", stv, src3[:, :, col0:col0 + ncols], writes=[st])
            assert scale_idx is None
            P.op(eng, lambda e: e.tensor_copy(out=wb.ap[:, 0:8 * ncols], in_=st.ap[:, 0:8 * ncols]), reads=[st], writes=[wb])
            return wb, wbv

        def rms_tile(xt, xn):
            ss, rstd = new_small()
            sm = small_cur["tok"]
            P.op("act", lambda e: e.activation(out=junk.ap, in_=xt.ap, func=AF.Square, accum_out=ss),
                 reads=[xt], writes=[junk, sm])
            P.op("act", lambda e: e.activation(out=rstd, in_=ss, func=AF.Sqrt, scale=1.0 / D, bias=EPS),
                 reads=[sm], writes=[sm])
            P.op("dve", lambda e: e.reciprocal(out=rstd, in_=rstd), reads=[sm], writes=[sm])
            if xn is not None:
                P.op("dve", lambda e: e.tensor_scalar(out=xn.ap, in0=xt.ap, scalar1=rstd, scalar2=None, op0=ALU.mult),
                     reads=[xt, sm], writes=[xn])
            return rstd

        def transpose8(src, dst_fn, dst_toks, evac_eng="act", scale_idx=None, bl=(4,)):
            bt, bt16, btok = nextbank(list(bl))
            for k in range(8):
                P.op("pe", lambda e, k=k: e.transpose(out=bt16[:, k * 128:(k + 1) * 128], in_=src.ap[:, k * 128:(k + 1) * 128],
                                                      identity=ident.ap), reads=[src, ident], writes=[btok])
            if scale_idx is None:
                P.op(evac_eng, lambda e: (e.copy if evac_eng == "act" else e.tensor_copy)(
                    out=dst_fn(None), in_=r3(bt16[:, 0:1024], a=8)), reads=[btok], writes=dst_toks)
            else:
                for k in range(8):
                    if evac_eng == "act" or (evac_eng == "mix" and k % 2 == 0):
                        P.op("act", lambda e, k=k: e.activation(out=dst_fn(k), in_=bt16[:, k * 128:(k + 1) * 128], func=AF.Copy,
                                                                scale=vec(scale_idx, k)), reads=[btok, vecs], writes=dst_toks)
                    else:
                        P.op("dve", lambda e, k=k: e.tensor_scalar(out=dst_fn(k), in0=bt16[:, k * 128:(k + 1) * 128],
                                                                   scalar1=vec(scale_idx, k), scalar2=None, op0=ALU.mult),
                             reads=[btok, vecs], writes=dst_toks)

        Df = Ef[0]
        Am = Ef[1]
        Mf = Ef[2]
        for pos in range(3):
            P.op("pool", lambda e, pos=pos: e.iota(Df.ap[:, 0:128], [[1, 128]], base=128 * (1 - pos), channel_multiplier=-1,
                                                   allow_small_or_imprecise_dtypes=True), writes=[Df])
            P.op("act", lambda e: e.activation(out=Df.ap[:, 0:128], in_=Df.ap[:, 0:128], func=AF.Abs),
                 reads=[Df], writes=[Df])
            P.op("dve", lambda e: e.tensor_scalar(out=Am.ap[:, 0:128], in0=Df.ap[:, 0:128], scalar1=128.0, scalar2=None,
                                                  op0=ALU.is_le), reads=[Df], writes=[Am])
            for h in range(16):
                P.op("act", lambda e, h=h: e.activation(out=Mf.ap[:, 0:128], in_=Df.ap[:, 0:128], func=AF.Exp, scale=-SLOPES[h]),
                     reads=[Df], writes=[Mf])
                P.op("dve", lambda e, pos=pos, h=h: e.tensor_tensor(out=Mv[:, pos, h, :], in0=Mf.ap[:, 0:128], in1=Am.ap[:, 0:128],
                                                                    op=ALU.mult), reads=[Mf, Am], writes=[Mtab])
        P.barrier()

        def chk(name):
            if stage == name:
                raise _Stop()

        try:
          for s in range(2):
            tb0 = s * SEQ
            chk('S0')
            def s1_pre(tt):
                xt = xts[tt % 2]
                xn = xns[tt % 2]
                P.dma("sp", xt.ap, x_d[tb0 + tt * 128:tb0 + (tt + 1) * 128, :], writes=[xt])
                rms_tile(xt, xn)

            s1_pre(0)
            for tt in range(16):
                if tt + 1 < 16:
                    s1_pre(tt + 1)
                transpose8(xns[tt % 2], lambda k, tt=tt: hTv[:, k, tt * 128:(tt + 1) * 128], [hT_tok[tt]], evac_eng="mix", scale_idx=G1,
                           bl=(4, 5, 6, 7))

            chk('S1')
            ev_i = 0
            for cp in range(4):
                wb, wbv = load_w(win_d, 2048 + cp * 256, 256)
                for cc in range(2):
                    c = cp * 2 + cc
                    for b in range(4):
                        bt, _, btok = nextbank([0, 1, 2, 3])
                        P.opn("pe", [lambda e, k=k, cc=cc, b=b, wbv=wbv, bt=bt: e.matmul(
                            bt[:, :], lhsT=wbv[:, k, cc * 128:(cc + 1) * 128], rhs=hTv[:, k, b * 512:(b + 1) * 512],
                            start=(k == 0), stop=(k == 7)) for k in range(8)], reads=[wb] + hT_tok[4 * b:4 * b + 4], writes=[btok])
                        dst = QTv[:, c, b * 512:(b + 1) * 512]
                        ev_i += 1
                        if ev_i % 2:
                            P.op("act", lambda e, dst=dst, bt=bt: e.mul(out=dst, in_=bt[:, :], mul=0.125),
                                 reads=[btok], writes=QT_tok[4 * b:4 * b + 4])
                        else:
                            P.op("dve", lambda e, dst=dst, bt=bt: e.tensor_scalar(out=dst, in0=bt[:, :], scalar1=0.125, scalar2=None,
                                                                                  op0=ALU.mult), reads=[btok], writes=QT_tok[4 * b:4 * b + 4])
            wst_i["i"] += 1
            st = wst[wst_i["i"] % 2]
            stv = r3(st.ap, a=8)
            P.dma("sp", stv, win_d[:, :, 3072:3328], writes=[st])
            for half in range(2):
                P.op("pool", lambda e, half=half: e.tensor_copy(
                    out=wkdv[:, :, :, half * 64:(half + 1) * 64], in_=st.ap.rearrange("p (k g e) -> p k g e", k=8, g=4)),
                    reads=[st], writes=[wkd])
            for g in range(4):
                for b in range(4):
                    bt, _, btok = nextbank([0, 1, 2, 3])
                    for k in range(8):
                        P.op("pe", lambda e, k=k, g=g, b=b, bt=bt: e.matmul(
                            bt[:, :], lhsT=wkdv[:, k, g, :], rhs=hTv[:, k, b * 512:(b + 1) * 512],
                            start=(k == 0), stop=(k == 7)), reads=[wkd] + hT_tok[4 * b:4 * b + 4], writes=[btok])
                    P.op("act", lambda e, g=g, b=b, bt=bt: e.copy(out=KTv[:, g, b * 512:(b + 1) * 512], in_=bt[:, :]),
                         reads=[btok], writes=[KT_tok])
            wb, wbv = load_w(win_d, 3328, 256)
            P.op("pool", lambda e: e.memset(Vv[:, :, :, 64:65], 1.0), writes=V_tok)
            for tt in range(16):
                bt, _, btok = nextbank([0, 1, 2, 3])
                for k in range(8):
                    P.op("pe", lambda e, k=k, tt=tt, wbv=wbv, bt=bt: e.matmul(
                        bt[:, 0:256], lhsT=hTv[:, k, tt * 128:(tt + 1) * 128], rhs=wbv[:, k, :],
                        start=(k == 0), stop=(k == 7)), reads=[wb, hT_tok[tt]], writes=[btok])
                P.op("dve", lambda e, tt=tt, bt=bt: e.tensor_copy(out=Vv[:, tt, :, 0:64],
                                                                  in_=bt[:, 0:256].rearrange("p (g e) -> p g e", g=4)),
                     reads=[btok], writes=[V_tok[tt]])

            chk('S2')
            cnt3 = {"pt": 0, "ef": 0}
            po = [banks[5], banks[6], banks[7]]

            def st_phase(i, g):
                kbs = [kb for kb in (i - 1, i, i + 1) if 0 <= kb < 16]
                pts = []
                for kb in kbs:
                    pos = kb - i + 1
                    rr["pair"] = rr.get("pair", 0) + 1
                    b0 = 2 * (rr["pair"] % 2)
                    pair = (banks[b0], banks[b0 + 1])
                    for j in range(4):
                        h = 4 * g + j
                        c, p = h // 2, h % 2
                        bt, _, btok = pair[p]
                        jj = j // 2
                        P.op("pe", lambda e, jj=jj, g=g, kb=kb, c=c, p=p, i=i, bt=bt: e.matmul(
                            bt[:, jj * 128:(jj + 1) * 128], lhsT=KTv[64 * p:64 * p + 64, g, kb * 128:(kb + 1) * 128],
                            rhs=QTv[64 * p:64 * p + 64, c, i * 128:(i + 1) * 128], start=True, stop=True),
                            reads=[KT_tok, QT_tok[i]], writes=[btok])
                    cnt3["ef"] += 1
                    ef = Ef[cnt3["ef"] % 3]
                    src2 = bc(pair[0][0][:, 0:1], [[512, 2], [1, 256]])
                    P.op("act", lambda e, ef=ef, src2=src2: e.activation(out=ef.ap.rearrange("p (a b) -> p a b", a=2), in_=src2,
                                                                       func=AF.Exp),
                         reads=[pair[0][2], pair[1][2]], writes=[ef])
                    cnt3["pt"] += 1
                    pt = PT[cnt3["pt"] % 6]
                    m4 = bc(Mv[:, pos, 4 * g, :], [[128, 2], [256, 2], [1, 128]])
                    P.op("dve", lambda e, ef=ef, pt=pt, m4=m4: e.tensor_tensor(
                        out=pt.ap.rearrange("p (a b q) -> p a b q", a=2, b=2), in0=ef.ap.rearrange("p (a b q) -> p a b q", a=2, b=2),
                        in1=m4, op=ALU.mult), reads=[ef, Mtab], writes=[pt])
                    pts.append(pt)
                return pts

            def pv_phase(i, g, pts):
                kbs = [kb for kb in (i - 1, i, i + 1) if 0 <= kb < 16]
                for j in range(4):
                    h = 4 * g + j
                    pb, _, pbtok = po[h // 7]
                    o0 = (h % 7) * 65
                    P.opn("pe", [lambda e, j=j, g=g, kb=kb, n=n, pb=pb, o0=o0, pt=pts[n], last=len(kbs) - 1: e.matmul(
                        pb[:, o0:o0 + 65], lhsT=pt.ap[:, (j % 2) * 256 + (j // 2) * 128:(j % 2) * 256 + (j // 2) * 128 + 128], rhs=Vv[:, kb, g, :],
                        start=(n == 0), stop=(n == last)) for n, kb in enumerate(kbs)],
                        reads=list(pts) + [V_tok[kb] for kb in kbs], writes=[pbtok])

            items3 = [(i, g) for i in range(16) for g in range(4)]
            pend3 = st_phase(*items3[0])
            for idx3, (i, g) in enumerate(items3):
                nxt3 = st_phase(*items3[idx3 + 1]) if idx3 + 1 < len(items3) else None
                pv_phase(i, g, pend3)
                pend3 = nxt3
                if g != 3:
                    continue
                if stage in ('S3a', 'S3b'):
                    continue
                ao = AO[i % 2]
                for b3, (h0, nh) in enumerate(((0, 7), (7, 7), (14, 2))):
                    pb, _, pbtok = po[b3]
                    pv = pb[:, 0:nh * 65].rearrange("p (h e) -> p h e", e=65)
                    P.op("dve", lambda e, pv=pv, h0=h0, nh=nh: e.tensor_tensor(
                        out=den.ap[:, h0:h0 + nh], in0=pv[:, :, 64], in1=esink.ap[:, h0:h0 + nh], op=ALU.add),
                        reads=[pbtok, esink], writes=[den])
                P.op("dve", lambda e: e.reciprocal(out=rec.ap, in_=den.ap), reads=[den], writes=[rec])
                for b3, (h0, nh) in enumerate(((0, 7), (7, 7), (14, 2))):
                    pb, _, pbtok = po[b3]
                    pv = pb[:, 0:nh * 65].rearrange("p (h e) -> p h e", e=65)
                    P.op("dve", lambda e, pv=pv, h0=h0, nh=nh, ao=ao: e.tensor_tensor(
                        out=ao.ap[:, h0 * 64:(h0 + nh) * 64].rearrange("p (h e) -> p h e", e=64), in0=pv[:, :, 0:64],
                        in1=bc(rec.ap[:, h0:h0 + nh], [[1, nh], [0, 64]]), op=ALU.mult),
                        reads=[pbtok, rec], writes=[ao])
                if stage == 'S3c':
                    continue
                transpose8(ao, lambda k, i=i: QTv[:, :, i * 128:(i + 1) * 128], [QT_tok[i]])
            chk('S3'); chk('S3a'); chk('S3b'); chk('S3c')
            P.barrier()

            for u in Ub:
                P.op("pool", lambda e, u=u: e.memset(u.ap[:, 0:15], 0.0), writes=[u])
                P.op("pool", lambda e, u=u: e.memset(u.ap[:, 2063:2078], 0.0), writes=[u])
            sg_i = 0
            for cp in range(4):
                wa, wav = load_w(win_d, cp * 256, 256)
                wg, wgv = load_w(win_d, 1024 + cp * 256, 256)
                for cc in range(2):
                    c = cp * 2 + cc
                    u = Ub[c % 2]
                    for b in range(4):
                        ba, _, batok = nextbank([0, 1, 2, 3])
                        bg, _, bgtok = nextbank([0, 1, 2, 3])
                        for (wt, wtv, bt, btok) in ((wa, wav, ba, batok), (wg, wgv, bg, bgtok)):
                            P.opn("pe", [lambda e, k=k, cc=cc, b=b, wtv=wtv, bt=bt: e.matmul(
                                bt[:, :], lhsT=wtv[:, k, cc * 128:(cc + 1) * 128], rhs=hTv[:, k, b * 512:(b + 1) * 512],
                                start=(k == 0), stop=(k == 7)) for k in range(8)], reads=[wt] + hT_tok[4 * b:4 * b + 4], writes=[btok])
                        sg_i += 1
                        sg = Ef[sg_i % 3]
                        P.op("act", lambda e, sg=sg, bg=bg: e.activation(out=sg.ap, in_=bg[:, :], func=AF.Sigmoid),
                             reads=[bgtok], writes=[sg])
                        P.op("dve", lambda e, sg=sg, ba=ba, u=u, b=b: e.tensor_tensor(
                            out=u.ap[:, 15 + b * 512:15 + (b + 1) * 512], in0=ba[:, :], in1=sg.ap, op=ALU.mult),
                            reads=[batok, sg], writes=[u])
                    dg = dgs[c % 2]
                    P.op("dve", lambda e, dg=dg, c=c: e.tensor_tensor(
                        out=dg.ap.rearrange("p (t j) -> p t j", t=31), in0=bc(ident.ap[:, 0:1], [[0, 31], [1, 128]]),
                        in1=bc(dww.ap[:, c * 31:c * 31 + 1], [[1, 31], [0, 128]]), op=ALU.mult), reads=[ident, dww], writes=[dg])
                    for b in range(4):
                        bt, _, btok = nextbank([0, 1, 2, 3])
                        P.opn("pe", [lambda e, tap=tap, dg=dg, u=u, b=b, bt=bt: e.matmul(
                            bt[:, :], lhsT=dg.ap[:, tap * 128:(tap + 1) * 128], rhs=u.ap[:, tap + b * 512:tap + b * 512 + 512],
                            start=(tap == 0), stop=(tap == 30)) for tap in range(31)], reads=[dg, u], writes=[btok])
                        P.op("act", lambda e, c=c, b=b, bt=bt: e.activation(out=cTv[:, c, b * 512:(b + 1) * 512], in_=bt[:, :],
                                                                            func=AF.Identity, bias=vec(DWB, c)),
                             reads=[btok, vecs], writes=[cT_tok[c][b]])
            P.barrier()
            for b in range(4):
                bs_, _, bstok = nextbank([0, 1, 2, 3])
                bq_, _, bqtok = nextbank([0, 1, 2, 3])
                blk = slice(b * 512, (b + 1) * 512)
                for c in range(8):
                    sq = lnq[c % 2]
                    P.op("act", lambda e, sq=sq, c=c, blk=blk: e.activation(out=sq.ap, in_=cTv[:, c, blk], func=AF.Square),
                         reads=[cT_tok[c][b]], writes=[sq])
                    P.op("pe", lambda e, c=c, blk=blk, bs_=bs_: e.matmul(bs_[:, :], lhsT=ones16.ap, rhs=cTv[:, c, blk],
                                                                       start=(c == 0), stop=(c == 7)),
                         reads=[ones16, cT_tok[c][b]], writes=[bstok])
                    P.op("pe", lambda e, c=c, sq=sq, bq_=bq_: e.matmul(bq_[:, :], lhsT=ones16.ap, rhs=sq.ap,
                                                                     start=(c == 0), stop=(c == 7)),
                         reads=[ones16, sq], writes=[bqtok])
                P.op("act", lambda e, bs_=bs_: e.copy(out=lnm.ap, in_=bs_[:, :]), reads=[bstok], writes=[lnm])
                P.op("dve", lambda e: e.tensor_tensor(out=lnr.ap, in0=lnm.ap, in1=lnm.ap, op=ALU.mult), reads=[lnm], writes=[lnr])
                P.op("dve", lambda e, bq_=bq_: e.tensor_tensor(out=lnr.ap, in0=bq_[:, :], in1=lnr.ap, op=ALU.subtract),
                     reads=[bqtok, lnr], writes=[lnr])
                P.op("act", lambda e: e.activation(out=lnr.ap, in_=lnr.ap, func=AF.Sqrt, bias=EPS), reads=[lnr], writes=[lnr])
                P.op("dve", lambda e: e.reciprocal(out=lnr.ap, in_=lnr.ap), reads=[lnr], writes=[lnr])
                for c in range(8):
                    t1 = lnt[c % 2]
                    P.op("dve", lambda e, t1=t1, c=c, blk=blk: e.tensor_tensor(out=t1.ap, in0=cTv[:, c, blk], in1=lnm.ap, op=ALU.subtract),
                         reads=[cT_tok[c][b], lnm], writes=[t1])
                    P.op("dve", lambda e, t1=t1: e.tensor_tensor(out=t1.ap, in0=t1.ap, in1=lnr.ap, op=ALU.mult),
                         reads=[t1, lnr], writes=[t1])
                    P.op("act", lambda e, t1=t1, c=c, blk=blk: e.activation(out=cTv[:, c, blk], in_=t1.ap, func=AF.Silu,
                                                                           scale=vec(LNG, c), bias=vec(LNB, c)),
                         reads=[t1, vecs], writes=[cT_tok[c][b]])
            chk('S4')
            P.barrier()

            sg_i = 0
            for c in range(8):
                w1, w1v = load_w(wpw_d, c * 128, 128, half=True)
                w2, w2v = load_w(wo_d, c * 128, 128, half=True)
                w3, w3v = load_w(win_d, 3584 + c * 128, 128, half=True)
                w4, w4v = load_w(win_d, 4608 + c * 128, 128, half=True)
                for b in range(4):
                    blk = slice(b * 512, (b + 1) * 512)
                    bks = [nextbank([0, 1, 2, 3]) for _ in range(4)]
                    srcs = ((w1, w1v, cTv, [cT_tok[k][b] for k in range(8)]),
                            (w2, w2v, QTv, QT_tok[4 * b:4 * b + 4]),
                            (w3, w3v, hTv, hT_tok[4 * b:4 * b + 4]),
                            (w4, w4v, hTv, hT_tok[4 * b:4 * b + 4]))
                    for (wt, wtv, src, stoks), (bt, _, btok) in zip(srcs, bks):
                        P.opn("pe", [lambda e, k=k, wtv=wtv, src=src, bt=bt, blk=blk: e.matmul(
                            bt[:, :], lhsT=wtv[:, k, 0:128], rhs=src[:, k, blk], start=(k == 0), stop=(k == 7)) for k in range(8)],
                            reads=[wt] + list(stoks), writes=[btok])
                    sa = Ef[0]
                    sb_ = Ef[1]
                    m1 = Ef[2]
                    P.op("act", lambda e, bt=bks[2][0]: e.activation(out=sa.ap, in_=bt[:, :], func=AF.Sigmoid),
                         reads=[bks[2][2]], writes=[sa])
                    P.op("act", lambda e, bt=bks[3][0]: e.activation(out=sb_.ap, in_=bt[:, :], func=AF.Sigmoid),
                         reads=[bks[3][2]], writes=[sb_])
                    P.op("dve", lambda e, bt=bks[0][0]: e.tensor_tensor(out=m1.ap, in0=bt[:, :], in1=sa.ap, op=ALU.mult),
                         reads=[bks[0][2], sa], writes=[m1])
                    P.op("dve", lambda e, bt=bks[1][0]: e.tensor_tensor(out=sb_.ap, in0=bt[:, :], in1=sb_.ap, op=ALU.mult),
                         reads=[bks[1][2], sb_], writes=[sb_])
                    P.op("pool", lambda e, c=c, blk=blk: e.tensor_tensor(out=mTv[:, c, blk], in0=m1.ap, in1=sb_.ap, op=ALU.add),
                         reads=[m1, sb_], writes=[mT_tok[b]])

            chk('S5')
            P.barrier()
            wo4 = [load_w(wout_d, q4 * 256, 256) for q4 in range(4)]
            for tt in range(16):
                xt = xts[tt % 2]
                gt = (tb0 // 128) + tt
                P.dma("sp", xt.ap, x_d[tb0 + tt * 128:tb0 + (tt + 1) * 128, :], writes=[xt])
                for half in range(2):
                    bt, _, btok = nextbank([0, 1, 2, 3])
                    for q2 in range(2):
                        wb, wbv = wo4[half * 2 + q2]
                        P.opn("pe", [lambda e, k=k, tt=tt, q2=q2, wbv=wbv, bt=bt: e.matmul(
                            bt[:, q2 * 256:(q2 + 1) * 256], lhsT=mTv[:, k, tt * 128:(tt + 1) * 128], rhs=wbv[:, k, :],
                            start=(k == 0), stop=(k == 7)) for k in range(8)], reads=[wb, mT_tok[tt // 4]], writes=[btok])
                    P.op("dve", lambda e, half=half, bt=bt, xt=xt: e.tensor_tensor(
                        out=xt.ap[:, half * 512:(half + 1) * 512], in0=bt[:, :], in1=xt.ap[:, half * 512:(half + 1) * 512],
                        op=ALU.add), reads=[btok, xt], writes=[xt])
                P.dma("act", out_d[gt * 128:(gt + 1) * 128, :], xt.ap, reads=[xt], writes=[out_tok[gt]], key=xt)
            chk('S6')
            P.barrier()
        except _Stop:
            pass

        if not stop_after_a:
            A.off = mark0
            build_peer(nc, P, A, banks, nextbank, dict(
                ident=ident, identf=identf, vecs=vecs, vec=vec, G2=G2, small=small, new_small=new_small, small_cur=small_cur,
                out_tok=out_tok, out_d=out_d, fg_d=fg_d, skT_d=skT_d, pu_d=pu_d, pv_d=pv_d, wq_d=wq_d,
                ut_d=ut_d, vb_d=vb_d, wqb_d=wqb_d))
        P.emit()
    return nc


def build_peer(nc, P, A, banks, nextbank, C):
    ident, identf, vecs, vec, G2 = C["ident"], C["identf"], C["vecs"], C["vec"], C["G2"]
    small, new_small, out_tok, out_d = C["small"], C["new_small"], C["out_tok"], C["out_d"]
    small_cur = C["small_cur"]
    ut_d, vb_d, wqb_d = C["ut_d"], C["vb_d"], C["wqb_d"]
    NBLK = TOK // TB
    NT = TB // 128
    mark = A.off
    NPB = 4
    pst = [A.f32(1024) for _ in range(NPB)]
    pst2 = [A.f32(1024) for _ in range(NPB)]
    pbf = [A.b16(1024) for _ in range(NPB)]
    pbf2 = [A.b16(1024) for _ in range(NPB)]
    utb = [A.b16(1024) for _ in range(2)]
    assert A.off - mark <= 128 * TB * 2
    A.off = mark
    G = A.b16(128 * TB)
    Gv = G.ap.rearrange("p (i t) -> p i t", i=128)
    h2T = [A.b16(8 * TB) for _ in range(2)]
    h2T_toks = [[Tok() for _ in range(NT)] for _ in range(2)]
    qT = A.f32(16 * TB)
    qTv = r3(qT.ap, a=16)
    sc = [A.f32(2048)] * 2
    sc2 = A.f32(256)
    v16 = A.f32(256)
    v16v = r3(v16.ap, a=16)
    ix = A.u32(256)
    ixv = r3(ix.ap, a=16)
    ixf = A.f32(256)
    cand = sc[0]
    eqb = A.f32(2048)
    eq2 = cand
    ts = A.f32(128)
    tsv = r3(ts.ap, a=8)
    pos = A.u32(128)
    posv = r3(pos.ap, a=8)
    posf = A.f32(128)
    k1f = A.f32(128)
    k2f = A.f32(128)
    Ivs = [A.f32(128) for _ in range(2)]
    Jvs = [A.f32(128) for _ in range(2)]
    Wvs = [A.f32(128) for _ in range(2)]
    ew = A.f32(128)
    zz = A.f32(16)
    SM = A.f32(3 * TB)
    SMv = r3(SM.ap, a=3)
    iota128 = A.b16(128)
    iotaf = A.f32(128)
    skT = A.f32(256)
    skTv = r3(skT.ap, a=2)
    fg = A.f32(1024)
    ohB = [A.b16(16 * 128) for _ in range(2)]
    ohE = [A.b16(16 * 128) for _ in range(2)]
    OAI = A.b16(TB * 8)
    OAJ = A.b16(TB * 8)
    OBI = A.b16(TB * 16)
    OBJ = A.b16(TB * 16)
    xh = A.f32(TB)
    xl = A.f32(TB)
    ubr = [A.b16(1024) for _ in range(4)]
    vbr = [A.b16(1024) for _ in range(4)]
    gelr = [A.f32(TB) for _ in range(3)]
    ATr = [A.b16(TB) for _ in range(4)]
    xts = [A.f32(1024) for _ in range(2)]
    xns = [A.b16(1024) for _ in range(2)]
    wqc = [A.b16(1024) for _ in range(3)]
    ut_tok = [Tok() for _ in range(128)]
    vb_tok = [Tok() for _ in range(128)]
    wqb_tok = [Tok() for _ in range(16)]

    P.dma("sp", skT.ap, C["skT_d"], writes=[skT])
    P.dma("sp", fg.ap, C["fg_d"], writes=[fg])
    P.op("pool", lambda e: e.iota(iotaf.ap, [[1, 128]], base=0, channel_multiplier=0, allow_small_or_imprecise_dtypes=True),
         writes=[iotaf])
    P.op("dve", lambda e: e.tensor_copy(out=iota128.ap, in_=iotaf.ap), reads=[iotaf], writes=[iota128])
    k16 = A.f32(16)
    nhalf = A.f32(1)
    P.op("pool", lambda e: e.memset(nhalf.ap, -0.5), writes=[nhalf])
    P.op("dve", lambda e: e.tensor_scalar(out=k16.ap, in0=iotaf.ap[:, 0:16], scalar1=16.0, scalar2=None, op0=ALU.mult),
         reads=[iotaf], writes=[k16])

    for m in range(16):
        st = pst[m % NPB]
        pb = pbf[m % NPB]
        P.dma("sp", r3(st.ap, a=8), C["wq_d"][:, :, m * 128:(m + 1) * 128], writes=[st])
        P.op("pool", lambda e, st=st, pb=pb: e.tensor_copy(out=pb.ap, in_=st.ap), reads=[st], writes=[pb])
        P.dma("act", wqb_d[m], pb.ap, reads=[pb], writes=[wqb_tok[m]], key=pb)
    def prep_uv(i):
        st = pst[i % NPB]
        pb = pbf[i % NPB]
        P.dma("sp", st.ap, C["pu_d"][i * 128:(i + 1) * 128, :], writes=[st])
        P.op("pool", lambda e, st=st, pb=pb: e.tensor_copy(out=pb.ap, in_=st.ap), reads=[st], writes=[pb])
        bt, bt16, btok = nextbank([4, 5, 6, 7])
        for k in range(8):
            P.op("pe", lambda e, k=k, pb=pb, bt16=bt16: e.transpose(out=bt16[:, k * 128:(k + 1) * 128], in_=pb.ap[:, k * 128:(k + 1) * 128],
                                                             identity=ident.ap), reads=[pb, ident], writes=[btok])
        ut = utb[i % 2]
        P.op("act", lambda e, ut=ut, bt16=bt16: e.copy(out=ut.ap, in_=bt16[:, 0:1024]), reads=[btok], writes=[ut])
        P.dma("act", ut_d[i], ut.ap, reads=[ut], writes=[ut_tok[i]], key=ut)
        st2 = pst2[i % NPB]
        pb2 = pbf2[i % NPB]
        P.dma("pool", st2.ap, C["pv_d"][i * 128:(i + 1) * 128, :], writes=[st2])
        P.op("dve", lambda e, st2=st2, pb2=pb2: e.tensor_copy(out=pb2.ap, in_=st2.ap), reads=[st2], writes=[pb2])
        P.dma("act", vb_d[i * 128:(i + 1) * 128, :], pb2.ap, reads=[pb2], writes=[vb_tok[i]], key=pb2)

    cnt = {"oh": 0, "ev": 0}

    def routing(nb):
        h2Tv = r3(h2T[nb % 2].ap, a=8)
        h2T_tok = h2T_toks[nb % 2]
        for tt in range(NT):
            gt = nb * NT + tt
            xt = xts[tt]
            xn = xns[tt]
            P.dma("sp", xt.ap, out_d[gt * 128:(gt + 1) * 128, :], reads=[out_tok[gt]], writes=[xt])
            ss, rstd = new_small()
            smt = small_cur["tok"]
            P.op("dve", lambda e, xt=xt, ss=ss: e.scalar_tensor_tensor(out=eqb.ap[:, 0:1024], in0=xt.ap, scalar=1.0, in1=xt.ap,
                                                                       op0=ALU.mult, op1=ALU.mult, accum_out=ss),
                 reads=[xt], writes=[eqb, smt])
            P.op("pool", lambda e, ss=ss: e.tensor_scalar(out=ss, in0=ss, scalar1=1.0 / D, scalar2=EPS, op0=ALU.mult, op1=ALU.add),
                 reads=[smt], writes=[smt])
            P.op("pool", lambda e, ss=ss, rstd=rstd: e.tensor_tensor(out=rstd, in0=ss, in1=nhalf.ap, op=ALU.pow),
                 reads=[smt, nhalf], writes=[smt])
            P.op("dve", lambda e, xt=xt, xn=xn, rstd=rstd: e.tensor_scalar(out=xn.ap, in0=xt.ap, scalar1=rstd, scalar2=None, op0=ALU.mult),
                 reads=[xt, smt], writes=[xn])
            bt, bt16, btok = nextbank([7])
            for k in range(8):
                P.op("pe", lambda e, k=k, xn=xn, bt16=bt16: e.transpose(out=bt16[:, k * 128:(k + 1) * 128], in_=xn.ap[:, k * 128:(k + 1) * 128],
                                                                 identity=ident.ap), reads=[xn, ident], writes=[btok])
            for k in range(8):
                P.op("dve", lambda e, k=k, tt=tt, bt16=bt16: e.tensor_scalar(
                    out=h2Tv[:, k, tt * 128:(tt + 1) * 128], in0=bt16[:, k * 128:(k + 1) * 128], scalar1=vec(G2, k), scalar2=None,
                    op0=ALU.mult), reads=[btok, vecs], writes=[h2T_tok[tt]])
        for m in range(16):
            wq = wqc[m % 3]
            P.dma("sp", wq.ap, wqb_d[m], reads=[wqb_tok[m]], writes=[wq])
            bt, _, btok = nextbank([7])
            for k in range(8):
                P.op("pe", lambda e, k=k, wq=wq, bt=bt: e.matmul(bt[:, 0:TB], lhsT=r3(wq.ap, a=8)[:, k, :], rhs=h2Tv[:, k, :],
                                                              start=(k == 0), stop=(k == 7)), reads=[wq] + h2T_tok, writes=[btok])
            cnt["ev"] += 1
            if cnt["ev"] % 2:
                P.op("act", lambda e, m=m, bt=bt: e.copy(out=qTv[:, m, :], in_=bt[:, 0:TB]), reads=[btok], writes=[qT])
            else:
                P.op("dve", lambda e, m=m, bt=bt: e.tensor_copy(out=qTv[:, m, :], in_=bt[:, 0:TB]), reads=[btok], writes=[qT])
        for tt in range(NT):
            Iv, Jv, Wv = Ivs[tt], Jvs[tt], Wvs[tt]
            scb = sc[tt]
            for bq in range(4):
                bt, _, btok = nextbank([7])
                for mm in range(4):
                    m = bq * 4 + mm
                    P.op("pe", lambda e, mm=mm, m=m, tt=tt, bt=bt: e.matmul(
                        bt[:, mm * 128:(mm + 1) * 128], lhsT=qTv[:, m, tt * 128:(tt + 1) * 128], rhs=skTv[:, m % 2, :],
                        start=True, stop=True), reads=[qT, skT], writes=[btok])
                P.op("dve", lambda e, bq=bq, bt=bt, scb=scb: e.tensor_copy(out=scb.ap[:, bq * 512:(bq + 1) * 512], in_=bt[:, :]),
                     reads=[btok], writes=[scb])
            for m in range(16):
                src = scb.ap[:, m * 128:(m + 1) * 128]
                P.op("dve", lambda e, m=m, src=src: e.max(out=v16v[:, m, 0:8], in_=src), reads=[scb], writes=[v16])
                P.op("dve", lambda e, m=m, src=src: e.max_index(out=ixv[:, m, 0:8], in_max=v16v[:, m, 0:8], in_values=src),
                     reads=[scb, v16], writes=[ix])
                P.op("dve", lambda e, m=m, src=src: e.match_replace(out=sc2.ap[:, 0:128], in_to_replace=v16v[:, m, 0:8], in_values=src,
                                                                    imm_value=-1e30), reads=[scb, v16], writes=[sc2])
                P.op("dve", lambda e, m=m: e.max(out=v16v[:, m, 8:16], in_=sc2.ap[:, 0:128]), reads=[sc2], writes=[v16])
                P.op("dve", lambda e, m=m: e.max_index(out=ixv[:, m, 8:16], in_max=v16v[:, m, 8:16], in_values=sc2.ap[:, 0:128]),
                     reads=[sc2, v16], writes=[ix])
            P.op("dve", lambda e: e.tensor_copy(out=ixf.ap, in_=ix.ap), reads=[ix], writes=[ixf])
            c4 = lambda b: b.ap.rearrange("p (h a b) -> p h a b", h=8, a=16)
            P.op("dve", lambda e: e.tensor_tensor(out=c4(cand), in0=bc(v16.ap[:, 0:1], [[32, 8], [1, 16], [0, 16]]),
                                                  in1=bc(v16.ap[:, 16:17], [[32, 8], [0, 16], [1, 16]]), op=ALU.add),
                 reads=[v16], writes=[cand])
            for h in range(8):
                src = cand.ap[:, h * 256:(h + 1) * 256]
                P.op("dve", lambda e, h=h, src=src: e.max(out=tsv[:, h, 0:8], in_=src), reads=[cand], writes=[ts])
                P.op("dve", lambda e, h=h, src=src: e.max_index(out=posv[:, h, 0:8], in_max=tsv[:, h, 0:8], in_values=src),
                     reads=[cand, ts], writes=[pos])
                P.op("dve", lambda e, h=h, src=src: e.match_replace(out=sc2.ap, in_to_replace=tsv[:, h, 0:8], in_values=src,
                                                                    imm_value=-1e30), reads=[cand, ts], writes=[sc2])
                P.op("dve", lambda e, h=h: e.max(out=tsv[:, h, 8:16], in_=sc2.ap), reads=[sc2], writes=[ts])
                P.op("dve", lambda e, h=h: e.max_index(out=posv[:, h, 8:16], in_max=tsv[:, h, 8:16], in_values=sc2.ap),
                     reads=[sc2, ts], writes=[pos])
            P.op("dve", lambda e: e.tensor_tensor(out=r3(ew.ap, a=8), in0=tsv, in1=bc(ts.ap[:, 0:1], [[16, 8], [0, 16]]), op=ALU.subtract),
                 reads=[ts], writes=[ew])
            P.op("act", lambda e: e.activation(out=ew.ap, in_=ew.ap, func=AF.Exp), reads=[ew], writes=[ew])
            P.op("dve", lambda e: e.tensor_reduce(out=zz.ap[:, 0:8], in_=r3(ew.ap, a=8), axis=AX.X, op=ALU.add), reads=[ew], writes=[zz])
            P.op("dve", lambda e: e.reciprocal(out=zz.ap[:, 8:16], in_=zz.ap[:, 0:8]), reads=[zz], writes=[zz])
            P.op("dve", lambda e, Wv=Wv: e.tensor_tensor(out=r3(Wv.ap, a=8), in0=r3(ew.ap, a=8), in1=bc(zz.ap[:, 8:9], [[1, 8], [0, 16]]), op=ALU.mult),
                 reads=[ew, zz], writes=[Wv])
            P.op("dve", lambda e: e.tensor_copy(out=posf.ap, in_=pos.ap), reads=[pos], writes=[posf])
            P.op("dve", lambda e: e.tensor_tensor(out=c4(eqb), in0=bc(posf.ap[:, 0:1], [[16, 8], [1, 16], [0, 16]]),
                                                  in1=bc(k16.ap[:, 0:1], [[0, 8], [0, 16], [1, 16]]), op=ALU.subtract),
                 reads=[k16, posf], writes=[eqb])
            P.op("dve", lambda e: e.tensor_scalar(out=eq2.ap, in0=eqb.ap, scalar1=0.0, scalar2=None, op0=ALU.is_ge),
                 reads=[eqb], writes=[eq2])
            P.op("dve", lambda e: e.scalar_tensor_tensor(out=eqb.ap, in0=eqb.ap, scalar=16.0, in1=eq2.ap, op0=ALU.is_lt, op1=ALU.mult),
                 reads=[eqb, eq2], writes=[eqb])
            P.op("dve", lambda e: e.tensor_tensor(out=c4(eq2), in0=c4(eqb), in1=bc(iotaf.ap[:, 0:1], [[0, 8], [0, 16], [1, 16]]), op=ALU.mult),
                 reads=[eqb, iotaf], writes=[eq2])
            P.op("dve", lambda e: e.tensor_reduce(out=k1f.ap, in_=c4(eq2), axis=AX.X, op=ALU.add), reads=[eq2], writes=[k1f])
            P.op("dve", lambda e: e.tensor_tensor(out=c4(eq2), in0=c4(eqb), in1=bc(ixf.ap[:, 0:1], [[32, 8], [0, 16], [1, 16]]), op=ALU.mult),
                 reads=[eqb, ixf], writes=[eq2])
            P.op("dve", lambda e, Iv=Iv: e.tensor_reduce(out=Iv.ap, in_=c4(eq2), axis=AX.X, op=ALU.add), reads=[eq2], writes=[Iv])
            P.op("dve", lambda e: e.scalar_tensor_tensor(out=k2f.ap, in0=k1f.ap, scalar=-16.0, in1=posf.ap, op0=ALU.mult, op1=ALU.add),
                 reads=[k1f, posf], writes=[k2f])
            P.op("dve", lambda e: e.tensor_tensor(out=c4(eqb), in0=bc(k2f.ap[:, 0:1], [[16, 8], [1, 16], [0, 16]]),
                                                  in1=bc(iotaf.ap[:, 0:1], [[0, 8], [0, 16], [1, 16]]), op=ALU.is_equal),
                 reads=[k2f, iotaf], writes=[eqb])
            P.op("dve", lambda e: e.tensor_tensor(out=c4(eq2), in0=c4(eqb), in1=bc(ixf.ap[:, 16:17], [[32, 8], [0, 16], [1, 16]]), op=ALU.mult),
                 reads=[eqb, ixf], writes=[eq2])
            P.op("dve", lambda e, Jv=Jv: e.tensor_reduce(out=Jv.ap, in_=c4(eq2), axis=AX.X, op=ALU.add), reads=[eq2], writes=[Jv])
        for tt in range(NT):
            Iv, Jv, Wv = Ivs[tt], Jvs[tt], Wvs[tt]
            bt, _, btok = nextbank([7])
            for qi, srcb in enumerate((Iv, Jv, Wv)):
                P.op("pe", lambda e, qi=qi, srcb=srcb, bt=bt: e.transpose(out=bt[:, qi * 128:(qi + 1) * 128], in_=srcb.ap, identity=identf.ap),
                     reads=[srcb, identf], writes=[btok])
            P.op("act", lambda e, tt=tt, bt=bt: e.copy(out=SMv[:, :, tt * 128:(tt + 1) * 128], in_=r3(bt[:, 0:384], a=3)),
                 reads=[btok], writes=[SM])
        t3 = lambda b, n: b.ap[:, 0:TB * n].rearrange("p (t a) -> p t a", a=n)
        for q, OA, OB in ((0, OAI, OBI), (1, OAJ, OBJ)):
            P.op("dve", lambda e, q=q: e.tensor_tensor(out=t3(eqb, 8), in0=bc(SMv[:, q, 0:1], [[1, TB], [0, 8]]),
                                                       in1=bc(k16.ap[:, 0:1], [[0, TB], [1, 8]]), op=ALU.subtract),
                 reads=[SM, k16], writes=[eqb])
            P.op("dve", lambda e: e.tensor_scalar(out=cand.ap, in0=eqb.ap, scalar1=0.0, scalar2=None, op0=ALU.is_ge),
                 reads=[eqb], writes=[cand])
            P.op("dve", lambda e, OA=OA: e.scalar_tensor_tensor(out=OA.ap, in0=eqb.ap, scalar=16.0, in1=cand.ap, op0=ALU.is_lt, op1=ALU.mult),
                 reads=[eqb, cand], writes=[OA])
            P.op("dve", lambda e, OA=OA: e.tensor_tensor(out=t3(eqb, 8), in0=t3(OA, 8), in1=bc(iotaf.ap[:, 0:1], [[0, TB], [1, 8]]), op=ALU.mult),
                 reads=[OA, iotaf], writes=[eqb])
            P.op("dve", lambda e: e.tensor_reduce(out=xh.ap, in_=t3(eqb, 8), axis=AX.X, op=ALU.add), reads=[eqb], writes=[xh])
            P.op("dve", lambda e, q=q: e.scalar_tensor_tensor(out=xl.ap, in0=xh.ap, scalar=-16.0, in1=SMv[:, q, :], op0=ALU.mult, op1=ALU.add),
                 reads=[xh, SM], writes=[xl])
            P.op("dve", lambda e, OB=OB: e.tensor_tensor(out=t3(OB, 16), in0=bc(xl.ap[:, 0:1], [[1, TB], [0, 16]]),
                                                         in1=bc(iotaf.ap[:, 0:1], [[0, TB], [1, 16]]), op=ALU.is_equal),
                 reads=[xl, iotaf], writes=[OB])
        P.op("dve", lambda e: e.tensor_tensor(out=t3(OAJ, 8), in0=t3(OAJ, 8), in1=bc(SMv[:, 2, 0:1], [[1, TB], [0, 8]]), op=ALU.mult),
             reads=[OAJ, SM], writes=[OAJ])

    def b5(nb):
        TG = 16
        for tg in range(TB // TG):
            t0 = tg * TG
            cnt["oh"] += 1
            ob, oc = ohB[cnt["oh"] % 2], ohE[cnt["oh"] % 2]
            o3 = lambda b: b.ap.rearrange("p (t i) -> p t i", t=TG)
            o4 = lambda b: b.ap.rearrange("p (t a c) -> p t a c", t=TG, a=8)
            P.op("dve", lambda e, oc=oc, t0=t0: e.tensor_tensor(
                out=o4(oc), in0=bc(OAI.ap[:, t0 * 8:t0 * 8 + 1], [[8, TG], [1, 8], [0, 16]]),
                in1=bc(OBI.ap[:, t0 * 16:t0 * 16 + 1], [[16, TG], [0, 8], [1, 16]]), op=ALU.mult), reads=[OAI, OBI], writes=[oc])
            P.op("pool", lambda e, ob=ob, t0=t0: e.tensor_tensor(
                out=o4(ob), in0=bc(OAJ.ap[:, t0 * 8:t0 * 8 + 1], [[8, TG], [1, 8], [0, 16]]),
                in1=bc(OBJ.ap[:, t0 * 16:t0 * 16 + 1], [[16, TG], [0, 8], [1, 16]]), op=ALU.mult), reads=[OAJ, OBJ], writes=[ob])
            for t4 in range(TG // 4):
                bt, _, btok = nextbank([6, 7])
                P.opn("pe", [lambda e, q=q, t4=t4, oc=oc, ob=ob, bt=bt: e.matmul(
                    bt[:, q * 128:(q + 1) * 128], lhsT=o3(ob)[:, t4 * 4 + q, :], rhs=o3(oc)[:, t4 * 4 + q, :], start=True, stop=True)
                    for q in range(4)], reads=[oc, ob], writes=[btok])
                ta = t0 + t4 * 4
                P.op("act", lambda e, ta=ta, bt=bt: e.copy(out=Gv[:, :, ta:ta + 4], in_=bc(bt[:, 0:1], [[1, 128], [128, 4]])),
                     reads=[btok], writes=[G])
    def b6(nb, pend):
        h2Tv = r3(h2T[nb % 2].ap, a=8)
        h2T_tok = h2T_toks[nb % 2]
        def u_side(i):
            ub = ubr[i % 4]
            vb = vbr[i % 4]
            P.dma("sp", ub.ap, ut_d[i], reads=[ut_tok[i]], writes=[ub])
            P.dma("sp", vb.ap, vb_d[i * 128:(i + 1) * 128, :], reads=[vb_tok[i]], writes=[vb])
            bs_, _, bstok = banks[4 + i % 3]
            P.opn("pe", [lambda e, k=k, ub=ub, bs_=bs_: e.matmul(bs_[:, 0:TB], lhsT=r3(ub.ap, a=8)[:, k, :], rhs=h2Tv[:, k, :],
                                                             start=(k == 0), stop=(k == 7)) for k in range(8)],
                  reads=[ub] + h2T_tok, writes=[bstok])
            return bs_, bstok, vb

        per = (len(pend) + 119) // 120 if pend else 0
        uq = [u_side(0), u_side(1)]
        for i in range(128):
            bs_, bstok, vb = uq.pop(0)
            if i + 2 < 128:
                uq.append(u_side(i + 2))
            gel = gelr[i % 3]
            P.op("act", lambda e, gel=gel, bs_=bs_: e.activation(out=gel.ap, in_=bs_[:, 0:TB], func=AF.Gelu), reads=[bstok], writes=[gel])
            at = ATr[i % 4]
            P.op("pool", lambda e, gel=gel, at=at, i=i: e.tensor_tensor(out=at.ap, in0=gel.ap, in1=Gv[:, i, :], op=ALU.mult),
                 reads=[gel, G], writes=[at])
            P.opn("pe", [lambda e, tt=tt, half=half, at=at, vb=vb, i=i: e.matmul(
                banks[tt * 2 + half][0][:, :], lhsT=at.ap[:, tt * 128:(tt + 1) * 128], rhs=vb.ap[:, half * 512:(half + 1) * 512],
                start=(i == 0), stop=(i == 127)) for tt in range(NT) for half in range(2)],
                reads=[at, vb], writes=[banks[b4][2] for b4 in range(2 * NT)])
            if pend:
                P.replay(pend, per)
        P.replay(pend, len(pend))

    def b7(nb):
        for tt in range(NT):
            gt = nb * NT + tt
            xt = xts[tt]
            P.dma("sp", xt.ap, out_d[gt * 128:(gt + 1) * 128, :], reads=[out_tok[gt]], writes=[xt])
            for half in range(2):
                ab, _, abtok = banks[tt * 2 + half]
                P.op("dve", lambda e, half=half, ab=ab, xt=xt: e.tensor_tensor(
                    out=xt.ap[:, half * 512:(half + 1) * 512], in0=ab[:, :], in1=xt.ap[:, half * 512:(half + 1) * 512], op=ALU.add),
                    reads=[abtok, xt], writes=[xt])
            ss, rstd = new_small()
            smt = small_cur["tok"]
            P.op("dve", lambda e, xt=xt, ss=ss: e.scalar_tensor_tensor(out=eqb.ap[:, 0:1024], in0=xt.ap, scalar=1.0, in1=xt.ap,
                                                                       op0=ALU.mult, op1=ALU.mult, accum_out=ss),
                 reads=[xt], writes=[eqb, smt])
            P.op("pool", lambda e, ss=ss: e.tensor_scalar(out=ss, in0=ss, scalar1=1.0 / D, scalar2=EPS, op0=ALU.mult, op1=ALU.add),
                 reads=[smt], writes=[smt])
            P.op("pool", lambda e, ss=ss, rstd=rstd: e.tensor_tensor(out=rstd, in0=ss, in1=nhalf.ap, op=ALU.pow),
                 reads=[smt, nhalf], writes=[smt])
            P.op("dve", lambda e, xt=xt, rstd=rstd: e.scalar_tensor_tensor(out=xt.ap, in0=xt.ap, scalar=rstd, in1=fg.ap,
                                                                           op0=ALU.mult, op1=ALU.mult), reads=[xt, smt, fg], writes=[xt])
            P.dma("act", out_d[gt * 128:(gt + 1) * 128, :], xt.ap, reads=[xt], writes=[out_tok[gt]], key=xt)


    routing(0)
    for i in range(128):
        prep_uv(i)
    P.barrier()
    for nb in range(NBLK):
        b5(nb)
        pend = []
        if nb + 1 < NBLK:
            P.capture()
            routing(nb + 1)
            pend = P.end_capture()
        b6(nb, pend)
        b7(nb)


def prep_inputs(inputs):
    f = lambda a: np.ascontiguousarray(np.asarray(a, dtype=np.float32))
    rk = lambda w: f(w.reshape(8, 128, -1).transpose(1, 0, 2))
    pv = lambda v: v.reshape(8, 128).T
    x = f(inputs["x"])
    vecs = np.concatenate([pv(np.asarray(inputs[n])[0]) for n in
                           ("norm1_g", "conv_dw_b", "conv_ln_g", "conv_ln_b", "norm2_g")], axis=1)
    dww = np.asarray(inputs["conv_dw_w"])[0].reshape(31, 8, 128).transpose(2, 1, 0).reshape(128, 248)
    shared = {
        "win": rk(np.asarray(inputs["w_in"])[0]),
        "wpw": rk(np.asarray(inputs["conv_w_pw"])[0]),
        "wo": rk(np.asarray(inputs["attn_w_o"])[0]),
        "wout": rk(np.asarray(inputs["w_out"])[0]),
        "wq": rk(np.asarray(inputs["peer_w_query"])[0]),
        "vecs": f(vecs),
        "dww": f(dww),
        "sink": f(np.broadcast_to(np.asarray(inputs["attn_sink"])[0][None, :], (128, 16))),
        "fg": f(np.broadcast_to(np.asarray(inputs["final_g"])[None, :], (128, 1024))),
        "skT": f(np.asarray(inputs["peer_sub_keys"])[0].transpose(2, 0, 1).reshape(128, 256)),
        "pu": f(np.asarray(inputs["peer_u"])[0]),
        "pv": f(np.asarray(inputs["peer_v"])[0]),
    }
    xs = x.reshape(NCORES, TOK, D)
    return [dict(shared, x=np.ascontiguousarray(xs[c])) for c in range(NCORES)]


_NC_CACHE = {}


def kernel(**inputs):
    in_maps = prep_inputs(inputs)
    if "nc" not in _NC_CACHE:
        _NC_CACHE["nc"] = build_program()
    res = run_bass_kernel_spmd(_NC_CACHE["nc"], in_maps, core_ids=list(range(NCORES)))
    out = np.stack([np.asarray(r["out"]) for r in res.results], axis=0)
    return out.reshape(16, SEQ, D).astype(np.float32)
```

```python
import os
import numpy as np
from contextlib import ExitStack
import concourse.bass as bass
import concourse.mybir as mybir
from concourse.bass_utils import run_bass_kernel_spmd

F32 = mybir.dt.float32
BF16 = mybir.dt.bfloat16
U32 = mybir.dt.uint32
AF = mybir.ActivationFunctionType
ALU = mybir.AluOpType
AX = mybir.AxisListType

NCORES = 8
TOK = 4096
SEQ = 2048
D = 1024
EPS = 1e-6
TB = 256


class Tok:
    __slots__ = ("w", "r", "sem", "cnt", "name")

    def __init__(self, name=""):
        self.w = None
        self.r = {}
        self.sem = None
        self.cnt = 0
        self.name = name


class Buf:
    __slots__ = ("ap", "tok")

    def __init__(self, ap, name=""):
        self.ap = ap
        self.tok = Tok(name)


class Prog:
    ENG = ("pe", "dve", "act", "pool", "sp")

    def __init__(self, nc, es):
        self.nc = nc
        self.es = es
        self.ops = {e: [] for e in self.ENG}
        self.cnt = {e: 0 for e in self.ENG}
        self.sems = {}
        for e in self.ENG:
            self.sems["E_" + e] = es.enter_context(nc.semaphore("s_" + e))
        self.final = {}
        self.waited = {e: {} for e in self.ENG}
        self.ndma = 0

    def _collect(self, eng, reads, writes):
        need = {}

        def add(ev, raw):
            if ev is None:
                return
            key, val, src = ev
            if src == eng and eng == "pe":
                return
            if need.get(key, 0) < val:
                need[key] = val

        for t in reads:
            add(t.w, True)
        for t in writes:
            add(t.w, False)
            for key, (val, src) in t.r.items():
                add((key, val, src), False)
        waits = []
        wd = self.waited[eng]
        for key, val in need.items():
            if wd.get(key, 0) < val:
                wd[key] = val
                waits.append((key, val))
        return waits

    def _commit(self, ev, reads, writes):
        key, val, src = ev
        for t in reads:
            old = t.r.get(key)
            if old is None or old[0] < val:
                t.r[key] = (val, src)
        for t in writes:
            t.w = ev
            t.r = {}

    cap = None

    def capture(self):
        self.cap = []

    def end_capture(self):
        c, self.cap = self.cap, None
        return c

    def replay(self, lst, n):
        for _ in range(min(n, len(lst))):
            kind, args = lst.pop(0)
            getattr(self, kind)(*args)

    def op(self, eng, fn, reads=(), writes=()):
        if self.cap is not None:
            self.cap.append(("op", (eng, fn, list(reads), list(writes))))
            return
        reads = [b.tok if isinstance(b, Buf) else b for b in reads]
        writes = [b.tok if isinstance(b, Buf) else b for b in writes]
        waits = self._collect(eng, reads, writes)
        self.cnt[eng] += 1
        key = "E_" + eng
        ev = (key, self.cnt[eng], eng)
        self.final[key] = self.cnt[eng]
        self.ops[eng].append((waits, fn, key, 1))
        self._commit(ev, reads, writes)

    def opn(self, eng, fns, reads=(), writes=()):
        fns = list(fns)

        def run(e, fns=fns):
            last = None
            for f in fns:
                last = f(e)
            return last

        self.op(eng, run, reads, writes)

    def dma(self, q, out, in_, reads=(), writes=(), key=None):
        if self.cap is not None:
            self.cap.append(("dma", (q, out, in_, list(reads), list(writes), key)))
            return
        reads = [b.tok if isinstance(b, Buf) else b for b in reads]
        writes = [b.tok if isinstance(b, Buf) else b for b in writes]
        kt = key if key is not None else (writes[0] if writes else reads[0])
        if isinstance(kt, Buf):
            kt = kt.tok
        if kt.sem is None:
            self.ndma += 1
            kt.sem = "D_%d" % self.ndma
            self.sems[kt.sem] = self.es.enter_context(self.nc.semaphore("d%d" % self.ndma))
        waits = self._collect(q, reads, writes)
        kt.cnt += 16
        ev = (kt.sem, kt.cnt, None)
        self.final[kt.sem] = kt.cnt
        self.ops[q].append((waits, lambda e: e.dma_start(out=out, in_=in_), kt.sem, 16))
        self._commit(ev, reads, writes)

    def barrier(self):
        for e in self.ENG:
            waits = []
            wd = self.waited[e]
            for key, val in self.final.items():
                if key == "E_" + e:
                    continue
                if wd.get(key, 0) < val:
                    wd[key] = val
                    waits.append((key, val))
            if waits:
                self.ops[e].append((waits, None, None, 0))

    def emit(self):
        nc = self.nc
        self.barrier()
        engmap = {"pe": "tensor", "dve": "vector", "act": "scalar", "pool": "gpsimd", "sp": "sync"}
        with nc.Block() as block:
            for e in self.ENG:
                def body(engine, ops=self.ops[e], sems=self.sems):
                    for waits, fn, key, inc in ops:
                        for wk, wv in waits:
                            engine.wait_ge(sems[wk], wv)
                        if fn is not None:
                            fn(engine).then_inc(sems[key], inc)

                getattr(block, engmap[e])(body)


class Arena:
    def __init__(self, nc, nbytes):
        self.t32 = nc.alloc_sbuf_tensor("arena", [128, nbytes // 4], F32)
        self.t16 = self.t32.bitcast(BF16)
        self.tu = self.t32.bitcast(U32)
        self.off = 0
        self.cap = nbytes

    def alloc(self, nbytes):
        off = (self.off + 63) // 64 * 64
        self.off = off + nbytes
        assert self.off <= self.cap, (self.off, self.cap)
        return off

    def f32(self, n, name=""):
        o = self.alloc(n * 4)
        return Buf(self.t32[:, o // 4:o // 4 + n], name)

    def b16(self, n, name=""):
        o = self.alloc(n * 2)
        return Buf(self.t16[:, o // 2:o // 2 + n], name)

    def u32(self, n, name=""):
        o = self.alloc(n * 4)
        return Buf(self.tu[:, o // 4:o // 4 + n], name)


def bc(ap, dims):
    return bass.AP(ap.tensor, ap.offset, [list(ap.ap[0])] + [list(d) for d in dims])


def r3(ap, **kw):
    return ap.rearrange("p (a b) -> p a b", **kw)


KDBG = os.environ.get('KDBG', '')
SLOPES = [2.0 ** (-8.0 * (h + 1) / 16.0) for h in range(16)]


class _Stop(Exception):
    pass


def build_program(stop_after_a=False, stage=None):
    nc = bass.Bass("TRN2", target_bir_lowering=False)
    es = ExitStack()
    dt = lambda name, shape, dtype=F32, kind="ExternalInput": nc.dram_tensor(name, shape, dtype, kind=kind).ap()
    x_d = dt("x", [TOK, D])
    win_d = dt("win", [128, 8, 5632])
    wpw_d = dt("wpw", [128, 8, 1024])
    wo_d = dt("wo", [128, 8, 1024])
    wout_d = dt("wout", [128, 8, 1024])
    wq_d = dt("wq", [128, 8, 2048])
    vecs_d = dt("vecs", [128, 40])
    dww_d = dt("dww", [128, 248])
    sink_d = dt("sink", [128, 16])
    fg_d = dt("fg", [128, 1024])
    skT_d = dt("skT", [128, 256])
    pu_d = dt("pu", [16384, 1024])
    pv_d = dt("pv", [16384, 1024])
    out_d = dt("out", [TOK, D], F32, "ExternalOutput")
    ut_d = dt("ut_scr", [128, 128, 1024], BF16, "Internal")
    vb_d = dt("vb_scr", [16384, 1024], BF16, "Internal")
    wqb_d = dt("wqb_scr", [16, 128, 1024], BF16, "Internal")

    with es:
        P = Prog(nc, es)
        A = Arena(nc, 207 * 1024)
        banks = []
        psall = nc.alloc_psum_tensor("psall", [128, 4096], F32)
        psall16 = psall.bitcast(BF16)
        for i in range(8):
            banks.append((psall[:, i * 512:(i + 1) * 512], psall16[:, i * 1024:(i + 1) * 1024], Tok(f"bank{i}")))
        rr = {"i": 0}

        def nextbank(lst):
            rr["i"] += 1
            return banks[lst[rr["i"] % len(lst)]]

        ident = A.b16(128, "ident")
        identf = A.f32(128, "identf")
        ones16 = A.b16(128, "ones")
        vecs = A.f32(40, "vecs")
        dww = A.f32(248, "dww")
        esink = A.f32(16, "esink")
        small = A.f32(64, "small")
        out_tok = [Tok(f"out{i}") for i in range(TOK // 128)]

        P.dma("sp", vecs.ap, vecs_d, writes=[vecs])
        P.dma("sp", dww.ap, dww_d, writes=[dww])
        P.dma("sp", esink.ap, sink_d, writes=[esink])
        P.op("act", lambda e: e.activation(out=esink.ap, in_=esink.ap, func=AF.Exp), reads=[esink], writes=[esink])
        P.op("pool", lambda e: e.iota(identf.ap, [[1, 128]], base=0, channel_multiplier=-1,
                                      allow_small_or_imprecise_dtypes=True), writes=[identf])
        P.op("dve", lambda e: e.tensor_scalar(out=identf.ap, in0=identf.ap, scalar1=0.0, scalar2=None,
                                              op0=ALU.is_equal), reads=[identf], writes=[identf])
        P.op("dve", lambda e: e.tensor_copy(out=ident.ap, in_=identf.ap), reads=[identf], writes=[ident])
        P.op("pool", lambda e: e.memset(ones16.ap, 1.0 / 1024.0), writes=[ones16])
        G1, DWB, LNG, LNB, G2 = range(5)
        vec = lambda idx, k: vecs.ap[:, idx * 8 + k:idx * 8 + k + 1]

        mark0 = A.off
        Mtab = A.b16(3 * 16 * 128, "Mtab")
        Mv = Mtab.ap.rearrange("p (a h q) -> p a h q", a=3, h=16)
        hT = A.b16(8 * SEQ)
        hTv = r3(hT.ap, a=8)
        hT_tok = [Tok() for _ in range(16)]
        QT = A.b16(8 * SEQ)
        QTv = r3(QT.ap, a=8)
        QT_tok = [Tok() for _ in range(16)]
        cT = A.b16(8 * SEQ)
        cTv = r3(cT.ap, a=8)
        cT_tok = [[Tok() for _ in range(4)] for _ in range(8)]
        r4 = A.alloc(32768)
        KTv = r3(A.t16[:, r4 // 2:r4 // 2 + 4 * SEQ], a=4)
        KT_tok = Tok()
        Vo = r4 + 16384
        Vv = A.t16[:, Vo // 2:Vo // 2 + 16 * 4 * 65].rearrange("p (t g e) -> p t g e", t=16, g=4)
        V_tok = [Tok() for _ in range(16)]
        dgs = [Buf(A.t16[:, (r4 + i * 8192) // 2:(r4 + i * 8192) // 2 + 31 * 128]) for i in range(2)]
        Ub = [Buf(A.t16[:, (r4 + 16384 + i * 4224) // 2:(r4 + 16384 + i * 4224) // 2 + 2078]) for i in range(2)]
        mTv = r3(A.t16[:, r4 // 2:r4 // 2 + 8 * SEQ], a=8)
        mT_tok = [Tok() for _ in range(4)]
        wst = [A.f32(2048) for _ in range(2)]
        wbf = [A.b16(2048) for _ in range(4)]
        wst_h = [Buf(b.ap[:, h * 1024:(h + 1) * 1024]) for b in wst for h in range(2)]
        wbf_h = [Buf(b.ap[:, h * 1024:(h + 1) * 1024]) for b in wbf for h in range(2)]
        xts = [A.f32(1024) for _ in range(2)]
        xns = [A.b16(1024) for _ in range(2)]
        junk = A.b16(1024)
        w12 = A.alloc(12288)
        Ef = [Buf(A.t32[:, (w12 + i * 2048) // 4:(w12 + i * 2048) // 4 + 512]) for i in range(3)]
        PT = [Buf(A.t16[:, (w12 + 6144 + i * 1024) // 2:(w12 + 6144 + i * 1024) // 2 + 512]) for i in range(6)]
        lnm = Buf(A.t32[:, (w12) // 4:(w12) // 4 + 512])
        lnr = Buf(A.t32[:, (w12 + 2048) // 4:(w12 + 2048) // 4 + 512])
        lnt = [Buf(A.t32[:, (w12 + 4096 + i * 2048) // 4:(w12 + 4096 + i * 2048) // 4 + 512]) for i in range(2)]
        lnq = [Buf(A.t16[:, (w12 + 8192 + i * 1024) // 2:(w12 + 8192 + i * 1024) // 2 + 512]) for i in range(2)]
        wkd = Buf(A.t16[:, w12 // 2:w12 // 2 + 4096])
        wkdv = wkd.ap.rearrange("p (k g e) -> p k g e", k=8, g=4)
        AO = [A.b16(1024) for _ in range(2)]
        den = A.f32(16)
        rec = A.f32(16)
        ss_i = {"i": 0}
        small_cur = {"tok": None}
        wst_i = {"i": 0}
        wbf_i = {"i": 0}

        small_toks = [Tok() for _ in range(32)]

        def new_small():
            ss_i["i"] = (ss_i["i"] + 1) % 32
            i = ss_i["i"]
            small_cur["tok"] = small_toks[i]
            return small.ap[:, 2 * i:2 * i + 1], small.ap[:, 2 * i + 1:2 * i + 2]

        def load_w(src3, col0, ncols, scale_idx=None, eng="pool", half=False):
            wst_i["i"] += 1
            wbf_i["i"] += 1
            if half:
                st = wst_h[wst_i["i"] % 4]
                wb = wbf_h[wbf_i["i"] % 8]
            else:
                st = wst[wst_i["i"] % 2]
                wb = wbf[wbf_i["i"] % 4]
            stv = r3(st.ap[:, 0:8 * ncols], a=8)
            wbv = r3(wb.ap[:, 0:8 * ncols], a=8)
            P.dma("sp", stv, src3[:, :, col0:col0 + ncols], writes=[st])
            assert scale_idx is None
            P.op(eng, lambda e: e.tensor_copy(out=wb.ap[:, 0:8 * ncols], in_=st.ap[:, 0:8 * ncols]), reads=[st], writes=[wb])
            return wb, wbv

        def rms_tile(xt, xn):
            ss, rstd = new_small()
            sm = small_cur["tok"]
            P.op("act", lambda e: e.activation(out=junk.ap, in_=xt.ap, func=AF.Square, accum_out=ss),
                 reads=[xt], writes=[junk, sm])
            P.op("act", lambda e: e.activation(out=rstd, in_=ss, func=AF.Sqrt, scale=1.0 / D, bias=EPS),
                 reads=[sm], writes=[sm])
            P.op("dve", lambda e: e.reciprocal(out=rstd, in_=rstd), reads=[sm], writes=[sm])
            if xn is not None:
                P.op("dve", lambda e: e.tensor_scalar(out=xn.ap, in0=xt.ap, scalar1=rstd, scalar2=None, op0=ALU.mult),
                     reads=[xt, sm], writes=[xn])
            return rstd

        def transpose8(src, dst_fn, dst_toks, evac_eng="act", scale_idx=None, bl=(4,)):
            bt, bt16, btok = nextbank(list(bl))
            for k in range(8):
                P.op("pe", lambda e, k=k: e.transpose(out=bt16[:, k * 128:(k + 1) * 128], in_=src.ap[:, k * 128:(k + 1) * 128],
                                                      identity=ident.ap), reads=[src, ident], writes=[btok])
            if scale_idx is None:
                P.op(evac_eng, lambda e: (e.copy if evac_eng == "act" else e.tensor_copy)(
                    out=dst_fn(None), in_=r3(bt16[:, 0:1024], a=8)), reads=[btok], writes=dst_toks)
            else:
                for k in range(8):
                    if evac_eng == "act" or (evac_eng == "mix" and k % 2 == 0):
                        P.op("act", lambda e, k=k: e.activation(out=dst_fn(k), in_=bt16[:, k * 128:(k + 1) * 128], func=AF.Copy,
                                                                scale=vec(scale_idx, k)), reads=[btok, vecs], writes=dst_toks)
                    else:
                        P.op("dve", lambda e, k=k: e.tensor_scalar(out=dst_fn(k), in0=bt16[:, k * 128:(k + 1) * 128],
                                                                   scalar1=vec(scale_idx, k), scalar2=None, op0=ALU.mult),
                             reads=[btok, vecs], writes=dst_toks)

        Df = Ef[0]
        Am = Ef[1]
        Mf = Ef[2]
        for pos in range(3):
            P.op("pool", lambda e, pos=pos: e.iota(Df.ap[:, 0:128], [[1, 128]], base=128 * (1 - pos), channel_multiplier=-1,
                                                   allow_small_or_imprecise_dtypes=True), writes=[Df])
            P.op("act", lambda e: e.activation(out=Df.ap[:, 0:128], in_=Df.ap[:, 0:128], func=AF.Abs),
                 reads=[Df], writes=[Df])
            P.op("dve", lambda e: e.tensor_scalar(out=Am.ap[:, 0:128], in0=Df.ap[:, 0:128], scalar1=128.0, scalar2=None,
                                                  op0=ALU.is_le), reads=[Df], writes=[Am])
            for h in range(16):
                P.op("act", lambda e, h=h: e.activation(out=Mf.ap[:, 0:128], in_=Df.ap[:, 0:128], func=AF.Exp, scale=-SLOPES[h]),
                     reads=[Df], writes=[Mf])
                P.op("dve", lambda e, pos=pos, h=h: e.tensor_tensor(out=Mv[:, pos, h, :], in0=Mf.ap[:, 0:128], in1=Am.ap[:, 0:128],
                                                                    op=ALU.mult), reads=[Mf, Am], writes=[Mtab])
        P.barrier()

        def chk(name):
            if stage == name:
                raise _Stop()

        try:
          for s in range(2):
            tb0 = s * SEQ
            chk('S0')
            def s1_pre(tt):
                xt = xts[tt % 2]
                xn = xns[tt % 2]
                P.dma("sp", xt.ap, x_d[tb0 + tt * 128:tb0 + (tt + 1) * 128, :], writes=[xt])
                rms_tile(xt, xn)

            s1_pre(0)
            for tt in range(16):
                if tt + 1 < 16:
                    s1_pre(tt + 1)
                transpose8(xns[tt % 2], lambda k, tt=tt: hTv[:, k, tt * 128:(tt + 1) * 128], [hT_tok[tt]], evac_eng="mix", scale_idx=G1,
                           bl=(4, 5, 6, 7))

            chk('S1')
            ev_i = 0
            for cp in range(4):
                wb, wbv = load_w(win_d, 2048 + cp * 256, 256)
                for cc in range(2):
                    c = cp * 2 + cc
                    for b in range(4):
                        bt, _, btok = nextbank([0, 1, 2, 3])
                        P.opn("pe", [lambda e, k=k, cc=cc, b=b, wbv=wbv, bt=bt: e.matmul(
                            bt[:, :], lhsT=wbv[:, k, cc * 128:(cc + 1) * 128], rhs=hTv[:, k, b * 512:(b + 1) * 512],
                            start=(k == 0), stop=(k == 7)) for k in range(8)], reads=[wb] + hT_tok[4 * b:4 * b + 4], writes=[btok])
                        dst = QTv[:, c, b * 512:(b + 1) * 512]
                        ev_i += 1
                        if ev_i % 2:
                            P.op("act", lambda e, dst=dst, bt=bt: e.mul(out=dst, in_=bt[:, :], mul=0.125),
                                 reads=[btok], writes=QT_tok[4 * b:4 * b + 4])
                        else:
                            P.op("dve", lambda e, dst=dst, bt=bt: e.tensor_scalar(out=dst, in0=bt[:, :], scalar1=0.125, scalar2=None,
                                                                                  op0=ALU.mult), reads=[btok], writes=QT_tok[4 * b:4 * b + 4])
            wst_i["i"] += 1
            st = wst[wst_i["i"] % 2]
            stv = r3(st.ap, a=8)
            P.dma("sp", stv, win_d[:, :, 3072:3328], writes=[st])
            for half in range(2):
                P.op("pool", lambda e, half=half: e.tensor_copy(
                    out=wkdv[:, :, :, half * 64:(half + 1) * 64], in_=st.ap.rearrange("p (k g e) -> p k g e", k=8, g=4)),
                    reads=[st], writes=[wkd])
            for g in range(4):
                for b in range(4):
                    bt, _, btok = nextbank([0, 1, 2, 3])
                    for k in range(8):
                        P.op("pe", lambda e, k=k, g=g, b=b, bt=bt: e.matmul(
                            bt[:, :], lhsT=wkdv[:, k, g, :], rhs=hTv[:, k, b * 512:(b + 1) * 512],
                            start=(k == 0), stop=(k == 7)), reads=[wkd] + hT_tok[4 * b:4 * b + 4], writes=[btok])
                    P.op("act", lambda e, g=g, b=b, bt=bt: e.copy(out=KTv[:, g, b * 512:(b + 1) * 512], in_=bt[:, :]),
                         reads=[btok], writes=[KT_tok])
            wb, wbv = load_w(win_d, 3328, 256)
            P.op("pool", lambda e: e.memset(Vv[:, :, :, 64:65], 1.0), writes=V_tok)
            for tt in range(16):
                bt, _, btok = nextbank([0, 1, 2, 3])
                for k in range(8):
                    P.op("pe", lambda e, k=k, tt=tt, wbv=wbv, bt=bt: e.matmul(
                        bt[:, 0:256], lhsT=hTv[:, k, tt * 128:(tt + 1) * 128], rhs=wbv[:, k, :],
                        start=(k == 0), stop=(k == 7)), reads=[wb, hT_tok[tt]], writes=[btok])
                P.op("dve", lambda e, tt=tt, bt=bt: e.tensor_copy(out=Vv[:, tt, :, 0:64],
                                                                  in_=bt[:, 0:256].rearrange("p (g e) -> p g e", g=4)),
                     reads=[btok], writes=[V_tok[tt]])

            chk('S2')
            cnt3 = {"pt": 0, "ef": 0}
            po = [banks[5], banks[6], banks[7]]

            def st_phase(i, g):
                kbs = [kb for kb in (i - 1, i, i + 1) if 0 <= kb < 16]
                pts = []
                for kb in kbs:
                    pos = kb - i + 1
                    rr["pair"] = rr.get("pair", 0) + 1
                    b0 = 2 * (rr["pair"] % 2)
                    pair = (banks[b0], banks[b0 + 1])
                    for j in range(4):
                        h = 4 * g + j
                        c, p = h // 2, h % 2
                        bt, _, btok = pair[p]
                        jj = j // 2
                        P.op("pe", lambda e, jj=jj, g=g, kb=kb, c=c, p=p, i=i, bt=bt: e.matmul(
                            bt[:, jj * 128:(jj + 1) * 128], lhsT=KTv[64 * p:64 * p + 64, g, kb * 128:(kb + 1) * 128],
                            rhs=QTv[64 * p:64 * p + 64, c, i * 128:(i + 1) * 128], start=True, stop=True),
                            reads=[KT_tok, QT_tok[i]], writes=[btok])
                    cnt3["ef"] += 1
                    ef = Ef[cnt3["ef"] % 3]
                    src2 = bc(pair[0][0][:, 0:1], [[512, 2], [1, 256]])
                    P.op("act", lambda e, ef=ef, src2=src2: e.activation(out=ef.ap.rearrange("p (a b) -> p a b", a=2), in_=src2,
                                                                       func=AF.Exp),
                         reads=[pair[0][2], pair[1][2]], writes=[ef])
                    cnt3["pt"] += 1
                    pt = PT[cnt3["pt"] % 6]
                    m4 = bc(Mv[:, pos, 4 * g, :], [[128, 2], [256, 2], [1, 128]])
                    P.op("dve", lambda e, ef=ef, pt=pt, m4=m4: e.tensor_tensor(
                        out=pt.ap.rearrange("p (a b q) -> p a b q", a=2, b=2), in0=ef.ap.rearrange("p (a b q) -> p a b q", a=2, b=2),
                        in1=m4, op=ALU.mult), reads=[ef, Mtab], writes=[pt])
                    pts.append(pt)
                return pts

            def pv_phase(i, g, pts):
                kbs = [kb for kb in (i - 1, i, i + 1) if 0 <= kb < 16]
                for j in range(4):
                    h = 4 * g + j
                    pb, _, pbtok = po[h // 7]
                    o0 = (h % 7) * 65
                    P.opn("pe", [lambda e, j=j, g=g, kb=kb, n=n, pb=pb, o0=o0, pt=pts[n], last=len(kbs) - 1: e.matmul(
                        pb[:, o0:o0 + 65], lhsT=pt.ap[:, (j % 2) * 256 + (j // 2) * 128:(j % 2) * 256 + (j // 2) * 128 + 128], rhs=Vv[:, kb, g, :],
                        start=(n == 0), stop=(n == last)) for n, kb in enumerate(kbs)],
                        reads=list(pts) + [V_tok[kb] for kb in kbs], writes=[pbtok])

            items3 = [(i, g) for i in range(16) for g in range(4)]
            pend3 = st_phase(*items3[0])
            for idx3, (i, g) in enumerate(items3):
                nxt3 = st_phase(*items3[idx3 + 1]) if idx3 + 1 < len(items3) else None
                pv_phase(i, g, pend3)
                pend3 = nxt3
                if g != 3:
                    continue
                if stage in ('S3a', 'S3b'):
                    continue
                ao = AO[i % 2]
                for b3, (h0, nh) in enumerate(((0, 7), (7, 7), (14, 2))):
                    pb, _, pbtok = po[b3]
                    pv = pb[:, 0:nh * 65].rearrange("p (h e) -> p h e", e=65)
                    P.op("dve", lambda e, pv=pv, h0=h0, nh=nh: e.tensor_tensor(
                        out=den.ap[:, h0:h0 + nh], in0=pv[:, :, 64], in1=esink.ap[:, h0:h0 + nh], op=ALU.add),
                        reads=[pbtok, esink], writes=[den])
                P.op("dve", lambda e: e.reciprocal(out=rec.ap, in_=den.ap), reads=[den], writes=[rec])
                for b3, (h0, nh) in enumerate(((0, 7), (7, 7), (14, 2))):
                    pb, _, pbtok = po[b3]
                    pv = pb[:, 0:nh * 65].rearrange("p (h e) -> p h e", e=65)
                    P.op("dve", lambda e, pv=pv, h0=h0, nh=nh, ao=ao: e.tensor_tensor(
                        out=ao.ap[:, h0 * 64:(h0 + nh) * 64].rearrange("p (h e) -> p h e", e=64), in0=pv[:, :, 0:64],
                        in1=bc(rec.ap[:, h0:h0 + nh], [[1, nh], [0, 64]]), op=ALU.mult),
                        reads=[pbtok, rec], writes=[ao])
                if stage == 'S3c':
                    continue
                transpose8(ao, lambda k, i=i: QTv[:, :, i * 128:(i + 1) * 128], [QT_tok[i]])
            chk('S3'); chk('S3a'); chk('S3b'); chk('S3c')
            P.barrier()

            for u in Ub:
                P.op("pool", lambda e, u=u: e.memset(u.ap[:, 0:15], 0.0), writes=[u])
                P.op("pool", lambda e, u=u: e.memset(u.ap[:, 2063:2078], 0.0), writes=[u])
            sg_i = 0
            for cp in range(4):
                wa, wav = load_w(win_d, cp * 256, 256)
                wg, wgv = load_w(win_d, 1024 + cp * 256, 256)
                for cc in range(2):
                    c = cp * 2 + cc
                    u = Ub[c % 2]
                    for b in range(4):
                        ba, _, batok = nextbank([0, 1, 2, 3])
                        bg, _, bgtok = nextbank([0, 1, 2, 3])
                        for (wt, wtv, bt, btok) in ((wa, wav, ba, batok), (wg, wgv, bg, bgtok)):
                            P.opn("pe", [lambda e, k=k, cc=cc, b=b, wtv=wtv, bt=bt: e.matmul(
                                bt[:, :], lhsT=wtv[:, k, cc * 128:(cc + 1) * 128], rhs=hTv[:, k, b * 512:(b + 1) * 512],
                                start=(k == 0), stop=(k == 7)) for k in range(8)], reads=[wt] + hT_tok[4 * b:4 * b + 4], writes=[btok])
                        sg_i += 1
                        sg = Ef[sg_i % 3]
                        P.op("act", lambda e, sg=sg, bg=bg: e.activation(out=sg.ap, in_=bg[:, :], func=AF.Sigmoid),
                             reads=[bgtok], writes=[sg])
                        P.op("dve", lambda e, sg=sg, ba=ba, u=u, b=b: e.tensor_tensor(
                            out=u.ap[:, 15 + b * 512:15 + (b + 1) * 512], in0=ba[:, :], in1=sg.ap, op=ALU.mult),
                            reads=[batok, sg], writes=[u])
                    dg = dgs[c % 2]
                    P.op("dve", lambda e, dg=dg, c=c: e.tensor_tensor(
                        out=dg.ap.rearrange("p (t j) -> p t j", t=31), in0=bc(ident.ap[:, 0:1], [[0, 31], [1, 128]]),
                        in1=bc(dww.ap[:, c * 31:c * 31 + 1], [[1, 31], [0, 128]]), op=ALU.mult), reads=[ident, dww], writes=[dg])
                    for b in range(4):
                        bt, _, btok = nextbank([0, 1, 2, 3])
                        P.opn("pe", [lambda e, tap=tap, dg=dg, u=u, b=b, bt=bt: e.matmul(
                            bt[:, :], lhsT=dg.ap[:, tap * 128:(tap + 1) * 128], rhs=u.ap[:, tap + b * 512:tap + b * 512 + 512],
                            start=(tap == 0), stop=(tap == 30)) for tap in range(31)], reads=[dg, u], writes=[btok])
                        P.op("act", lambda e, c=c, b=b, bt=bt: e.activation(out=cTv[:, c, b * 512:(b + 1) * 512], in_=bt[:, :],
                                                                            func=AF.Identity, bias=vec(DWB, c)),
                             reads=[btok, vecs], writes=[cT_tok[c][b]])
            P.barrier()
            for b in range(4):
                bs_, _, bstok = nextbank([0, 1, 2, 3])
                bq_, _, bqtok = nextbank([0, 1, 2, 3])
                blk = slice(b * 512, (b + 1) * 512)
                for c in range(8):
                    sq = lnq[c % 2]
                    P.op("act", lambda e, sq=sq, c=c, blk=blk: e.activation(out=sq.ap, in_=cTv[:, c, blk], func=AF.Square),
                         reads=[cT_tok[c][b]], writes=[sq])
                    P.op("pe", lambda e, c=c, blk=blk, bs_=bs_: e.matmul(bs_[:, :], lhsT=ones16.ap, rhs=cTv[:, c, blk],
                                                                       start=(c == 0), stop=(c == 7)),
                         reads=[ones16, cT_tok[c][b]], writes=[bstok])
                    P.op("pe", lambda e, c=c, sq=sq, bq_=bq_: e.matmul(bq_[:, :], lhsT=ones16.ap, rhs=sq.ap,
                                                                     start=(c == 0), stop=(c == 7)),
                         reads=[ones16, sq], writes=[bqtok])
                P.op("act", lambda e, bs_=bs_: e.copy(out=lnm.ap, in_=bs_[:, :]), reads=[bstok], writes=[lnm])
                P.op("dve", lambda e: e.tensor_tensor(out=lnr.ap, in0=lnm.ap, in1=lnm.ap, op=ALU.mult), reads=[lnm], writes=[lnr])
                P.op("dve", lambda e, bq_=bq_: e.tensor_tensor(out=lnr.ap, in0=bq_[:, :], in1=lnr.ap, op=ALU.subtract),
                     reads=[bqtok, lnr], writes=[lnr])
                P.op("act", lambda e: e.activation(out=lnr.ap, in_=lnr.ap, func=AF.Sqrt, bias=EPS), reads=[lnr], writes=[lnr])
                P.op("dve", lambda e: e.reciprocal(out=lnr.ap, in_=lnr.ap), reads=[lnr], writes=[lnr])
                for c in range(8):
                    t1 = lnt[c % 2]
                    P.op("dve", lambda e, t1=t1, c=c, blk=blk: e.tensor_tensor(out=t1.ap, in0=cTv[:, c, blk], in1=lnm.ap, op=ALU.subtract),
                         reads=[cT_tok[c][b], lnm], writes=[t1])
                    P.op("dve", lambda e, t1=t1: e.tensor_tensor(out=t1.ap, in0=t1.ap, in1=lnr.ap, op=ALU.mult),
                         reads=[t1, lnr], writes=[t1])
                    P.op("act", lambda e, t1=t1, c=c, blk=blk: e.activation(out=cTv[:, c, blk], in_=t1.ap, func=AF.Silu,
                                                                           scale=vec(LNG, c), bias=vec(LNB, c)),
                         reads=[t1, vecs], writes=[cT_tok[c][b]])
            chk('S4')
            P.barrier()

            sg_i = 0
            for c in range(8):
                w1, w1v = load_w(wpw_d, c * 128, 128, half=True)
                w2, w2v = load_w(wo_d, c * 128, 128, half=True)
                w3, w3v = load_w(win_d, 3584 + c * 128, 128, half=True)
                w4, w4v = load_w(win_d, 4608 + c * 128, 128, half=True)
                for b in range(4):
                    blk = slice(b * 512, (b + 1) * 512)
                    bks = [nextbank([0, 1, 2, 3]) for _ in range(4)]
                    srcs = ((w1, w1v, cTv, [cT_tok[k][b] for k in range(8)]),
                            (w2, w2v, QTv, QT_tok[4 * b:4 * b + 4]),
                            (w3, w3v, hTv, hT_tok[4 * b:4 * b + 4]),
                            (w4, w4v, hTv, hT_tok[4 * b:4 * b + 4]))
                    for (wt, wtv, src, stoks), (bt, _, btok) in zip(srcs, bks):
                        P.opn("pe", [lambda e, k=k, wtv=wtv, src=src, bt=bt, blk=blk: e.matmul(
                            bt[:, :], lhsT=wtv[:, k, 0:128], rhs=src[:, k, blk], start=(k == 0), stop=(k == 7)) for k in range(8)],
                            reads=[wt] + list(stoks), writes=[btok])
                    sa = Ef[0]
                    sb_ = Ef[1]
                    m1 = Ef[2]
                    P.op("act", lambda e, bt=bks[2][0]: e.activation(out=sa.ap, in_=bt[:, :], func=AF.Sigmoid),
                         reads=[bks[2][2]], writes=[sa])
                    P.op("act", lambda e, bt=bks[3][0]: e.activation(out=sb_.ap, in_=bt[:, :], func=AF.Sigmoid),
                         reads=[bks[3][2]], writes=[sb_])
                    P.op("dve", lambda e, bt=bks[0][0]: e.tensor_tensor(out=m1.ap, in0=bt[:, :], in1=sa.ap, op=ALU.mult),
                         reads=[bks[0][2], sa], writes=[m1])
                    P.op("dve", lambda e, bt=bks[1][0]: e.tensor_tensor(out=sb_.ap, in0=bt[:, :], in1=sb_.ap, op=ALU.mult),
                         reads=[bks[1][2], sb_], writes=[sb_])
                    P.op("pool", lambda e, c=c, blk=blk: e.tensor_tensor(out=mTv[:, c, blk], in0=m1.ap, in1=sb_.ap, op=ALU.add),
                         reads=[m1, sb_], writes=[mT_tok[b]])

            chk('S5')
            P.barrier()
            wo4 = [load_w(wout_d, q4 * 256, 256) for q4 in range(4)]
            for tt in range(16):
                xt = xts[tt % 2]
                gt = (tb0 // 128) + tt
                P.dma("sp", xt.ap, x_d[tb0 + tt * 128:tb0 + (tt + 1) * 128, :], writes=[xt])
                for half in range(2):
                    bt, _, btok = nextbank([0, 1, 2, 3])
                    for q2 in range(2):
                        wb, wbv = wo4[half * 2 + q2]
                        P.opn("pe", [lambda e, k=k, tt=tt, q2=q2, wbv=wbv, bt=bt: e.matmul(
                            bt[:, q2 * 256:(q2 + 1) * 256], lhsT=mTv[:, k, tt * 128:(tt + 1) * 128], rhs=wbv[:, k, :],
                            start=(k == 0), stop=(k == 7)) for k in range(8)], reads=[wb, mT_tok[tt // 4]], writes=[btok])
                    P.op("dve", lambda e, half=half, bt=bt, xt=xt: e.tensor_tensor(
                        out=xt.ap[:, half * 512:(half + 1) * 512], in0=bt[:, :], in1=xt.ap[:, half * 512:(half + 1) * 512],
                        op=ALU.add), reads=[btok, xt], writes=[xt])
                P.dma("act", out_d[gt * 128:(gt + 1) * 128, :], xt.ap, reads=[xt], writes=[out_tok[gt]], key=xt)
            chk('S6')
            P.barrier()
        except _Stop:
            pass

        if not stop_after_a:
            A.off = mark0
            build_peer(nc, P, A, banks, nextbank, dict(
                ident=ident, identf=identf, vecs=vecs, vec=vec, G2=G2, small=small, new_small=new_small, small_cur=small_cur,
                out_tok=out_tok, out_d=out_d, fg_d=fg_d, skT_d=skT_d, pu_d=pu_d, pv_d=pv_d, wq_d=wq_d,
                ut_d=ut_d, vb_d=vb_d, wqb_d=wqb_d))
        P.emit()
    return nc


def build_peer(nc, P, A, banks, nextbank, C):
    ident, identf, vecs, vec, G2 = C["ident"], C["identf"], C["vecs"], C["vec"], C["G2"]
    small, new_small, out_tok, out_d = C["small"], C["new_small"], C["out_tok"], C["out_d"]
    small_cur = C["small_cur"]
    ut_d, vb_d, wqb_d = C["ut_d"], C["vb_d"], C["wqb_d"]
    NBLK = TOK // TB
    NT = TB // 128
    mark = A.off
    NPB = 4
    pst = [A.f32(1024) for _ in range(NPB)]
    pst2 = [A.f32(1024) for _ in range(NPB)]
    pbf = [A.b16(1024) for _ in range(NPB)]
    pbf2 = [A.b16(1024) for _ in range(NPB)]
    utb = [A.b16(1024) for _ in range(2)]
    assert A.off - mark <= 128 * TB * 2
    A.off = mark
    G = A.b16(128 * TB)
    Gv = G.ap.rearrange("p (i t) -> p i t", i=128)
    h2T = [A.b16(8 * TB) for _ in range(2)]
    h2T_toks = [[Tok() for _ in range(NT)] for _ in range(2)]
    qT = A.f32(16 * TB)
    qTv = r3(qT.ap, a=16)
    sc = [A.f32(2048)] * 2
    sc2 = A.f32(256)
    v16 = A.f32(256)
    v16v = r3(v16.ap, a=16)
    ix = A.u32(256)
    ixv = r3(ix.ap, a=16)
    ixf = A.f32(256)
    cand = sc[0]
    eqb = A.f32(2048)
    eq2 = cand
    ts = A.f32(128)
    tsv = r3(ts.ap, a=8)
    pos = A.u32(128)
    posv = r3(pos.ap, a=8)
    posf = A.f32(128)
    k1f = A.f32(128)
    k2f = A.f32(128)
    Ivs = [A.f32(128) for _ in range(2)]
    Jvs = [A.f32(128) for _ in range(2)]
    Wvs = [A.f32(128) for _ in range(2)]
    ew = A.f32(128)
    zz = A.f32(16)
    SM = A.f32(3 * TB)
    SMv = r3(SM.ap, a=3)
    iota128 = A.b16(128)
    iotaf = A.f32(128)
    skT = A.f32(256)
    skTv = r3(skT.ap, a=2)
    fg = A.f32(1024)
    ohB = [A.b16(16 * 128) for _ in range(2)]
    ohE = [A.b16(16 * 128) for _ in range(2)]
    OAI = A.b16(TB * 8)
    OAJ = A.b16(TB * 8)
    OBI = A.b16(TB * 16)
    OBJ = A.b16(TB * 16)
    xh = A.f32(TB)
    xl = A.f32(TB)
    ubr = [A.b16(1024) for _ in range(4)]
    vbr = [A.b16(1024) for _ in range(4)]
    gelr = [A.f32(TB) for _ in range(3)]
    ATr = [A.b16(TB) for _ in range(4)]
    xts = [A.f32(1024) for _ in range(2)]
    xns = [A.b16(1024) for _ in range(2)]
    wqc = [A.b16(1024) for _ in range(3)]
    ut_tok = [Tok() for _ in range(128)]
    vb_tok = [Tok() for _ in range(128)]
    wqb_tok = [Tok() for _ in range(16)]

    P.dma("sp", skT.ap, C["skT_d"], writes=[skT])
    P.dma("sp", fg.ap, C["fg_d"], writes=[fg])
    P.op("pool", lambda e: e.iota(iotaf.ap, [[1, 128]], base=0, channel_multiplier=0, allow_small_or_imprecise_dtypes=True),
         writes=[iotaf])
    P.op("dve", lambda e: e.tensor_copy(out=iota128.ap, in_=iotaf.ap), reads=[iotaf], writes=[iota128])
    k16 = A.f32(16)
    nhalf = A.f32(1)
    P.op("pool", lambda e: e.memset(nhalf.ap, -0.5), writes=[nhalf])
    P.op("dve", lambda e: e.tensor_scalar(out=k16.ap, in0=iotaf.ap[:, 0:16], scalar1=16.0, scalar2=None, op0=ALU.mult),
         reads=[iotaf], writes=[k16])

    for m in range(16):
        st = pst[m % NPB]
        pb = pbf[m % NPB]
        P.dma("sp", r3(st.ap, a=8), C["wq_d"][:, :, m * 128:(m + 1) * 128], writes=[st])
        P.op("pool", lambda e, st=st, pb=pb: e.tensor_copy(out=pb.ap, in_=st.ap), reads=[st], writes=[pb])
        P.dma("act", wqb_d[m], pb.ap, reads=[pb], writes=[wqb_tok[m]], key=pb)
    def prep_uv(i):
        st = pst[i % NPB]
        pb = pbf[i % NPB]
        P.dma("sp", st.ap, C["pu_d"][i * 128:(i + 1) * 128, :], writes=[st])
        P.op("pool", lambda e, st=st, pb=pb: e.tensor_copy(out=pb.ap, in_=st.ap), reads=[st], writes=[pb])
        bt, bt16, btok = nextbank([4, 5, 6, 7])
        for k in range(8):
            P.op("pe", lambda e, k=k, pb=pb, bt16=bt16: e.transpose(out=bt16[:, k * 128:(k + 1) * 128], in_=pb.ap[:, k * 128:(k + 1) * 128],
                                                             identity=ident.ap), reads=[pb, ident], writes=[btok])
        ut = utb[i % 2]
        P.op("act", lambda e, ut=ut, bt16=bt16: e.copy(out=ut.ap, in_=bt16[:, 0:1024]), reads=[btok], writes=[ut])
        P.dma("act", ut_d[i], ut.ap, reads=[ut], writes=[ut_tok[i]], key=ut)
        st2 = pst2[i % NPB]
        pb2 = pbf2[i % NPB]
        P.dma("pool", st2.ap, C["pv_d"][i * 128:(i + 1) * 128, :], writes=[st2])
        P.op("dve", lambda e, st2=st2, pb2=pb2: e.tensor_copy(out=pb2.ap, in_=st2.ap), reads=[st2], writes=[pb2])
        P.dma("act", vb_d[i * 128:(i + 1) * 128, :], pb2.ap, reads=[pb2], writes=[vb_tok[i]], key=pb2)

    cnt = {"oh": 0, "ev": 0}

    def routing(nb):
        h2Tv = r3(h2T[nb % 2].ap, a=8)
        h2T_tok = h2T_toks[nb % 2]
        for tt in range(NT):
            gt = nb * NT + tt
            xt = xts[tt]
            xn = xns[tt]
            P.dma("sp", xt.ap, out_d[gt * 128:(gt + 1) * 128, :], reads=[out_tok[gt]], writes=[xt])
            ss, rstd = new_small()
            smt = small_cur["tok"]
            P.op("dve", lambda e, xt=xt, ss=ss: e.scalar_tensor_tensor(out=eqb.ap[:, 0:1024], in0=xt.ap, scalar=1.0, in1=xt.ap,
                                                                       op0=ALU.mult, op1=ALU.mult, accum_out=ss),
                 reads=[xt], writes=[eqb, smt])
            P.op("pool", lambda e, ss=ss: e.tensor_scalar(out=ss, in0=ss, scalar1=1.0 / D, scalar2=EPS, op0=ALU.mult, op1=ALU.add),
                 reads=[smt], writes=[smt])
            P.op("pool", lambda e, ss=ss, rstd=rstd: e.tensor_tensor(out=rstd, in0=ss, in1=nhalf.ap, op=ALU.pow),
                 reads=[smt, nhalf], writes=[smt])
            P.op("dve", lambda e, xt=xt, xn=xn, rstd=rstd: e.tensor_scalar(out=xn.ap, in0=xt.ap, scalar1=rstd, scalar2=None, op0=ALU.mult),
                 reads=[xt, smt], writes=[xn])
            bt, bt16, btok = nextbank([7])
            for k in range(8):
                P.op("pe", lambda e, k=k, xn=xn, bt16=bt16: e.transpose(out=bt16[:, k * 128:(k + 1) * 128], in_=xn.ap[:, k * 128:(k + 1) * 128],
                                                                 identity=ident.ap), reads=[xn, ident], writes=[btok])
            for k in range(8):
                P.op("dve", lambda e, k=k, tt=tt, bt16=bt16: e.tensor_scalar(
                    out=h2Tv[:, k, tt * 128:(tt + 1) * 128], in0=bt16[:, k * 128:(k + 1) * 128], scalar1=vec(G2, k), scalar2=None,
                    op0=ALU.mult), reads=[btok, vecs], writes=[h2T_tok[tt]])
        for m in range(16):
            wq = wqc[m % 3]
            P.dma("sp", wq.ap, wqb_d[m], reads=[wqb_tok[m]], writes=[wq])
            bt, _, btok = nextbank([7])
            for k in range(8):
                P.op("pe", lambda e, k=k, wq=wq, bt=bt: e.matmul(bt[:, 0:TB], lhsT=r3(wq.ap, a=8)[:, k, :], rhs=h2Tv[:, k, :],
                                                              start=(k == 0), stop=(k == 7)), reads=[wq] + h2T_tok, writes=[btok])
            cnt["ev"] += 1
            if cnt["ev"] % 2:
                P.op("act", lambda e, m=m, bt=bt: e.copy(out=qTv[:, m, :], in_=bt[:, 0:TB]), reads=[btok], writes=[qT])
            else:
                P.op("dve", lambda e, m=m, bt=bt: e.tensor_copy(out=qTv[:, m, :], in_=bt[:, 0:TB]), reads=[btok], writes=[qT])
        for tt in range(NT):
            Iv, Jv, Wv = Ivs[tt], Jvs[tt], Wvs[tt]
            scb = sc[tt]
            for bq in range(4):
                bt, _, btok = nextbank([7])
                for mm in range(4):
                    m = bq * 4 + mm
                    P.op("pe", lambda e, mm=mm, m=m, tt=tt, bt=bt: e.matmul(
                        bt[:, mm * 128:(mm + 1) * 128], lhsT=qTv[:, m, tt * 128:(tt + 1) * 128], rhs=skTv[:, m % 2, :],
                        start=True, stop=True), reads=[qT, skT], writes=[btok])
                P.op("dve", lambda e, bq=bq, bt=bt, scb=scb: e.tensor_copy(out=scb.ap[:, bq * 512:(bq + 1) * 512], in_=bt[:, :]),
                     reads=[btok], writes=[scb])
            for m in range(16):
                src = scb.ap[:, m * 128:(m + 1) * 128]
                P.op("dve", lambda e, m=m, src=src: e.max(out=v16v[:, m, 0:8], in_=src), reads=[scb], writes=[v16])
                P.op("dve", lambda e, m=m, src=src: e.max_index(out=ixv[:, m, 0:8], in_max=v16v[:, m, 0:8], in_values=src),
                     reads=[scb, v16], writes=[ix])
                P.op("dve", lambda e, m=m, src=src: e.match_replace(out=sc2.ap[:, 0:128], in_to_replace=v16v[:, m, 0:8], in_values=src,
                                                                    imm_value=-1e30), reads=[scb, v16], writes=[sc2])
                P.op("dve", lambda e, m=m: e.max(out=v16v[:, m, 8:16], in_=sc2.ap[:, 0:128]), reads=[sc2], writes=[v16])
                P.op("dve", lambda e, m=m: e.max_index(out=ixv[:, m, 8:16], in_max=v16v[:, m, 8:16], in_values=sc2.ap[:, 0:128]),
                     reads=[sc2, v16], writes=[ix])
            P.op("dve", lambda e: e.tensor_copy(out=ixf.ap, in_=ix.ap), reads=[ix], writes=[ixf])
            c4 = lambda b: b.ap.rearrange("p (h a b) -> p h a b", h=8, a=16)
            P.op("dve", lambda e: e.tensor_tensor(out=c4(cand), in0=bc(v16.ap[:, 0:1], [[32, 8], [1, 16], [0, 16]]),
                                                  in1=bc(v16.ap[:, 16:17], [[32, 8], [0, 16], [1, 16]]), op=ALU.add),
                 reads=[v16], writes=[cand])
            for h in range(8):
                src = cand.ap[:, h * 256:(h + 1) * 256]
                P.op("dve", lambda e, h=h, src=src: e.max(out=tsv[:, h, 0:8], in_=src), reads=[cand], writes=[ts])
                P.op("dve", lambda e, h=h, src=src: e.max_index(out=posv[:, h, 0:8], in_max=tsv[:, h, 0:8], in_values=src),
                     reads=[cand, ts], writes=[pos])
                P.op("dve", lambda e, h=h, src=src: e.match_replace(out=sc2.ap, in_to_replace=tsv[:, h, 0:8], in_values=src,
                                                                    imm_value=-1e30), reads=[cand, ts], writes=[sc2])
                P.op("dve", lambda e, h=h: e.max(out=tsv[:, h, 8:16], in_=sc2.ap), reads=[sc2], writes=[ts])
                P.op("dve", lambda e, h=h: e.max_index(out=posv[:, h, 8:16], in_max=tsv[:, h, 8:16], in_values=sc2.ap),
                     reads=[sc2, ts], writes=[pos])
            P.op("dve", lambda e: e.tensor_tensor(out=r3(ew.ap, a=8), in0=tsv, in1=bc(ts.ap[:, 0:1], [[16, 8], [0, 16]]), op=ALU.subtract),
                 reads=[ts], writes=[ew])
            P.op("act", lambda e: e.activation(out=ew.ap, in_=ew.ap, func=AF.Exp), reads=[ew], writes=[ew])
            P.op("dve", lambda e: e.tensor_reduce(out=zz.ap[:, 0:8], in_=r3(ew.ap, a=8), axis=AX.X, op=ALU.add), reads=[ew], writes=[zz])
            P.op("dve", lambda e: e.reciprocal(out=zz.ap[:, 8:16], in_=zz.ap[:, 0:8]), reads=[zz], writes=[zz])
            P.op("dve", lambda e, Wv=Wv: e.tensor_tensor(out=r3(Wv.ap, a=8), in0=r3(ew.ap, a=8), in1=bc(zz.ap[:, 8:9], [[1, 8], [0, 16]]), op=ALU.mult),
                 reads=[ew, zz], writes=[Wv])
            P.op("dve", lambda e: e.tensor_copy(out=posf.ap, in_=pos.ap), reads=[pos], writes=[posf])
            P.op("dve", lambda e: e.tensor_tensor(out=c4(eqb), in0=bc(posf.ap[:, 0:1], [[16, 8], [1, 16], [0, 16]]),
                                                  in1=bc(k16.ap[:, 0:1], [[0, 8], [0, 16], [1, 16]]), op=ALU.subtract),
                 reads=[k16, posf], writes=[eqb])
            P.op("dve", lambda e: e.tensor_scalar(out=eq2.ap, in0=eqb.ap, scalar1=0.0, scalar2=None, op0=ALU.is_ge),
                 reads=[eqb], writes=[eq2])
            P.op("dve", lambda e: e.scalar_tensor_tensor(out=eqb.ap, in0=eqb.ap, scalar=16.0, in1=eq2.ap, op0=ALU.is_lt, op1=ALU.mult),
                 reads=[eqb, eq2], writes=[eqb])
            P.op("dve", lambda e: e.tensor_tensor(out=c4(eq2), in0=c4(eqb), in1=bc(iotaf.ap[:, 0:1], [[0, 8], [0, 16], [1, 16]]), op=ALU.mult),
                 reads=[eqb, iotaf], writes=[eq2])
            P.op("dve", lambda e: e.tensor_reduce(out=k1f.ap, in_=c4(eq2), axis=AX.X, op=ALU.add), reads=[eq2], writes=[k1f])
            P.op("dve", lambda e: e.tensor_tensor(out=c4(eq2), in0=c4(eqb), in1=bc(ixf.ap[:, 0:1], [[32, 8], [0, 16], [1, 16]]), op=ALU.mult),
                 reads=[eqb, ixf], writes=[eq2])
            P.op("dve", lambda e, Iv=Iv: e.tensor_reduce(out=Iv.ap, in_=c4(eq2), axis=AX.X, op=ALU.add), reads=[eq2], writes=[Iv])
            P.op("dve", lambda e: e.scalar_tensor_tensor(out=k2f.ap, in0=k1f.ap, scalar=-16.0, in1=posf.ap, op0=ALU.mult, op1=ALU.add),
                 reads=[k1f, posf], writes=[k2f])
            P.op("dve", lambda e: e.tensor_tensor(out=c4(eqb), in0=bc(k2f.ap[:, 0:1], [[16, 8], [1, 16], [0, 16]]),
                                                  in1=bc(iotaf.ap[:, 0:1], [[0, 8], [0, 16], [1, 16]]), op=ALU.is_equal),
                 reads=[k2f, iotaf], writes=[eqb])
            P.op("dve", lambda e: e.tensor_tensor(out=c4(eq2), in0=c4(eqb), in1=bc(ixf.ap[:, 16:17], [[32, 8], [0, 16], [1, 16]]), op=ALU.mult),
                 reads=[eqb, ixf], writes=[eq2])
            P.op("dve", lambda e, Jv=Jv: e.tensor_reduce(out=Jv.ap, in_=c4(eq2), axis=AX.X, op=ALU.add), reads=[eq2], writes=[Jv])
        for tt in range(NT):
            Iv, Jv, Wv = Ivs[tt], Jvs[tt], Wvs[tt]
            bt, _, btok = nextbank([7])
            for qi, srcb in enumerate((Iv, Jv, Wv)):
                P.op("pe", lambda e, qi=qi, srcb=srcb, bt=bt: e.transpose(out=bt[:, qi * 128:(qi + 1) * 128], in_=srcb.ap, identity=identf.ap),
                     reads=[srcb, identf], writes=[btok])
            P.op("act", lambda e, tt=tt, bt=bt: e.copy(out=SMv[:, :, tt * 128:(tt + 1) * 128], in_=r3(bt[:, 0:384], a=3)),
                 reads=[btok], writes=[SM])
        t3 = lambda b, n: b.ap[:, 0:TB * n].rearrange("p (t a) -> p t a", a=n)
        for q, OA, OB in ((0, OAI, OBI), (1, OAJ, OBJ)):
            P.op("dve", lambda e, q=q: e.tensor_tensor(out=t3(eqb, 8), in0=bc(SMv[:, q, 0:1], [[1, TB], [0, 8]]),
                                                       in1=bc(k16.ap[:, 0:1], [[0, TB], [1, 8]]), op=ALU.subtract),
                 reads=[SM, k16], writes=[eqb])
            P.op("dve", lambda e: e.tensor_scalar(out=cand.ap, in0=eqb.ap, scalar1=0.0, scalar2=None, op0=ALU.is_ge),
                 reads=[eqb], writes=[cand])
            P.op("dve", lambda e, OA=OA: e.scalar_tensor_tensor(out=OA.ap, in0=eqb.ap, scalar=16.0, in1=cand.ap, op0=ALU.is_lt, op1=ALU.mult),
                 reads=[eqb, cand], writes=[OA])
            P.op("dve", lambda e, OA=OA: e.tensor_tensor(out=t3(eqb, 8), in0=t3(OA, 8), in1=bc(iotaf.ap[:, 0:1], [[0, TB], [1, 8]]), op=ALU.mult),
                 reads=[OA, iotaf], writes=[eqb])
            P.op("dve", lambda e: e.tensor_reduce(out=xh.ap, in_=t3(eqb, 8), axis=AX.X, op=ALU.add), reads=[eqb], writes=[xh])
            P.op("dve", lambda e, q=q: e.scalar_tensor_tensor(out=xl.ap, in0=xh.ap, scalar=-16.0, in1=SMv[:, q, :], op0=ALU.mult, op1=ALU.add),
                 reads=[xh, SM], writes=[xl])
            P.op("dve", lambda e, OB=OB: e.tensor_tensor(out=t3(OB, 16), in0=bc(xl.ap[:, 0:1], [[1, TB], [0, 16]]),
                                                         in1=bc(iotaf.ap[:, 0:1], [[0, TB], [1, 16]]), op=ALU.is_equal),
                 reads=[xl, iotaf], writes=[OB])
        P.op("dve", lambda e: e.tensor_tensor(out=t3(OAJ, 8), in0=t3(OAJ, 8), in1=bc(SMv[:, 2, 0:1], [[1, TB], [0, 8]]), op=ALU.mult),
             reads=[OAJ, SM], writes=[OAJ])

    def b5(nb):
        TG = 16
        for tg in range(TB // TG):
            t0 = tg * TG
            cnt["oh"] += 1
            ob, oc = ohB[cnt["oh"] % 2], ohE[cnt["oh"] % 2]
            o3 = lambda b: b.ap.rearrange("p (t i) -> p t i", t=TG)
            o4 = lambda b: b.ap.rearrange("p (t a c) -> p t a c", t=TG, a=8)
            P.op("dve", lambda e, oc=oc, t0=t0: e.tensor_tensor(
                out=o4(oc), in0=bc(OAI.ap[:, t0 * 8:t0 * 8 + 1], [[8, TG], [1, 8], [0, 16]]),
                in1=bc(OBI.ap[:, t0 * 16:t0 * 16 + 1], [[16, TG], [0, 8], [1, 16]]), op=ALU.mult), reads=[OAI, OBI], writes=[oc])
            P.op("dve" if tg % 5 == 4 else "pool", lambda e, ob=ob, t0=t0: e.tensor_tensor(
                out=o4(ob), in0=bc(OAJ.ap[:, t0 * 8:t0 * 8 + 1], [[8, TG], [1, 8], [0, 16]]),
                in1=bc(OBJ.ap[:, t0 * 16:t0 * 16 + 1], [[16, TG], [0, 8], [1, 16]]), op=ALU.mult), reads=[OAJ, OBJ], writes=[ob])
            for t4 in range(TG // 4):
                bt, _, btok = nextbank([6, 7])
                P.opn("pe", [lambda e, q=q, t4=t4, oc=oc, ob=ob, bt=bt: e.matmul(
                    bt[:, q * 128:(q + 1) * 128], lhsT=o3(ob)[:, t4 * 4 + q, :], rhs=o3(oc)[:, t4 * 4 + q, :], start=True, stop=True)
                    for q in range(4)], reads=[oc, ob], writes=[btok])
                ta = t0 + t4 * 4
                P.op("act", lambda e, ta=ta, bt=bt: e.copy(out=Gv[:, :, ta:ta + 4], in_=bc(bt[:, 0:1], [[1, 128], [128, 4]])),
                     reads=[btok], writes=[G])
    def b6(nb, pend):
        h2Tv = r3(h2T[nb % 2].ap, a=8)
        h2T_tok = h2T_toks[nb % 2]
        def u_side(i):
            ub = ubr[i % 4]
            vb = vbr[i % 4]
            P.dma("sp", ub.ap, ut_d[i], reads=[ut_tok[i]], writes=[ub])
            P.dma("sp", vb.ap, vb_d[i * 128:(i + 1) * 128, :], reads=[vb_tok[i]], writes=[vb])
            bs_, _, bstok = banks[4 + i % 3]
            P.opn("pe", [lambda e, k=k, ub=ub, bs_=bs_: e.matmul(bs_[:, 0:TB], lhsT=r3(ub.ap, a=8)[:, k, :], rhs=h2Tv[:, k, :],
                                                             start=(k == 0), stop=(k == 7)) for k in range(8)],
                  reads=[ub] + h2T_tok, writes=[bstok])
            return bs_, bstok, vb

        per = (len(pend) + 119) // 120 if pend else 0
        uq = [u_side(0), u_side(1)]
        for i in range(128):
            bs_, bstok, vb = uq.pop(0)
            if i + 2 < 128:
                uq.append(u_side(i + 2))
            gel = gelr[i % 3]
            P.op("act", lambda e, gel=gel, bs_=bs_: e.activation(out=gel.ap, in_=bs_[:, 0:TB], func=AF.Gelu), reads=[bstok], writes=[gel])
            at = ATr[i % 4]
            P.op("pool", lambda e, gel=gel, at=at, i=i: e.tensor_tensor(out=at.ap, in0=gel.ap, in1=Gv[:, i, :], op=ALU.mult),
                 reads=[gel, G], writes=[at])
            P.opn("pe", [lambda e, tt=tt, half=half, at=at, vb=vb, i=i: e.matmul(
                banks[tt * 2 + half][0][:, :], lhsT=at.ap[:, tt * 128:(tt + 1) * 128], rhs=vb.ap[:, half * 512:(half + 1) * 512],
                start=(i == 0), stop=(i == 127)) for tt in range(NT) for half in range(2)],
                reads=[at, vb], writes=[banks[b4][2] for b4 in range(2 * NT)])
            if pend:
                P.replay(pend, per)
        P.replay(pend, len(pend))

    def b7(nb):
        for tt in range(NT):
            gt = nb * NT + tt
            xt = xts[tt]
            P.dma("sp", xt.ap, out_d[gt * 128:(gt + 1) * 128, :], reads=[out_tok[gt]], writes=[xt])
            for half in range(2):
                ab, _, abtok = banks[tt * 2 + half]
                P.op("dve", lambda e, half=half, ab=ab, xt=xt: e.tensor_tensor(
                    out=xt.ap[:, half * 512:(half + 1) * 512], in0=ab[:, :], in1=xt.ap[:, half * 512:(half + 1) * 512], op=ALU.add),
                    reads=[abtok, xt], writes=[xt])
            ss, rstd = new_small()
            smt = small_cur["tok"]
            P.op("dve", lambda e, xt=xt, ss=ss: e.scalar_tensor_tensor(out=eqb.ap[:, 0:1024], in0=xt.ap, scalar=1.0, in1=xt.ap,
                                                                       op0=ALU.mult, op1=ALU.mult, accum_out=ss),
                 reads=[xt], writes=[eqb, smt])
            P.op("pool", lambda e, ss=ss: e.tensor_scalar(out=ss, in0=ss, scalar1=1.0 / D, scalar2=EPS, op0=ALU.mult, op1=ALU.add),
                 reads=[smt], writes=[smt])
            P.op("pool", lambda e, ss=ss, rstd=rstd: e.tensor_tensor(out=rstd, in0=ss, in1=nhalf.ap, op=ALU.pow),
                 reads=[smt, nhalf], writes=[smt])
            P.op("dve", lambda e, xt=xt, rstd=rstd: e.scalar_tensor_tensor(out=xt.ap, in0=xt.ap, scalar=rstd, in1=fg.ap,
                                                                           op0=ALU.mult, op1=ALU.mult), reads=[xt, smt, fg], writes=[xt])
            P.dma("act", out_d[gt * 128:(gt + 1) * 128, :], xt.ap, reads=[xt], writes=[out_tok[gt]], key=xt)


    routing(0)
    for i in range(128):
        prep_uv(i)
    P.barrier()
    for nb in range(NBLK):
        b5(nb)
        pend = []
        if nb + 1 < NBLK:
            P.capture()
            routing(nb + 1)
            pend = P.end_capture()
        b6(nb, pend)
        b7(nb)


def prep_inputs(inputs):
    f = lambda a: np.ascontiguousarray(np.asarray(a, dtype=np.float32))
    rk = lambda w: f(w.reshape(8, 128, -1).transpose(1, 0, 2))
    pv = lambda v: v.reshape(8, 128).T
    x = f(inputs["x"])
    vecs = np.concatenate([pv(np.asarray(inputs[n])[0]) for n in
                           ("norm1_g", "conv_dw_b", "conv_ln_g", "conv_ln_b", "norm2_g")], axis=1)
    dww = np.asarray(inputs["conv_dw_w"])[0].reshape(31, 8, 128).transpose(2, 1, 0).reshape(128, 248)
    shared = {
        "win": rk(np.asarray(inputs["w_in"])[0]),
        "wpw": rk(np.asarray(inputs["conv_w_pw"])[0]),
        "wo": rk(np.asarray(inputs["attn_w_o"])[0]),
        "wout": rk(np.asarray(inputs["w_out"])[0]),
        "wq": rk(np.asarray(inputs["peer_w_query"])[0]),
        "vecs": f(vecs),
        "dww": f(dww),
        "sink": f(np.broadcast_to(np.asarray(inputs["attn_sink"])[0][None, :], (128, 16))),
        "fg": f(np.broadcast_to(np.asarray(inputs["final_g"])[None, :], (128, 1024))),
        "skT": f(np.asarray(inputs["peer_sub_keys"])[0].transpose(2, 0, 1).reshape(128, 256)),
        "pu": f(np.asarray(inputs["peer_u"])[0]),
        "pv": f(np.asarray(inputs["peer_v"])[0]),
    }
    xs = x.reshape(NCORES, TOK, D)
    return [dict(shared, x=np.ascontiguousarray(xs[c])) for c in range(NCORES)]


_NC_CACHE = {}


def kernel(**inputs):
    in_maps = prep_inputs(inputs)
    if "nc" not in _NC_CACHE:
        _NC_CACHE["nc"] = build_program()
    res = run_bass_kernel_spmd(_NC_CACHE["nc"], in_maps, core_ids=list(range(NCORES)))
    out = np.stack([np.asarray(r["out"]) for r in res.results], axis=0)
    return out.reshape(16, SEQ, D).astype(np.float32)
```

```python
import os
import numpy as np
from contextlib import ExitStack
import concourse.bass as bass
import concourse.mybir as mybir
from concourse.bass_utils import run_bass_kernel_spmd

F32 = mybir.dt.float32
BF16 = mybir.dt.bfloat16
U32 = mybir.dt.uint32
AF = mybir.ActivationFunctionType
ALU = mybir.AluOpType
AX = mybir.AxisListType

NCORES = 8
TOK = 4096
SEQ = 2048
D = 1024
EPS = 1e-6
TB = 256


class Tok:
    __slots__ = ("w", "r", "sem", "cnt", "name")

    def __init__(self, name=""):
        self.w = None
        self.r = {}
        self.sem = None
        self.cnt = 0
        self.name = name


class Buf:
    __slots__ = ("ap", "tok")

    def __init__(self, ap, name=""):
        self.ap = ap
        self.tok = Tok(name)


class Prog:
    ENG = ("pe", "dve", "act", "pool", "sp")

    def __init__(self, nc, es):
        self.nc = nc
        self.es = es
        self.ops = {e: [] for e in self.ENG}
        self.cnt = {e: 0 for e in self.ENG}
        self.sems = {}
        for e in self.ENG:
            self.sems["E_" + e] = es.enter_context(nc.semaphore("s_" + e))
        self.final = {}
        self.waited = {e: {} for e in self.ENG}
        self.ndma = 0

    def _collect(self, eng, reads, writes):
        need = {}

        def add(ev, raw):
            if ev is None:
                return
            key, val, src = ev
            if src == eng and eng == "pe":
                return
            if need.get(key, 0) < val:
                need[key] = val

        for t in reads:
            add(t.w, True)
        for t in writes:
            add(t.w, False)
            for key, (val, src) in t.r.items():
                add((key, val, src), False)
        waits = []
        wd = self.waited[eng]
        for key, val in need.items():
            if wd.get(key, 0) < val:
                wd[key] = val
                waits.append((key, val))
        return waits

    def _commit(self, ev, reads, writes):
        key, val, src = ev
        for t in reads:
            old = t.r.get(key)
            if old is None or old[0] < val:
                t.r[key] = (val, src)
        for t in writes:
            t.w = ev
            t.r = {}

    cap = None

    def capture(self):
        self.cap = []

    def end_capture(self):
        c, self.cap = self.cap, None
        return c

    def replay(self, lst, n):
        for _ in range(min(n, len(lst))):
            kind, args = lst.pop(0)
            getattr(self, kind)(*args)

    def op(self, eng, fn, reads=(), writes=()):
        if self.cap is not None:
            self.cap.append(("op", (eng, fn, list(reads), list(writes))))
            return
        reads = [b.tok if isinstance(b, Buf) else b for b in reads]
        writes = [b.tok if isinstance(b, Buf) else b for b in writes]
        waits = self._collect(eng, reads, writes)
        self.cnt[eng] += 1
        key = "E_" + eng
        ev = (key, self.cnt[eng], eng)
        self.final[key] = self.cnt[eng]
        self.ops[eng].append((waits, fn, key, 1))
        self._commit(ev, reads, writes)

    def opn(self, eng, fns, reads=(), writes=()):
        fns = list(fns)

        def run(e, fns=fns):
            last = None
            for f in fns:
                last = f(e)
            return last

        self.op(eng, run, reads, writes)

    def dma(self, q, out, in_, reads=(), writes=(), key=None):
        if self.cap is not None:
            self.cap.append(("dma", (q, out, in_, list(reads), list(writes), key)))
            return
        reads = [b.tok if isinstance(b, Buf) else b for b in reads]
        writes = [b.tok if isinstance(b, Buf) else b for b in writes]
        kt = key if key is not None else (writes[0] if writes else reads[0])
        if isinstance(kt, Buf):
            kt = kt.tok
        if kt.sem is None:
            self.ndma += 1
            kt.sem = "D_%d" % self.ndma
            self.sems[kt.sem] = self.es.enter_context(self.nc.semaphore("d%d" % self.ndma))
        waits = self._collect(q, reads, writes)
        kt.cnt += 16
        ev = (kt.sem, kt.cnt, None)
        self.final[kt.sem] = kt.cnt
        self.ops[q].append((waits, lambda e: e.dma_start(out=out, in_=in_), kt.sem, 16))
        self._commit(ev, reads, writes)

    def barrier(self):
        for e in self.ENG:
            waits = []
            wd = self.waited[e]
            for key, val in self.final.items():
                if key == "E_" + e:
                    continue
                if wd.get(key, 0) < val:
                    wd[key] = val
                    waits.append((key, val))
            if waits:
                self.ops[e].append((waits, None, None, 0))

    def emit(self):
        nc = self.nc
        self.barrier()
        engmap = {"pe": "tensor", "dve": "vector", "act": "scalar", "pool": "gpsimd", "sp": "sync"}
        with nc.Block() as block:
            for e in self.ENG:
                def body(engine, ops=self.ops[e], sems=self.sems):
                    for waits, fn, key, inc in ops:
                        for wk, wv in waits:
                            engine.wait_ge(sems[wk], wv)
                        if fn is not None:
                            fn(engine).then_inc(sems[key], inc)

                getattr(block, engmap[e])(body)


class Arena:
    def __init__(self, nc, nbytes):
        self.t32 = nc.alloc_sbuf_tensor("arena", [128, nbytes // 4], F32)
        self.t16 = self.t32.bitcast(BF16)
        self.tu = self.t32.bitcast(U32)
        self.off = 0
        self.cap = nbytes

    def alloc(self, nbytes):
        off = (self.off + 63) // 64 * 64
        self.off = off + nbytes
        assert self.off <= self.cap, (self.off, self.cap)
        return off

    def f32(self, n, name=""):
        o = self.alloc(n * 4)
        return Buf(self.t32[:, o // 4:o // 4 + n], name)

    def b16(self, n, name=""):
        o = self.alloc(n * 2)
        return Buf(self.t16[:, o // 2:o // 2 + n], name)

    def u32(self, n, name=""):
        o = self.alloc(n * 4)
        return Buf(self.tu[:, o // 4:o // 4 + n], name)


def bc(ap, dims):
    return bass.AP(ap.tensor, ap.offset, [list(ap.ap[0])] + [list(d) for d in dims])


def r3(ap, **kw):
    return ap.rearrange("p (a b) -> p a b", **kw)


KDBG = os.environ.get('KDBG', '')
SLOPES = [2.0 ** (-8.0 * (h + 1) / 16.0) for h in range(16)]


class _Stop(Exception):
    pass


def build_program(stop_after_a=False, stage=None):
    nc = bass.Bass("TRN2", target_bir_lowering=False)
    es = ExitStack()
    dt = lambda name, shape, dtype=F32, kind="ExternalInput": nc.dram_tensor(name, shape, dtype, kind=kind).ap()
    x_d = dt("x", [TOK, D])
    win_d = dt("win", [128, 8, 5632])
    wpw_d = dt("wpw", [128, 8, 1024])
    wo_d = dt("wo", [128, 8, 1024])
    wout_d = dt("wout", [128, 8, 1024])
    wq_d = dt("wq", [128, 8, 2048])
    vecs_d = dt("vecs", [128, 40])
    dww_d = dt("dww", [128, 248])
    sink_d = dt("sink", [128, 16])
    fg_d = dt("fg", [128, 1024])
    skT_d = dt("skT", [128, 256])
    pu_d = dt("pu", [16384, 1024])
    pv_d = dt("pv", [16384, 1024])
    out_d = dt("out", [TOK, D], F32, "ExternalOutput")
    ut_d = dt("ut_scr", [128, 128, 1024], BF16, "Internal")
    vb_d = dt("vb_scr", [16384, 1024], BF16, "Internal")
    wqb_d = dt("wqb_scr", [16, 128, 1024], BF16, "Internal")

    with es:
        P = Prog(nc, es)
        A = Arena(nc, 207 * 1024)
        banks = []
        psall = nc.alloc_psum_tensor("psall", [128, 4096], F32)
        psall16 = psall.bitcast(BF16)
        for i in range(8):
            banks.append((psall[:, i * 512:(i + 1) * 512], psall16[:, i * 1024:(i + 1) * 1024], Tok(f"bank{i}")))
        rr = {"i": 0}

        def nextbank(lst):
            rr["i"] += 1
            return banks[lst[rr["i"] % len(lst)]]

        ident = A.b16(128, "ident")
        identf = A.f32(128, "identf")
        ones16 = A.b16(128, "ones")
        vecs = A.f32(40, "vecs")
        dww = A.f32(248, "dww")
        esink = A.f32(16, "esink")
        small = A.f32(64, "small")
        out_tok = [Tok(f"out{i}") for i in range(TOK // 128)]

        P.dma("sp", vecs.ap, vecs_d, writes=[vecs])
        P.dma("sp", dww.ap, dww_d, writes=[dww])
        P.dma("sp", esink.ap, sink_d, writes=[esink])
        P.op("act", lambda e: e.activation(out=esink.ap, in_=esink.ap, func=AF.Exp), reads=[esink], writes=[esink])
        P.op("pool", lambda e: e.iota(identf.ap, [[1, 128]], base=0, channel_multiplier=-1,
                                      allow_small_or_imprecise_dtypes=True), writes=[identf])
        P.op("dve", lambda e: e.tensor_scalar(out=identf.ap, in0=identf.ap, scalar1=0.0, scalar2=None,
                                              op0=ALU.is_equal), reads=[identf], writes=[identf])
        P.op("dve", lambda e: e.tensor_copy(out=ident.ap, in_=identf.ap), reads=[identf], writes=[ident])
        P.op("pool", lambda e: e.memset(ones16.ap, 1.0 / 1024.0), writes=[ones16])
        G1, DWB, LNG, LNB, G2 = range(5)
        vec = lambda idx, k: vecs.ap[:, idx * 8 + k:idx * 8 + k + 1]

        mark0 = A.off
        Mtab = A.b16(3 * 16 * 128, "Mtab")
        Mv = Mtab.ap.rearrange("p (a h q) -> p a h q", a=3, h=16)
        hT = A.b16(8 * SEQ)
        hTv = r3(hT.ap, a=8)
        hT_tok = [Tok() for _ in range(16)]
        QT = A.b16(8 * SEQ)
        QTv = r3(QT.ap, a=8)
        QT_tok = [Tok() for _ in range(16)]
        cT = A.b16(8 * SEQ)
        cTv = r3(cT.ap, a=8)
        cT_tok = [[Tok() for _ in range(4)] for _ in range(8)]
        r4 = A.alloc(32768)
        KTv = r3(A.t16[:, r4 // 2:r4 // 2 + 4 * SEQ], a=4)
        KT_tok = Tok()
        Vo = r4 + 16384
        Vv = A.t16[:, Vo // 2:Vo // 2 + 16 * 4 * 65].rearrange("p (t g e) -> p t g e", t=16, g=4)
        V_tok = [Tok() for _ in range(16)]
        dgs = [Buf(A.t16[:, (r4 + i * 8192) // 2:(r4 + i * 8192) // 2 + 31 * 128]) for i in range(2)]
        Ub = [Buf(A.t16[:, (r4 + 16384 + i * 4224) // 2:(r4 + 16384 + i * 4224) // 2 + 2078]) for i in range(2)]
        mTv = r3(A.t16[:, r4 // 2:r4 // 2 + 8 * SEQ], a=8)
        mT_tok = [Tok() for _ in range(4)]
        wst = [A.f32(2048) for _ in range(2)]
        wbf = [A.b16(2048) for _ in range(4)]
        wst_h = [Buf(b.ap[:, h * 1024:(h + 1) * 1024]) for b in wst for h in range(2)]
        wbf_h = [Buf(b.ap[:, h * 1024:(h + 1) * 1024]) for b in wbf for h in range(2)]
        xts = [A.f32(1024) for _ in range(2)]
        xns = [A.b16(1024) for _ in range(2)]
        junk = A.b16(1024)
        w12 = A.alloc(12288)
        Ef = [Buf(A.t32[:, (w12 + i * 2048) // 4:(w12 + i * 2048) // 4 + 512]) for i in range(3)]
        PT = [Buf(A.t16[:, (w12 + 6144 + i * 1024) // 2:(w12 + 6144 + i * 1024) // 2 + 512]) for i in range(6)]
        lnm = Buf(A.t32[:, (w12) // 4:(w12) // 4 + 512])
        lnr = Buf(A.t32[:, (w12 + 2048) // 4:(w12 + 2048) // 4 + 512])
        lnt = [Buf(A.t32[:, (w12 + 4096 + i * 2048) // 4:(w12 + 4096 + i * 2048) // 4 + 512]) for i in range(2)]
        lnq = [Buf(A.t16[:, (w12 + 8192 + i * 1024) // 2:(w12 + 8192 + i * 1024) // 2 + 512]) for i in range(2)]
        wkd = Buf(A.t16[:, w12 // 2:w12 // 2 + 4096])
        wkdv = wkd.ap.rearrange("p (k g e) -> p k g e", k=8, g=4)
        AO = [A.b16(1024) for _ in range(2)]
        den = A.f32(16)
        rec = A.f32(16)
        ss_i = {"i": 0}
        small_cur = {"tok": None}
        wst_i = {"i": 0}
        wbf_i = {"i": 0}

        small_toks = [Tok() for _ in range(32)]

        def new_small():
            ss_i["i"] = (ss_i["i"] + 1) % 32
            i = ss_i["i"]
            small_cur["tok"] = small_toks[i]
            return small.ap[:, 2 * i:2 * i + 1], small.ap[:, 2 * i + 1:2 * i + 2]

        def load_w(src3, col0, ncols, scale_idx=None, eng="pool", half=False):
            wst_i["i"] += 1
            wbf_i["i"] += 1
            if half:
                st = wst_h[wst_i["i"] % 4]
                wb = wbf_h[wbf_i["i"] % 8]
            else:
                st = wst[wst_i["i"] % 2]
                wb = wbf[wbf_i["i"] % 4]
            stv = r3(st.ap[:, 0:8 * ncols], a=8)
            wbv = r3(wb.ap[:, 0:8 * ncols], a=8)
            P.dma("sp", stv, src3[:, :, col0:col0 + ncols], writes=[st])
            assert scale_idx is None
            P.op(eng, lambda e: e.tensor_copy(out=wb.ap[:, 0:8 * ncols], in_=st.ap[:, 0:8 * ncols]), reads=[st], writes=[wb])
            return wb, wbv

        def rms_tile(xt, xn):
            ss, rstd = new_small()
            sm = small_cur["tok"]
            P.op("act", lambda e: e.activation(out=junk.ap, in_=xt.ap, func=AF.Square, accum_out=ss),
                 reads=[xt], writes=[junk, sm])
            P.op("act", lambda e: e.activation(out=rstd, in_=ss, func=AF.Sqrt, scale=1.0 / D, bias=EPS),
                 reads=[sm], writes=[sm])
            P.op("dve", lambda e: e.reciprocal(out=rstd, in_=rstd), reads=[sm], writes=[sm])
            if xn is not None:
                P.op("dve", lambda e: e.tensor_scalar(out=xn.ap, in0=xt.ap, scalar1=rstd, scalar2=None, op0=ALU.mult),
                     reads=[xt, sm], writes=[xn])
            return rstd

        def transpose8(src, dst_fn, dst_toks, evac_eng="act", scale_idx=None, bl=(4,)):
            bt, bt16, btok = nextbank(list(bl))
            for k in range(8):
                P.op("pe", lambda e, k=k: e.transpose(out=bt16[:, k * 128:(k + 1) * 128], in_=src.ap[:, k * 128:(k + 1) * 128],
                                                      identity=ident.ap), reads=[src, ident], writes=[btok])
            if scale_idx is None:
                P.op(evac_eng, lambda e: (e.copy if evac_eng == "act" else e.tensor_copy)(
                    out=dst_fn(None), in_=r3(bt16[:, 0:1024], a=8)), reads=[btok], writes=dst_toks)
            else:
                for k in range(8):
                    if evac_eng == "act" or (evac_eng == "mix" and k % 2 == 0):
                        P.op("act", lambda e, k=k: e.activation(out=dst_fn(k), in_=bt16[:, k * 128:(k + 1) * 128], func=AF.Copy,
                                                                scale=vec(scale_idx, k)), reads=[btok, vecs], writes=dst_toks)
                    else:
                        P.op("dve", lambda e, k=k: e.tensor_scalar(out=dst_fn(k), in0=bt16[:, k * 128:(k + 1) * 128],
                                                                   scalar1=vec(scale_idx, k), scalar2=None, op0=ALU.mult),
                             reads=[btok, vecs], writes=dst_toks)

        Df = Ef[0]
        Am = Ef[1]
        Mf = Ef[2]
        for pos in range(3):
            P.op("pool", lambda e, pos=pos: e.iota(Df.ap[:, 0:128], [[1, 128]], base=128 * (1 - pos), channel_multiplier=-1,
                                                   allow_small_or_imprecise_dtypes=True), writes=[Df])
            P.op("act", lambda e: e.activation(out=Df.ap[:, 0:128], in_=Df.ap[:, 0:128], func=AF.Abs),
                 reads=[Df], writes=[Df])
            P.op("dve", lambda e: e.tensor_scalar(out=Am.ap[:, 0:128], in0=Df.ap[:, 0:128], scalar1=128.0, scalar2=None,
                                                  op0=ALU.is_le), reads=[Df], writes=[Am])
            for h in range(16):
                P.op("act", lambda e, h=h: e.activation(out=Mf.ap[:, 0:128], in_=Df.ap[:, 0:128], func=AF.Exp, scale=-SLOPES[h]),
                     reads=[Df], writes=[Mf])
                P.op("dve", lambda e, pos=pos, h=h: e.tensor_tensor(out=Mv[:, pos, h, :], in0=Mf.ap[:, 0:128], in1=Am.ap[:, 0:128],
                                                                    op=ALU.mult), reads=[Mf, Am], writes=[Mtab])
        P.barrier()

        def chk(name):
            if stage == name:
                raise _Stop()

        try:
          for s in range(2):
            tb0 = s * SEQ
            chk('S0')
            def s1_pre(tt):
                xt = xts[tt % 2]
                xn = xns[tt % 2]
                P.dma("sp", xt.ap, x_d[tb0 + tt * 128:tb0 + (tt + 1) * 128, :], writes=[xt])
                rms_tile(xt, xn)

            s1_pre(0)
            for tt in range(16):
                if tt + 1 < 16:
                    s1_pre(tt + 1)
                transpose8(xns[tt % 2], lambda k, tt=tt: hTv[:, k, tt * 128:(tt + 1) * 128], [hT_tok[tt]], evac_eng="mix", scale_idx=G1,
                           bl=(4, 5, 6, 7))

            chk('S1')
            ev_i = 0
            for cp in range(4):
                wb, wbv = load_w(win_d, 2048 + cp * 256, 256)
                for cc in range(2):
                    c = cp * 2 + cc
                    for b in range(4):
                        bt, _, btok = nextbank([0, 1, 2, 3])
                        P.opn("pe", [lambda e, k=k, cc=cc, b=b, wbv=wbv, bt=bt: e.matmul(
                            bt[:, :], lhsT=wbv[:, k, cc * 128:(cc + 1) * 128], rhs=hTv[:, k, b * 512:(b + 1) * 512],
                            start=(k == 0), stop=(k == 7)) for k in range(8)], reads=[wb] + hT_tok[4 * b:4 * b + 4], writes=[btok])
                        dst = QTv[:, c, b * 512:(b + 1) * 512]
                        ev_i += 1
                        if ev_i % 2:
                            P.op("act", lambda e, dst=dst, bt=bt: e.mul(out=dst, in_=bt[:, :], mul=0.125),
                                 reads=[btok], writes=QT_tok[4 * b:4 * b + 4])
                        else:
                            P.op("dve", lambda e, dst=dst, bt=bt: e.tensor_scalar(out=dst, in0=bt[:, :], scalar1=0.125, scalar2=None,
                                                                                  op0=ALU.mult), reads=[btok], writes=QT_tok[4 * b:4 * b + 4])
            wst_i["i"] += 1
            st = wst[wst_i["i"] % 2]
            stv = r3(st.ap, a=8)
            P.dma("sp", stv, win_d[:, :, 3072:3328], writes=[st])
            for half in range(2):
                P.op("pool", lambda e, half=half: e.tensor_copy(
                    out=wkdv[:, :, :, half * 64:(half + 1) * 64], in_=st.ap.rearrange("p (k g e) -> p k g e", k=8, g=4)),
                    reads=[st], writes=[wkd])
            for g in range(4):
                for b in range(4):
                    bt, _, btok = nextbank([0, 1, 2, 3])
                    for k in range(8):
                        P.op("pe", lambda e, k=k, g=g, b=b, bt=bt: e.matmul(
                            bt[:, :], lhsT=wkdv[:, k, g, :], rhs=hTv[:, k, b * 512:(b + 1) * 512],
                            start=(k == 0), stop=(k == 7)), reads=[wkd] + hT_tok[4 * b:4 * b + 4], writes=[btok])
                    P.op("act", lambda e, g=g, b=b, bt=bt: e.copy(out=KTv[:, g, b * 512:(b + 1) * 512], in_=bt[:, :]),
                         reads=[btok], writes=[KT_tok])
            wb, wbv = load_w(win_d, 3328, 256)
            P.op("pool", lambda e: e.memset(Vv[:, :, :, 64:65], 1.0), writes=V_tok)
            for tt in range(16):
                bt, _, btok = nextbank([0, 1, 2, 3])
                for k in range(8):
                    P.op("pe", lambda e, k=k, tt=tt, wbv=wbv, bt=bt: e.matmul(
                        bt[:, 0:256], lhsT=hTv[:, k, tt * 128:(tt + 1) * 128], rhs=wbv[:, k, :],
                        start=(k == 0), stop=(k == 7)), reads=[wb, hT_tok[tt]], writes=[btok])
                P.op("dve", lambda e, tt=tt, bt=bt: e.tensor_copy(out=Vv[:, tt, :, 0:64],
                                                                  in_=bt[:, 0:256].rearrange("p (g e) -> p g e", g=4)),
                     reads=[btok], writes=[V_tok[tt]])

            chk('S2')
            cnt3 = {"pt": 0, "ef": 0}
            po = [banks[5], banks[6], banks[7]]

            def st_phase(i, g):
                kbs = [kb for kb in (i - 1, i, i + 1) if 0 <= kb < 16]
                pts = []
                for kb in kbs:
                    pos = kb - i + 1
                    rr["pair"] = rr.get("pair", 0) + 1
                    b0 = 2 * (rr["pair"] % 2)
                    pair = (banks[b0], banks[b0 + 1])
                    for j in range(4):
                        h = 4 * g + j
                        c, p = h // 2, h % 2
                        bt, _, btok = pair[p]
                        jj = j // 2
                        P.op("pe", lambda e, jj=jj, g=g, kb=kb, c=c, p=p, i=i, bt=bt: e.matmul(
                            bt[:, jj * 128:(jj + 1) * 128], lhsT=KTv[64 * p:64 * p + 64, g, kb * 128:(kb + 1) * 128],
                            rhs=QTv[64 * p:64 * p + 64, c, i * 128:(i + 1) * 128], start=True, stop=True),
                            reads=[KT_tok, QT_tok[i]], writes=[btok])
                    cnt3["ef"] += 1
                    ef = Ef[cnt3["ef"] % 3]
                    src2 = bc(pair[0][0][:, 0:1], [[512, 2], [1, 256]])
                    P.op("act", lambda e, ef=ef, src2=src2: e.activation(out=ef.ap.rearrange("p (a b) -> p a b", a=2), in_=src2,
                                                                       func=AF.Exp),
                         reads=[pair[0][2], pair[1][2]], writes=[ef])
                    cnt3["pt"] += 1
                    pt = PT[cnt3["pt"] % 6]
                    m4 = bc(Mv[:, pos, 4 * g, :], [[128, 2], [256, 2], [1, 128]])
                    P.op("dve", lambda e, ef=ef, pt=pt, m4=m4: e.tensor_tensor(
                        out=pt.ap.rearrange("p (a b q) -> p a b q", a=2, b=2), in0=ef.ap.rearrange("p (a b q) -> p a b q", a=2, b=2),
                        in1=m4, op=ALU.mult), reads=[ef, Mtab], writes=[pt])
                    pts.append(pt)
                return pts

            def pv_phase(i, g, pts):
                kbs = [kb for kb in (i - 1, i, i + 1) if 0 <= kb < 16]
                for j in range(4):
                    h = 4 * g + j
                    pb, _, pbtok = po[h // 7]
                    o0 = (h % 7) * 65
                    P.opn("pe", [lambda e, j=j, g=g, kb=kb, n=n, pb=pb, o0=o0, pt=pts[n], last=len(kbs) - 1: e.matmul(
                        pb[:, o0:o0 + 65], lhsT=pt.ap[:, (j % 2) * 256 + (j // 2) * 128:(j % 2) * 256 + (j // 2) * 128 + 128], rhs=Vv[:, kb, g, :],
                        start=(n == 0), stop=(n == last)) for n, kb in enumerate(kbs)],
                        reads=list(pts) + [V_tok[kb] for kb in kbs], writes=[pbtok])

            items3 = [(i, g) for i in range(16) for g in range(4)]
            pend3 = st_phase(*items3[0])
            for idx3, (i, g) in enumerate(items3):
                nxt3 = st_phase(*items3[idx3 + 1]) if idx3 + 1 < len(items3) else None
                pv_phase(i, g, pend3)
                pend3 = nxt3
                if g != 3:
                    continue
                if stage in ('S3a', 'S3b'):
                    continue
                ao = AO[i % 2]
                for b3, (h0, nh) in enumerate(((0, 7), (7, 7), (14, 2))):
                    pb, _, pbtok = po[b3]
                    pv = pb[:, 0:nh * 65].rearrange("p (h e) -> p h e", e=65)
                    P.op("dve", lambda e, pv=pv, h0=h0, nh=nh: e.tensor_tensor(
                        out=den.ap[:, h0:h0 + nh], in0=pv[:, :, 64], in1=esink.ap[:, h0:h0 + nh], op=ALU.add),
                        reads=[pbtok, esink], writes=[den])
                P.op("dve", lambda e: e.reciprocal(out=rec.ap, in_=den.ap), reads=[den], writes=[rec])
                for b3, (h0, nh) in enumerate(((0, 7), (7, 7), (14, 2))):
                    pb, _, pbtok = po[b3]
                    pv = pb[:, 0:nh * 65].rearrange("p (h e) -> p h e", e=65)
                    P.op("dve", lambda e, pv=pv, h0=h0, nh=nh, ao=ao: e.tensor_tensor(
                        out=ao.ap[:, h0 * 64:(h0 + nh) * 64].rearrange("p (h e) -> p h e", e=64), in0=pv[:, :, 0:64],
                        in1=bc(rec.ap[:, h0:h0 + nh], [[1, nh], [0, 64]]), op=ALU.mult),
                        reads=[pbtok, rec], writes=[ao])
                if stage == 'S3c':
                    continue
                transpose8(ao, lambda k, i=i: QTv[:, :, i * 128:(i + 1) * 128], [QT_tok[i]])
            chk('S3'); chk('S3a'); chk('S3b'); chk('S3c')
            P.barrier()

            for u in Ub:
                P.op("pool", lambda e, u=u: e.memset(u.ap[:, 0:15], 0.0), writes=[u])
                P.op("pool", lambda e, u=u: e.memset(u.ap[:, 2063:2078], 0.0), writes=[u])
            sg_i = 0
            for cp in range(4):
                wa, wav = load_w(win_d, cp * 256, 256)
                wg, wgv = load_w(win_d, 1024 + cp * 256, 256)
                for cc in range(2):
                    c = cp * 2 + cc
                    u = Ub[c % 2]
                    for b in range(4):
                        ba, _, batok = nextbank([0, 1, 2, 3])
                        bg, _, bgtok = nextbank([0, 1, 2, 3])
                        for (wt, wtv, bt, btok) in ((wa, wav, ba, batok), (wg, wgv, bg, bgtok)):
                            P.opn("pe", [lambda e, k=k, cc=cc, b=b, wtv=wtv, bt=bt: e.matmul(
                                bt[:, :], lhsT=wtv[:, k, cc * 128:(cc + 1) * 128], rhs=hTv[:, k, b * 512:(b + 1) * 512],
                                start=(k == 0), stop=(k == 7)) for k in range(8)], reads=[wt] + hT_tok[4 * b:4 * b + 4], writes=[btok])
                        sg_i += 1
                        sg = Ef[sg_i % 3]
                        P.op("act", lambda e, sg=sg, bg=bg: e.activation(out=sg.ap, in_=bg[:, :], func=AF.Sigmoid),
                             reads=[bgtok], writes=[sg])
                        P.op("dve", lambda e, sg=sg, ba=ba, u=u, b=b: e.tensor_tensor(
                            out=u.ap[:, 15 + b * 512:15 + (b + 1) * 512], in0=ba[:, :], in1=sg.ap, op=ALU.mult),
                            reads=[batok, sg], writes=[u])
                    dg = dgs[c % 2]
                    P.op("dve", lambda e, dg=dg, c=c: e.tensor_tensor(
                        out=dg.ap.rearrange("p (t j) -> p t j", t=31), in0=bc(ident.ap[:, 0:1], [[0, 31], [1, 128]]),
                        in1=bc(dww.ap[:, c * 31:c * 31 + 1], [[1, 31], [0, 128]]), op=ALU.mult), reads=[ident, dww], writes=[dg])
                    for b in range(4):
                        bt, _, btok = nextbank([0, 1, 2, 3])
                        P.opn("pe", [lambda e, tap=tap, dg=dg, u=u, b=b, bt=bt: e.matmul(
                            bt[:, :], lhsT=dg.ap[:, tap * 128:(tap + 1) * 128], rhs=u.ap[:, tap + b * 512:tap + b * 512 + 512],
                            start=(tap == 0), stop=(tap == 30)) for tap in range(31)], reads=[dg, u], writes=[btok])
                        P.op("act", lambda e, c=c, b=b, bt=bt: e.activation(out=cTv[:, c, b * 512:(b + 1) * 512], in_=bt[:, :],
                                                                            func=AF.Identity, bias=vec(DWB, c)),
                             reads=[btok, vecs], writes=[cT_tok[c][b]])
            P.barrier()
            for b in range(4):
                bs_, _, bstok = nextbank([0, 1, 2, 3])
                bq_, _, bqtok = nextbank([0, 1, 2, 3])
                blk = slice(b * 512, (b + 1) * 512)
                for c in range(8):
                    sq = lnq[c % 2]
                    P.op("act", lambda e, sq=sq, c=c, blk=blk: e.activation(out=sq.ap, in_=cTv[:, c, blk], func=AF.Square),
                         reads=[cT_tok[c][b]], writes=[sq])
                    P.op("pe", lambda e, c=c, blk=blk, bs_=bs_: e.matmul(bs_[:, :], lhsT=ones16.ap, rhs=cTv[:, c, blk],
                                                                       start=(c == 0), stop=(c == 7)),
                         reads=[ones16, cT_tok[c][b]], writes=[bstok])
                    P.op("pe", lambda e, c=c, sq=sq, bq_=bq_: e.matmul(bq_[:, :], lhsT=ones16.ap, rhs=sq.ap,
                                                                     start=(c == 0), stop=(c == 7)),
                         reads=[ones16, sq], writes=[bqtok])
                P.op("act", lambda e, bs_=bs_: e.copy(out=lnm.ap, in_=bs_[:, :]), reads=[bstok], writes=[lnm])
                P.op("dve", lambda e: e.tensor_tensor(out=lnr.ap, in0=lnm.ap, in1=lnm.ap, op=ALU.mult), reads=[lnm], writes=[lnr])
                P.op("dve", lambda e, bq_=bq_: e.tensor_tensor(out=lnr.ap, in0=bq_[:, :], in1=lnr.ap, op=ALU.subtract),
                     reads=[bqtok, lnr], writes=[lnr])
                P.op("act", lambda e: e.activation(out=lnr.ap, in_=lnr.ap, func=AF.Sqrt, bias=EPS), reads=[lnr], writes=[lnr])
                P.op("dve", lambda e: e.reciprocal(out=lnr.ap, in_=lnr.ap), reads=[lnr], writes=[lnr])
                for c in range(8):
                    t1 = lnt[c % 2]
                    P.op("dve", lambda e, t1=t1, c=c, blk=blk: e.tensor_tensor(out=t1.ap, in0=cTv[:, c, blk], in1=lnm.ap, op=ALU.subtract),
                         reads=[cT_tok[c][b], lnm], writes=[t1])
                    P.op("dve", lambda e, t1=t1: e.tensor_tensor(out=t1.ap, in0=t1.ap, in1=lnr.ap, op=ALU.mult),
                         reads=[t1, lnr], writes=[t1])
                    P.op("act", lambda e, t1=t1, c=c, blk=blk: e.activation(out=cTv[:, c, blk], in_=t1.ap, func=AF.Silu,
                                                                           scale=vec(LNG, c), bias=vec(LNB, c)),
                         reads=[t1, vecs], writes=[cT_tok[c][b]])
            chk('S4')
            P.barrier()

            sg_i = 0
            for c in range(8):
                w1, w1v = load_w(wpw_d, c * 128, 128, half=True)
                w2, w2v = load_w(wo_d, c * 128, 128, half=True)
                w3, w3v = load_w(win_d, 3584 + c * 128, 128, half=True)
                w4, w4v = load_w(win_d, 4608 + c * 128, 128, half=True)
                for b in range(4):
                    blk = slice(b * 512, (b + 1) * 512)
                    bks = [nextbank([0, 1, 2, 3]) for _ in range(4)]
                    srcs = ((w1, w1v, cTv, [cT_tok[k][b] for k in range(8)]),
                            (w2, w2v, QTv, QT_tok[4 * b:4 * b + 4]),
                            (w3, w3v, hTv, hT_tok[4 * b:4 * b + 4]),
                            (w4, w4v, hTv, hT_tok[4 * b:4 * b + 4]))
                    for (wt, wtv, src, stoks), (bt, _, btok) in zip(srcs, bks):
                        P.opn("pe", [lambda e, k=k, wtv=wtv, src=src, bt=bt, blk=blk: e.matmul(
                            bt[:, :], lhsT=wtv[:, k, 0:128], rhs=src[:, k, blk], start=(k == 0), stop=(k == 7)) for k in range(8)],
                            reads=[wt] + list(stoks), writes=[btok])
                    sa = Ef[0]
                    sb_ = Ef[1]
                    m1 = Ef[2]
                    P.op("act", lambda e, bt=bks[2][0]: e.activation(out=sa.ap, in_=bt[:, :], func=AF.Sigmoid),
                         reads=[bks[2][2]], writes=[sa])
                    P.op("act", lambda e, bt=bks[3][0]: e.activation(out=sb_.ap, in_=bt[:, :], func=AF.Sigmoid),
                         reads=[bks[3][2]], writes=[sb_])
                    P.op("dve", lambda e, bt=bks[0][0]: e.tensor_tensor(out=m1.ap, in0=bt[:, :], in1=sa.ap, op=ALU.mult),
                         reads=[bks[0][2], sa], writes=[m1])
                    P.op("dve", lambda e, bt=bks[1][0]: e.tensor_tensor(out=sb_.ap, in0=bt[:, :], in1=sb_.ap, op=ALU.mult),
                         reads=[bks[1][2], sb_], writes=[sb_])
                    P.op("pool", lambda e, c=c, blk=blk: e.tensor_tensor(out=mTv[:, c, blk], in0=m1.ap, in1=sb_.ap, op=ALU.add),
                         reads=[m1, sb_], writes=[mT_tok[b]])

            chk('S5')
            P.barrier()
            wo4 = [load_w(wout_d, q4 * 256, 256) for q4 in range(4)]
            for tt in range(16):
                xt = xts[tt % 2]
                gt = (tb0 // 128) + tt
                P.dma("sp", xt.ap, x_d[tb0 + tt * 128:tb0 + (tt + 1) * 128, :], writes=[xt])
                for half in range(2):
                    bt, _, btok = nextbank([0, 1, 2, 3])
                    for q2 in range(2):
                        wb, wbv = wo4[half * 2 + q2]
                        P.opn("pe", [lambda e, k=k, tt=tt, q2=q2, wbv=wbv, bt=bt: e.matmul(
                            bt[:, q2 * 256:(q2 + 1) * 256], lhsT=mTv[:, k, tt * 128:(tt + 1) * 128], rhs=wbv[:, k, :],
                            start=(k == 0), stop=(k == 7)) for k in range(8)], reads=[wb, mT_tok[tt // 4]], writes=[btok])
                    P.op("dve", lambda e, half=half, bt=bt, xt=xt: e.tensor_tensor(
                        out=xt.ap[:, half * 512:(half + 1) * 512], in0=bt[:, :], in1=xt.ap[:, half * 512:(half + 1) * 512],
                        op=ALU.add), reads=[btok, xt], writes=[xt])
                P.dma("act", out_d[gt * 128:(gt + 1) * 128, :], xt.ap, reads=[xt], writes=[out_tok[gt]], key=xt)
            chk('S6')
            P.barrier()
        except _Stop:
            pass

        if not stop_after_a:
            A.off = mark0
            build_peer(nc, P, A, banks, nextbank, dict(
                ident=ident, identf=identf, vecs=vecs, vec=vec, G2=G2, small=small, new_small=new_small, small_cur=small_cur,
                out_tok=out_tok, out_d=out_d, fg_d=fg_d, skT_d=skT_d, pu_d=pu_d, pv_d=pv_d, wq_d=wq_d,
                ut_d=ut_d, vb_d=vb_d, wqb_d=wqb_d))
        P.emit()
    return nc


def build_peer(nc, P, A, banks, nextbank, C):
    ident, identf, vecs, vec, G2 = C["ident"], C["identf"], C["vecs"], C["vec"], C["G2"]
    small, new_small, out_tok, out_d = C["small"], C["new_small"], C["out_tok"], C["out_d"]
    small_cur = C["small_cur"]
    ut_d, vb_d, wqb_d = C["ut_d"], C["vb_d"], C["wqb_d"]
    NBLK = TOK // TB
    NT = TB // 128
    mark = A.off
    NPB = 4
    pst = [A.f32(1024) for _ in range(NPB)]
    pst2 = [A.f32(1024) for _ in range(NPB)]
    pbf = [A.b16(1024) for _ in range(NPB)]
    pbf2 = [A.b16(1024) for _ in range(NPB)]
    utb = [A.b16(1024) for _ in range(2)]
    assert A.off - mark <= 128 * TB * 2
    A.off = mark
    G = A.b16(128 * TB)
    Gv = G.ap.rearrange("p (i t) -> p i t", i=128)
    h2T = [A.b16(8 * TB) for _ in range(2)]
    h2T_toks = [[Tok() for _ in range(NT)] for _ in range(2)]
    qT = A.f32(16 * TB)
    qTv = r3(qT.ap, a=16)
    sc = [A.f32(2048)] * 2
    sc2 = A.f32(256)
    v16 = A.f32(256)
    v16v = r3(v16.ap, a=16)
    ix = A.u32(256)
    ixv = r3(ix.ap, a=16)
    ixf = A.f32(256)
    cand = sc[0]
    eqb = A.f32(2048)
    eq2 = cand
    ts = A.f32(128)
    tsv = r3(ts.ap, a=8)
    pos = A.u32(128)
    posv = r3(pos.ap, a=8)
    posf = A.f32(128)
    k1f = A.f32(128)
    k2f = A.f32(128)
    Ivs = [A.f32(128) for _ in range(2)]
    Jvs = [A.f32(128) for _ in range(2)]
    Wvs = [A.f32(128) for _ in range(2)]
    ew = A.f32(128)
    zz = A.f32(16)
    SM = A.f32(3 * TB)
    SMv = r3(SM.ap, a=3)
    iota128 = A.b16(128)
    iotaf = A.f32(128)
    skT = A.f32(256)
    skTv = r3(skT.ap, a=2)
    fg = A.f32(1024)
    ohB = [A.b16(16 * 128) for _ in range(2)]
    ohE = [A.b16(16 * 128) for _ in range(2)]
    OAI = A.b16(TB * 8)
    OAJ = A.b16(TB * 8)
    OBI = A.b16(TB * 16)
    OBJ = A.b16(TB * 16)
    xh = A.f32(TB)
    xl = A.f32(TB)
    ubr = [A.b16(1024) for _ in range(4)]
    vbr = [A.b16(1024) for _ in range(4)]
    gelr = [A.f32(TB) for _ in range(3)]
    ATr = [A.b16(TB) for _ in range(4)]
    xts = [A.f32(1024) for _ in range(2)]
    xns = [A.b16(1024) for _ in range(2)]
    wqc = [A.b16(1024) for _ in range(3)]
    ut_tok = [Tok() for _ in range(128)]
    vb_tok = [Tok() for _ in range(128)]
    wqb_tok = [Tok() for _ in range(16)]

    P.dma("sp", skT.ap, C["skT_d"], writes=[skT])
    P.dma("sp", fg.ap, C["fg_d"], writes=[fg])
    P.op("pool", lambda e: e.iota(iotaf.ap, [[1, 128]], base=0, channel_multiplier=0, allow_small_or_imprecise_dtypes=True),
         writes=[iotaf])
    P.op("dve", lambda e: e.tensor_copy(out=iota128.ap, in_=iotaf.ap), reads=[iotaf], writes=[iota128])
    k16 = A.f32(16)
    nhalf = A.f32(1)
    P.op("pool", lambda e: e.memset(nhalf.ap, -0.5), writes=[nhalf])
    P.op("dve", lambda e: e.tensor_scalar(out=k16.ap, in0=iotaf.ap[:, 0:16], scalar1=16.0, scalar2=None, op0=ALU.mult),
         reads=[iotaf], writes=[k16])

    for m in range(16):
        st = pst[m % NPB]
        pb = pbf[m % NPB]
        P.dma("sp", r3(st.ap, a=8), C["wq_d"][:, :, m * 128:(m + 1) * 128], writes=[st])
        P.op("pool", lambda e, st=st, pb=pb: e.tensor_copy(out=pb.ap, in_=st.ap), reads=[st], writes=[pb])
        P.dma("act", wqb_d[m], pb.ap, reads=[pb], writes=[wqb_tok[m]], key=pb)
    def prep_uv(i):
        st = pst[i % NPB]
        pb = pbf[i % NPB]
        P.dma("sp", st.ap, C["pu_d"][i * 128:(i + 1) * 128, :], writes=[st])
        P.op("pool", lambda e, st=st, pb=pb: e.tensor_copy(out=pb.ap, in_=st.ap), reads=[st], writes=[pb])
        bt, bt16, btok = nextbank([4, 5, 6, 7])
        for k in range(8):
            P.op("pe", lambda e, k=k, pb=pb, bt16=bt16: e.transpose(out=bt16[:, k * 128:(k + 1) * 128], in_=pb.ap[:, k * 128:(k + 1) * 128],
                                                             identity=ident.ap), reads=[pb, ident], writes=[btok])
        ut = utb[i % 2]
        P.op("act", lambda e, ut=ut, bt16=bt16: e.copy(out=ut.ap, in_=bt16[:, 0:1024]), reads=[btok], writes=[ut])
        P.dma("act", ut_d[i], ut.ap, reads=[ut], writes=[ut_tok[i]], key=ut)
        st2 = pst2[i % NPB]
        pb2 = pbf2[i % NPB]
        P.dma("pool", st2.ap, C["pv_d"][i * 128:(i + 1) * 128, :], writes=[st2])
        P.op("dve", lambda e, st2=st2, pb2=pb2: e.tensor_copy(out=pb2.ap, in_=st2.ap), reads=[st2], writes=[pb2])
        P.dma("act", vb_d[i * 128:(i + 1) * 128, :], pb2.ap, reads=[pb2], writes=[vb_tok[i]], key=pb2)

    cnt = {"oh": 0, "ev": 0}

    def routing(nb):
        h2Tv = r3(h2T[nb % 2].ap, a=8)
        h2T_tok = h2T_toks[nb % 2]
        for tt in range(NT):
            gt = nb * NT + tt
            xt = xts[tt]
            xn = xns[tt]
            P.dma("sp", xt.ap, out_d[gt * 128:(gt + 1) * 128, :], reads=[out_tok[gt]], writes=[xt])
            ss, rstd = new_small()
            smt = small_cur["tok"]
            P.op("dve", lambda e, xt=xt, ss=ss: e.scalar_tensor_tensor(out=eqb.ap[:, 0:1024], in0=xt.ap, scalar=1.0, in1=xt.ap,
                                                                       op0=ALU.mult, op1=ALU.mult, accum_out=ss),
                 reads=[xt], writes=[eqb, smt])
            P.op("pool", lambda e, ss=ss: e.tensor_scalar(out=ss, in0=ss, scalar1=1.0 / D, scalar2=EPS, op0=ALU.mult, op1=ALU.add),
                 reads=[smt], writes=[smt])
            P.op("pool", lambda e, ss=ss, rstd=rstd: e.tensor_tensor(out=rstd, in0=ss, in1=nhalf.ap, op=ALU.pow),
                 reads=[smt, nhalf], writes=[smt])
            P.op("dve", lambda e, xt=xt, xn=xn, rstd=rstd: e.tensor_scalar(out=xn.ap, in0=xt.ap, scalar1=rstd, scalar2=None, op0=ALU.mult),
                 reads=[xt, smt], writes=[xn])
            bt, bt16, btok = nextbank([7])
            for k in range(8):
                P.op("pe", lambda e, k=k, xn=xn, bt16=bt16: e.transpose(out=bt16[:, k * 128:(k + 1) * 128], in_=xn.ap[:, k * 128:(k + 1) * 128],
                                                                 identity=ident.ap), reads=[xn, ident], writes=[btok])
            for k in range(8):
                P.op("dve", lambda e, k=k, tt=tt, bt16=bt16: e.tensor_scalar(
                    out=h2Tv[:, k, tt * 128:(tt + 1) * 128], in0=bt16[:, k * 128:(k + 1) * 128], scalar1=vec(G2, k), scalar2=None,
                    op0=ALU.mult), reads=[btok, vecs], writes=[h2T_tok[tt]])
        for m in range(3):
            P.dma("act", wqc[m].ap, wqb_d[m], reads=[wqb_tok[m]], writes=[wqc[m]])
        for m in range(16):
            wq = wqc[m % 3]
            bt, _, btok = nextbank([7])
            P.opn("pe", [lambda e, k=k, wq=wq, bt=bt: e.matmul(bt[:, 0:TB], lhsT=r3(wq.ap, a=8)[:, k, :], rhs=h2Tv[:, k, :],
                                                           start=(k == 0), stop=(k == 7)) for k in range(8)],
                  reads=[wq] + h2T_tok, writes=[btok])
            P.op("act", lambda e, m=m, bt=bt: e.copy(out=qTv[:, m, :], in_=bt[:, 0:TB]), reads=[btok], writes=[qT])
            if m + 3 < 16:
                P.dma("act", wq.ap, wqb_d[m + 3], reads=[wqb_tok[m + 3]], writes=[wq])
        for tt in range(NT):
            Iv, Jv, Wv = Ivs[tt], Jvs[tt], Wvs[tt]
            scb = sc[tt]
            for bq in range(4):
                bt, _, btok = nextbank([7])
                for mm in range(4):
                    m = bq * 4 + mm
                    P.op("pe", lambda e, mm=mm, m=m, tt=tt, bt=bt: e.matmul(
                        bt[:, mm * 128:(mm + 1) * 128], lhsT=qTv[:, m, tt * 128:(tt + 1) * 128], rhs=skTv[:, m % 2, :],
                        start=True, stop=True), reads=[qT, skT], writes=[btok])
                P.op("dve", lambda e, bq=bq, bt=bt, scb=scb: e.tensor_copy(out=scb.ap[:, bq * 512:(bq + 1) * 512], in_=bt[:, :]),
                     reads=[btok], writes=[scb])
            for m in range(16):
                src = scb.ap[:, m * 128:(m + 1) * 128]
                P.op("dve", lambda e, m=m, src=src: e.max(out=v16v[:, m, 0:8], in_=src), reads=[scb], writes=[v16])
                P.op("dve", lambda e, m=m, src=src: e.max_index(out=ixv[:, m, 0:8], in_max=v16v[:, m, 0:8], in_values=src),
                     reads=[scb, v16], writes=[ix])
                P.op("dve", lambda e, m=m, src=src: e.match_replace(out=sc2.ap[:, 0:128], in_to_replace=v16v[:, m, 0:8], in_values=src,
                                                                    imm_value=-1e30), reads=[scb, v16], writes=[sc2])
                P.op("dve", lambda e, m=m: e.max(out=v16v[:, m, 8:16], in_=sc2.ap[:, 0:128]), reads=[sc2], writes=[v16])
                P.op("dve", lambda e, m=m: e.max_index(out=ixv[:, m, 8:16], in_max=v16v[:, m, 8:16], in_values=sc2.ap[:, 0:128]),
                     reads=[sc2, v16], writes=[ix])
            P.op("dve", lambda e: e.tensor_copy(out=ixf.ap, in_=ix.ap), reads=[ix], writes=[ixf])
            c4 = lambda b: b.ap.rearrange("p (h a b) -> p h a b", h=8, a=16)
            P.op("dve", lambda e: e.tensor_tensor(out=c4(cand), in0=bc(v16.ap[:, 0:1], [[32, 8], [1, 16], [0, 16]]),
                                                  in1=bc(v16.ap[:, 16:17], [[32, 8], [0, 16], [1, 16]]), op=ALU.add),
                 reads=[v16], writes=[cand])
            for h in range(8):
                src = cand.ap[:, h * 256:(h + 1) * 256]
                P.op("dve", lambda e, h=h, src=src: e.max(out=tsv[:, h, 0:8], in_=src), reads=[cand], writes=[ts])
                P.op("dve", lambda e, h=h, src=src: e.max_index(out=posv[:, h, 0:8], in_max=tsv[:, h, 0:8], in_values=src),
                     reads=[cand, ts], writes=[pos])
                P.op("dve", lambda e, h=h, src=src: e.match_replace(out=sc2.ap, in_to_replace=tsv[:, h, 0:8], in_values=src,
                                                                    imm_value=-1e30), reads=[cand, ts], writes=[sc2])
                P.op("dve", lambda e, h=h: e.max(out=tsv[:, h, 8:16], in_=sc2.ap), reads=[sc2], writes=[ts])
                P.op("dve", lambda e, h=h: e.max_index(out=posv[:, h, 8:16], in_max=tsv[:, h, 8:16], in_values=sc2.ap),
                     reads=[sc2, ts], writes=[pos])
            P.op("dve", lambda e: e.tensor_tensor(out=r3(ew.ap, a=8), in0=tsv, in1=bc(ts.ap[:, 0:1], [[16, 8], [0, 16]]), op=ALU.subtract),
                 reads=[ts], writes=[ew])
            P.op("act", lambda e: e.activation(out=ew.ap, in_=ew.ap, func=AF.Exp), reads=[ew], writes=[ew])
            P.op("dve", lambda e: e.tensor_reduce(out=zz.ap[:, 0:8], in_=r3(ew.ap, a=8), axis=AX.X, op=ALU.add), reads=[ew], writes=[zz])
            P.op("dve", lambda e: e.reciprocal(out=zz.ap[:, 8:16], in_=zz.ap[:, 0:8]), reads=[zz], writes=[zz])
            P.op("dve", lambda e, Wv=Wv: e.tensor_tensor(out=r3(Wv.ap, a=8), in0=r3(ew.ap, a=8), in1=bc(zz.ap[:, 8:9], [[1, 8], [0, 16]]), op=ALU.mult),
                 reads=[ew, zz], writes=[Wv])
            P.op("dve", lambda e: e.tensor_copy(out=posf.ap, in_=pos.ap), reads=[pos], writes=[posf])
            P.op("dve", lambda e: e.tensor_tensor(out=c4(eqb), in0=bc(posf.ap[:, 0:1], [[16, 8], [1, 16], [0, 16]]),
                                                  in1=bc(k16.ap[:, 0:1], [[0, 8], [0, 16], [1, 16]]), op=ALU.subtract),
                 reads=[k16, posf], writes=[eqb])
            P.op("dve", lambda e: e.tensor_scalar(out=eq2.ap, in0=eqb.ap, scalar1=0.0, scalar2=None, op0=ALU.is_ge),
                 reads=[eqb], writes=[eq2])
            P.op("dve", lambda e: e.scalar_tensor_tensor(out=eqb.ap, in0=eqb.ap, scalar=16.0, in1=eq2.ap, op0=ALU.is_lt, op1=ALU.mult),
                 reads=[eqb, eq2], writes=[eqb])
            P.op("dve", lambda e: e.tensor_tensor(out=c4(eq2), in0=c4(eqb), in1=bc(iotaf.ap[:, 0:1], [[0, 8], [0, 16], [1, 16]]), op=ALU.mult),
                 reads=[eqb, iotaf], writes=[eq2])
            P.op("dve", lambda e: e.tensor_reduce(out=k1f.ap, in_=c4(eq2), axis=AX.X, op=ALU.add), reads=[eq2], writes=[k1f])
            P.op("dve", lambda e: e.tensor_tensor(out=c4(eq2), in0=c4(eqb), in1=bc(ixf.ap[:, 0:1], [[32, 8], [0, 16], [1, 16]]), op=ALU.mult),
                 reads=[eqb, ixf], writes=[eq2])
            P.op("dve", lambda e, Iv=Iv: e.tensor_reduce(out=Iv.ap, in_=c4(eq2), axis=AX.X, op=ALU.add), reads=[eq2], writes=[Iv])
            P.op("dve", lambda e: e.scalar_tensor_tensor(out=k2f.ap, in0=k1f.ap, scalar=-16.0, in1=posf.ap, op0=ALU.mult, op1=ALU.add),
                 reads=[k1f, posf], writes=[k2f])
            P.op("dve", lambda e: e.tensor_tensor(out=c4(eqb), in0=bc(k2f.ap[:, 0:1], [[16, 8], [1, 16], [0, 16]]),
                                                  in1=bc(iotaf.ap[:, 0:1], [[0, 8], [0, 16], [1, 16]]), op=ALU.is_equal),
                 reads=[k2f, iotaf], writes=[eqb])
            P.op("dve", lambda e: e.tensor_tensor(out=c4(eq2), in0=c4(eqb), in1=bc(ixf.ap[:, 16:17], [[32, 8], [0, 16], [1, 16]]), op=ALU.mult),
                 reads=[eqb, ixf], writes=[eq2])
            P.op("dve", lambda e, Jv=Jv: e.tensor_reduce(out=Jv.ap, in_=c4(eq2), axis=AX.X, op=ALU.add), reads=[eq2], writes=[Jv])
        for tt in range(NT):
            Iv, Jv, Wv = Ivs[tt], Jvs[tt], Wvs[tt]
            bt, _, btok = nextbank([7])
            for qi, srcb in enumerate((Iv, Jv, Wv)):
                P.op("pe", lambda e, qi=qi, srcb=srcb, bt=bt: e.transpose(out=bt[:, qi * 128:(qi + 1) * 128], in_=srcb.ap, identity=identf.ap),
                     reads=[srcb, identf], writes=[btok])
            P.op("act", lambda e, tt=tt, bt=bt: e.copy(out=SMv[:, :, tt * 128:(tt + 1) * 128], in_=r3(bt[:, 0:384], a=3)),
                 reads=[btok], writes=[SM])
        t3 = lambda b, n: b.ap[:, 0:TB * n].rearrange("p (t a) -> p t a", a=n)
        for q, OA, OB in ((0, OAI, OBI), (1, OAJ, OBJ)):
            P.op("dve", lambda e, q=q: e.tensor_tensor(out=t3(eqb, 8), in0=bc(SMv[:, q, 0:1], [[1, TB], [0, 8]]),
                                                       in1=bc(k16.ap[:, 0:1], [[0, TB], [1, 8]]), op=ALU.subtract),
                 reads=[SM, k16], writes=[eqb])
            P.op("dve", lambda e: e.tensor_scalar(out=cand.ap, in0=eqb.ap, scalar1=0.0, scalar2=None, op0=ALU.is_ge),
                 reads=[eqb], writes=[cand])
            P.op("dve", lambda e, OA=OA: e.scalar_tensor_tensor(out=OA.ap, in0=eqb.ap, scalar=16.0, in1=cand.ap, op0=ALU.is_lt, op1=ALU.mult),
                 reads=[eqb, cand], writes=[OA])
            P.op("dve", lambda e, OA=OA: e.tensor_tensor(out=t3(eqb, 8), in0=t3(OA, 8), in1=bc(iotaf.ap[:, 0:1], [[0, TB], [1, 8]]), op=ALU.mult),
                 reads=[OA, iotaf], writes=[eqb])
            P.op("dve", lambda e: e.tensor_reduce(out=xh.ap, in_=t3(eqb, 8), axis=AX.X, op=ALU.add), reads=[eqb], writes=[xh])
            P.op("dve", lambda e, q=q: e.scalar_tensor_tensor(out=xl.ap, in0=xh.ap, scalar=-16.0, in1=SMv[:, q, :], op0=ALU.mult, op1=ALU.add),
                 reads=[xh, SM], writes=[xl])
            P.op("dve", lambda e, OB=OB: e.tensor_tensor(out=t3(OB, 16), in0=bc(xl.ap[:, 0:1], [[1, TB], [0, 16]]),
                                                         in1=bc(iotaf.ap[:, 0:1], [[0, TB], [1, 16]]), op=ALU.is_equal),
                 reads=[xl, iotaf], writes=[OB])
        P.op("dve", lambda e: e.tensor_tensor(out=t3(OAJ, 8), in0=t3(OAJ, 8), in1=bc(SMv[:, 2, 0:1], [[1, TB], [0, 8]]), op=ALU.mult),
             reads=[OAJ, SM], writes=[OAJ])

    def b5(nb):
        TG = 16
        for tg in range(TB // TG):
            t0 = tg * TG
            cnt["oh"] += 1
            ob, oc = ohB[cnt["oh"] % 2], ohE[cnt["oh"] % 2]
            o3 = lambda b: b.ap.rearrange("p (t i) -> p t i", t=TG)
            o4 = lambda b: b.ap.rearrange("p (t a c) -> p t a c", t=TG, a=8)
            P.op("dve", lambda e, oc=oc, t0=t0: e.tensor_tensor(
                out=o4(oc), in0=bc(OAI.ap[:, t0 * 8:t0 * 8 + 1], [[8, TG], [1, 8], [0, 16]]),
                in1=bc(OBI.ap[:, t0 * 16:t0 * 16 + 1], [[16, TG], [0, 8], [1, 16]]), op=ALU.mult), reads=[OAI, OBI], writes=[oc])
            P.op("dve" if tg % 5 == 4 else "pool", lambda e, ob=ob, t0=t0: e.tensor_tensor(
                out=o4(ob), in0=bc(OAJ.ap[:, t0 * 8:t0 * 8 + 1], [[8, TG], [1, 8], [0, 16]]),
                in1=bc(OBJ.ap[:, t0 * 16:t0 * 16 + 1], [[16, TG], [0, 8], [1, 16]]), op=ALU.mult), reads=[OAJ, OBJ], writes=[ob])
            for t4 in range(TG // 4):
                bt, _, btok = nextbank([6, 7])
                P.opn("pe", [lambda e, q=q, t4=t4, oc=oc, ob=ob, bt=bt: e.matmul(
                    bt[:, q * 128:(q + 1) * 128], lhsT=o3(ob)[:, t4 * 4 + q, :], rhs=o3(oc)[:, t4 * 4 + q, :], start=True, stop=True)
                    for q in range(4)], reads=[oc, ob], writes=[btok])
                ta = t0 + t4 * 4
                P.op("act", lambda e, ta=ta, bt=bt: e.copy(out=Gv[:, :, ta:ta + 4], in_=bc(bt[:, 0:1], [[1, 128], [128, 4]])),
                     reads=[btok], writes=[G])
    def b6(nb, pend):
        h2Tv = r3(h2T[nb % 2].ap, a=8)
        h2T_tok = h2T_toks[nb % 2]
        def u_side(i):
            ub = ubr[i % 4]
            vb = vbr[i % 4]
            P.dma("sp", ub.ap, ut_d[i], reads=[ut_tok[i]], writes=[ub])
            P.dma("sp", vb.ap, vb_d[i * 128:(i + 1) * 128, :], reads=[vb_tok[i]], writes=[vb])
            bs_, _, bstok = banks[4 + i % 3]
            P.opn("pe", [lambda e, k=k, ub=ub, bs_=bs_: e.matmul(bs_[:, 0:TB], lhsT=r3(ub.ap, a=8)[:, k, :], rhs=h2Tv[:, k, :],
                                                             start=(k == 0), stop=(k == 7)) for k in range(8)],
                  reads=[ub] + h2T_tok, writes=[bstok])
            return bs_, bstok, vb

        per = (len(pend) + 119) // 120 if pend else 0
        uq = [u_side(0), u_side(1)]
        for i in range(128):
            bs_, bstok, vb = uq.pop(0)
            if i + 2 < 128:
                uq.append(u_side(i + 2))
            gel = gelr[i % 3]
            P.op("act", lambda e, gel=gel, bs_=bs_: e.activation(out=gel.ap, in_=bs_[:, 0:TB], func=AF.Gelu), reads=[bstok], writes=[gel])
            at = ATr[i % 4]
            P.op("pool", lambda e, gel=gel, at=at, i=i: e.tensor_tensor(out=at.ap, in0=gel.ap, in1=Gv[:, i, :], op=ALU.mult),
                 reads=[gel, G], writes=[at])
            P.opn("pe", [lambda e, tt=tt, half=half, at=at, vb=vb, i=i: e.matmul(
                banks[tt * 2 + half][0][:, :], lhsT=at.ap[:, tt * 128:(tt + 1) * 128], rhs=vb.ap[:, half * 512:(half + 1) * 512],
                start=(i == 0), stop=(i == 127)) for tt in range(NT) for half in range(2)],
                reads=[at, vb], writes=[banks[b4][2] for b4 in range(2 * NT)])
            if pend:
                P.replay(pend, per)
        P.replay(pend, len(pend))

    def b7(nb):
        for tt in range(NT):
            gt = nb * NT + tt
            xt = xts[tt]
            P.dma("sp", xt.ap, out_d[gt * 128:(gt + 1) * 128, :], reads=[out_tok[gt]], writes=[xt])
            for half in range(2):
                ab, _, abtok = banks[tt * 2 + half]
                P.op("dve", lambda e, half=half, ab=ab, xt=xt: e.tensor_tensor(
                    out=xt.ap[:, half * 512:(half + 1) * 512], in0=ab[:, :], in1=xt.ap[:, half * 512:(half + 1) * 512], op=ALU.add),
                    reads=[abtok, xt], writes=[xt])
            ss, rstd = new_small()
            smt = small_cur["tok"]
            P.op("dve", lambda e, xt=xt, ss=ss: e.scalar_tensor_tensor(out=eqb.ap[:, 0:1024], in0=xt.ap, scalar=1.0, in1=xt.ap,
                                                                       op0=ALU.mult, op1=ALU.mult, accum_out=ss),
                 reads=[xt], writes=[eqb, smt])
            P.op("pool", lambda e, ss=ss: e.tensor_scalar(out=ss, in0=ss, scalar1=1.0 / D, scalar2=EPS, op0=ALU.mult, op1=ALU.add),
                 reads=[smt], writes=[smt])
            P.op("pool", lambda e, ss=ss, rstd=rstd: e.tensor_tensor(out=rstd, in0=ss, in1=nhalf.ap, op=ALU.pow),
                 reads=[smt, nhalf], writes=[smt])
            P.op("dve", lambda e, xt=xt, rstd=rstd: e.scalar_tensor_tensor(out=xt.ap, in0=xt.ap, scalar=rstd, in1=fg.ap,
                                                                           op0=ALU.mult, op1=ALU.mult), reads=[xt, smt, fg], writes=[xt])
            P.dma("act", out_d[gt * 128:(gt + 1) * 128, :], xt.ap, reads=[xt], writes=[out_tok[gt]], key=xt)


    routing(0)
    for i in range(128):
        prep_uv(i)
    P.barrier()
    for nb in range(NBLK):
        b5(nb)
        pend = []
        if nb + 1 < NBLK:
            P.capture()
            routing(nb + 1)
            pend = P.end_capture()
        b6(nb, pend)
        b7(nb)


def prep_inputs(inputs):
    f = lambda a: np.ascontiguousarray(np.asarray(a, dtype=np.float32))
    rk = lambda w: f(w.reshape(8, 128, -1).transpose(1, 0, 2))
    pv = lambda v: v.reshape(8, 128).T
    x = f(inputs["x"])
    vecs = np.concatenate([pv(np.asarray(inputs[n])[0]) for n in
                           ("norm1_g", "conv_dw_b", "conv_ln_g", "conv_ln_b", "norm2_g")], axis=1)
    dww = np.asarray(inputs["conv_dw_w"])[0].reshape(31, 8, 128).transpose(2, 1, 0).reshape(128, 248)
    shared = {
        "win": rk(np.asarray(inputs["w_in"])[0]),
        "wpw": rk(np.asarray(inputs["conv_w_pw"])[0]),
        "wo": rk(np.asarray(inputs["attn_w_o"])[0]),
        "wout": rk(np.asarray(inputs["w_out"])[0]),
        "wq": rk(np.asarray(inputs["peer_w_query"])[0]),
        "vecs": f(vecs),
        "dww": f(dww),
        "sink": f(np.broadcast_to(np.asarray(inputs["attn_sink"])[0][None, :], (128, 16))),
        "fg": f(np.broadcast_to(np.asarray(inputs["final_g"])[None, :], (128, 1024))),
        "skT": f(np.asarray(inputs["peer_sub_keys"])[0].transpose(2, 0, 1).reshape(128, 256)),
        "pu": f(np.asarray(inputs["peer_u"])[0]),
        "pv": f(np.asarray(inputs["peer_v"])[0]),
    }
    xs = x.reshape(NCORES, TOK, D)
    return [dict(shared, x=np.ascontiguousarray(xs[c])) for c in range(NCORES)]


_NC_CACHE = {}


def kernel(**inputs):
    in_maps = prep_inputs(inputs)
    if "nc" not in _NC_CACHE:
        _NC_CACHE["nc"] = build_program()
    res = run_bass_kernel_spmd(_NC_CACHE["nc"], in_maps, core_ids=list(range(NCORES)))
    out = np.stack([np.asarray(r["out"]) for r in res.results], axis=0)
    return out.reshape(16, SEQ, D).astype(np.float32)
```

```python
import os
import numpy as np
from contextlib import ExitStack
import concourse.bass as bass
import concourse.mybir as mybir
from concourse.bass_utils import run_bass_kernel_spmd

F32 = mybir.dt.float32
BF16 = mybir.dt.bfloat16
U32 = mybir.dt.uint32
AF = mybir.ActivationFunctionType
ALU = mybir.AluOpType
AX = mybir.AxisListType

NCORES = 8
TOK = 4096
SEQ = 2048
D = 1024
EPS = 1e-6
TB = 256


class Tok:
    __slots__ = ("w", "r", "sem", "cnt", "name")

    def __init__(self, name=""):
        self.w = None
        self.r = {}
        self.sem = None
        self.cnt = 0
        self.name = name


class Buf:
    __slots__ = ("ap", "tok")

    def __init__(self, ap, name=""):
        self.ap = ap
        self.tok = Tok(name)


class Prog:
    ENG = ("pe", "dve", "act", "pool", "sp")

    def __init__(self, nc, es):
        self.nc = nc
        self.es = es
        self.ops = {e: [] for e in self.ENG}
        self.cnt = {e: 0 for e in self.ENG}
        self.sems = {}
        for e in self.ENG:
            self.sems["E_" + e] = es.enter_context(nc.semaphore("s_" + e))
        self.final = {}
        self.waited = {e: {} for e in self.ENG}
        self.ndma = 0

    def _collect(self, eng, reads, writes):
        need = {}

        def add(ev, raw):
            if ev is None:
                return
            key, val, src = ev
            if src == eng and eng == "pe":
                return
            if need.get(key, 0) < val:
                need[key] = val

        for t in reads:
            add(t.w, True)
        for t in writes:
            add(t.w, False)
            for key, (val, src) in t.r.items():
                add((key, val, src), False)
        waits = []
        wd = self.waited[eng]
        for key, val in need.items():
            if wd.get(key, 0) < val:
                wd[key] = val
                waits.append((key, val))
        return waits

    def _commit(self, ev, reads, writes):
        key, val, src = ev
        for t in reads:
            old = t.r.get(key)
            if old is None or old[0] < val:
                t.r[key] = (val, src)
        for t in writes:
            t.w = ev
            t.r = {}

    cap = None

    def capture(self):
        self.cap = []

    def end_capture(self):
        c, self.cap = self.cap, None
        return c

    def replay(self, lst, n):
        for _ in range(min(n, len(lst))):
            kind, args = lst.pop(0)
            getattr(self, kind)(*args)

    def op(self, eng, fn, reads=(), writes=()):
        if self.cap is not None:
            self.cap.append(("op", (eng, fn, list(reads), list(writes))))
            return
        reads = [b.tok if isinstance(b, Buf) else b for b in reads]
        writes = [b.tok if isinstance(b, Buf) else b for b in writes]
        waits = self._collect(eng, reads, writes)
        self.cnt[eng] += 1
        key = "E_" + eng
        ev = (key, self.cnt[eng], eng)
        self.final[key] = self.cnt[eng]
        self.ops[eng].append((waits, fn, key, 1))
        self._commit(ev, reads, writes)

    def opn(self, eng, fns, reads=(), writes=()):
        fns = list(fns)

        def run(e, fns=fns):
            last = None
            for f in fns:
                last = f(e)
            return last

        self.op(eng, run, reads, writes)

    def dma(self, q, out, in_, reads=(), writes=(), key=None):
        if self.cap is not None:
            self.cap.append(("dma", (q, out, in_, list(reads), list(writes), key)))
            return
        reads = [b.tok if isinstance(b, Buf) else b for b in reads]
        writes = [b.tok if isinstance(b, Buf) else b for b in writes]
        kt = key if key is not None else (writes[0] if writes else reads[0])
        if isinstance(kt, Buf):
            kt = kt.tok
        if kt.sem is None:
            self.ndma += 1
            kt.sem = "D_%d" % self.ndma
            self.sems[kt.sem] = self.es.enter_context(self.nc.semaphore("d%d" % self.ndma))
        waits = self._collect(q, reads, writes)
        kt.cnt += 16
        ev = (kt.sem, kt.cnt, None)
        self.final[kt.sem] = kt.cnt
        self.ops[q].append((waits, lambda e: e.dma_start(out=out, in_=in_), kt.sem, 16))
        self._commit(ev, reads, writes)

    def barrier(self):
        for e in self.ENG:
            waits = []
            wd = self.waited[e]
            for key, val in self.final.items():
                if key == "E_" + e:
                    continue
                if wd.get(key, 0) < val:
                    wd[key] = val
                    waits.append((key, val))
            if waits:
                self.ops[e].append((waits, None, None, 0))

    def emit(self):
        nc = self.nc
        self.barrier()
        engmap = {"pe": "tensor", "dve": "vector", "act": "scalar", "pool": "gpsimd", "sp": "sync"}
        with nc.Block() as block:
            for e in self.ENG:
                def body(engine, ops=self.ops[e], sems=self.sems):
                    for waits, fn, key, inc in ops:
                        for wk, wv in waits:
                            engine.wait_ge(sems[wk], wv)
                        if fn is not None:
                            fn(engine).then_inc(sems[key], inc)

                getattr(block, engmap[e])(body)


class Arena:
    def __init__(self, nc, nbytes):
        self.t32 = nc.alloc_sbuf_tensor("arena", [128, nbytes // 4], F32)
        self.t16 = self.t32.bitcast(BF16)
        self.tu = self.t32.bitcast(U32)
        self.off = 0
        self.cap = nbytes

    def alloc(self, nbytes):
        off = (self.off + 63) // 64 * 64
        self.off = off + nbytes
        assert self.off <= self.cap, (self.off, self.cap)
        return off

    def f32(self, n, name=""):
        o = self.alloc(n * 4)
        return Buf(self.t32[:, o // 4:o // 4 + n], name)

    def b16(self, n, name=""):
        o = self.alloc(n * 2)
        return Buf(self.t16[:, o // 2:o // 2 + n], name)

    def u32(self, n, name=""):
        o = self.alloc(n * 4)
        return Buf(self.tu[:, o // 4:o // 4 + n], name)


def bc(ap, dims):
    return bass.AP(ap.tensor, ap.offset, [list(ap.ap[0])] + [list(d) for d in dims])


def r3(ap, **kw):
    return ap.rearrange("p (a b) -> p a b", **kw)


KDBG = os.environ.get('KDBG', '')
SLOPES = [2.0 ** (-8.0 * (h + 1) / 16.0) for h in range(16)]


class _Stop(Exception):
    pass


def build_program(stop_after_a=False, stage=None):
    nc = bass.Bass("TRN2", target_bir_lowering=False)
    es = ExitStack()
    dt = lambda name, shape, dtype=F32, kind="ExternalInput": nc.dram_tensor(name, shape, dtype, kind=kind).ap()
    x_d = dt("x", [TOK, D])
    win_d = dt("win", [128, 8, 5632])
    wpw_d = dt("wpw", [128, 8, 1024])
    wo_d = dt("wo", [128, 8, 1024])
    wout_d = dt("wout", [128, 8, 1024])
    wq_d = dt("wq", [128, 8, 2048])
    vecs_d = dt("vecs", [128, 40])
    dww_d = dt("dww", [128, 248])
    sink_d = dt("sink", [128, 16])
    fg_d = dt("fg", [128, 1024])
    skT_d = dt("skT", [128, 256])
    pu_d = dt("pu", [16384, 1024])
    pv_d = dt("pv", [16384, 1024])
    out_d = dt("out", [TOK, D], F32, "ExternalOutput")
    ut_d = dt("ut_scr", [128, 128, 1024], BF16, "Internal")
    vb_d = dt("vb_scr", [16384, 1024], BF16, "Internal")
    wqb_d = dt("wqb_scr", [16, 128, 1024], BF16, "Internal")

    with es:
        P = Prog(nc, es)
        A = Arena(nc, 207 * 1024)
        banks = []
        psall = nc.alloc_psum_tensor("psall", [128, 4096], F32)
        psall16 = psall.bitcast(BF16)
        for i in range(8):
            banks.append((psall[:, i * 512:(i + 1) * 512], psall16[:, i * 1024:(i + 1) * 1024], Tok(f"bank{i}")))
        rr = {"i": 0}

        def nextbank(lst):
            rr["i"] += 1
            return banks[lst[rr["i"] % len(lst)]]

        ident = A.b16(128, "ident")
        identf = A.f32(128, "identf")
        ones16 = A.b16(128, "ones")
        vecs = A.f32(40, "vecs")
        dww = A.f32(248, "dww")
        esink = A.f32(16, "esink")
        small = A.f32(64, "small")
        out_tok = [Tok(f"out{i}") for i in range(TOK // 128)]

        P.dma("sp", vecs.ap, vecs_d, writes=[vecs])
        P.dma("sp", dww.ap, dww_d, writes=[dww])
        P.dma("sp", esink.ap, sink_d, writes=[esink])
        P.op("act", lambda e: e.activation(out=esink.ap, in_=esink.ap, func=AF.Exp), reads=[esink], writes=[esink])
        P.op("pool", lambda e: e.iota(identf.ap, [[1, 128]], base=0, channel_multiplier=-1,
                                      allow_small_or_imprecise_dtypes=True), writes=[identf])
        P.op("dve", lambda e: e.tensor_scalar(out=identf.ap, in0=identf.ap, scalar1=0.0, scalar2=None,
                                              op0=ALU.is_equal), reads=[identf], writes=[identf])
        P.op("dve", lambda e: e.tensor_copy(out=ident.ap, in_=identf.ap), reads=[identf], writes=[ident])
        P.op("pool", lambda e: e.memset(ones16.ap, 1.0 / 1024.0), writes=[ones16])
        G1, DWB, LNG, LNB, G2 = range(5)
        vec = lambda idx, k: vecs.ap[:, idx * 8 + k:idx * 8 + k + 1]

        mark0 = A.off
        Mtab = A.b16(3 * 16 * 128, "Mtab")
        Mv = Mtab.ap.rearrange("p (a h q) -> p a h q", a=3, h=16)
        hT = A.b16(8 * SEQ)
        hTv = r3(hT.ap, a=8)
        hT_tok = [Tok() for _ in range(16)]
        QT = A.b16(8 * SEQ)
        QTv = r3(QT.ap, a=8)
        QT_tok = [Tok() for _ in range(16)]
        cT = A.b16(8 * SEQ)
        cTv = r3(cT.ap, a=8)
        cT_tok = [[Tok() for _ in range(4)] for _ in range(8)]
        r4 = A.alloc(32768)
        KTv = r3(A.t16[:, r4 // 2:r4 // 2 + 4 * SEQ], a=4)
        KT_tok = Tok()
        Vo = r4 + 16384
        Vv = A.t16[:, Vo // 2:Vo // 2 + 16 * 4 * 65].rearrange("p (t g e) -> p t g e", t=16, g=4)
        V_tok = [Tok() for _ in range(16)]
        dgs = [Buf(A.t16[:, (r4 + i * 8192) // 2:(r4 + i * 8192) // 2 + 31 * 128]) for i in range(2)]
        Ub = [Buf(A.t16[:, (r4 + 16384 + i * 4224) // 2:(r4 + 16384 + i * 4224) // 2 + 2078]) for i in range(2)]
        mTv = r3(A.t16[:, r4 // 2:r4 // 2 + 8 * SEQ], a=8)
        mT_tok = [Tok() for _ in range(4)]
        wst = [A.f32(2048) for _ in range(2)]
        wbf = [A.b16(2048) for _ in range(4)]
        wst_h = [Buf(b.ap[:, h * 1024:(h + 1) * 1024]) for b in wst for h in range(2)]
        wbf_h = [Buf(b.ap[:, h * 1024:(h + 1) * 1024]) for b in wbf for h in range(2)]
        xts = [A.f32(1024) for _ in range(2)]
        xns = [A.b16(1024) for _ in range(2)]
        junk = A.b16(1024)
        w12 = A.alloc(12288)
        Ef = [Buf(A.t32[:, (w12 + i * 2048) // 4:(w12 + i * 2048) // 4 + 512]) for i in range(3)]
        PT = [Buf(A.t16[:, (w12 + 6144 + i * 1024) // 2:(w12 + 6144 + i * 1024) // 2 + 512]) for i in range(6)]
        lnm = Buf(A.t32[:, (w12) // 4:(w12) // 4 + 512])
        lnr = Buf(A.t32[:, (w12 + 2048) // 4:(w12 + 2048) // 4 + 512])
        lnt = [Buf(A.t32[:, (w12 + 4096 + i * 2048) // 4:(w12 + 4096 + i * 2048) // 4 + 512]) for i in range(2)]
        lnq = [Buf(A.t16[:, (w12 + 8192 + i * 1024) // 2:(w12 + 8192 + i * 1024) // 2 + 512]) for i in range(2)]
        wkd = Buf(A.t16[:, w12 // 2:w12 // 2 + 4096])
        wkdv = wkd.ap.rearrange("p (k g e) -> p k g e", k=8, g=4)
        AO = [A.b16(1024) for _ in range(2)]
        den = A.f32(16)
        rec = A.f32(16)
        ss_i = {"i": 0}
        small_cur = {"tok": None}
        wst_i = {"i": 0}
        wbf_i = {"i": 0}

        small_toks = [Tok() for _ in range(32)]

        def new_small():
            ss_i["i"] = (ss_i["i"] + 1) % 32
            i = ss_i["i"]
            small_cur["tok"] = small_toks[i]
            return small.ap[:, 2 * i:2 * i + 1], small.ap[:, 2 * i + 1:2 * i + 2]

        def load_w(src3, col0, ncols, scale_idx=None, eng="pool", half=False):
            wst_i["i"] += 1
            wbf_i["i"] += 1
            if half:
                st = wst_h[wst_i["i"] % 4]
                wb = wbf_h[wbf_i["i"] % 8]
            else:
                st = wst[wst_i["i"] % 2]
                wb = wbf[wbf_i["i"] % 4]
            stv = r3(st.ap[:, 0:8 * ncols], a=8)
            wbv = r3(wb.ap[:, 0:8 * ncols], a=8)
            P.dma("sp", stv, src3[:, :, col0:col0 + ncols], writes=[st])
            assert scale_idx is None
            P.op(eng, lambda e: e.tensor_copy(out=wb.ap[:, 0:8 * ncols], in_=st.ap[:, 0:8 * ncols]), reads=[st], writes=[wb])
            return wb, wbv

        def rms_tile(xt, xn):
            ss, rstd = new_small()
            sm = small_cur["tok"]
            P.op("act", lambda e: e.activation(out=junk.ap, in_=xt.ap, func=AF.Square, accum_out=ss),
                 reads=[xt], writes=[junk, sm])
            P.op("act", lambda e: e.activation(out=rstd, in_=ss, func=AF.Sqrt, scale=1.0 / D, bias=EPS),
                 reads=[sm], writes=[sm])
            P.op("dve", lambda e: e.reciprocal(out=rstd, in_=rstd), reads=[sm], writes=[sm])
            if xn is not None:
                P.op("dve", lambda e: e.tensor_scalar(out=xn.ap, in0=xt.ap, scalar1=rstd, scalar2=None, op0=ALU.mult),
                     reads=[xt, sm], writes=[xn])
            return rstd

        def transpose8(src, dst_fn, dst_toks, evac_eng="act", scale_idx=None, bl=(4,)):
            bt, bt16, btok = nextbank(list(bl))
            for k in range(8):
                P.op("pe", lambda e, k=k: e.transpose(out=bt16[:, k * 128:(k + 1) * 128], in_=src.ap[:, k * 128:(k + 1) * 128],
                                                      identity=ident.ap), reads=[src, ident], writes=[btok])
            if scale_idx is None:
                P.op(evac_eng, lambda e: (e.copy if evac_eng == "act" else e.tensor_copy)(
                    out=dst_fn(None), in_=r3(bt16[:, 0:1024], a=8)), reads=[btok], writes=dst_toks)
            else:
                for k in range(8):
                    if evac_eng == "act" or (evac_eng == "mix" and k % 2 == 0):
                        P.op("act", lambda e, k=k: e.activation(out=dst_fn(k), in_=bt16[:, k * 128:(k + 1) * 128], func=AF.Copy,
                                                                scale=vec(scale_idx, k)), reads=[btok, vecs], writes=dst_toks)
                    else:
                        P.op("dve", lambda e, k=k: e.tensor_scalar(out=dst_fn(k), in0=bt16[:, k * 128:(k + 1) * 128],
                                                                   scalar1=vec(scale_idx, k), scalar2=None, op0=ALU.mult),
                             reads=[btok, vecs], writes=dst_toks)

        Df = Ef[0]
        Am = Ef[1]
        Mf = Ef[2]
        for pos in range(3):
            P.op("pool", lambda e, pos=pos: e.iota(Df.ap[:, 0:128], [[1, 128]], base=128 * (1 - pos), channel_multiplier=-1,
                                                   allow_small_or_imprecise_dtypes=True), writes=[Df])
            P.op("act", lambda e: e.activation(out=Df.ap[:, 0:128], in_=Df.ap[:, 0:128], func=AF.Abs),
                 reads=[Df], writes=[Df])
            P.op("dve", lambda e: e.tensor_scalar(out=Am.ap[:, 0:128], in0=Df.ap[:, 0:128], scalar1=128.0, scalar2=None,
                                                  op0=ALU.is_le), reads=[Df], writes=[Am])
            for h in range(16):
                P.op("act", lambda e, h=h: e.activation(out=Mf.ap[:, 0:128], in_=Df.ap[:, 0:128], func=AF.Exp, scale=-SLOPES[h]),
                     reads=[Df], writes=[Mf])
                P.op("dve", lambda e, pos=pos, h=h: e.tensor_tensor(out=Mv[:, pos, h, :], in0=Mf.ap[:, 0:128], in1=Am.ap[:, 0:128],
                                                                    op=ALU.mult), reads=[Mf, Am], writes=[Mtab])
        P.barrier()

        def chk(name):
            if stage == name:
                raise _Stop()

        try:
          for s in range(2):
            tb0 = s * SEQ
            chk('S0')
            def s1_pre(tt):
                xt = xts[tt % 2]
                xn = xns[tt % 2]
                P.dma("sp", xt.ap, x_d[tb0 + tt * 128:tb0 + (tt + 1) * 128, :], writes=[xt])
                rms_tile(xt, xn)

            s1_pre(0)
            for tt in range(16):
                if tt + 1 < 16:
                    s1_pre(tt + 1)
                transpose8(xns[tt % 2], lambda k, tt=tt: hTv[:, k, tt * 128:(tt + 1) * 128], [hT_tok[tt]], evac_eng="mix", scale_idx=G1,
                           bl=(4, 5, 6, 7))

            chk('S1')
            ev_i = 0
            for cp in range(4):
                wb, wbv = load_w(win_d, 2048 + cp * 256, 256)
                for cc in range(2):
                    c = cp * 2 + cc
                    for b in range(4):
                        bt, _, btok = nextbank([0, 1, 2, 3])
                        P.opn("pe", [lambda e, k=k, cc=cc, b=b, wbv=wbv, bt=bt: e.matmul(
                            bt[:, :], lhsT=wbv[:, k, cc * 128:(cc + 1) * 128], rhs=hTv[:, k, b * 512:(b + 1) * 512],
                            start=(k == 0), stop=(k == 7)) for k in range(8)], reads=[wb] + hT_tok[4 * b:4 * b + 4], writes=[btok])
                        dst = QTv[:, c, b * 512:(b + 1) * 512]
                        ev_i += 1
                        if ev_i % 2:
                            P.op("act", lambda e, dst=dst, bt=bt: e.mul(out=dst, in_=bt[:, :], mul=0.125),
                                 reads=[btok], writes=QT_tok[4 * b:4 * b + 4])
                        else:
                            P.op("dve", lambda e, dst=dst, bt=bt: e.tensor_scalar(out=dst, in0=bt[:, :], scalar1=0.125, scalar2=None,
                                                                                  op0=ALU.mult), reads=[btok], writes=QT_tok[4 * b:4 * b + 4])
            wst_i["i"] += 1
            st = wst[wst_i["i"] % 2]
            stv = r3(st.ap, a=8)
            P.dma("sp", stv, win_d[:, :, 3072:3328], writes=[st])
            for half in range(2):
                P.op("pool", lambda e, half=half: e.tensor_copy(
                    out=wkdv[:, :, :, half * 64:(half + 1) * 64], in_=st.ap.rearrange("p (k g e) -> p k g e", k=8, g=4)),
                    reads=[st], writes=[wkd])
            for g in range(4):
                for b in range(4):
                    bt, _, btok = nextbank([0, 1, 2, 3])
                    for k in range(8):
                        P.op("pe", lambda e, k=k, g=g, b=b, bt=bt: e.matmul(
                            bt[:, :], lhsT=wkdv[:, k, g, :], rhs=hTv[:, k, b * 512:(b + 1) * 512],
                            start=(k == 0), stop=(k == 7)), reads=[wkd] + hT_tok[4 * b:4 * b + 4], writes=[btok])
                    P.op("act", lambda e, g=g, b=b, bt=bt: e.copy(out=KTv[:, g, b * 512:(b + 1) * 512], in_=bt[:, :]),
                         reads=[btok], writes=[KT_tok])
            wb, wbv = load_w(win_d, 3328, 256)
            P.op("pool", lambda e: e.memset(Vv[:, :, :, 64:65], 1.0), writes=V_tok)
            for tt in range(16):
                bt, _, btok = nextbank([0, 1, 2, 3])
                for k in range(8):
                    P.op("pe", lambda e, k=k, tt=tt, wbv=wbv, bt=bt: e.matmul(
                        bt[:, 0:256], lhsT=hTv[:, k, tt * 128:(tt + 1) * 128], rhs=wbv[:, k, :],
                        start=(k == 0), stop=(k == 7)), reads=[wb, hT_tok[tt]], writes=[btok])
                P.op("dve", lambda e, tt=tt, bt=bt: e.tensor_copy(out=Vv[:, tt, :, 0:64],
                                                                  in_=bt[:, 0:256].rearrange("p (g e) -> p g e", g=4)),
                     reads=[btok], writes=[V_tok[tt]])

            chk('S2')
            cnt3 = {"pt": 0, "ef": 0}
            po = [banks[5], banks[6], banks[7]]

            def st_phase(i, g):
                kbs = [kb for kb in (i - 1, i, i + 1) if 0 <= kb < 16]
                pts = []
                for kb in kbs:
                    pos = kb - i + 1
                    rr["pair"] = rr.get("pair", 0) + 1
                    b0 = 2 * (rr["pair"] % 2)
                    pair = (banks[b0], banks[b0 + 1])
                    for j in range(4):
                        h = 4 * g + j
                        c, p = h // 2, h % 2
                        bt, _, btok = pair[p]
                        jj = j // 2
                        P.op("pe", lambda e, jj=jj, g=g, kb=kb, c=c, p=p, i=i, bt=bt: e.matmul(
                            bt[:, jj * 128:(jj + 1) * 128], lhsT=KTv[64 * p:64 * p + 64, g, kb * 128:(kb + 1) * 128],
                            rhs=QTv[64 * p:64 * p + 64, c, i * 128:(i + 1) * 128], start=True, stop=True),
                            reads=[KT_tok, QT_tok[i]], writes=[btok])
                    cnt3["ef"] += 1
                    ef = Ef[cnt3["ef"] % 3]
                    src2 = bc(pair[0][0][:, 0:1], [[512, 2], [1, 256]])
                    P.op("act", lambda e, ef=ef, src2=src2: e.activation(out=ef.ap.rearrange("p (a b) -> p a b", a=2), in_=src2,
                                                                       func=AF.Exp),
                         reads=[pair[0][2], pair[1][2]], writes=[ef])
                    cnt3["pt"] += 1
                    pt = PT[cnt3["pt"] % 6]
                    m4 = bc(Mv[:, pos, 4 * g, :], [[128, 2], [256, 2], [1, 128]])
                    P.op("dve", lambda e, ef=ef, pt=pt, m4=m4: e.tensor_tensor(
                        out=pt.ap.rearrange("p (a b q) -> p a b q", a=2, b=2), in0=ef.ap.rearrange("p (a b q) -> p a b q", a=2, b=2),
                        in1=m4, op=ALU.mult), reads=[ef, Mtab], writes=[pt])
                    pts.append(pt)
                return pts

            def pv_phase(i, g, pts):
                kbs = [kb for kb in (i - 1, i, i + 1) if 0 <= kb < 16]
                for j in range(4):
                    h = 4 * g + j
                    pb, _, pbtok = po[h // 7]
                    o0 = (h % 7) * 65
                    P.opn("pe", [lambda e, j=j, g=g, kb=kb, n=n, pb=pb, o0=o0, pt=pts[n], last=len(kbs) - 1: e.matmul(
                        pb[:, o0:o0 + 65], lhsT=pt.ap[:, (j % 2) * 256 + (j // 2) * 128:(j % 2) * 256 + (j // 2) * 128 + 128], rhs=Vv[:, kb, g, :],
                        start=(n == 0), stop=(n == last)) for n, kb in enumerate(kbs)],
                        reads=list(pts) + [V_tok[kb] for kb in kbs], writes=[pbtok])

            items3 = [(i, g) for i in range(16) for g in range(4)]
            pend3 = st_phase(*items3[0])
            for idx3, (i, g) in enumerate(items3):
                nxt3 = st_phase(*items3[idx3 + 1]) if idx3 + 1 < len(items3) else None
                pv_phase(i, g, pend3)
                pend3 = nxt3
                if g != 3:
                    continue
                if stage in ('S3a', 'S3b'):
                    continue
                ao = AO[i % 2]
                for b3, (h0, nh) in enumerate(((0, 7), (7, 7), (14, 2))):
                    pb, _, pbtok = po[b3]
                    pv = pb[:, 0:nh * 65].rearrange("p (h e) -> p h e", e=65)
                    P.op("dve", lambda e, pv=pv, h0=h0, nh=nh: e.tensor_tensor(
                        out=den.ap[:, h0:h0 + nh], in0=pv[:, :, 64], in1=esink.ap[:, h0:h0 + nh], op=ALU.add),
                        reads=[pbtok, esink], writes=[den])
                P.op("dve", lambda e: e.reciprocal(out=rec.ap, in_=den.ap), reads=[den], writes=[rec])
                for b3, (h0, nh) in enumerate(((0, 7), (7, 7), (14, 2))):
                    pb, _, pbtok = po[b3]
                    pv = pb[:, 0:nh * 65].rearrange("p (h e) -> p h e", e=65)
                    P.op("dve", lambda e, pv=pv, h0=h0, nh=nh, ao=ao: e.tensor_tensor(
                        out=ao.ap[:, h0 * 64:(h0 + nh) * 64].rearrange("p (h e) -> p h e", e=64), in0=pv[:, :, 0:64],
                        in1=bc(rec.ap[:, h0:h0 + nh], [[1, nh], [0, 64]]), op=ALU.mult),
                        reads=[pbtok, rec], writes=[ao])
                if stage == 'S3c':
                    continue
                transpose8(ao, lambda k, i=i: QTv[:, :, i * 128:(i + 1) * 128], [QT_tok[i]])
            chk('S3'); chk('S3a'); chk('S3b'); chk('S3c')
            P.barrier()

            for u in Ub:
                P.op("pool", lambda e, u=u: e.memset(u.ap[:, 0:15], 0.0), writes=[u])
                P.op("pool", lambda e, u=u: e.memset(u.ap[:, 2063:2078], 0.0), writes=[u])
            sg_i = 0
            for cp in range(4):
                wa, wav = load_w(win_d, cp * 256, 256)
                wg, wgv = load_w(win_d, 1024 + cp * 256, 256)
                for cc in range(2):
                    c = cp * 2 + cc
                    u = Ub[c % 2]
                    for b in range(4):
                        ba, _, batok = nextbank([0, 1, 2, 3])
                        bg, _, bgtok = nextbank([0, 1, 2, 3])
                        for (wt, wtv, bt, btok) in ((wa, wav, ba, batok), (wg, wgv, bg, bgtok)):
                            P.opn("pe", [lambda e, k=k, cc=cc, b=b, wtv=wtv, bt=bt: e.matmul(
                                bt[:, :], lhsT=wtv[:, k, cc * 128:(cc + 1) * 128], rhs=hTv[:, k, b * 512:(b + 1) * 512],
                                start=(k == 0), stop=(k == 7)) for k in range(8)], reads=[wt] + hT_tok[4 * b:4 * b + 4], writes=[btok])
                        sg_i += 1
                        sg = Ef[sg_i % 3]
                        P.op("act", lambda e, sg=sg, bg=bg: e.activation(out=sg.ap, in_=bg[:, :], func=AF.Sigmoid),
                             reads=[bgtok], writes=[sg])
                        P.op("dve", lambda e, sg=sg, ba=ba, u=u, b=b: e.tensor_tensor(
                            out=u.ap[:, 15 + b * 512:15 + (b + 1) * 512], in0=ba[:, :], in1=sg.ap, op=ALU.mult),
                            reads=[batok, sg], writes=[u])
                    dg = dgs[c % 2]
                    P.op("dve", lambda e, dg=dg, c=c: e.tensor_tensor(
                        out=dg.ap.rearrange("p (t j) -> p t j", t=31), in0=bc(ident.ap[:, 0:1], [[0, 31], [1, 128]]),
                        in1=bc(dww.ap[:, c * 31:c * 31 + 1], [[1, 31], [0, 128]]), op=ALU.mult), reads=[ident, dww], writes=[dg])
                    for b in range(4):
                        bt, _, btok = nextbank([0, 1, 2, 3])
                        P.opn("pe", [lambda e, tap=tap, dg=dg, u=u, b=b, bt=bt: e.matmul(
                            bt[:, :], lhsT=dg.ap[:, tap * 128:(tap + 1) * 128], rhs=u.ap[:, tap + b * 512:tap + b * 512 + 512],
                            start=(tap == 0), stop=(tap == 30)) for tap in range(31)], reads=[dg, u], writes=[btok])
                        P.op("act", lambda e, c=c, b=b, bt=bt: e.activation(out=cTv[:, c, b * 512:(b + 1) * 512], in_=bt[:, :],
                                                                            func=AF.Identity, bias=vec(DWB, c)),
                             reads=[btok, vecs], writes=[cT_tok[c][b]])
            P.barrier()
            for b in range(4):
                bs_, _, bstok = nextbank([0, 1, 2, 3])
                bq_, _, bqtok = nextbank([0, 1, 2, 3])
                blk = slice(b * 512, (b + 1) * 512)
                for c in range(8):
                    sq = lnq[c % 2]
                    P.op("act", lambda e, sq=sq, c=c, blk=blk: e.activation(out=sq.ap, in_=cTv[:, c, blk], func=AF.Square),
                         reads=[cT_tok[c][b]], writes=[sq])
                    P.op("pe", lambda e, c=c, blk=blk, bs_=bs_: e.matmul(bs_[:, :], lhsT=ones16.ap, rhs=cTv[:, c, blk],
                                                                       start=(c == 0), stop=(c == 7)),
                         reads=[ones16, cT_tok[c][b]], writes=[bstok])
                    P.op("pe", lambda e, c=c, sq=sq, bq_=bq_: e.matmul(bq_[:, :], lhsT=ones16.ap, rhs=sq.ap,
                                                                     start=(c == 0), stop=(c == 7)),
                         reads=[ones16, sq], writes=[bqtok])
                P.op("act", lambda e, bs_=bs_: e.copy(out=lnm.ap, in_=bs_[:, :]), reads=[bstok], writes=[lnm])
                P.op("dve", lambda e: e.tensor_tensor(out=lnr.ap, in0=lnm.ap, in1=lnm.ap, op=ALU.mult), reads=[lnm], writes=[lnr])
                P.op("dve", lambda e, bq_=bq_: e.tensor_tensor(out=lnr.ap, in0=bq_[:, :], in1=lnr.ap, op=ALU.subtract),
                     reads=[bqtok, lnr], writes=[lnr])
                P.op("act", lambda e: e.activation(out=lnr.ap, in_=lnr.ap, func=AF.Sqrt, bias=EPS), reads=[lnr], writes=[lnr])
                P.op("dve", lambda e: e.reciprocal(out=lnr.ap, in_=lnr.ap), reads=[lnr], writes=[lnr])
                for c in range(8):
                    t1 = lnt[c % 2]
                    P.op("dve", lambda e, t1=t1, c=c, blk=blk: e.tensor_tensor(out=t1.ap, in0=cTv[:, c, blk], in1=lnm.ap, op=ALU.subtract),
                         reads=[cT_tok[c][b], lnm], writes=[t1])
                    P.op("dve", lambda e, t1=t1: e.tensor_tensor(out=t1.ap, in0=t1.ap, in1=lnr.ap, op=ALU.mult),
                         reads=[t1, lnr], writes=[t1])
                    P.op("act", lambda e, t1=t1, c=c, blk=blk: e.activation(out=cTv[:, c, blk], in_=t1.ap, func=AF.Silu,
                                                                           scale=vec(LNG, c), bias=vec(LNB, c)),
                         reads=[t1, vecs], writes=[cT_tok[c][b]])
            chk('S4')
            P.barrier()

            sg_i = 0
            for c in range(8):
                w1, w1v = load_w(wpw_d, c * 128, 128, half=True)
                w2, w2v = load_w(wo_d, c * 128, 128, half=True)
                w3, w3v = load_w(win_d, 3584 + c * 128, 128, half=True)
                w4, w4v = load_w(win_d, 4608 + c * 128, 128, half=True)
                for b in range(4):
                    blk = slice(b * 512, (b + 1) * 512)
                    bks = [nextbank([0, 1, 2, 3]) for _ in range(4)]
                    srcs = ((w1, w1v, cTv, [cT_tok[k][b] for k in range(8)]),
                            (w2, w2v, QTv, QT_tok[4 * b:4 * b + 4]),
                            (w3, w3v, hTv, hT_tok[4 * b:4 * b + 4]),
                            (w4, w4v, hTv, hT_tok[4 * b:4 * b + 4]))
                    for (wt, wtv, src, stoks), (bt, _, btok) in zip(srcs, bks):
                        P.opn("pe", [lambda e, k=k, wtv=wtv, src=src, bt=bt, blk=blk: e.matmul(
                            bt[:, :], lhsT=wtv[:, k, 0:128], rhs=src[:, k, blk], start=(k == 0), stop=(k == 7)) for k in range(8)],
                            reads=[wt] + list(stoks), writes=[btok])
                    sa = Ef[0]
                    sb_ = Ef[1]
                    m1 = Ef[2]
                    P.op("act", lambda e, bt=bks[2][0]: e.activation(out=sa.ap, in_=bt[:, :], func=AF.Sigmoid),
                         reads=[bks[2][2]], writes=[sa])
                    P.op("act", lambda e, bt=bks[3][0]: e.activation(out=sb_.ap, in_=bt[:, :], func=AF.Sigmoid),
                         reads=[bks[3][2]], writes=[sb_])
                    P.op("dve", lambda e, bt=bks[0][0]: e.tensor_tensor(out=m1.ap, in0=bt[:, :], in1=sa.ap, op=ALU.mult),
                         reads=[bks[0][2], sa], writes=[m1])
                    P.op("dve", lambda e, bt=bks[1][0]: e.tensor_tensor(out=sb_.ap, in0=bt[:, :], in1=sb_.ap, op=ALU.mult),
                         reads=[bks[1][2], sb_], writes=[sb_])
                    P.op("pool", lambda e, c=c, blk=blk: e.tensor_tensor(out=mTv[:, c, blk], in0=m1.ap, in1=sb_.ap, op=ALU.add),
                         reads=[m1, sb_], writes=[mT_tok[b]])

            chk('S5')
            P.barrier()
            wo4 = [load_w(wout_d, q4 * 256, 256) for q4 in range(4)]
            for tt in range(16):
                xt = xts[tt % 2]
                gt = (tb0 // 128) + tt
                P.dma("sp", xt.ap, x_d[tb0 + tt * 128:tb0 + (tt + 1) * 128, :], writes=[xt])
                for half in range(2):
                    bt, _, btok = nextbank([0, 1, 2, 3])
                    for q2 in range(2):
                        wb, wbv = wo4[half * 2 + q2]
                        P.opn("pe", [lambda e, k=k, tt=tt, q2=q2, wbv=wbv, bt=bt: e.matmul(
                            bt[:, q2 * 256:(q2 + 1) * 256], lhsT=mTv[:, k, tt * 128:(tt + 1) * 128], rhs=wbv[:, k, :],
                            start=(k == 0), stop=(k == 7)) for k in range(8)], reads=[wb, mT_tok[tt // 4]], writes=[btok])
                    P.op("dve", lambda e, half=half, bt=bt, xt=xt: e.tensor_tensor(
                        out=xt.ap[:, half * 512:(half + 1) * 512], in0=bt[:, :], in1=xt.ap[:, half * 512:(half + 1) * 512],
                        op=ALU.add), reads=[btok, xt], writes=[xt])
                P.dma("act", out_d[gt * 128:(gt + 1) * 128, :], xt.ap, reads=[xt], writes=[out_tok[gt]], key=xt)
            chk('S6')
            P.barrier()
        except _Stop:
            pass

        if not stop_after_a:
            A.off = mark0
            build_peer(nc, P, A, banks, nextbank, dict(
                ident=ident, identf=identf, vecs=vecs, vec=vec, G2=G2, small=small, new_small=new_small, small_cur=small_cur,
                out_tok=out_tok, out_d=out_d, fg_d=fg_d, skT_d=skT_d, pu_d=pu_d, pv_d=pv_d, wq_d=wq_d,
                ut_d=ut_d, vb_d=vb_d, wqb_d=wqb_d))
        P.emit()
    return nc


def build_peer(nc, P, A, banks, nextbank, C):
    ident, identf, vecs, vec, G2 = C["ident"], C["identf"], C["vecs"], C["vec"], C["G2"]
    small, new_small, out_tok, out_d = C["small"], C["new_small"], C["out_tok"], C["out_d"]
    small_cur = C["small_cur"]
    ut_d, vb_d, wqb_d = C["ut_d"], C["vb_d"], C["wqb_d"]
    NBLK = TOK // TB
    NT = TB // 128
    mark = A.off
    NPB = 4
    pst = [A.f32(1024) for _ in range(NPB)]
    pst2 = [A.f32(1024) for _ in range(NPB)]
    pbf = [A.b16(1024) for _ in range(NPB)]
    pbf2 = [A.b16(1024) for _ in range(NPB)]
    utb = [A.b16(1024) for _ in range(2)]
    assert A.off - mark <= 128 * TB * 2
    A.off = mark
    G = A.b16(128 * TB)
    Gv = G.ap.rearrange("p (i t) -> p i t", i=128)
    h2T = [A.b16(8 * TB) for _ in range(2)]
    h2T_toks = [[Tok() for _ in range(NT)] for _ in range(2)]
    qT = A.f32(16 * TB)
    qTv = r3(qT.ap, a=16)
    sc = [A.f32(2048)] * 2
    sc2 = A.f32(256)
    v16 = A.f32(256)
    v16v = r3(v16.ap, a=16)
    ix = A.u32(256)
    ixv = r3(ix.ap, a=16)
    ixf = A.f32(256)
    cand = sc[0]
    eqb = A.f32(2048)
    eq2 = cand
    ts = A.f32(128)
    tsv = r3(ts.ap, a=8)
    pos = A.u32(128)
    posv = r3(pos.ap, a=8)
    posf = A.f32(128)
    k1f = A.f32(128)
    k2f = A.f32(128)
    Ivs = [A.f32(128) for _ in range(2)]
    Jvs = [A.f32(128) for _ in range(2)]
    Wvs = [A.f32(128) for _ in range(2)]
    ew = A.f32(128)
    zz = A.f32(16)
    SM = A.f32(3 * TB)
    SMv = r3(SM.ap, a=3)
    iota128 = A.b16(128)
    iotaf = A.f32(128)
    skT = A.f32(256)
    skTv = r3(skT.ap, a=2)
    fg = A.f32(1024)
    ohB = [A.b16(16 * 128) for _ in range(2)]
    ohE = [A.b16(16 * 128) for _ in range(2)]
    OAI = A.b16(TB * 8)
    OAJ = A.b16(TB * 8)
    OBI = A.b16(TB * 16)
    OBJ = A.b16(TB * 16)
    xh = A.f32(TB)
    xl = A.f32(TB)
    ubr = [A.b16(1024) for _ in range(4)]
    vbr = [A.b16(1024) for _ in range(4)]
    gelr = [A.f32(TB) for _ in range(3)]
    ATr = [A.b16(TB) for _ in range(4)]
    xts = [A.f32(1024) for _ in range(2)]
    xns = [A.b16(1024) for _ in range(2)]
    wqc = [A.b16(1024) for _ in range(3)]
    ut_tok = [Tok() for _ in range(128)]
    vb_tok = [Tok() for _ in range(128)]
    wqb_tok = [Tok() for _ in range(16)]

    P.dma("sp", skT.ap, C["skT_d"], writes=[skT])
    P.dma("sp", fg.ap, C["fg_d"], writes=[fg])
    P.op("pool", lambda e: e.iota(iotaf.ap, [[1, 128]], base=0, channel_multiplier=0, allow_small_or_imprecise_dtypes=True),
         writes=[iotaf])
    P.op("dve", lambda e: e.tensor_copy(out=iota128.ap, in_=iotaf.ap), reads=[iotaf], writes=[iota128])
    k16 = A.f32(16)
    nhalf = A.f32(1)
    P.op("pool", lambda e: e.memset(nhalf.ap, -0.5), writes=[nhalf])
    P.op("dve", lambda e: e.tensor_scalar(out=k16.ap, in0=iotaf.ap[:, 0:16], scalar1=16.0, scalar2=None, op0=ALU.mult),
         reads=[iotaf], writes=[k16])

    for m in range(16):
        st = pst[m % NPB]
        pb = pbf[m % NPB]
        P.dma("sp", r3(st.ap, a=8), C["wq_d"][:, :, m * 128:(m + 1) * 128], writes=[st])
        P.op("pool", lambda e, st=st, pb=pb: e.tensor_copy(out=pb.ap, in_=st.ap), reads=[st], writes=[pb])
        P.dma("act", wqb_d[m], pb.ap, reads=[pb], writes=[wqb_tok[m]], key=pb)
    def prep_uv(i):
        st = pst[i % NPB]
        pb = pbf[i % NPB]
        P.dma("sp", st.ap, C["pu_d"][i * 128:(i + 1) * 128, :], writes=[st])
        P.op("pool", lambda e, st=st, pb=pb: e.tensor_copy(out=pb.ap, in_=st.ap), reads=[st], writes=[pb])
        bt, bt16, btok = nextbank([4, 5, 6, 7])
        for k in range(8):
            P.op("pe", lambda e, k=k, pb=pb, bt16=bt16: e.transpose(out=bt16[:, k * 128:(k + 1) * 128], in_=pb.ap[:, k * 128:(k + 1) * 128],
                                                             identity=ident.ap), reads=[pb, ident], writes=[btok])
        ut = utb[i % 2]
        P.op("act", lambda e, ut=ut, bt16=bt16: e.copy(out=ut.ap, in_=bt16[:, 0:1024]), reads=[btok], writes=[ut])
        P.dma("act", ut_d[i], ut.ap, reads=[ut], writes=[ut_tok[i]], key=ut)
        st2 = pst2[i % NPB]
        pb2 = pbf2[i % NPB]
        P.dma("pool", st2.ap, C["pv_d"][i * 128:(i + 1) * 128, :], writes=[st2])
        P.op("dve", lambda e, st2=st2, pb2=pb2: e.tensor_copy(out=pb2.ap, in_=st2.ap), reads=[st2], writes=[pb2])
        P.dma("act", vb_d[i * 128:(i + 1) * 128, :], pb2.ap, reads=[pb2], writes=[vb_tok[i]], key=pb2)

    cnt = {"oh": 0, "ev": 0}

    def routing(nb):
        h2Tv = r3(h2T[nb % 2].ap, a=8)
        h2T_tok = h2T_toks[nb % 2]
        for tt in range(NT):
            gt = nb * NT + tt
            xt = xts[tt]
            xn = xns[tt]
            P.dma("sp", xt.ap, out_d[gt * 128:(gt + 1) * 128, :], reads=[out_tok[gt]], writes=[xt])
            ss, rstd = new_small()
            smt = small_cur["tok"]
            P.op("dve", lambda e, xt=xt, ss=ss: e.scalar_tensor_tensor(out=eqb.ap[:, 0:1024], in0=xt.ap, scalar=1.0, in1=xt.ap,
                                                                       op0=ALU.mult, op1=ALU.mult, accum_out=ss),
                 reads=[xt], writes=[eqb, smt])
            P.op("pool", lambda e, ss=ss: e.tensor_scalar(out=ss, in0=ss, scalar1=1.0 / D, scalar2=EPS, op0=ALU.mult, op1=ALU.add),
                 reads=[smt], writes=[smt])
            P.op("pool", lambda e, ss=ss, rstd=rstd: e.tensor_tensor(out=rstd, in0=ss, in1=nhalf.ap, op=ALU.pow),
                 reads=[smt, nhalf], writes=[smt])
            P.op("dve", lambda e, xt=xt, xn=xn, rstd=rstd: e.tensor_scalar(out=xn.ap, in0=xt.ap, scalar1=rstd, scalar2=None, op0=ALU.mult),
                 reads=[xt, smt], writes=[xn])
            bt, bt16, btok = nextbank([6, 7])
            for k in range(8):
                P.op("pe", lambda e, k=k, xn=xn, bt16=bt16: e.transpose(out=bt16[:, k * 128:(k + 1) * 128], in_=xn.ap[:, k * 128:(k + 1) * 128],
                                                                 identity=ident.ap), reads=[xn, ident], writes=[btok])
            for k in range(8):
                P.op("dve", lambda e, k=k, tt=tt, bt16=bt16: e.tensor_scalar(
                    out=h2Tv[:, k, tt * 128:(tt + 1) * 128], in0=bt16[:, k * 128:(k + 1) * 128], scalar1=vec(G2, k), scalar2=None,
                    op0=ALU.mult), reads=[btok, vecs], writes=[h2T_tok[tt]])
        for m in range(3):
            P.dma("act", wqc[m].ap, wqb_d[m], reads=[wqb_tok[m]], writes=[wqc[m]])
        for m in range(16):
            wq = wqc[m % 3]
            bt, _, btok = nextbank([6, 7])
            P.opn("pe", [lambda e, k=k, wq=wq, bt=bt: e.matmul(bt[:, 0:TB], lhsT=r3(wq.ap, a=8)[:, k, :], rhs=h2Tv[:, k, :],
                                                           start=(k == 0), stop=(k == 7)) for k in range(8)],
                  reads=[wq] + h2T_tok, writes=[btok])
            P.op("act", lambda e, m=m, bt=bt: e.copy(out=qTv[:, m, :], in_=bt[:, 0:TB]), reads=[btok], writes=[qT])
            if m + 3 < 16:
                P.dma("act", wq.ap, wqb_d[m + 3], reads=[wqb_tok[m + 3]], writes=[wq])
        for tt in range(NT):
            Iv, Jv, Wv = Ivs[tt], Jvs[tt], Wvs[tt]
            scb = sc[tt]
            for bq in range(4):
                bt, _, btok = nextbank([6, 7])
                for mm in range(4):
                    m = bq * 4 + mm
                    P.op("pe", lambda e, mm=mm, m=m, tt=tt, bt=bt: e.matmul(
                        bt[:, mm * 128:(mm + 1) * 128], lhsT=qTv[:, m, tt * 128:(tt + 1) * 128], rhs=skTv[:, m % 2, :],
                        start=True, stop=True), reads=[qT, skT], writes=[btok])
                P.op("dve", lambda e, bq=bq, bt=bt, scb=scb: e.tensor_copy(out=scb.ap[:, bq * 512:(bq + 1) * 512], in_=bt[:, :]),
                     reads=[btok], writes=[scb])
            for m in range(16):
                src = scb.ap[:, m * 128:(m + 1) * 128]
                P.op("dve", lambda e, m=m, src=src: e.max(out=v16v[:, m, 0:8], in_=src), reads=[scb], writes=[v16])
                P.op("dve", lambda e, m=m, src=src: e.max_index(out=ixv[:, m, 0:8], in_max=v16v[:, m, 0:8], in_values=src),
                     reads=[scb, v16], writes=[ix])
                P.op("dve", lambda e, m=m, src=src: e.match_replace(out=sc2.ap[:, 0:128], in_to_replace=v16v[:, m, 0:8], in_values=src,
                                                                    imm_value=-1e30), reads=[scb, v16], writes=[sc2])
                P.op("dve", lambda e, m=m: e.max(out=v16v[:, m, 8:16], in_=sc2.ap[:, 0:128]), reads=[sc2], writes=[v16])
                P.op("dve", lambda e, m=m: e.max_index(out=ixv[:, m, 8:16], in_max=v16v[:, m, 8:16], in_values=sc2.ap[:, 0:128]),
                     reads=[sc2, v16], writes=[ix])
            P.op("dve", lambda e: e.tensor_copy(out=ixf.ap, in_=ix.ap), reads=[ix], writes=[ixf])
            c4 = lambda b: b.ap.rearrange("p (h a b) -> p h a b", h=8, a=16)
            P.op("dve", lambda e: e.tensor_tensor(out=c4(cand), in0=bc(v16.ap[:, 0:1], [[32, 8], [1, 16], [0, 16]]),
                                                  in1=bc(v16.ap[:, 16:17], [[32, 8], [0, 16], [1, 16]]), op=ALU.add),
                 reads=[v16], writes=[cand])
            for h in range(8):
                src = cand.ap[:, h * 256:(h + 1) * 256]
                P.op("dve", lambda e, h=h, src=src: e.max(out=tsv[:, h, 0:8], in_=src), reads=[cand], writes=[ts])
                P.op("dve", lambda e, h=h, src=src: e.max_index(out=posv[:, h, 0:8], in_max=tsv[:, h, 0:8], in_values=src),
                     reads=[cand, ts], writes=[pos])
                P.op("dve", lambda e, h=h, src=src: e.match_replace(out=sc2.ap, in_to_replace=tsv[:, h, 0:8], in_values=src,
                                                                    imm_value=-1e30), reads=[cand, ts], writes=[sc2])
                P.op("dve", lambda e, h=h: e.max(out=tsv[:, h, 8:16], in_=sc2.ap), reads=[sc2], writes=[ts])
                P.op("dve", lambda e, h=h: e.max_index(out=posv[:, h, 8:16], in_max=tsv[:, h, 8:16], in_values=sc2.ap),
                     reads=[sc2, ts], writes=[pos])
            P.op("dve", lambda e: e.tensor_tensor(out=r3(ew.ap, a=8), in0=tsv, in1=bc(ts.ap[:, 0:1], [[16, 8], [0, 16]]), op=ALU.subtract),
                 reads=[ts], writes=[ew])
            P.op("act", lambda e: e.activation(out=ew.ap, in_=ew.ap, func=AF.Exp), reads=[ew], writes=[ew])
            P.op("dve", lambda e: e.tensor_reduce(out=zz.ap[:, 0:8], in_=r3(ew.ap, a=8), axis=AX.X, op=ALU.add), reads=[ew], writes=[zz])
            P.op("dve", lambda e: e.reciprocal(out=zz.ap[:, 8:16], in_=zz.ap[:, 0:8]), reads=[zz], writes=[zz])
            P.op("dve", lambda e, Wv=Wv: e.tensor_tensor(out=r3(Wv.ap, a=8), in0=r3(ew.ap, a=8), in1=bc(zz.ap[:, 8:9], [[1, 8], [0, 16]]), op=ALU.mult),
                 reads=[ew, zz], writes=[Wv])
            P.op("dve", lambda e: e.tensor_copy(out=posf.ap, in_=pos.ap), reads=[pos], writes=[posf])
            P.op("dve", lambda e: e.tensor_tensor(out=c4(eqb), in0=bc(posf.ap[:, 0:1], [[16, 8], [1, 16], [0, 16]]),
                                                  in1=bc(k16.ap[:, 0:1], [[0, 8], [0, 16], [1, 16]]), op=ALU.subtract),
                 reads=[k16, posf], writes=[eqb])
            P.op("dve", lambda e: e.tensor_scalar(out=eq2.ap, in0=eqb.ap, scalar1=0.0, scalar2=None, op0=ALU.is_ge),
                 reads=[eqb], writes=[eq2])
            P.op("dve", lambda e: e.scalar_tensor_tensor(out=eqb.ap, in0=eqb.ap, scalar=16.0, in1=eq2.ap, op0=ALU.is_lt, op1=ALU.mult),
                 reads=[eqb, eq2], writes=[eqb])
            P.op("dve", lambda e: e.tensor_tensor(out=c4(eq2), in0=c4(eqb), in1=bc(iotaf.ap[:, 0:1], [[0, 8], [0, 16], [1, 16]]), op=ALU.mult),
                 reads=[eqb, iotaf], writes=[eq2])
            P.op("dve", lambda e: e.tensor_reduce(out=k1f.ap, in_=c4(eq2), axis=AX.X, op=ALU.add), reads=[eq2], writes=[k1f])
            P.op("dve", lambda e: e.tensor_tensor(out=c4(eq2), in0=c4(eqb), in1=bc(ixf.ap[:, 0:1], [[32, 8], [0, 16], [1, 16]]), op=ALU.mult),
                 reads=[eqb, ixf], writes=[eq2])
            P.op("dve", lambda e, Iv=Iv: e.tensor_reduce(out=Iv.ap, in_=c4(eq2), axis=AX.X, op=ALU.add), reads=[eq2], writes=[Iv])
            P.op("dve", lambda e: e.scalar_tensor_tensor(out=k2f.ap, in0=k1f.ap, scalar=-16.0, in1=posf.ap, op0=ALU.mult, op1=ALU.add),
                 reads=[k1f, posf], writes=[k2f])
            P.op("dve", lambda e: e.tensor_tensor(out=c4(eqb), in0=bc(k2f.ap[:, 0:1], [[16, 8], [1, 16], [0, 16]]),
                                                  in1=bc(iotaf.ap[:, 0:1], [[0, 8], [0, 16], [1, 16]]), op=ALU.is_equal),
                 reads=[k2f, iotaf], writes=[eqb])
            P.op("dve", lambda e: e.tensor_tensor(out=c4(eq2), in0=c4(eqb), in1=bc(ixf.ap[:, 16:17], [[32, 8], [0, 16], [1, 16]]), op=ALU.mult),
                 reads=[eqb, ixf], writes=[eq2])
            P.op("dve", lambda e, Jv=Jv: e.tensor_reduce(out=Jv.ap, in_=c4(eq2), axis=AX.X, op=ALU.add), reads=[eq2], writes=[Jv])
        for tt in range(NT):
            Iv, Jv, Wv = Ivs[tt], Jvs[tt], Wvs[tt]
            bt, _, btok = nextbank([6, 7])
            for qi, srcb in enumerate((Iv, Jv, Wv)):
                P.op("pe", lambda e, qi=qi, srcb=srcb, bt=bt: e.transpose(out=bt[:, qi * 128:(qi + 1) * 128], in_=srcb.ap, identity=identf.ap),
                     reads=[srcb, identf], writes=[btok])
            P.op("act", lambda e, tt=tt, bt=bt: e.copy(out=SMv[:, :, tt * 128:(tt + 1) * 128], in_=r3(bt[:, 0:384], a=3)),
                 reads=[btok], writes=[SM])
        t3 = lambda b, n: b.ap[:, 0:TB * n].rearrange("p (t a) -> p t a", a=n)
        for q, OA, OB in ((0, OAI, OBI), (1, OAJ, OBJ)):
            P.op("dve", lambda e, q=q: e.tensor_tensor(out=t3(eqb, 8), in0=bc(SMv[:, q, 0:1], [[1, TB], [0, 8]]),
                                                       in1=bc(k16.ap[:, 0:1], [[0, TB], [1, 8]]), op=ALU.subtract),
                 reads=[SM, k16], writes=[eqb])
            P.op("dve", lambda e: e.tensor_scalar(out=cand.ap, in0=eqb.ap, scalar1=0.0, scalar2=None, op0=ALU.is_ge),
                 reads=[eqb], writes=[cand])
            P.op("dve", lambda e, OA=OA: e.scalar_tensor_tensor(out=OA.ap, in0=eqb.ap, scalar=16.0, in1=cand.ap, op0=ALU.is_lt, op1=ALU.mult),
                 reads=[eqb, cand], writes=[OA])
            P.op("dve", lambda e, OA=OA: e.tensor_tensor(out=t3(eqb, 8), in0=t3(OA, 8), in1=bc(iotaf.ap[:, 0:1], [[0, TB], [1, 8]]), op=ALU.mult),
                 reads=[OA, iotaf], writes=[eqb])
            P.op("dve", lambda e: e.tensor_reduce(out=xh.ap, in_=t3(eqb, 8), axis=AX.X, op=ALU.add), reads=[eqb], writes=[xh])
            P.op("dve", lambda e, q=q: e.scalar_tensor_tensor(out=xl.ap, in0=xh.ap, scalar=-16.0, in1=SMv[:, q, :], op0=ALU.mult, op1=ALU.add),
                 reads=[xh, SM], writes=[xl])
            P.op("dve", lambda e, OB=OB: e.tensor_tensor(out=t3(OB, 16), in0=bc(xl.ap[:, 0:1], [[1, TB], [0, 16]]),
                                                         in1=bc(iotaf.ap[:, 0:1], [[0, TB], [1, 16]]), op=ALU.is_equal),
                 reads=[xl, iotaf], writes=[OB])
        P.op("dve", lambda e: e.tensor_tensor(out=t3(OAJ, 8), in0=t3(OAJ, 8), in1=bc(SMv[:, 2, 0:1], [[1, TB], [0, 8]]), op=ALU.mult),
             reads=[OAJ, SM], writes=[OAJ])

    def b5(nb):
        TG = 16
        for tg in range(TB // TG):
            t0 = tg * TG
            cnt["oh"] += 1
            ob, oc = ohB[cnt["oh"] % 2], ohE[cnt["oh"] % 2]
            o3 = lambda b: b.ap.rearrange("p (t i) -> p t i", t=TG)
            o4 = lambda b: b.ap.rearrange("p (t a c) -> p t a c", t=TG, a=8)
            P.op("dve", lambda e, oc=oc, t0=t0: e.tensor_tensor(
                out=o4(oc), in0=bc(OAI.ap[:, t0 * 8:t0 * 8 + 1], [[8, TG], [1, 8], [0, 16]]),
                in1=bc(OBI.ap[:, t0 * 16:t0 * 16 + 1], [[16, TG], [0, 8], [1, 16]]), op=ALU.mult), reads=[OAI, OBI], writes=[oc])
            P.op("dve" if tg % 5 == 4 else "pool", lambda e, ob=ob, t0=t0: e.tensor_tensor(
                out=o4(ob), in0=bc(OAJ.ap[:, t0 * 8:t0 * 8 + 1], [[8, TG], [1, 8], [0, 16]]),
                in1=bc(OBJ.ap[:, t0 * 16:t0 * 16 + 1], [[16, TG], [0, 8], [1, 16]]), op=ALU.mult), reads=[OAJ, OBJ], writes=[ob])
            for t4 in range(TG // 4):
                bt, _, btok = nextbank([6, 7])
                P.opn("pe", [lambda e, q=q, t4=t4, oc=oc, ob=ob, bt=bt: e.matmul(
                    bt[:, q * 128:(q + 1) * 128], lhsT=o3(ob)[:, t4 * 4 + q, :], rhs=o3(oc)[:, t4 * 4 + q, :], start=True, stop=True)
                    for q in range(4)], reads=[oc, ob], writes=[btok])
                ta = t0 + t4 * 4
                P.op("act", lambda e, ta=ta, bt=bt: e.copy(out=Gv[:, :, ta:ta + 4], in_=bc(bt[:, 0:1], [[1, 128], [128, 4]])),
                     reads=[btok], writes=[G])
    def b6(nb, pend):
        h2Tv = r3(h2T[nb % 2].ap, a=8)
        h2T_tok = h2T_toks[nb % 2]
        def u_side(i):
            ub = ubr[i % 4]
            vb = vbr[i % 4]
            P.dma("sp", ub.ap, ut_d[i], reads=[ut_tok[i]], writes=[ub])
            P.dma("sp", vb.ap, vb_d[i * 128:(i + 1) * 128, :], reads=[vb_tok[i]], writes=[vb])
            bs_, _, bstok = banks[4 + i % 2]
            P.opn("pe", [lambda e, k=k, ub=ub, bs_=bs_: e.matmul(bs_[:, 0:TB], lhsT=r3(ub.ap, a=8)[:, k, :], rhs=h2Tv[:, k, :],
                                                             start=(k == 0), stop=(k == 7)) for k in range(8)],
                  reads=[ub] + h2T_tok, writes=[bstok])
            return bs_, bstok, vb

        per = (len(pend) + 119) // 120 if pend else 0
        uq = [u_side(0)]
        for i in range(128):
            bs_, bstok, vb = uq.pop(0)
            if i + 1 < 128:
                uq.append(u_side(i + 1))
            gel = gelr[i % 3]
            P.op("act", lambda e, gel=gel, bs_=bs_: e.activation(out=gel.ap, in_=bs_[:, 0:TB], func=AF.Gelu), reads=[bstok], writes=[gel])
            at = ATr[i % 4]
            P.op("pool", lambda e, gel=gel, at=at, i=i: e.tensor_tensor(out=at.ap, in0=gel.ap, in1=Gv[:, i, :], op=ALU.mult),
                 reads=[gel, G], writes=[at])
            P.opn("pe", [lambda e, tt=tt, half=half, at=at, vb=vb, i=i: e.matmul(
                banks[tt * 2 + half][0][:, :], lhsT=at.ap[:, tt * 128:(tt + 1) * 128], rhs=vb.ap[:, half * 512:(half + 1) * 512],
                start=(i == 0), stop=(i == 127)) for tt in range(NT) for half in range(2)],
                reads=[at, vb], writes=[banks[b4][2] for b4 in range(2 * NT)])
            if pend:
                P.replay(pend, per)
        P.replay(pend, len(pend))

    def b7(nb):
        for tt in range(NT):
            gt = nb * NT + tt
            xt = xts[tt]
            P.dma("sp", xt.ap, out_d[gt * 128:(gt + 1) * 128, :], reads=[out_tok[gt]], writes=[xt])
            for half in range(2):
                ab, _, abtok = banks[tt * 2 + half]
                P.op("dve", lambda e, half=half, ab=ab, xt=xt: e.tensor_tensor(
                    out=xt.ap[:, half * 512:(half + 1) * 512], in0=ab[:, :], in1=xt.ap[:, half * 512:(half + 1) * 512], op=ALU.add),
                    reads=[abtok, xt], writes=[xt])
            ss, rstd = new_small()
            smt = small_cur["tok"]
            P.op("dve", lambda e, xt=xt, ss=ss: e.scalar_tensor_tensor(out=eqb.ap[:, 0:1024], in0=xt.ap, scalar=1.0, in1=xt.ap,
                                                                       op0=ALU.mult, op1=ALU.mult, accum_out=ss),
                 reads=[xt], writes=[eqb, smt])
            P.op("pool", lambda e, ss=ss: e.tensor_scalar(out=ss, in0=ss, scalar1=1.0 / D, scalar2=EPS, op0=ALU.mult, op1=ALU.add),
                 reads=[smt], writes=[smt])
            P.op("pool", lambda e, ss=ss, rstd=rstd: e.tensor_tensor(out=rstd, in0=ss, in1=nhalf.ap, op=ALU.pow),
                 reads=[smt, nhalf], writes=[smt])
            P.op("dve", lambda e, xt=xt, rstd=rstd: e.scalar_tensor_tensor(out=xt.ap, in0=xt.ap, scalar=rstd, in1=fg.ap,
                                                                           op0=ALU.mult, op1=ALU.mult), reads=[xt, smt, fg], writes=[xt])
            P.dma("act", out_d[gt * 128:(gt + 1) * 128, :], xt.ap, reads=[xt], writes=[out_tok[gt]], key=xt)


    routing(0)
    for i in range(128):
        prep_uv(i)
    P.barrier()
    for nb in range(NBLK):
        b5(nb)
        pend = []
        if nb + 1 < NBLK:
            P.capture()
            routing(nb + 1)
            pend = P.end_capture()
        b6(nb, pend)
        b7(nb)


def prep_inputs(inputs):
    f = lambda a: np.ascontiguousarray(np.asarray(a, dtype=np.float32))
    rk = lambda w: f(w.reshape(8, 128, -1).transpose(1, 0, 2))
    pv = lambda v: v.reshape(8, 128).T
    x = f(inputs["x"])
    vecs = np.concatenate([pv(np.asarray(inputs[n])[0]) for n in
                           ("norm1_g", "conv_dw_b", "conv_ln_g", "conv_ln_b", "norm2_g")], axis=1)
    dww = np.asarray(inputs["conv_dw_w"])[0].reshape(31, 8, 128).transpose(2, 1, 0).reshape(128, 248)
    shared = {
        "win": rk(np.asarray(inputs["w_in"])[0]),
        "wpw": rk(np.asarray(inputs["conv_w_pw"])[0]),
        "wo": rk(np.asarray(inputs["attn_w_o"])[0]),
        "wout": rk(np.asarray(inputs["w_out"])[0]),
        "wq": rk(np.asarray(inputs["peer_w_query"])[0]),
        "vecs": f(vecs),
        "dww": f(dww),
        "sink": f(np.broadcast_to(np.asarray(inputs["attn_sink"])[0][None, :], (128, 16))),
        "fg": f(np.broadcast_to(np.asarray(inputs["final_g"])[None, :], (128, 1024))),
        "skT": f(np.asarray(inputs["peer_sub_keys"])[0].transpose(2, 0, 1).reshape(128, 256)),
        "pu": f(np.asarray(inputs["peer_u"])[0]),
        "pv": f(np.asarray(inputs["peer_v"])[0]),
    }
    xs = x.reshape(NCORES, TOK, D)
    return [dict(shared, x=np.ascontiguousarray(xs[c])) for c in range(NCORES)]


_NC_CACHE = {}


def kernel(**inputs):
    in_maps = prep_inputs(inputs)
    if "nc" not in _NC_CACHE:
        _NC_CACHE["nc"] = build_program()
    res = run_bass_kernel_spmd(_NC_CACHE["nc"], in_maps, core_ids=list(range(NCORES)))
    out = np.stack([np.asarray(r["out"]) for r in res.results], axis=0)
    return out.reshape(16, SEQ, D).astype(np.float32)
```

```python
import os
import numpy as np
from contextlib import ExitStack
import concourse.bass as bass
import concourse.mybir as mybir
from concourse.bass_utils import run_bass_kernel_spmd

F32 = mybir.dt.float32
BF16 = mybir.dt.bfloat16
U32 = mybir.dt.uint32
AF = mybir.ActivationFunctionType
ALU = mybir.AluOpType
AX = mybir.AxisListType

NCORES = 8
TOK = 4096
SEQ = 2048
D = 1024
EPS = 1e-6
TB = 256


class Tok:
    __slots__ = ("w", "r", "sem", "cnt", "name")

    def __init__(self, name=""):
        self.w = None
        self.r = {}
        self.sem = None
        self.cnt = 0
        self.name = name


class Buf:
    __slots__ = ("ap", "tok")

    def __init__(self, ap, name=""):
        self.ap = ap
        self.tok = Tok(name)


class Prog:
    ENG = ("pe", "dve", "act", "pool", "sp")

    def __init__(self, nc, es):
        self.nc = nc
        self.es = es
        self.ops = {e: [] for e in self.ENG}
        self.cnt = {e: 0 for e in self.ENG}
        self.sems = {}
        for e in self.ENG:
            self.sems["E_" + e] = es.enter_context(nc.semaphore("s_" + e))
        self.final = {}
        self.waited = {e: {} for e in self.ENG}
        self.ndma = 0

    def _collect(self, eng, reads, writes):
        need = {}

        def add(ev, raw):
            if ev is None:
                return
            key, val, src = ev
            if src == eng and eng == "pe":
                return
            if need.get(key, 0) < val:
                need[key] = val

        for t in reads:
            add(t.w, True)
        for t in writes:
            add(t.w, False)
            for key, (val, src) in t.r.items():
                add((key, val, src), False)
        waits = []
        wd = self.waited[eng]
        for key, val in need.items():
            if wd.get(key, 0) < val:
                wd[key] = val
                waits.append((key, val))
        return waits

    def _commit(self, ev, reads, writes):
        key, val, src = ev
        for t in reads:
            old = t.r.get(key)
            if old is None or old[0] < val:
                t.r[key] = (val, src)
        for t in writes:
            t.w = ev
            t.r = {}

    cap = None

    def capture(self):
        self.cap = []

    def end_capture(self):
        c, self.cap = self.cap, None
        return c

    def replay(self, lst, n):
        for _ in range(min(n, len(lst))):
            kind, args = lst.pop(0)
            getattr(self, kind)(*args)

    def op(self, eng, fn, reads=(), writes=()):
        if self.cap is not None:
            self.cap.append(("op", (eng, fn, list(reads), list(writes))))
            return
        reads = [b.tok if isinstance(b, Buf) else b for b in reads]
        writes = [b.tok if isinstance(b, Buf) else b for b in writes]
        waits = self._collect(eng, reads, writes)
        self.cnt[eng] += 1
        key = "E_" + eng
        ev = (key, self.cnt[eng], eng)
        self.final[key] = self.cnt[eng]
        self.ops[eng].append((waits, fn, key, 1))
        self._commit(ev, reads, writes)

    def opn(self, eng, fns, reads=(), writes=()):
        fns = list(fns)

        def run(e, fns=fns):
            last = None
            for f in fns:
                last = f(e)
            return last

        self.op(eng, run, reads, writes)

    def dma(self, q, out, in_, reads=(), writes=(), key=None):
        if self.cap is not None:
            self.cap.append(("dma", (q, out, in_, list(reads), list(writes), key)))
            return
        reads = [b.tok if isinstance(b, Buf) else b for b in reads]
        writes = [b.tok if isinstance(b, Buf) else b for b in writes]
        kt = key if key is not None else (writes[0] if writes else reads[0])
        if isinstance(kt, Buf):
            kt = kt.tok
        if kt.sem is None:
            self.ndma += 1
            kt.sem = "D_%d" % self.ndma
            self.sems[kt.sem] = self.es.enter_context(self.nc.semaphore("d%d" % self.ndma))
        waits = self._collect(q, reads, writes)
        kt.cnt += 16
        ev = (kt.sem, kt.cnt, None)
        self.final[kt.sem] = kt.cnt
        self.ops[q].append((waits, lambda e: e.dma_start(out=out, in_=in_), kt.sem, 16))
        self._commit(ev, reads, writes)

    def barrier(self):
        for e in self.ENG:
            waits = []
            wd = self.waited[e]
            for key, val in self.final.items():
                if key == "E_" + e:
                    continue
                if wd.get(key, 0) < val:
                    wd[key] = val
                    waits.append((key, val))
            if waits:
                self.ops[e].append((waits, None, None, 0))

    def emit(self):
        nc = self.nc
        self.barrier()
        engmap = {"pe": "tensor", "dve": "vector", "act": "scalar", "pool": "gpsimd", "sp": "sync"}
        with nc.Block() as block:
            for e in self.ENG:
                def body(engine, ops=self.ops[e], sems=self.sems):
                    for waits, fn, key, inc in ops:
                        for wk, wv in waits:
                            engine.wait_ge(sems[wk], wv)
                        if fn is not None:
                            fn(engine).then_inc(sems[key], inc)

                getattr(block, engmap[e])(body)


class Arena:
    def __init__(self, nc, nbytes):
        self.t32 = nc.alloc_sbuf_tensor("arena", [128, nbytes // 4], F32)
        self.t16 = self.t32.bitcast(BF16)
        self.tu = self.t32.bitcast(U32)
        self.off = 0
        self.cap = nbytes

    def alloc(self, nbytes):
        off = (self.off + 63) // 64 * 64
        self.off = off + nbytes
        assert self.off <= self.cap, (self.off, self.cap)
        return off

    def f32(self, n, name=""):
        o = self.alloc(n * 4)
        return Buf(self.t32[:, o // 4:o // 4 + n], name)

    def b16(self, n, name=""):
        o = self.alloc(n * 2)
        return Buf(self.t16[:, o // 2:o // 2 + n], name)

    def u32(self, n, name=""):
        o = self.alloc(n * 4)
        return Buf(self.tu[:, o // 4:o // 4 + n], name)


def bc(ap, dims):
    return bass.AP(ap.tensor, ap.offset, [list(ap.ap[0])] + [list(d) for d in dims])


def r3(ap, **kw):
    return ap.rearrange("p (a b) -> p a b", **kw)


KDBG = os.environ.get('KDBG', '')
SLOPES = [2.0 ** (-8.0 * (h + 1) / 16.0) for h in range(16)]


class _Stop(Exception):
    pass


def build_program(stop_after_a=False, stage=None):
    nc = bass.Bass("TRN2", target_bir_lowering=False)
    es = ExitStack()
    dt = lambda name, shape, dtype=F32, kind="ExternalInput": nc.dram_tensor(name, shape, dtype, kind=kind).ap()
    x_d = dt("x", [TOK, D])
    win_d = dt("win", [128, 8, 5632])
    wpw_d = dt("wpw", [128, 8, 1024])
    wo_d = dt("wo", [128, 8, 1024])
    wout_d = dt("wout", [128, 8, 1024])
    wq_d = dt("wq", [128, 8, 2048])
    vecs_d = dt("vecs", [128, 40])
    dww_d = dt("dww", [128, 248])
    sink_d = dt("sink", [128, 16])
    fg_d = dt("fg", [128, 1024])
    skT_d = dt("skT", [128, 256])
    pu_d = dt("pu", [16384, 1024])
    pv_d = dt("pv", [16384, 1024])
    out_d = dt("out", [TOK, D], F32, "ExternalOutput")
    ut_d = dt("ut_scr", [128, 128, 1024], BF16, "Internal")
    vb_d = dt("vb_scr", [16384, 1024], BF16, "Internal")
    wqb_d = dt("wqb_scr", [16, 128, 1024], BF16, "Internal")

    with es:
        P = Prog(nc, es)
        A = Arena(nc, 207 * 1024)
        banks = []
        psall = nc.alloc_psum_tensor("psall", [128, 4096], F32)
        psall16 = psall.bitcast(BF16)
        for i in range(8):
            banks.append((psall[:, i * 512:(i + 1) * 512], psall16[:, i * 1024:(i + 1) * 1024], Tok(f"bank{i}")))
        rr = {"i": 0}

        def nextbank(lst):
            rr["i"] += 1
            return banks[lst[rr["i"] % len(lst)]]

        ident = A.b16(128, "ident")
        identf = A.f32(128, "identf")
        ones16 = A.b16(128, "ones")
        vecs = A.f32(40, "vecs")
        dww = A.f32(248, "dww")
        esink = A.f32(16, "esink")
        small = A.f32(64, "small")
        out_tok = [Tok(f"out{i}") for i in range(TOK // 128)]

        P.dma("sp", vecs.ap, vecs_d, writes=[vecs])
        P.dma("sp", dww.ap, dww_d, writes=[dww])
        P.dma("sp", esink.ap, sink_d, writes=[esink])
        P.op("act", lambda e: e.activation(out=esink.ap, in_=esink.ap, func=AF.Exp), reads=[esink], writes=[esink])
        P.op("pool", lambda e: e.iota(identf.ap, [[1, 128]], base=0, channel_multiplier=-1,
                                      allow_small_or_imprecise_dtypes=True), writes=[identf])
        P.op("dve", lambda e: e.tensor_scalar(out=identf.ap, in0=identf.ap, scalar1=0.0, scalar2=None,
                                              op0=ALU.is_equal), reads=[identf], writes=[identf])
        P.op("dve", lambda e: e.tensor_copy(out=ident.ap, in_=identf.ap), reads=[identf], writes=[ident])
        P.op("pool", lambda e: e.memset(ones16.ap, 1.0 / 1024.0), writes=[ones16])
        G1, DWB, LNG, LNB, G2 = range(5)
        vec = lambda idx, k: vecs.ap[:, idx * 8 + k:idx * 8 + k + 1]

        mark0 = A.off
        Mtab = A.b16(3 * 16 * 128, "Mtab")
        Mv = Mtab.ap.rearrange("p (a h q) -> p a h q", a=3, h=16)
        hT = A.b16(8 * SEQ)
        hTv = r3(hT.ap, a=8)
        hT_tok = [Tok() for _ in range(16)]
        QT = A.b16(8 * SEQ)
        QTv = r3(QT.ap, a=8)
        QT_tok = [Tok() for _ in range(16)]
        cT = A.b16(8 * SEQ)
        cTv = r3(cT.ap, a=8)
        cT_tok = [[Tok() for _ in range(4)] for _ in range(8)]
        r4 = A.alloc(32768)
        KTv = r3(A.t16[:, r4 // 2:r4 // 2 + 4 * SEQ], a=4)
        KT_tok = Tok()
        Vo = r4 + 16384
        Vv = A.t16[:, Vo // 2:Vo // 2 + 16 * 4 * 65].rearrange("p (t g e) -> p t g e", t=16, g=4)
        V_tok = [Tok() for _ in range(16)]
        dgs = [Buf(A.t16[:, (r4 + i * 8192) // 2:(r4 + i * 8192) // 2 + 31 * 128]) for i in range(2)]
        Ub = [Buf(A.t16[:, (r4 + 16384 + i * 4224) // 2:(r4 + 16384 + i * 4224) // 2 + 2078]) for i in range(2)]
        mTv = r3(A.t16[:, r4 // 2:r4 // 2 + 8 * SEQ], a=8)
        mT_tok = [Tok() for _ in range(4)]
        wst = [A.f32(2048) for _ in range(2)]
        wbf = [A.b16(2048) for _ in range(4)]
        wst_h = [Buf(b.ap[:, h * 1024:(h + 1) * 1024]) for b in wst for h in range(2)]
        wbf_h = [Buf(b.ap[:, h * 1024:(h + 1) * 1024]) for b in wbf for h in range(2)]
        xts = [A.f32(1024) for _ in range(2)]
        xns = [A.b16(1024) for _ in range(2)]
        junk = A.b16(1024)
        w12 = A.alloc(12288)
        Ef = [Buf(A.t32[:, (w12 + i * 2048) // 4:(w12 + i * 2048) // 4 + 512]) for i in range(3)]
        PT = [Buf(A.t16[:, (w12 + 6144 + i * 1024) // 2:(w12 + 6144 + i * 1024) // 2 + 512]) for i in range(6)]
        lnm = Buf(A.t32[:, (w12) // 4:(w12) // 4 + 512])
        lnr = Buf(A.t32[:, (w12 + 2048) // 4:(w12 + 2048) // 4 + 512])
        lnt = [Buf(A.t32[:, (w12 + 4096 + i * 2048) // 4:(w12 + 4096 + i * 2048) // 4 + 512]) for i in range(2)]
        lnq = [Buf(A.t16[:, (w12 + 8192 + i * 1024) // 2:(w12 + 8192 + i * 1024) // 2 + 512]) for i in range(2)]
        wkd = Buf(A.t16[:, w12 // 2:w12 // 2 + 4096])
        wkdv = wkd.ap.rearrange("p (k g e) -> p k g e", k=8, g=4)
        AO = [A.b16(1024) for _ in range(2)]
        den = A.f32(16)
        rec = A.f32(16)
        ss_i = {"i": 0}
        small_cur = {"tok": None}
        wst_i = {"i": 0}
        wbf_i = {"i": 0}

        small_toks = [Tok() for _ in range(32)]

        def new_small():
            ss_i["i"] = (ss_i["i"] + 1) % 32
            i = ss_i["i"]
            small_cur["tok"] = small_toks[i]
            return small.ap[:, 2 * i:2 * i + 1], small.ap[:, 2 * i + 1:2 * i + 2]

        def load_w(src3, col0, ncols, scale_idx=None, eng="pool", half=False):
            wst_i["i"] += 1
            wbf_i["i"] += 1
            if half:
                st = wst_h[wst_i["i"] % 4]
                wb = wbf_h[wbf_i["i"] % 8]
            else:
                st = wst[wst_i["i"] % 2]
                wb = wbf[wbf_i["i"] % 4]
            stv = r3(st.ap[:, 0:8 * ncols], a=8)
            wbv = r3(wb.ap[:, 0:8 * ncols], a=8)
            P.dma("sp", stv, src3[:, :, col0:col0 + ncols], writes=[st])
            assert scale_idx is None
            P.op(eng, lambda e: e.tensor_copy(out=wb.ap[:, 0:8 * ncols], in_=st.ap[:, 0:8 * ncols]), reads=[st], writes=[wb])
            return wb, wbv

        def rms_tile(xt, xn):
            ss, rstd = new_small()
            sm = small_cur["tok"]
            P.op("act", lambda e: e.activation(out=junk.ap, in_=xt.ap, func=AF.Square, accum_out=ss),
                 reads=[xt], writes=[junk, sm])
            P.op("act", lambda e: e.activation(out=rstd, in_=ss, func=AF.Sqrt, scale=1.0 / D, bias=EPS),
                 reads=[sm], writes=[sm])
            P.op("dve", lambda e: e.reciprocal(out=rstd, in_=rstd), reads=[sm], writes=[sm])
            if xn is not None:
                P.op("dve", lambda e: e.tensor_scalar(out=xn.ap, in0=xt.ap, scalar1=rstd, scalar2=None, op0=ALU.mult),
                     reads=[xt, sm], writes=[xn])
            return rstd

        def transpose8(src, dst_fn, dst_toks, evac_eng="act", scale_idx=None, bl=(4,)):
            bt, bt16, btok = nextbank(list(bl))
            for k in range(8):
                P.op("pe", lambda e, k=k: e.transpose(out=bt16[:, k * 128:(k + 1) * 128], in_=src.ap[:, k * 128:(k + 1) * 128],
                                                      identity=ident.ap), reads=[src, ident], writes=[btok])
            if scale_idx is None:
                P.op(evac_eng, lambda e: (e.copy if evac_eng == "act" else e.tensor_copy)(
                    out=dst_fn(None), in_=r3(bt16[:, 0:1024], a=8)), reads=[btok], writes=dst_toks)
            else:
                for k in range(8):
                    if evac_eng == "act" or (evac_eng == "mix" and k % 2 == 0):
                        P.op("act", lambda e, k=k: e.activation(out=dst_fn(k), in_=bt16[:, k * 128:(k + 1) * 128], func=AF.Copy,
                                                                scale=vec(scale_idx, k)), reads=[btok, vecs], writes=dst_toks)
                    else:
                        P.op("dve", lambda e, k=k: e.tensor_scalar(out=dst_fn(k), in0=bt16[:, k * 128:(k + 1) * 128],
                                                                   scalar1=vec(scale_idx, k), scalar2=None, op0=ALU.mult),
                             reads=[btok, vecs], writes=dst_toks)

        Df = Ef[0]
        Am = Ef[1]
        Mf = Ef[2]
        for pos in range(3):
            P.op("pool", lambda e, pos=pos: e.iota(Df.ap[:, 0:128], [[1, 128]], base=128 * (1 - pos), channel_multiplier=-1,
                                                   allow_small_or_imprecise_dtypes=True), writes=[Df])
            P.op("act", lambda e: e.activation(out=Df.ap[:, 0:128], in_=Df.ap[:, 0:128], func=AF.Abs),
                 reads=[Df], writes=[Df])
            P.op("dve", lambda e: e.tensor_scalar(out=Am.ap[:, 0:128], in0=Df.ap[:, 0:128], scalar1=128.0, scalar2=None,
                                                  op0=ALU.is_le), reads=[Df], writes=[Am])
            for h in range(16):
                P.op("act", lambda e, h=h: e.activation(out=Mf.ap[:, 0:128], in_=Df.ap[:, 0:128], func=AF.Exp, scale=-SLOPES[h]),
                     reads=[Df], writes=[Mf])
                P.op("dve", lambda e, pos=pos, h=h: e.tensor_tensor(out=Mv[:, pos, h, :], in0=Mf.ap[:, 0:128], in1=Am.ap[:, 0:128],
                                                                    op=ALU.mult), reads=[Mf, Am], writes=[Mtab])
        P.barrier()

        def chk(name):
            if stage == name:
                raise _Stop()

        try:
          for s in range(2):
            tb0 = s * SEQ
            chk('S0')
            def s1_pre(tt):
                xt = xts[tt % 2]
                xn = xns[tt % 2]
                P.dma("sp", xt.ap, x_d[tb0 + tt * 128:tb0 + (tt + 1) * 128, :], writes=[xt])
                rms_tile(xt, xn)

            s1_pre(0)
            for tt in range(16):
                if tt + 1 < 16:
                    s1_pre(tt + 1)
                transpose8(xns[tt % 2], lambda k, tt=tt: hTv[:, k, tt * 128:(tt + 1) * 128], [hT_tok[tt]], evac_eng="mix", scale_idx=G1,
                           bl=(4, 5, 6, 7))

            chk('S1')
            ev_i = 0
            for cp in range(4):
                wb, wbv = load_w(win_d, 2048 + cp * 256, 256)
                for cc in range(2):
                    c = cp * 2 + cc
                    for b in range(4):
                        bt, _, btok = nextbank([0, 1, 2, 3])
                        P.opn("pe", [lambda e, k=k, cc=cc, b=b, wbv=wbv, bt=bt: e.matmul(
                            bt[:, :], lhsT=wbv[:, k, cc * 128:(cc + 1) * 128], rhs=hTv[:, k, b * 512:(b + 1) * 512],
                            start=(k == 0), stop=(k == 7)) for k in range(8)], reads=[wb] + hT_tok[4 * b:4 * b + 4], writes=[btok])
                        dst = QTv[:, c, b * 512:(b + 1) * 512]
                        ev_i += 1
                        if ev_i % 2:
                            P.op("act", lambda e, dst=dst, bt=bt: e.mul(out=dst, in_=bt[:, :], mul=0.125),
                                 reads=[btok], writes=QT_tok[4 * b:4 * b + 4])
                        else:
                            P.op("dve", lambda e, dst=dst, bt=bt: e.tensor_scalar(out=dst, in0=bt[:, :], scalar1=0.125, scalar2=None,
                                                                                  op0=ALU.mult), reads=[btok], writes=QT_tok[4 * b:4 * b + 4])
            wst_i["i"] += 1
            st = wst[wst_i["i"] % 2]
            stv = r3(st.ap, a=8)
            P.dma("sp", stv, win_d[:, :, 3072:3328], writes=[st])
            for half in range(2):
                P.op("pool", lambda e, half=half: e.tensor_copy(
                    out=wkdv[:, :, :, half * 64:(half + 1) * 64], in_=st.ap.rearrange("p (k g e) -> p k g e", k=8, g=4)),
                    reads=[st], writes=[wkd])
            for g in range(4):
                for b in range(4):
                    bt, _, btok = nextbank([0, 1, 2, 3])
                    for k in range(8):
                        P.op("pe", lambda e, k=k, g=g, b=b, bt=bt: e.matmul(
                            bt[:, :], lhsT=wkdv[:, k, g, :], rhs=hTv[:, k, b * 512:(b + 1) * 512],
                            start=(k == 0), stop=(k == 7)), reads=[wkd] + hT_tok[4 * b:4 * b + 4], writes=[btok])
                    P.op("act", lambda e, g=g, b=b, bt=bt: e.copy(out=KTv[:, g, b * 512:(b + 1) * 512], in_=bt[:, :]),
                         reads=[btok], writes=[KT_tok])
            wb, wbv = load_w(win_d, 3328, 256)
            P.op("pool", lambda e: e.memset(Vv[:, :, :, 64:65], 1.0), writes=V_tok)
            for tt in range(16):
                bt, _, btok = nextbank([0, 1, 2, 3])
                for k in range(8):
                    P.op("pe", lambda e, k=k, tt=tt, wbv=wbv, bt=bt: e.matmul(
                        bt[:, 0:256], lhsT=hTv[:, k, tt * 128:(tt + 1) * 128], rhs=wbv[:, k, :],
                        start=(k == 0), stop=(k == 7)), reads=[wb, hT_tok[tt]], writes=[btok])
                P.op("dve", lambda e, tt=tt, bt=bt: e.tensor_copy(out=Vv[:, tt, :, 0:64],
                                                                  in_=bt[:, 0:256].rearrange("p (g e) -> p g e", g=4)),
                     reads=[btok], writes=[V_tok[tt]])

            chk('S2')
            cnt3 = {"pt": 0, "ef": 0}
            po = [banks[5], banks[6], banks[7]]

            def st_phase(i, g):
                kbs = [kb for kb in (i - 1, i, i + 1) if 0 <= kb < 16]
                pts = []
                for kb in kbs:
                    pos = kb - i + 1
                    rr["pair"] = rr.get("pair", 0) + 1
                    b0 = 2 * (rr["pair"] % 2)
                    pair = (banks[b0], banks[b0 + 1])
                    for j in range(4):
                        h = 4 * g + j
                        c, p = h // 2, h % 2
                        bt, _, btok = pair[p]
                        jj = j // 2
                        P.op("pe", lambda e, jj=jj, g=g, kb=kb, c=c, p=p, i=i, bt=bt: e.matmul(
                            bt[:, jj * 128:(jj + 1) * 128], lhsT=KTv[64 * p:64 * p + 64, g, kb * 128:(kb + 1) * 128],
                            rhs=QTv[64 * p:64 * p + 64, c, i * 128:(i + 1) * 128], start=True, stop=True),
                            reads=[KT_tok, QT_tok[i]], writes=[btok])
                    cnt3["ef"] += 1
                    ef = Ef[cnt3["ef"] % 3]
                    src2 = bc(pair[0][0][:, 0:1], [[512, 2], [1, 256]])
                    P.op("act", lambda e, ef=ef, src2=src2: e.activation(out=ef.ap.rearrange("p (a b) -> p a b", a=2), in_=src2,
                                                                       func=AF.Exp),
                         reads=[pair[0][2], pair[1][2]], writes=[ef])
                    cnt3["pt"] += 1
                    pt = PT[cnt3["pt"] % 6]
                    m4 = bc(Mv[:, pos, 4 * g, :], [[128, 2], [256, 2], [1, 128]])
                    P.op("dve", lambda e, ef=ef, pt=pt, m4=m4: e.tensor_tensor(
                        out=pt.ap.rearrange("p (a b q) -> p a b q", a=2, b=2), in0=ef.ap.rearrange("p (a b q) -> p a b q", a=2, b=2),
                        in1=m4, op=ALU.mult), reads=[ef, Mtab], writes=[pt])
                    pts.append(pt)
                return pts

            def pv_phase(i, g, pts):
                kbs = [kb for kb in (i - 1, i, i + 1) if 0 <= kb < 16]
                for j in range(4):
                    h = 4 * g + j
                    pb, _, pbtok = po[h // 7]
                    o0 = (h % 7) * 65
                    P.opn("pe", [lambda e, j=j, g=g, kb=kb, n=n, pb=pb, o0=o0, pt=pts[n], last=len(kbs) - 1: e.matmul(
                        pb[:, o0:o0 + 65], lhsT=pt.ap[:, (j % 2) * 256 + (j // 2) * 128:(j % 2) * 256 + (j // 2) * 128 + 128], rhs=Vv[:, kb, g, :],
                        start=(n == 0), stop=(n == last)) for n, kb in enumerate(kbs)],
                        reads=list(pts) + [V_tok[kb] for kb in kbs], writes=[pbtok])

            items3 = [(i, g) for i in range(16) for g in range(4)]
            pend3 = st_phase(*items3[0])
            for idx3, (i, g) in enumerate(items3):
                nxt3 = st_phase(*items3[idx3 + 1]) if idx3 + 1 < len(items3) else None
                pv_phase(i, g, pend3)
                pend3 = nxt3
                if g != 3:
                    continue
                if stage in ('S3a', 'S3b'):
                    continue
                ao = AO[i % 2]
                for b3, (h0, nh) in enumerate(((0, 7), (7, 7), (14, 2))):
                    pb, _, pbtok = po[b3]
                    pv = pb[:, 0:nh * 65].rearrange("p (h e) -> p h e", e=65)
                    P.op("dve", lambda e, pv=pv, h0=h0, nh=nh: e.tensor_tensor(
                        out=den.ap[:, h0:h0 + nh], in0=pv[:, :, 64], in1=esink.ap[:, h0:h0 + nh], op=ALU.add),
                        reads=[pbtok, esink], writes=[den])
                P.op("dve", lambda e: e.reciprocal(out=rec.ap, in_=den.ap), reads=[den], writes=[rec])
                for b3, (h0, nh) in enumerate(((0, 7), (7, 7), (14, 2))):
                    pb, _, pbtok = po[b3]
                    pv = pb[:, 0:nh * 65].rearrange("p (h e) -> p h e", e=65)
                    P.op("dve", lambda e, pv=pv, h0=h0, nh=nh, ao=ao: e.tensor_tensor(
                        out=ao.ap[:, h0 * 64:(h0 + nh) * 64].rearrange("p (h e) -> p h e", e=64), in0=pv[:, :, 0:64],
                        in1=bc(rec.ap[:, h0:h0 + nh], [[1, nh], [0, 64]]), op=ALU.mult),
                        reads=[pbtok, rec], writes=[ao])
                if stage == 'S3c':
                    continue
                transpose8(ao, lambda k, i=i: QTv[:, :, i * 128:(i + 1) * 128], [QT_tok[i]])
            chk('S3'); chk('S3a'); chk('S3b'); chk('S3c')
            P.barrier()

            for u in Ub:
                P.op("pool", lambda e, u=u: e.memset(u.ap[:, 0:15], 0.0), writes=[u])
                P.op("pool", lambda e, u=u: e.memset(u.ap[:, 2063:2078], 0.0), writes=[u])
            sg_i = 0
            for cp in range(4):
                wa, wav = load_w(win_d, cp * 256, 256)
                wg, wgv = load_w(win_d, 1024 + cp * 256, 256)
                for cc in range(2):
                    c = cp * 2 + cc
                    u = Ub[c % 2]
                    for b in range(4):
                        ba, _, batok = nextbank([0, 1, 2, 3])
                        bg, _, bgtok = nextbank([0, 1, 2, 3])
                        for (wt, wtv, bt, btok) in ((wa, wav, ba, batok), (wg, wgv, bg, bgtok)):
                            P.opn("pe", [lambda e, k=k, cc=cc, b=b, wtv=wtv, bt=bt: e.matmul(
                                bt[:, :], lhsT=wtv[:, k, cc * 128:(cc + 1) * 128], rhs=hTv[:, k, b * 512:(b + 1) * 512],
                                start=(k == 0), stop=(k == 7)) for k in range(8)], reads=[wt] + hT_tok[4 * b:4 * b + 4], writes=[btok])
                        sg_i += 1
                        sg = Ef[sg_i % 3]
                        P.op("act", lambda e, sg=sg, bg=bg: e.activation(out=sg.ap, in_=bg[:, :], func=AF.Sigmoid),
                             reads=[bgtok], writes=[sg])
                        P.op("dve", lambda e, sg=sg, ba=ba, u=u, b=b: e.tensor_tensor(
                            out=u.ap[:, 15 + b * 512:15 + (b + 1) * 512], in0=ba[:, :], in1=sg.ap, op=ALU.mult),
                            reads=[batok, sg], writes=[u])
                    dg = dgs[c % 2]
                    P.op("dve", lambda e, dg=dg, c=c: e.tensor_tensor(
                        out=dg.ap.rearrange("p (t j) -> p t j", t=31), in0=bc(ident.ap[:, 0:1], [[0, 31], [1, 128]]),
                        in1=bc(dww.ap[:, c * 31:c * 31 + 1], [[1, 31], [0, 128]]), op=ALU.mult), reads=[ident, dww], writes=[dg])
                    for b in range(4):
                        bt, _, btok = nextbank([0, 1, 2, 3])
                        P.opn("pe", [lambda e, tap=tap, dg=dg, u=u, b=b, bt=bt: e.matmul(
                            bt[:, :], lhsT=dg.ap[:, tap * 128:(tap + 1) * 128], rhs=u.ap[:, tap + b * 512:tap + b * 512 + 512],
                            start=(tap == 0), stop=(tap == 30)) for tap in range(31)], reads=[dg, u], writes=[btok])
                        P.op("act", lambda e, c=c, b=b, bt=bt: e.activation(out=cTv[:, c, b * 512:(b + 1) * 512], in_=bt[:, :],
                                                                            func=AF.Identity, bias=vec(DWB, c)),
                             reads=[btok, vecs], writes=[cT_tok[c][b]])
            P.barrier()
            for b in range(4):
                bs_, _, bstok = nextbank([0, 1, 2, 3])
                bq_, _, bqtok = nextbank([0, 1, 2, 3])
                blk = slice(b * 512, (b + 1) * 512)
                for c in range(8):
                    sq = lnq[c % 2]
                    P.op("act", lambda e, sq=sq, c=c, blk=blk: e.activation(out=sq.ap, in_=cTv[:, c, blk], func=AF.Square),
                         reads=[cT_tok[c][b]], writes=[sq])
                    P.op("pe", lambda e, c=c, blk=blk, bs_=bs_: e.matmul(bs_[:, :], lhsT=ones16.ap, rhs=cTv[:, c, blk],
                                                                       start=(c == 0), stop=(c == 7)),
                         reads=[ones16, cT_tok[c][b]], writes=[bstok])
                    P.op("pe", lambda e, c=c, sq=sq, bq_=bq_: e.matmul(bq_[:, :], lhsT=ones16.ap, rhs=sq.ap,
                                                                     start=(c == 0), stop=(c == 7)),
                         reads=[ones16, sq], writes=[bqtok])
                P.op("act", lambda e, bs_=bs_: e.copy(out=lnm.ap, in_=bs_[:, :]), reads=[bstok], writes=[lnm])
                P.op("dve", lambda e: e.tensor_tensor(out=lnr.ap, in0=lnm.ap, in1=lnm.ap, op=ALU.mult), reads=[lnm], writes=[lnr])
                P.op("dve", lambda e, bq_=bq_: e.tensor_tensor(out=lnr.ap, in0=bq_[:, :], in1=lnr.ap, op=ALU.subtract),
                     reads=[bqtok, lnr], writes=[lnr])
                P.op("act", lambda e: e.activation(out=lnr.ap, in_=lnr.ap, func=AF.Sqrt, bias=EPS), reads=[lnr], writes=[lnr])
                P.op("dve", lambda e: e.reciprocal(out=lnr.ap, in_=lnr.ap), reads=[lnr], writes=[lnr])
                for c in range(8):
                    t1 = lnt[c % 2]
                    P.op("dve", lambda e, t1=t1, c=c, blk=blk: e.tensor_tensor(out=t1.ap, in0=cTv[:, c, blk], in1=lnm.ap, op=ALU.subtract),
                         reads=[cT_tok[c][b], lnm], writes=[t1])
                    P.op("dve", lambda e, t1=t1: e.tensor_tensor(out=t1.ap, in0=t1.ap, in1=lnr.ap, op=ALU.mult),
                         reads=[t1, lnr], writes=[t1])
                    P.op("act", lambda e, t1=t1, c=c, blk=blk: e.activation(out=cTv[:, c, blk], in_=t1.ap, func=AF.Silu,
                                                                           scale=vec(LNG, c), bias=vec(LNB, c)),
                         reads=[t1, vecs], writes=[cT_tok[c][b]])
            chk('S4')
            P.barrier()

            sg_i = 0
            for c in range(8):
                w1, w1v = load_w(wpw_d, c * 128, 128, half=True)
                w2, w2v = load_w(wo_d, c * 128, 128, half=True)
                w3, w3v = load_w(win_d, 3584 + c * 128, 128, half=True)
                w4, w4v = load_w(win_d, 4608 + c * 128, 128, half=True)
                for b in range(4):
                    blk = slice(b * 512, (b + 1) * 512)
                    bks = [nextbank([0, 1, 2, 3]) for _ in range(4)]
                    srcs = ((w1, w1v, cTv, [cT_tok[k][b] for k in range(8)]),
                            (w2, w2v, QTv, QT_tok[4 * b:4 * b + 4]),
                            (w3, w3v, hTv, hT_tok[4 * b:4 * b + 4]),
                            (w4, w4v, hTv, hT_tok[4 * b:4 * b + 4]))
                    for (wt, wtv, src, stoks), (bt, _, btok) in zip(srcs, bks):
                        P.opn("pe", [lambda e, k=k, wtv=wtv, src=src, bt=bt, blk=blk: e.matmul(
                            bt[:, :], lhsT=wtv[:, k, 0:128], rhs=src[:, k, blk], start=(k == 0), stop=(k == 7)) for k in range(8)],
                            reads=[wt] + list(stoks), writes=[btok])
                    sa = Ef[0]
                    sb_ = Ef[1]
                    m1 = Ef[2]
                    P.op("act", lambda e, bt=bks[2][0]: e.activation(out=sa.ap, in_=bt[:, :], func=AF.Sigmoid),
                         reads=[bks[2][2]], writes=[sa])
                    P.op("act", lambda e, bt=bks[3][0]: e.activation(out=sb_.ap, in_=bt[:, :], func=AF.Sigmoid),
                         reads=[bks[3][2]], writes=[sb_])
                    P.op("dve", lambda e, bt=bks[0][0]: e.tensor_tensor(out=m1.ap, in0=bt[:, :], in1=sa.ap, op=ALU.mult),
                         reads=[bks[0][2], sa], writes=[m1])
                    P.op("dve", lambda e, bt=bks[1][0]: e.tensor_tensor(out=sb_.ap, in0=bt[:, :], in1=sb_.ap, op=ALU.mult),
                         reads=[bks[1][2], sb_], writes=[sb_])
                    P.op("dve", lambda e, c=c, blk=blk: e.tensor_tensor(out=mTv[:, c, blk], in0=m1.ap, in1=sb_.ap, op=ALU.add),
                         reads=[m1, sb_], writes=[mT_tok[b]])

            chk('S5')
            P.barrier()
            wo4 = [load_w(wout_d, q4 * 256, 256) for q4 in range(4)]
            for tt in range(16):
                xt = xts[tt % 2]
                gt = (tb0 // 128) + tt
                P.dma("sp", xt.ap, x_d[tb0 + tt * 128:tb0 + (tt + 1) * 128, :], writes=[xt])
                for half in range(2):
                    bt, _, btok = nextbank([0, 1, 2, 3])
                    for q2 in range(2):
                        wb, wbv = wo4[half * 2 + q2]
                        P.opn("pe", [lambda e, k=k, tt=tt, q2=q2, wbv=wbv, bt=bt: e.matmul(
                            bt[:, q2 * 256:(q2 + 1) * 256], lhsT=mTv[:, k, tt * 128:(tt + 1) * 128], rhs=wbv[:, k, :],
                            start=(k == 0), stop=(k == 7)) for k in range(8)], reads=[wb, mT_tok[tt // 4]], writes=[btok])
                    P.op("dve", lambda e, half=half, bt=bt, xt=xt: e.tensor_tensor(
                        out=xt.ap[:, half * 512:(half + 1) * 512], in0=bt[:, :], in1=xt.ap[:, half * 512:(half + 1) * 512],
                        op=ALU.add), reads=[btok, xt], writes=[xt])
                P.dma("act", out_d[gt * 128:(gt + 1) * 128, :], xt.ap, reads=[xt], writes=[out_tok[gt]], key=xt)
            chk('S6')
            P.barrier()
        except _Stop:
            pass

        if not stop_after_a:
            A.off = mark0
            build_peer(nc, P, A, banks, nextbank, dict(
                ident=ident, identf=identf, vecs=vecs, vec=vec, G2=G2, small=small, new_small=new_small, small_cur=small_cur,
                out_tok=out_tok, out_d=out_d, fg_d=fg_d, skT_d=skT_d, pu_d=pu_d, pv_d=pv_d, wq_d=wq_d,
                ut_d=ut_d, vb_d=vb_d, wqb_d=wqb_d))
        P.emit()
    return nc


def build_peer(nc, P, A, banks, nextbank, C):
    ident, identf, vecs, vec, G2 = C["ident"], C["identf"], C["vecs"], C["vec"], C["G2"]
    small, new_small, out_tok, out_d = C["small"], C["new_small"], C["out_tok"], C["out_d"]
    small_cur = C["small_cur"]
    ut_d, vb_d, wqb_d = C["ut_d"], C["vb_d"], C["wqb_d"]
    NBLK = TOK // TB
    NT = TB // 128
    mark = A.off
    NPB = 4
    pst = [A.f32(1024) for _ in range(NPB)]
    pst2 = [A.f32(1024) for _ in range(NPB)]
    pbf = [A.b16(1024) for _ in range(NPB)]
    pbf2 = [A.b16(1024) for _ in range(NPB)]
    utb = [A.b16(1024) for _ in range(2)]
    assert A.off - mark <= 128 * TB * 2
    A.off = mark
    G = A.b16(128 * TB)
    Gv = G.ap.rearrange("p (i t) -> p i t", i=128)
    h2T = [A.b16(8 * TB) for _ in range(2)]
    h2T_toks = [[Tok() for _ in range(NT)] for _ in range(2)]
    qT = A.f32(16 * TB)
    qTv = r3(qT.ap, a=16)
    sc = [A.f32(2048)] * 2
    sc2 = A.f32(256)
    v16 = A.f32(256)
    v16v = r3(v16.ap, a=16)
    ix = A.u32(256)
    ixv = r3(ix.ap, a=16)
    ixf = A.f32(256)
    cand = sc[0]
    eqb = A.f32(2048)
    eq2 = cand
    ts = A.f32(128)
    tsv = r3(ts.ap, a=8)
    pos = A.u32(128)
    posv = r3(pos.ap, a=8)
    posf = A.f32(128)
    k1f = A.f32(128)
    k2f = A.f32(128)
    Ivs = [A.f32(128) for _ in range(2)]
    Jvs = [A.f32(128) for _ in range(2)]
    Wvs = [A.f32(128) for _ in range(2)]
    ew = A.f32(128)
    zz = A.f32(16)
    SM = A.f32(3 * TB)
    SMv = r3(SM.ap, a=3)
    iota128 = A.b16(128)
    iotaf = A.f32(128)
    skT = A.f32(256)
    skTv = r3(skT.ap, a=2)
    fg = A.f32(1024)
    ohB = [A.b16(16 * 128) for _ in range(2)]
    ohE = [A.b16(16 * 128) for _ in range(2)]
    OAI = A.b16(TB * 8)
    OAJ = A.b16(TB * 8)
    OBI = A.b16(TB * 16)
    OBJ = A.b16(TB * 16)
    xh = A.f32(TB)
    xl = A.f32(TB)
    ubr = [A.b16(1024) for _ in range(4)]
    vbr = [A.b16(1024) for _ in range(4)]
    gelr = [A.f32(TB) for _ in range(3)]
    ATr = [A.b16(TB) for _ in range(4)]
    xts = [A.f32(1024) for _ in range(2)]
    xns = [A.b16(1024) for _ in range(2)]
    wqc = [A.b16(1024) for _ in range(3)]
    ut_tok = [Tok() for _ in range(128)]
    vb_tok = [Tok() for _ in range(128)]
    wqb_tok = [Tok() for _ in range(16)]

    P.dma("sp", skT.ap, C["skT_d"], writes=[skT])
    P.dma("sp", fg.ap, C["fg_d"], writes=[fg])
    P.op("pool", lambda e: e.iota(iotaf.ap, [[1, 128]], base=0, channel_multiplier=0, allow_small_or_imprecise_dtypes=True),
         writes=[iotaf])
    P.op("dve", lambda e: e.tensor_copy(out=iota128.ap, in_=iotaf.ap), reads=[iotaf], writes=[iota128])
    k16 = A.f32(16)
    nhalf = A.f32(1)
    P.op("pool", lambda e: e.memset(nhalf.ap, -0.5), writes=[nhalf])
    P.op("dve", lambda e: e.tensor_scalar(out=k16.ap, in0=iotaf.ap[:, 0:16], scalar1=16.0, scalar2=None, op0=ALU.mult),
         reads=[iotaf], writes=[k16])

    for m in range(16):
        st = pst[m % NPB]
        pb = pbf[m % NPB]
        P.dma("sp", r3(st.ap, a=8), C["wq_d"][:, :, m * 128:(m + 1) * 128], writes=[st])
        P.op("pool", lambda e, st=st, pb=pb: e.tensor_copy(out=pb.ap, in_=st.ap), reads=[st], writes=[pb])
        P.dma("act", wqb_d[m], pb.ap, reads=[pb], writes=[wqb_tok[m]], key=pb)
    def prep_uv(i):
        st = pst[i % NPB]
        pb = pbf[i % NPB]
        P.dma("sp", st.ap, C["pu_d"][i * 128:(i + 1) * 128, :], writes=[st])
        P.op("pool", lambda e, st=st, pb=pb: e.tensor_copy(out=pb.ap, in_=st.ap), reads=[st], writes=[pb])
        bt, bt16, btok = nextbank([4, 5, 6, 7])
        for k in range(8):
            P.op("pe", lambda e, k=k, pb=pb, bt16=bt16: e.transpose(out=bt16[:, k * 128:(k + 1) * 128], in_=pb.ap[:, k * 128:(k + 1) * 128],
                                                             identity=ident.ap), reads=[pb, ident], writes=[btok])
        ut = utb[i % 2]
        P.op("act", lambda e, ut=ut, bt16=bt16: e.copy(out=ut.ap, in_=bt16[:, 0:1024]), reads=[btok], writes=[ut])
        P.dma("act", ut_d[i], ut.ap, reads=[ut], writes=[ut_tok[i]], key=ut)
        st2 = pst2[i % NPB]
        pb2 = pbf2[i % NPB]
        P.dma("pool", st2.ap, C["pv_d"][i * 128:(i + 1) * 128, :], writes=[st2])
        P.op("dve", lambda e, st2=st2, pb2=pb2: e.tensor_copy(out=pb2.ap, in_=st2.ap), reads=[st2], writes=[pb2])
        P.dma("act", vb_d[i * 128:(i + 1) * 128, :], pb2.ap, reads=[pb2], writes=[vb_tok[i]], key=pb2)

    cnt = {"oh": 0, "ev": 0}

    def routing(nb):
        h2Tv = r3(h2T[nb % 2].ap, a=8)
        h2T_tok = h2T_toks[nb % 2]
        for tt in range(NT):
            gt = nb * NT + tt
            xt = xts[tt]
            xn = xns[tt]
            P.dma("sp", xt.ap, out_d[gt * 128:(gt + 1) * 128, :], reads=[out_tok[gt]], writes=[xt])
            ss, rstd = new_small()
            smt = small_cur["tok"]
            P.op("dve", lambda e, xt=xt, ss=ss: e.scalar_tensor_tensor(out=eqb.ap[:, 0:1024], in0=xt.ap, scalar=1.0, in1=xt.ap,
                                                                       op0=ALU.mult, op1=ALU.mult, accum_out=ss),
                 reads=[xt], writes=[eqb, smt])
            P.op("pool", lambda e, ss=ss: e.tensor_scalar(out=ss, in0=ss, scalar1=1.0 / D, scalar2=EPS, op0=ALU.mult, op1=ALU.add),
                 reads=[smt], writes=[smt])
            P.op("pool", lambda e, ss=ss, rstd=rstd: e.tensor_tensor(out=rstd, in0=ss, in1=nhalf.ap, op=ALU.pow),
                 reads=[smt, nhalf], writes=[smt])
            P.op("dve", lambda e, xt=xt, xn=xn, rstd=rstd: e.tensor_scalar(out=xn.ap, in0=xt.ap, scalar1=rstd, scalar2=None, op0=ALU.mult),
                 reads=[xt, smt], writes=[xn])
            bt, bt16, btok = nextbank([6, 7])
            for k in range(8):
                P.op("pe", lambda e, k=k, xn=xn, bt16=bt16: e.transpose(out=bt16[:, k * 128:(k + 1) * 128], in_=xn.ap[:, k * 128:(k + 1) * 128],
                                                                 identity=ident.ap), reads=[xn, ident], writes=[btok])
            for k in range(8):
                P.op("dve", lambda e, k=k, tt=tt, bt16=bt16: e.tensor_scalar(
                    out=h2Tv[:, k, tt * 128:(tt + 1) * 128], in0=bt16[:, k * 128:(k + 1) * 128], scalar1=vec(G2, k), scalar2=None,
                    op0=ALU.mult), reads=[btok, vecs], writes=[h2T_tok[tt]])
        for m in range(3):
            P.dma("act", wqc[m].ap, wqb_d[m], reads=[wqb_tok[m]], writes=[wqc[m]])
        for m in range(16):
            wq = wqc[m % 3]
            bt, _, btok = nextbank([6, 7])
            P.opn("pe", [lambda e, k=k, wq=wq, bt=bt: e.matmul(bt[:, 0:TB], lhsT=r3(wq.ap, a=8)[:, k, :], rhs=h2Tv[:, k, :],
                                                           start=(k == 0), stop=(k == 7)) for k in range(8)],
                  reads=[wq] + h2T_tok, writes=[btok])
            P.op("act", lambda e, m=m, bt=bt: e.copy(out=qTv[:, m, :], in_=bt[:, 0:TB]), reads=[btok], writes=[qT])
            if m + 3 < 16:
                P.dma("act", wq.ap, wqb_d[m + 3], reads=[wqb_tok[m + 3]], writes=[wq])
        for tt in range(NT):
            Iv, Jv, Wv = Ivs[tt], Jvs[tt], Wvs[tt]
            scb = sc[tt]
            for bq in range(4):
                bt, _, btok = nextbank([6, 7])
                for mm in range(4):
                    m = bq * 4 + mm
                    P.op("pe", lambda e, mm=mm, m=m, tt=tt, bt=bt: e.matmul(
                        bt[:, mm * 128:(mm + 1) * 128], lhsT=qTv[:, m, tt * 128:(tt + 1) * 128], rhs=skTv[:, m % 2, :],
                        start=True, stop=True), reads=[qT, skT], writes=[btok])
                P.op("dve", lambda e, bq=bq, bt=bt, scb=scb: e.tensor_copy(out=scb.ap[:, bq * 512:(bq + 1) * 512], in_=bt[:, :]),
                     reads=[btok], writes=[scb])
            for m in range(16):
                src = scb.ap[:, m * 128:(m + 1) * 128]
                P.op("dve", lambda e, m=m, src=src: e.max(out=v16v[:, m, 0:8], in_=src), reads=[scb], writes=[v16])
                P.op("dve", lambda e, m=m, src=src: e.max_index(out=ixv[:, m, 0:8], in_max=v16v[:, m, 0:8], in_values=src),
                     reads=[scb, v16], writes=[ix])
                P.op("dve", lambda e, m=m, src=src: e.match_replace(out=sc2.ap[:, 0:128], in_to_replace=v16v[:, m, 0:8], in_values=src,
                                                                    imm_value=-1e30), reads=[scb, v16], writes=[sc2])
                P.op("dve", lambda e, m=m: e.max(out=v16v[:, m, 8:16], in_=sc2.ap[:, 0:128]), reads=[sc2], writes=[v16])
                P.op("dve", lambda e, m=m: e.max_index(out=ixv[:, m, 8:16], in_max=v16v[:, m, 8:16], in_values=sc2.ap[:, 0:128]),
                     reads=[sc2, v16], writes=[ix])
            P.op("dve", lambda e: e.tensor_copy(out=ixf.ap, in_=ix.ap), reads=[ix], writes=[ixf])
            c4 = lambda b: b.ap.rearrange("p (h a b) -> p h a b", h=8, a=16)
            P.op("dve", lambda e: e.tensor_tensor(out=c4(cand), in0=bc(v16.ap[:, 0:1], [[32, 8], [1, 16], [0, 16]]),
                                                  in1=bc(v16.ap[:, 16:17], [[32, 8], [0, 16], [1, 16]]), op=ALU.add),
                 reads=[v16], writes=[cand])
            for h in range(8):
                src = cand.ap[:, h * 256:(h + 1) * 256]
                P.op("dve", lambda e, h=h, src=src: e.max(out=tsv[:, h, 0:8], in_=src), reads=[cand], writes=[ts])
                P.op("dve", lambda e, h=h, src=src: e.max_index(out=posv[:, h, 0:8], in_max=tsv[:, h, 0:8], in_values=src),
                     reads=[cand, ts], writes=[pos])
                P.op("dve", lambda e, h=h, src=src: e.match_replace(out=sc2.ap, in_to_replace=tsv[:, h, 0:8], in_values=src,
                                                                    imm_value=-1e30), reads=[cand, ts], writes=[sc2])
                P.op("dve", lambda e, h=h: e.max(out=tsv[:, h, 8:16], in_=sc2.ap), reads=[sc2], writes=[ts])
                P.op("dve", lambda e, h=h: e.max_index(out=posv[:, h, 8:16], in_max=tsv[:, h, 8:16], in_values=sc2.ap),
                     reads=[sc2, ts], writes=[pos])
            P.op("dve", lambda e: e.tensor_tensor(out=r3(ew.ap, a=8), in0=tsv, in1=bc(ts.ap[:, 0:1], [[16, 8], [0, 16]]), op=ALU.subtract),
                 reads=[ts], writes=[ew])
            P.op("act", lambda e: e.activation(out=ew.ap, in_=ew.ap, func=AF.Exp), reads=[ew], writes=[ew])
            P.op("dve", lambda e: e.tensor_reduce(out=zz.ap[:, 0:8], in_=r3(ew.ap, a=8), axis=AX.X, op=ALU.add), reads=[ew], writes=[zz])
            P.op("dve", lambda e: e.reciprocal(out=zz.ap[:, 8:16], in_=zz.ap[:, 0:8]), reads=[zz], writes=[zz])
            P.op("dve", lambda e, Wv=Wv: e.tensor_tensor(out=r3(Wv.ap, a=8), in0=r3(ew.ap, a=8), in1=bc(zz.ap[:, 8:9], [[1, 8], [0, 16]]), op=ALU.mult),
                 reads=[ew, zz], writes=[Wv])
            P.op("dve", lambda e: e.tensor_copy(out=posf.ap, in_=pos.ap), reads=[pos], writes=[posf])
            P.op("dve", lambda e: e.tensor_tensor(out=c4(eqb), in0=bc(posf.ap[:, 0:1], [[16, 8], [1, 16], [0, 16]]),
                                                  in1=bc(k16.ap[:, 0:1], [[0, 8], [0, 16], [1, 16]]), op=ALU.subtract),
                 reads=[k16, posf], writes=[eqb])
            P.op("dve", lambda e: e.tensor_scalar(out=eq2.ap, in0=eqb.ap, scalar1=0.0, scalar2=None, op0=ALU.is_ge),
                 reads=[eqb], writes=[eq2])
            P.op("dve", lambda e: e.scalar_tensor_tensor(out=eqb.ap, in0=eqb.ap, scalar=16.0, in1=eq2.ap, op0=ALU.is_lt, op1=ALU.mult),
                 reads=[eqb, eq2], writes=[eqb])
            P.op("dve", lambda e: e.tensor_tensor(out=c4(eq2), in0=c4(eqb), in1=bc(iotaf.ap[:, 0:1], [[0, 8], [0, 16], [1, 16]]), op=ALU.mult),
                 reads=[eqb, iotaf], writes=[eq2])
            P.op("dve", lambda e: e.tensor_reduce(out=k1f.ap, in_=c4(eq2), axis=AX.X, op=ALU.add), reads=[eq2], writes=[k1f])
            P.op("dve", lambda e: e.tensor_tensor(out=c4(eq2), in0=c4(eqb), in1=bc(ixf.ap[:, 0:1], [[32, 8], [0, 16], [1, 16]]), op=ALU.mult),
                 reads=[eqb, ixf], writes=[eq2])
            P.op("dve", lambda e, Iv=Iv: e.tensor_reduce(out=Iv.ap, in_=c4(eq2), axis=AX.X, op=ALU.add), reads=[eq2], writes=[Iv])
            P.op("dve", lambda e: e.scalar_tensor_tensor(out=k2f.ap, in0=k1f.ap, scalar=-16.0, in1=posf.ap, op0=ALU.mult, op1=ALU.add),
                 reads=[k1f, posf], writes=[k2f])
            P.op("dve", lambda e: e.tensor_tensor(out=c4(eqb), in0=bc(k2f.ap[:, 0:1], [[16, 8], [1, 16], [0, 16]]),
                                                  in1=bc(iotaf.ap[:, 0:1], [[0, 8], [0, 16], [1, 16]]), op=ALU.is_equal),
                 reads=[k2f, iotaf], writes=[eqb])
            P.op("dve", lambda e: e.tensor_tensor(out=c4(eq2), in0=c4(eqb), in1=bc(ixf.ap[:, 16:17], [[32, 8], [0, 16], [1, 16]]), op=ALU.mult),
                 reads=[eqb, ixf], writes=[eq2])
            P.op("dve", lambda e, Jv=Jv: e.tensor_reduce(out=Jv.ap, in_=c4(eq2), axis=AX.X, op=ALU.add), reads=[eq2], writes=[Jv])
        for tt in range(NT):
            Iv, Jv, Wv = Ivs[tt], Jvs[tt], Wvs[tt]
            bt, _, btok = nextbank([6, 7])
            for qi, srcb in enumerate((Iv, Jv, Wv)):
                P.op("pe", lambda e, qi=qi, srcb=srcb, bt=bt: e.transpose(out=bt[:, qi * 128:(qi + 1) * 128], in_=srcb.ap, identity=identf.ap),
                     reads=[srcb, identf], writes=[btok])
            P.op("act", lambda e, tt=tt, bt=bt: e.copy(out=SMv[:, :, tt * 128:(tt + 1) * 128], in_=r3(bt[:, 0:384], a=3)),
                 reads=[btok], writes=[SM])
        t3 = lambda b, n: b.ap[:, 0:TB * n].rearrange("p (t a) -> p t a", a=n)
        for q, OA, OB in ((0, OAI, OBI), (1, OAJ, OBJ)):
            P.op("dve", lambda e, q=q: e.tensor_tensor(out=t3(eqb, 8), in0=bc(SMv[:, q, 0:1], [[1, TB], [0, 8]]),
                                                       in1=bc(k16.ap[:, 0:1], [[0, TB], [1, 8]]), op=ALU.subtract),
                 reads=[SM, k16], writes=[eqb])
            P.op("dve", lambda e: e.tensor_scalar(out=cand.ap, in0=eqb.ap, scalar1=0.0, scalar2=None, op0=ALU.is_ge),
                 reads=[eqb], writes=[cand])
            P.op("dve", lambda e, OA=OA: e.scalar_tensor_tensor(out=OA.ap, in0=eqb.ap, scalar=16.0, in1=cand.ap, op0=ALU.is_lt, op1=ALU.mult),
                 reads=[eqb, cand], writes=[OA])
            P.op("dve", lambda e, OA=OA: e.tensor_tensor(out=t3(eqb, 8), in0=t3(OA, 8), in1=bc(iotaf.ap[:, 0:1], [[0, TB], [1, 8]]), op=ALU.mult),
                 reads=[OA, iotaf], writes=[eqb])
            P.op("dve", lambda e: e.tensor_reduce(out=xh.ap, in_=t3(eqb, 8), axis=AX.X, op=ALU.add), reads=[eqb], writes=[xh])
            P.op("dve", lambda e, q=q: e.scalar_tensor_tensor(out=xl.ap, in0=xh.ap, scalar=-16.0, in1=SMv[:, q, :], op0=ALU.mult, op1=ALU.add),
                 reads=[xh, SM], writes=[xl])
            P.op("dve", lambda e, OB=OB: e.tensor_tensor(out=t3(OB, 16), in0=bc(xl.ap[:, 0:1], [[1, TB], [0, 16]]),
                                                         in1=bc(iotaf.ap[:, 0:1], [[0, TB], [1, 16]]), op=ALU.is_equal),
                 reads=[xl, iotaf], writes=[OB])
        P.op("dve", lambda e: e.tensor_tensor(out=t3(OAJ, 8), in0=t3(OAJ, 8), in1=bc(SMv[:, 2, 0:1], [[1, TB], [0, 8]]), op=ALU.mult),
             reads=[OAJ, SM], writes=[OAJ])

    def b5(nb):
        TG = 16
        for tg in range(TB // TG):
            t0 = tg * TG
            cnt["oh"] += 1
            ob, oc = ohB[cnt["oh"] % 2], ohE[cnt["oh"] % 2]
            o3 = lambda b: b.ap.rearrange("p (t i) -> p t i", t=TG)
            o4 = lambda b: b.ap.rearrange("p (t a c) -> p t a c", t=TG, a=8)
            P.op("dve", lambda e, oc=oc, t0=t0: e.tensor_tensor(
                out=o4(oc), in0=bc(OAI.ap[:, t0 * 8:t0 * 8 + 1], [[8, TG], [1, 8], [0, 16]]),
                in1=bc(OBI.ap[:, t0 * 16:t0 * 16 + 1], [[16, TG], [0, 8], [1, 16]]), op=ALU.mult), reads=[OAI, OBI], writes=[oc])
            P.op("dve" if tg % 5 == 4 else "pool", lambda e, ob=ob, t0=t0: e.tensor_tensor(
                out=o4(ob), in0=bc(OAJ.ap[:, t0 * 8:t0 * 8 + 1], [[8, TG], [1, 8], [0, 16]]),
                in1=bc(OBJ.ap[:, t0 * 16:t0 * 16 + 1], [[16, TG], [0, 8], [1, 16]]), op=ALU.mult), reads=[OAJ, OBJ], writes=[ob])
            for t4 in range(TG // 4):
                bt, _, btok = nextbank([6, 7])
                P.opn("pe", [lambda e, q=q, t4=t4, oc=oc, ob=ob, bt=bt: e.matmul(
                    bt[:, q * 128:(q + 1) * 128], lhsT=o3(ob)[:, t4 * 4 + q, :], rhs=o3(oc)[:, t4 * 4 + q, :], start=True, stop=True)
                    for q in range(4)], reads=[oc, ob], writes=[btok])
                ta = t0 + t4 * 4
                P.op("act", lambda e, ta=ta, bt=bt: e.copy(out=Gv[:, :, ta:ta + 4], in_=bc(bt[:, 0:1], [[1, 128], [128, 4]])),
                     reads=[btok], writes=[G])
    def b6(nb, pend):
        h2Tv = r3(h2T[nb % 2].ap, a=8)
        h2T_tok = h2T_toks[nb % 2]
        def u_side(i):
            ub = ubr[i % 4]
            vb = vbr[i % 4]
            P.dma("sp", ub.ap, ut_d[i], reads=[ut_tok[i]], writes=[ub])
            P.dma("sp", vb.ap, vb_d[i * 128:(i + 1) * 128, :], reads=[vb_tok[i]], writes=[vb])
            bs_, _, bstok = banks[4 + i % 2]
            P.opn("pe", [lambda e, k=k, ub=ub, bs_=bs_: e.matmul(bs_[:, 0:TB], lhsT=r3(ub.ap, a=8)[:, k, :], rhs=h2Tv[:, k, :],
                                                             start=(k == 0), stop=(k == 7)) for k in range(8)],
                  reads=[ub] + h2T_tok, writes=[bstok])
            return bs_, bstok, vb

        per = (len(pend) + 119) // 120 if pend else 0
        uq = [u_side(0)]
        for i in range(128):
            bs_, bstok, vb = uq.pop(0)
            if i + 1 < 128:
                uq.append(u_side(i + 1))
            gel = gelr[i % 3]
            P.op("act", lambda e, gel=gel, bs_=bs_: e.activation(out=gel.ap, in_=bs_[:, 0:TB], func=AF.Gelu), reads=[bstok], writes=[gel])
            at = ATr[i % 4]
            P.op("pool", lambda e, gel=gel, at=at, i=i: e.tensor_tensor(out=at.ap, in0=gel.ap, in1=Gv[:, i, :], op=ALU.mult),
                 reads=[gel, G], writes=[at])
            P.opn("pe", [lambda e, tt=tt, half=half, at=at, vb=vb, i=i: e.matmul(
                banks[tt * 2 + half][0][:, :], lhsT=at.ap[:, tt * 128:(tt + 1) * 128], rhs=vb.ap[:, half * 512:(half + 1) * 512],
                start=(i == 0), stop=(i == 127)) for tt in range(NT) for half in range(2)],
                reads=[at, vb], writes=[banks[b4][2] for b4 in range(2 * NT)])
            if pend:
                P.replay(pend, per)
        P.replay(pend, len(pend))

    def b7(nb):
        for tt in range(NT):
            gt = nb * NT + tt
            xt = xts[tt]
            P.dma("sp", xt.ap, out_d[gt * 128:(gt + 1) * 128, :], reads=[out_tok[gt]], writes=[xt])
            for half in range(2):
                ab, _, abtok = banks[tt * 2 + half]
                P.op("dve", lambda e, half=half, ab=ab, xt=xt: e.tensor_tensor(
                    out=xt.ap[:, half * 512:(half + 1) * 512], in0=ab[:, :], in1=xt.ap[:, half * 512:(half + 1) * 512], op=ALU.add),
                    reads=[abtok, xt], writes=[xt])
            ss, rstd = new_small()
            smt = small_cur["tok"]
            P.op("dve", lambda e, xt=xt, ss=ss: e.scalar_tensor_tensor(out=eqb.ap[:, 0:1024], in0=xt.ap, scalar=1.0, in1=xt.ap,
                                                                       op0=ALU.mult, op1=ALU.mult, accum_out=ss),
                 reads=[xt], writes=[eqb, smt])
            P.op("pool", lambda e, ss=ss: e.tensor_scalar(out=ss, in0=ss, scalar1=1.0 / D, scalar2=EPS, op0=ALU.mult, op1=ALU.add),
                 reads=[smt], writes=[smt])
            P.op("pool", lambda e, ss=ss, rstd=rstd: e.tensor_tensor(out=rstd, in0=ss, in1=nhalf.ap, op=ALU.pow),
                 reads=[smt, nhalf], writes=[smt])
            P.op("dve", lambda e, xt=xt, rstd=rstd: e.scalar_tensor_tensor(out=xt.ap, in0=xt.ap, scalar=rstd, in1=fg.ap,
                                                                           op0=ALU.mult, op1=ALU.mult), reads=[xt, smt, fg], writes=[xt])
            P.dma("act", out_d[gt * 128:(gt + 1) * 128, :], xt.ap, reads=[xt], writes=[out_tok[gt]], key=xt)


    routing(0)
    for i in range(128):
        prep_uv(i)
    P.barrier()
    for nb in range(NBLK):
        b5(nb)
        pend = []
        if nb + 1 < NBLK:
            P.capture()
            routing(nb + 1)
            pend = P.end_capture()
        b6(nb, pend)
        b7(nb)


def prep_inputs(inputs):
    f = lambda a: np.ascontiguousarray(np.asarray(a, dtype=np.float32))
    rk = lambda w: f(w.reshape(8, 128, -1).transpose(1, 0, 2))
    pv = lambda v: v.reshape(8, 128).T
    x = f(inputs["x"])
    vecs = np.concatenate([pv(np.asarray(inputs[n])[0]) for n in
                           ("norm1_g", "conv_dw_b", "conv_ln_g", "conv_ln_b", "norm2_g")], axis=1)
    dww = np.asarray(inputs["conv_dw_w"])[0].reshape(31, 8, 128).transpose(2, 1, 0).reshape(128, 248)
    shared = {
        "win": rk(np.asarray(inputs["w_in"])[0]),
        "wpw": rk(np.asarray(inputs["conv_w_pw"])[0]),
        "wo": rk(np.asarray(inputs["attn_w_o"])[0]),
        "wout": rk(np.asarray(inputs["w_out"])[0]),
        "wq": rk(np.asarray(inputs["peer_w_query"])[0]),
        "vecs": f(vecs),
        "dww": f(dww),
        "sink": f(np.broadcast_to(np.asarray(inputs["attn_sink"])[0][None, :], (128, 16))),
        "fg": f(np.broadcast_to(np.asarray(inputs["final_g"])[None, :], (128, 1024))),
        "skT": f(np.asarray(inputs["peer_sub_keys"])[0].transpose(2, 0, 1).reshape(128, 256)),
        "pu": f(np.asarray(inputs["peer_u"])[0]),
        "pv": f(np.asarray(inputs["peer_v"])[0]),
    }
    xs = x.reshape(NCORES, TOK, D)
    return [dict(shared, x=np.ascontiguousarray(xs[c])) for c in range(NCORES)]


_NC_CACHE = {}


def kernel(**inputs):
    in_maps = prep_inputs(inputs)
    if "nc" not in _NC_CACHE:
        _NC_CACHE["nc"] = build_program()
    res = run_bass_kernel_spmd(_NC_CACHE["nc"], in_maps, core_ids=list(range(NCORES)))
    out = np.stack([np.asarray(r["out"]) for r in res.results], axis=0)
    return out.reshape(16, SEQ, D).astype(np.float32)
```
